# Optimizing a Trainium2 kernel written in Bass

```python
import math
import jax, jax.numpy as jnp
from jax import lax
import numpy as np

D_MODEL = 2048
BATCH = 4
SEQ = 4096
DEPTH = 2

N_MIXERS = 2
NORM_EPS = 1e-6

ATT_HEADS = 16
ATT_HEAD_DIM = 128
ATT_WIDTH = ATT_HEADS * ATT_HEAD_DIM
IDX_HEADS = 16
IDX_HEAD_DIM = 64
TOPK_MAX = 256
Q_BLOCK = 128
A_IN = 4 * ATT_WIDTH + IDX_HEADS * IDX_HEAD_DIM + IDX_HEAD_DIM + IDX_HEADS

REL_BUCKETS = 32
REL_MAX_DIST = 128

SSM_EXPAND = 2
SSM_INNER = SSM_EXPAND * D_MODEL
SSM_HEAD_DIM = 64
SSM_HEADS = SSM_INNER // SSM_HEAD_DIM
SSM_GROUPS = 8
SSM_STATE = 128
SSM_CONV = 4
SSM_CHUNK = 128
SSM_CONV_CH = SSM_INNER + 2 * SSM_GROUPS * SSM_STATE
B_IN = SSM_INNER + SSM_CONV_CH + SSM_HEADS

N_A_LAYERS = (DEPTH + N_MIXERS - 1) // N_MIXERS
N_B_LAYERS = DEPTH // N_MIXERS

kernel_name = "hybrid_dsa_ssd_interleaved"


def rmsnorm(x, w):
    xf = x.astype(jnp.float32)
    y = xf * lax.rsqrt(jnp.mean(xf * xf, axis=-1, keepdims=True) + NORM_EPS)
    return (y * w.astype(jnp.float32)).astype(x.dtype)


def rel_bucket(n):
    n = jnp.maximum(n, 0)
    max_exact = REL_BUCKETS // 2
    nf = jnp.maximum(n, 1).astype(jnp.float32)
    large = max_exact + (jnp.log(nf / max_exact) / math.log(REL_MAX_DIST / max_exact)
                         * (REL_BUCKETS - max_exact)).astype(jnp.int32)
    large = jnp.minimum(large, REL_BUCKETS - 1)
    return jnp.where(n < max_exact, n, large)


def dsa_mixer(h, w_in, w_out, rel_bias):
    b, s, _ = h.shape
    proj = h @ w_in
    sizes = [ATT_WIDTH, ATT_WIDTH, ATT_WIDTH, ATT_WIDTH, IDX_HEADS * IDX_HEAD_DIM, IDX_HEAD_DIM, IDX_HEADS]
    offs = list(np.cumsum(sizes)[:-1])
    q, k, v, g, iq, ik, iw = jnp.split(proj, offs, axis=-1)
    q = q.reshape(b, s, ATT_HEADS, ATT_HEAD_DIM)
    k = k.reshape(b, s, ATT_HEADS, ATT_HEAD_DIM)
    v = v.reshape(b, s, ATT_HEADS, ATT_HEAD_DIM)
    iq = iq.reshape(b, s, IDX_HEADS, IDX_HEAD_DIM)
    iw = iw * (IDX_HEADS ** -0.5 * IDX_HEAD_DIM ** -0.5)
    topk = min(TOPK_MAX, s // 4)
    nblk = s // Q_BLOCK
    pos = jnp.arange(s, dtype=jnp.int32)
    scale = ATT_HEAD_DIM ** -0.5

    def blockify(a):
        return jnp.moveaxis(a.reshape(b, nblk, Q_BLOCK, *a.shape[2:]), 1, 0)

    def attend_block(args):
        qb, iqb, iwb, tb = args
        dots = jnp.einsum('bqhd,bsd->bqsh', iqb, ik)
        score = jnp.einsum('bqsh,bqh->bqs', jax.nn.relu(dots), iwb).astype(jnp.float32)
        causal = pos[None, :] <= tb[:, None]
        score = jnp.where(causal[None], score, -jnp.inf)
        _, idx = lax.top_k(score, topk)
        valid = idx <= tb[None, :, None]
        k_sel = jax.vmap(lambda kb, ib: kb[ib])(k, idx)
        v_sel = jax.vmap(lambda vb, ib: vb[ib])(v, idx)
        bias = rel_bias[rel_bucket(tb[None, :, None] - idx)]
        logits = (jnp.einsum('bqhd,bqkhd->bqhk', qb, k_sel).astype(jnp.float32) * scale
                  + jnp.swapaxes(bias, -1, -2).astype(jnp.float32))
        logits = jnp.where(valid[:, :, None, :], logits, -jnp.inf)
        p = jax.nn.softmax(logits, axis=-1).astype(v.dtype)
        return jnp.einsum('bqhk,bqkhd->bqhd', p, v_sel)

    out = lax.map(attend_block, (blockify(q), blockify(iq), blockify(iw), pos.reshape(nblk, Q_BLOCK)))
    out = jnp.moveaxis(out, 0, 1).reshape(b, s, ATT_WIDTH)
    return (out * jax.nn.silu(g)) @ w_out


def segsum(a):
    t = a.shape[-1]
    cs = jnp.cumsum(a, axis=-1)
    diff = cs[..., :, None] - cs[..., None, :]
    mask = jnp.tril(jnp.ones((t, t), dtype=bool))
    return jnp.where(mask, diff, -jnp.inf)


def ssd_scan(xdt, adt, bm, cm):
    b, s, nh, p = xdt.shape
    ng, n = bm.shape[-2:]
    r = nh // ng
    c, l = s // SSM_CHUNK, SSM_CHUNK
    x_ = xdt.reshape(b, c, l, ng, r, p)
    a_ = adt.reshape(b, c, l, ng, r).transpose(0, 3, 4, 1, 2)
    b_ = bm.reshape(b, c, l, ng, n).astype(jnp.float32)
    c_ = cm.reshape(b, c, l, ng, n).astype(jnp.float32)
    a_cs = jnp.cumsum(a_, axis=-1)
    lmat = jnp.exp(segsum(a_))
    cb = jnp.einsum('bclgn,bcsgn->bgcls', c_, b_)
    y_diag = jnp.einsum('bgcls,bgrcls,bcsgrp->bclgrp', cb, lmat, x_)
    decay_states = jnp.exp(a_cs[..., -1:] - a_cs)
    states = jnp.einsum('bclgn,bgrcl,bclgrp->bcgrpn', b_, decay_states, x_)
    chunk_decay = jnp.exp(a_cs[..., -1])

    def step(carry, inp):
        st, dec = inp
        return carry * dec[..., None, None] + st, carry

    init = jnp.zeros((b, ng, r, p, n), jnp.float32)
    _, prev = lax.scan(step, init, (jnp.moveaxis(states, 1, 0), jnp.moveaxis(chunk_decay, 3, 0)))
    prev = jnp.moveaxis(prev, 0, 1)
    y_off = jnp.einsum('bclgn,bcgrpn,bgrcl->bclgrp', c_, prev, jnp.exp(a_cs))
    return (y_diag + y_off).reshape(b, s, nh, p)


def ssd_mixer(h, w_in, conv_w, conv_b, dt_bias, a_log, d_skip, norm_w, w_out):
    b, s, _ = h.shape
    proj = h @ w_in
    z = proj[..., :SSM_INNER]
    xbc = proj[..., SSM_INNER:SSM_INNER + SSM_CONV_CH]
    dt = proj[..., SSM_INNER + SSM_CONV_CH:]
    xbc = lax.conv_general_dilated(xbc, conv_w[:, None, :].astype(xbc.dtype), window_strides=(1,),
                                   padding=[(SSM_CONV - 1, 0)], dimension_numbers=('NWC', 'WIO', 'NWC'),
                                   feature_group_count=SSM_CONV_CH) + conv_b
    xbc = jax.nn.silu(xbc)
    xs = xbc[..., :SSM_INNER].reshape(b, s, SSM_HEADS, SSM_HEAD_DIM)
    gn = SSM_GROUPS * SSM_STATE
    bm = xbc[..., SSM_INNER:SSM_INNER + gn].reshape(b, s, SSM_GROUPS, SSM_STATE)
    cm = xbc[..., SSM_INNER + gn:].reshape(b, s, SSM_GROUPS, SSM_STATE)
    dt = jax.nn.softplus((dt + dt_bias).astype(jnp.float32))
    a = -jnp.exp(a_log.astype(jnp.float32))
    xf = xs.astype(jnp.float32)
    y = ssd_scan(xf * dt[..., None], dt * a, bm, cm)
    y = y + xf * d_skip.astype(jnp.float32)[:, None]
    y = y.reshape(b, s, SSM_INNER).astype(h.dtype)
    y = rmsnorm(y * jax.nn.silu(z), norm_w)
    return y @ w_out


def setup_inputs(seed: int = 0) -> dict:
    key = jax.random.key(seed)
    ks = jax.random.split(key, 16)
    f32 = jnp.float32
    x = jax.random.normal(ks[0], (BATCH, SEQ, D_MODEL), f32)
    norm_w = 1.0 + 0.02 * jax.random.normal(ks[1], (DEPTH, D_MODEL), f32)
    a_w_in = jax.random.normal(ks[2], (N_A_LAYERS, D_MODEL, A_IN), f32) * D_MODEL ** -0.5
    a_w_out = jax.random.normal(ks[3], (N_A_LAYERS, ATT_WIDTH, D_MODEL), f32) * ATT_WIDTH ** -0.5
    rel_bias = 0.5 * jax.random.normal(ks[4], (REL_BUCKETS, ATT_HEADS), f32)
    b_w_in = jax.random.normal(ks[5], (N_B_LAYERS, D_MODEL, B_IN), f32) * D_MODEL ** -0.5
    b_conv_w = jax.random.normal(ks[6], (N_B_LAYERS, SSM_CONV, SSM_CONV_CH), f32) * SSM_CONV ** -0.5
    b_conv_b = 0.01 * jax.random.normal(ks[7], (N_B_LAYERS, SSM_CONV_CH), f32)
    dt0 = jnp.exp(jax.random.uniform(ks[8], (N_B_LAYERS, SSM_HEADS), f32,
                                     math.log(1e-3), math.log(1e-1)))
    b_dt_bias = dt0 + jnp.log(-jnp.expm1(-dt0))
    b_a_log = jnp.log(jax.random.uniform(ks[9], (N_B_LAYERS, SSM_HEADS), f32, 1.0, 16.0))
    b_d = 1.0 + 0.1 * jax.random.normal(ks[10], (N_B_LAYERS, SSM_HEADS), f32)
    b_norm_w = 1.0 + 0.02 * jax.random.normal(ks[11], (N_B_LAYERS, SSM_INNER), f32)
    b_w_out = jax.random.normal(ks[12], (N_B_LAYERS, SSM_INNER, D_MODEL), f32) * SSM_INNER ** -0.5
    final_norm_w = 1.0 + 0.02 * jax.random.normal(ks[13], (D_MODEL,), f32)
    return {"x": x, "norm_w": norm_w, "a_w_in": a_w_in, "a_w_out": a_w_out, "rel_bias": rel_bias,
            "b_w_in": b_w_in, "b_conv_w": b_conv_w, "b_conv_b": b_conv_b, "b_dt_bias": b_dt_bias,
            "b_a_log": b_a_log, "b_d": b_d, "b_norm_w": b_norm_w, "b_w_out": b_w_out,
            "final_norm_w": final_norm_w}


def reference(x, norm_w, a_w_in, a_w_out, rel_bias, b_w_in, b_conv_w, b_conv_b, b_dt_bias,
              b_a_log, b_d, b_norm_w, b_w_out, final_norm_w):
    for i in range(DEPTH):
        h = rmsnorm(x, norm_w[i])
        j = i // N_MIXERS
        if i % N_MIXERS == 0:
            x = x + dsa_mixer(h, a_w_in[j], a_w_out[j], rel_bias)
        else:
            x = x + ssd_mixer(h, b_w_in[j], b_conv_w[j], b_conv_b[j], b_dt_bias[j], b_a_log[j],
                              b_d[j], b_norm_w[j], b_w_out[j])
    return rmsnorm(x, final_norm_w)
```

```python
import contextlib
import numpy as np
import ml_dtypes
import concourse.bass as bass
import concourse.mybir as mybir
from concourse.bass_utils import run_bass_kernel_spmd

F32 = mybir.dt.float32
BF16 = mybir.dt.bfloat16
ALU = mybir.AluOpType
AF = mybir.ActivationFunctionType
AX = mybir.AxisListType

ENGS = ['pe', 'act', 'dve', 'pool', 'sp']


def _is_psum(r):
    n = r[0] if isinstance(r, tuple) else r
    return isinstance(n, str) and (n.startswith('ps') or n.startswith('acc'))


class Prog:
    def __init__(self, nc, stack):
        self.nc = nc
        self.stack = stack
        self.q = {e: [] for e in ENGS}
        self.semval = {}
        self.waited = {e: {} for e in ENGS}
        self.res = {}
        self.dma_keys = set()

    def op(self, eng, meth, kw, reads=(), writes=(), dma=None):
        fn = (meth, kw)
        waits = {}
        own = 'c_' + eng
        xr = [r for r in reads if _is_psum(r)]
        if xr:
            writes = list(writes) + [r for r in xr if r not in writes]

        def need(kv, raw):
            if kv is None:
                return
            k, v = kv
            if k == own and not raw:
                return
            if k == own and eng == 'pe':
                return
            if self.waited[eng].get(k, 0) >= v:
                return
            if waits.get(k, 0) < v:
                waits[k] = v

        for r in reads:
            st = self.res.get(r)
            if st:
                need(st['w'], True)
        for w in writes:
            st = self.res.get(w)
            if st:
                need(st['w'], False)
                for kv in st['r'].items():
                    need(kv, False)
        if dma:
            key, inc = dma, 16
            self.dma_keys.add(key)
        else:
            key, inc = own, 1
        self.semval[key] = self.semval.get(key, 0) + inc
        val = self.semval[key]
        for k, v in waits.items():
            self.waited[eng][k] = v
        self.q[eng].append([fn, sorted(waits.items()), key, inc, val])
        for r in reads:
            st = self.res.setdefault(r, {'w': None, 'r': {}})
            st['r'][key] = val
        for w in writes:
            self.res[w] = {'w': (key, val), 'r': {}}
        return (key, val)

    def finish(self, eng='sp'):
        waits = [(k, self.semval[k]) for k in sorted(self.dma_keys)]
        for e in ENGS:
            k = 'c_' + e
            if k in self.semval:
                waits.append((k, self.semval[k]))
        self.q[eng].append([None, waits, None, 0, 0])

    def emit(self):
        nc = self.nc
        waited_vals = {}
        for e in ENGS:
            for fn, waits, key, inc, val in self.q[e]:
                for k, v in waits:
                    waited_vals.setdefault(k, set()).add(v)
        remap = {}
        for k, vs in waited_vals.items():
            if k in self.dma_keys:
                continue
            svs = sorted(vs)
            remap[k] = {v: i + 1 for i, v in enumerate(svs)}
        sems = {}
        for k in list(self.semval.keys()):
            sems[k] = self.stack.enter_context(nc.semaphore("s_" + k))
        block = self.stack.enter_context(nc.Block())
        engmap = {'pe': block.tensor, 'act': block.scalar, 'dve': block.vector,
                  'pool': block.gpsimd, 'sp': block.sync}
        self.n_instr = {}
        for e in ENGS:
            ops = self.q[e]
            self.n_instr[e] = len(ops)
            if not ops:
                continue

            def body(engine, ops=ops):
                for fn, waits, key, inc, val in ops:
                    for k, v in waits:
                        if k in self.dma_keys:
                            engine.wait_ge(sems[k], v)
                        else:
                            engine.wait_ge(sems[k], remap[k][v])
                    if fn is None:
                        continue
                    ins = getattr(engine, fn[0])(**fn[1])
                    if key in self.dma_keys:
                        ins.then_inc(sems[key], 16)
                    elif key in remap and val in remap[key]:
                        ins.then_inc(sems[key], 1)
            engmap[e](body)


def MM(P, out, lhsT, rhs, start, stop, reads, writes, **kw):
    return P.op('pe', 'matmul', dict(out=out, lhsT=lhsT, rhs=rhs, start=start, stop=stop, **kw), reads, writes)


def ACTV(P, out, in_, func, reads, writes, **kw):
    return P.op('act', 'activation', dict(out=out, in_=in_, func=func, **kw), reads, writes)


def DMA(P, eng, out, in_, reads, writes, key):
    return P.op(eng, 'dma_start', dict(out=out, in_=in_), reads, writes, dma=key)


def TS(P, eng, out, in0, s1, s2, op0, op1, reads, writes, **kw):
    d = dict(out=out, in0=in0, scalar1=s1, scalar2=s2, op0=op0, **kw)
    if op1 is not None:
        d['op1'] = op1
    return P.op(eng, 'tensor_scalar', d, reads, writes)


def TT(P, eng, out, in0, in1, op, reads, writes):
    return P.op(eng, 'tensor_tensor', dict(out=out, in0=in0, in1=in1, op=op), reads, writes)


def STT(P, out, in0, scalar, in1, op0, op1, reads, writes, **kw):
    return P.op('dve', 'scalar_tensor_tensor', dict(out=out, in0=in0, scalar=scalar, in1=in1, op0=op0, op1=op1, **kw),
                reads, writes)


def CP(P, eng, out, in_, reads, writes):
    if eng == 'act':
        return P.op('act', 'activation', dict(out=out, in_=in_, func=AF.Copy), reads, writes)
    return P.op(eng, 'tensor_copy', dict(out=out, in_=in_), reads, writes)


D = 2048
KC = D // 128
EPS = 1e-6


S = 4096
NIT = 26
SCALE = 128 ** -0.5


def own_blocks(par):
    return [i for i in range(32) if (i % 4 in (0, 3)) == (par == 0)]


def k2_consts(rel_bias, par):
    n = np.arange(256)
    nf = np.maximum(n, 1).astype(np.float32)
    large = 16 + (np.log(nf / 16) / np.log(128 / 16) * 16).astype(np.int32)
    large = np.minimum(large, 31)
    bucket = np.where(n < 16, n, large)
    s = np.arange(128)[:, None]
    q = np.arange(128)[None, :]
    biasT = np.zeros((128, 16, 2, 128), np.float32)
    for d in range(2):
        dist = np.clip(d * 128 + q - s, 0, 255)
        biasT[:, :, d, :] = rel_bias[bucket[dist]].transpose(0, 2, 1)
    bfar = np.ascontiguousarray(np.broadcast_to(rel_bias[31][None, :], (128, 16))).astype(np.float32)
    negtri = np.where(np.arange(128)[None, :] <= np.arange(128)[:, None], 0.0, -1e30).astype(np.float32)
    ident = np.eye(128, dtype=np.float32).astype(ml_dtypes.bfloat16)
    dtab = lambda sp, w: (1 - w) if (sp == par) else (2 - w)
    cm = np.zeros((128, 2, 2, 128), np.float32)
    biasN = np.zeros((16, 128, 2, 3, 128), np.float32)
    for sp in range(2):
        for w in range(3):
            d = dtab(sp, w)
            if d >= 2:
                biasN[:, :, sp, w, :] = rel_bias[31][:, None, None]
            elif d >= 0:
                biasN[:, :, sp, w, :] = biasT[:, :, d, :].transpose(1, 0, 2)
            if w >= 1:
                if d == 0:
                    cm[:, sp, w - 1, :] = negtri
                elif d < 0:
                    cm[:, sp, w - 1, :] = -1e30
    return dict(bfar=bfar, ident=ident, cm=cm.reshape(128, 512), biasN=biasN.reshape(16, 128, 768))


def build_k2(nslots=16, nheads=16, nit=NIT):
    nc = bass.Bass("TRN2", target_bir_lowering=False)
    NQB = nslots
    nkbs = [2 * (qi + 1) for qi in range(nslots)]
    NQ = NQB * 128
    kT = nc.dram_tensor("kT", [2048, S], BF16, kind="ExternalInput").ap()
    v = nc.dram_tensor("v", [S, 2048], BF16, kind="ExternalInput").ap()
    ikT = nc.dram_tensor("ikT", [64, S], BF16, kind="ExternalInput").ap()
    qT = nc.dram_tensor("qT", [2048, NQ], BF16, kind="ExternalInput").ap()
    iqT = nc.dram_tensor("iqT", [1024, NQ], BF16, kind="ExternalInput").ap()
    iw = nc.dram_tensor("iw", [NQ, 16], F32, kind="ExternalInput").ap()
    sg = nc.dram_tensor("sg", [NQ, 2048], BF16, kind="ExternalInput").ap()
    biasNd = nc.dram_tensor("biasN", [16, 128, 768], F32, kind="ExternalInput").ap()
    bfard = nc.dram_tensor("bfar", [128, 16], F32, kind="ExternalInput").ap()
    cmd = nc.dram_tensor("cm", [128, 512], F32, kind="ExternalInput").ap()
    identd = nc.dram_tensor("ident", [128, 128], BF16, kind="ExternalInput").ap()
    u = nc.dram_tensor("u", [NQ, 2048], BF16, kind="ExternalOutput").ap()
    offs = []
    T = 0
    for nkb in nkbs:
        offs.append(T)
        T += nkb
    with contextlib.ExitStack() as st:
        sb = lambda name, shape, dt: st.enter_context(nc.sbuf_tensor(name, shape, dt))
        maskT = sb("maskT", [128, T, 128], BF16)
        score = sb("score", [128, S], F32)
        maskq = sb("maskq", [128, S], BF16)
        relu = [sb(f"relu{i}", [128, 512], F32) for i in range(2)]
        ikTs = sb("ikTs", [64, S], BF16)
        iqb = [sb(f"iqb{i}", [64, 16, 128], BF16) for i in range(2)]
        iwb = [sb(f"iwb{i}", [128, 16], F32) for i in range(2)]
        kTh = [sb(f"kTh{i}", [128, S], BF16) for i in range(2)]
        vh = [sb(f"vh{i}", [128, 32, 129], BF16) for i in range(2)]
        qTh = [sb(f"qTh{i}", [128, NQ], BF16) for i in range(2)]
        sgb = [sb(f"sgb{i}", [128, NQB, 128], BF16) for i in range(2)]
        biasN = [sb(f"biasN{i}", [128, 2, 3, 128], F32) for i in range(2)]
        bfar = sb("bfars", [128, 16], F32)
        cm = sb("cms", [128, 2, 256], F32)
        ident = sb("idents", [128, 128], BF16)
        hi = sb("hi", [128, 1], F32)
        lo = sb("lo", [128, 1], F32)
        w = sb("w", [128, 1], F32)
        mid = sb("mid", [128, 1], F32)
        cnt = sb("cnt", [128, 1], F32)
        ge = sb("ge", [128, 1], F32)
        rinv = sb("rinv", [128, 1], F32)
        NE = 4
        tb = [sb(f"tb{i}", [128, 128], F32) for i in range(2)]
        eb = [sb(f"eb{i}", [128, 128], BF16) for i in range(NE)]
        pt = [sb(f"pt{i}", [128, 128], BF16) for i in range(NE)]
        o32 = [sb(f"o32{i}", [128, 128], F32) for i in range(2)]
        ust = [sb(f"ust{i}", [128, 128], BF16) for i in range(4)]
        psb = [st.enter_context(nc.psum_tensor(f"ps{i}", [128, 512], F32)) for i in range(4)]
        pst = [st.enter_context(nc.psum_tensor(f"pst{i}", [128, 1024], BF16)) for i in range(2)]
        acc = [st.enter_context(nc.psum_tensor(f"acc{i}", [128, 512], F32)) for i in range(2)]
        P = Prog(nc, st)
        cn = {}

        def nxt(k, n):
            vv = cn.get(k, 0)
            cn[k] = vv + 1
            return vv % n
        DMA(P, 'sp', ikTs[:], ikT[:, :], [], ['ikT'], 'd_c0')
        DMA(P, 'sp', cm[:], cmd[:, :].rearrange("p (a b) -> p a b", a=2), [], ['cm'], 'd_c1')
        DMA(P, 'sp', ident[:], identd[:, :], [], ['ident'], 'd_c2')
        DMA(P, 'sp', bfar[:], bfard[:, :], [], ['bfar'], 'd_c3')
        for i in range(2):
            P.op('pool', 'memset', dict(ap=vh[i][:, :, 128:129], constant=1.0), [], [('vh1', i)])

        for qi in range(nslots):
            nkb = nkbs[qi]
            nk = nkb * 128
            s2 = qi % 2
            DMA(P, 'sp', iqb[s2][:], iqT[:, qi * 128:(qi + 1) * 128].rearrange("(h d) q -> d h q", d=64),
                [], [('iqb', s2)], f'd_iq{s2}')
            DMA(P, 'sp', iwb[s2][:], iw[qi * 128:(qi + 1) * 128, :], [], [('iwb', s2)], f'd_iw{s2}')
            nch = (nk + 511) // 512
            for c in range(nch):
                kw = min(512, nk - c * 512)
                for hh in range(16):
                    b = nxt('ps', 4)
                    MM(P, psb[b][:, :kw], iqb[s2][:, hh, :], ikTs[:, c * 512:c * 512 + kw], True, True,
                       [('iqb', s2), 'ikT'], [('ps', b)])
                    rb = nxt('relu', 2)
                    ACTV(P, relu[rb][:, :kw], psb[b][:, :kw], AF.Relu, [('ps', b)], [('relu', rb)])
                    sc = score[:, c * 512:c * 512 + kw]
                    if hh == 0:
                        TS(P, 'dve', sc, relu[rb][:, :kw], iwb[s2][:, 0:1], None, ALU.mult, None,
                           [('relu', rb), ('iwb', s2)], [('score', c)])
                    else:
                        STT(P, sc, relu[rb][:, :kw], iwb[s2][:, hh:hh + 1], sc, ALU.mult, ALU.add,
                            [('relu', rb), ('iwb', s2), ('score', c)], [('score', c)])
            scr = [('score', c) for c in range(nch)]
            P.op('dve', 'tensor_reduce', dict(out=hi[:], in_=score[:, :nk], axis=AX.X, op=ALU.max), scr, ['hi'])
            P.op('dve', 'tensor_reduce', dict(out=lo[:], in_=score[:, :nk], axis=AX.X, op=ALU.min), scr, ['lo'])
            TT(P, 'dve', w[:], hi[:], lo[:], ALU.subtract, ['hi', 'lo'], ['w'])
            dg = score[:, (nkb - 2) * 128:nkb * 128]
            TT(P, 'dve', dg, dg, cm[:, qi % 2, :], ALU.add, [('score', (nkb - 2) // 4), 'cm'], [('score', (nkb - 2) // 4)])
            for it in range(nit):
                TS(P, 'dve', w[:], w[:], 0.5, None, ALU.mult, None, ['w'], ['w'])
                TT(P, 'dve', mid[:], lo[:], w[:], ALU.add, ['lo', 'w'], ['mid'])
                TS(P, 'dve', maskq[:, :nk], score[:, :nk], mid[:, 0:1], 0.0, ALU.is_ge, ALU.add,
                   scr + ['mid'], ['maskq', 'cnt'], accum_out=cnt[:, 0:1])
                TS(P, 'dve', ge[:], cnt[:], 255.5, None, ALU.is_ge, None, ['cnt'], ['ge'])
                STT(P, lo[:], ge[:], w[:, 0:1], lo[:], ALU.mult, ALU.add, ['ge', 'w', 'lo'], ['lo'])
            TS(P, 'dve', maskq[:, :nk], score[:, :nk], lo[:, 0:1], None, ALU.is_ge, None, scr + ['lo'], ['maskq'])
            for j in range(nkb):
                b = nxt('pst', 2)
                P.op('pe', 'transpose', dict(out=pst[b][:, :128], in_=maskq[:, j * 128:(j + 1) * 128], identity=ident[:]),
                     ['maskq', 'ident'], [('pst', b)])
                CP(P, 'act', maskT[:, offs[qi] + j, :], pst[b][:, :128], [('pst', b)], [('maskT', qi)])

        for h in range(nheads):
            s2 = h % 2
            DMA(P, 'sp', kTh[s2][:], kT[h * 128:(h + 1) * 128, :], [], [('kTh', s2)], f'd_k{s2}')
            DMA(P, 'sp', vh[s2][:, :, 0:128], v[:, h * 128:(h + 1) * 128].rearrange("(j p) d -> p j d", p=128),
                [], [('vh', s2)], f'd_v{s2}')
            DMA(P, 'sp', qTh[s2][:], qT[h * 128:(h + 1) * 128, :], [], [('qTh', s2)], f'd_q{s2}')
            DMA(P, 'sp', sgb[s2][:], sg[:, h * 128:(h + 1) * 128].rearrange("(qi p) d -> p qi d", p=128),
                [], [('sgb', s2)], f'd_sg{s2}')
            DMA(P, 'sp', biasN[s2][:], biasNd[h].rearrange("p (a w q) -> p a w q", a=2, w=3), [], [('biasN', s2)], f'd_bn{s2}')
            for qi in range(nslots):
                nkb = nkbs[qi]
                ab = nxt('acc', 2)
                for j in range(nkb):
                    b = nxt('ps', 4)
                    MM(P, psb[b][:, :128], kTh[s2][:, j * 128:(j + 1) * 128], qTh[s2][:, qi * 128:(qi + 1) * 128],
                       True, True, [('kTh', s2), ('qTh', s2)], [('ps', b)])
                    wv = j - (nkb - 3)
                    e = nxt('eb', NE)
                    if wv < 0:
                        ACTV(P, eb[e][:], psb[b][:, :128], AF.Exp, [('ps', b), 'bfar'], [('eb', e)],
                             scale=SCALE, bias=bfar[:, h:h + 1])
                    else:
                        t = nxt('tb', 2)
                        STT(P, tb[t][:], psb[b][:, :128], SCALE, biasN[s2][:, qi % 2, wv, :], ALU.mult, ALU.add,
                            [('ps', b), ('biasN', s2)], [('tb', t)])
                        ACTV(P, eb[e][:], tb[t][:], AF.Exp, [('tb', t)], [('eb', e)])
                    p = nxt('pt', NE)
                    me = 'dve' if nxt('me', 2) == 0 else 'pool'
                    TT(P, me, pt[p][:], eb[e][:], maskT[:, offs[qi] + j, :], ALU.mult,
                       [('eb', e), ('maskT', qi)], [('pt', p)])
                    MM(P, acc[ab][:, :129], pt[p][:], vh[s2][:, j, :], j == 0, j == nkb - 1,
                       [('pt', p), ('vh', s2), ('vh1', s2)], [('acc', ab)])
                P.op('dve', 'reciprocal', dict(out=rinv[:], in_=acc[ab][:, 128:129]), [('acc', ab)], ['rinv'])
                o = nxt('o32', 2)
                TS(P, 'dve', o32[o][:], acc[ab][:, :128], rinv[:, 0:1], None, ALU.mult, None,
                   [('acc', ab), 'rinv'], [('o32', o)])
                us = nxt('ust', 4)
                TT(P, 'pool', ust[us][:], o32[o][:], sgb[s2][:, qi, :], ALU.mult, [('o32', o), ('sgb', s2)], [('ust', us)])
                DMA(P, 'sp', u[qi * 128:(qi + 1) * 128, h * 128:(h + 1) * 128], ust[us][:], [('ust', us)], [], f'd_u{us}')
        P.finish()
        P.emit()
        print("K2 instr counts", P.n_instr)
    return nc


NH = 32
NG = 4
NCT = 24


def k4_consts():
    U = np.triu(np.ones((128, 128), np.float32))
    sel = np.zeros((32, 32, 128), np.float32)
    for h in range(32):
        sel[h, h, :] = 1.0
    return dict(U=U, ones128=np.ones((128, 128), np.float32), sel=sel.reshape(32, 32 * 128),
                tri=U.copy(), identf=np.eye(128, dtype=np.float32),
                identb=np.eye(128, dtype=np.float32).astype(ml_dtypes.bfloat16))


class _Stop(Exception):
    pass


def build_k4(npieces=8, stop=99):
    nc = bass.Bass("TRN2", target_bir_lowering=False)
    SS = npieces * 512
    xbcT = nc.dram_tensor("xbcT", [NCT * 128, SS], F32, kind="ExternalInput").ap()
    dtd = nc.dram_tensor("dt", [SS, NH], F32, kind="ExternalInput").ap()
    cwd = nc.dram_tensor("cw", [128, NCT * 4], F32, kind="ExternalInput").ap()
    cbd = nc.dram_tensor("cb", [128, NCT], F32, kind="ExternalInput").ap()
    dtbd = nc.dram_tensor("dtb", [128, NH], F32, kind="ExternalInput").ap()
    alogd = nc.dram_tensor("alog", [128, NH], F32, kind="ExternalInput").ap()
    Dd = nc.dram_tensor("Dv", [128, NH], F32, kind="ExternalInput").ap()
    Ud = nc.dram_tensor("U", [128, 128], F32, kind="ExternalInput").ap()
    onesd = nc.dram_tensor("ones128", [128, 128], F32, kind="ExternalInput").ap()
    seld = nc.dram_tensor("sel", [32, 32 * 128], F32, kind="ExternalInput").ap()
    trid = nc.dram_tensor("tri", [128, 128], F32, kind="ExternalInput").ap()
    identfd = nc.dram_tensor("identf", [128, 128], F32, kind="ExternalInput").ap()
    identbd = nc.dram_tensor("identb", [128, 128], BF16, kind="ExternalInput").ap()
    yd_out = nc.dram_tensor("y", [SS, NH * 64], F32, kind="ExternalOutput").ap()
    with contextlib.ExitStack() as st:
        sb = lambda name, shape, dt: st.enter_context(nc.sbuf_tensor(name, shape, dt))
        xin = sb("xin", [128, 12, 515], F32)
        xc = sb("xc", [128, 16, 512], F32)
        BTc = sb("BTc", [128, 4, 512], BF16)
        CTc = sb("CTc", [128, 4, 512], BF16)
        cacc = [sb(f"cacc{i}", [128, 512], F32) for i in range(2)]
        cw = sb("cws", [128, NCT, 4], F32)
        cb = sb("cbs", [128, NCT], F32)
        dtb = sb("dtbs", [128, NH], F32)
        Aneg = sb("Aneg", [128, NH], F32)
        Dv = sb("Dvs", [128, NH], F32)
        U = sb("Us", [128, 128], F32)
        ones128 = sb("ones128s", [128, 128], F32)
        sel = sb("sels", [32, 32, 128], F32)
        tri = sb("tris", [128, 128], F32)
        identf = sb("identfs", [128, 128], F32)
        identb = sb("identbs", [128, 128], BF16)
        dtr = sb("dtr", [128, 4, NH], F32)
        dtp = sb("dtp", [128, 4, NH], F32)
        adt = sb("adt", [128, 4, NH], F32)
        acs = sb("acs", [128, NH], F32)
        ea = sb("ea", [128, NH], F32)
        dec = sb("dec", [128, NH], F32)
        cdec = sb("cdec", [128, NH], F32)
        dtdec = sb("dtdec", [128, NH], F32)
        acsT = sb("acsT", [32, 128], F32)
        x_tm = sb("x_tm", [128, NH, 64], F32)
        xdt = sb("xdt", [128, NH, 64], BF16)
        xdtd = sb("xdtd", [128, NH, 64], BF16)
        B_tm = sb("B_tm", [128, 4, 128], BF16)
        prev32 = sb("prev32", [128, NG, 512], F32)
        prevbf = sb("prevbf", [128, NG, 512], BF16)
        cbTm = [sb(f"cbTm{i}", [128, 128], F32) for i in range(2)]
        tcl = [sb(f"tcl{i}", [128, 4, 128], F32) for i in range(2)]
        Lt = [sb(f"Lt{i}", [128, 4, 128], F32) for i in range(2)]
        MT = [sb(f"MT{i}", [128, 4, 128], BF16) for i in range(2)]
        yos = [sb(f"yos{i}", [128, 8, 64], F32) for i in range(2)]
        dsk = [sb(f"dsk{i}", [128, 8, 64], F32) for i in range(2)]
        ystage = [sb(f"ystage{i}", [128, NH, 64], F32) for i in range(2)]
        ps = [st.enter_context(nc.psum_tensor(f"ps{i}", [128, 512], F32)) for i in range(3)]
        psmisc = st.enter_context(nc.psum_tensor("psmisc", [128, 512], F32))
        psbt = st.enter_context(nc.psum_tensor("psbt", [128, 1024], BF16))
        psbc = [st.enter_context(nc.psum_tensor(f"psbc{i}", [128, 512], F32)) for i in range(2)]
        psyd = st.enter_context(nc.psum_tensor("psyd", [128, 512], F32))
        P = Prog(nc, st)
        cn = {}

        def nxt(k, n):
            vv = cn.get(k, 0)
            cn[k] = vv + 1
            return vv % n
        for name, t, d in [('cw', cw, cwd[:, :].rearrange("p (c k) -> p c k", k=4)), ('cb', cb, cbd[:, :]),
                           ('dtb', dtb, dtbd[:, :]), ('Dv', Dv, Dd[:, :]), ('U', U, Ud[:, :]),
                           ('ones128', ones128, onesd[:, :]), ('sel', sel, seld[:, :].rearrange("p (h m) -> p h m", m=128)),
                           ('tri', tri, trid[:, :]), ('identf', identf, identfd[:, :]), ('identb', identb, identbd[:, :]),
                           ('Aneg', Aneg, alogd[:, :])]:
            DMA(P, 'sp', t[:], d, [], [name], 'd_' + name)
        ACTV(P, Aneg[:], Aneg[:], AF.Exp, ['Aneg'], ['Aneg'])
        TS(P, 'dve', Aneg[:], Aneg[:], -1.0, None, ALU.mult, None, ['Aneg'], ['Aneg'])
        P.op('dve', 'memset', dict(ap=prev32[:], constant=0.0), [], [('prev32', g) for g in range(NG)])
        P.op('dve', 'memset', dict(ap=prevbf[:], constant=0.0), [], [('prevbf', g) for g in range(NG)])

        def chk(n):
            if stop <= n:
                raise _Stop()
        try:
          for pc in range(npieces):
              t0 = pc * 512
              for half in range(2):
                  src = xbcT[half * 12 * 128:(half + 1) * 12 * 128, :]
                  if pc == 0:
                      P.op('dve', 'memset', dict(ap=xin[:, :, 0:3], constant=0.0), [], ['xin'])
                      DMA(P, 'sp', xin[:, :, 3:515], src[:, 0:512].rearrange("(c p) t -> p c t", p=128), [], ['xin'], 'd_xin')
                  else:
                      DMA(P, 'sp', xin[:, :, :], src[:, t0 - 3:t0 + 512].rearrange("(c p) t -> p c t", p=128), [], ['xin'], 'd_xin')
                  for cl in range(12):
                      ct = half * 12 + cl
                      a = nxt('cacc', 2)
                      TS(P, 'dve', cacc[a][:], xin[:, cl, 3:515], cw[:, ct, 3:4], None, ALU.mult, None,
                         ['xin', 'cw'], [('cacc', a)])
                      for k in (2, 1, 0):
                          STT(P, cacc[a][:], xin[:, cl, k:k + 512], cw[:, ct, k:k + 1], cacc[a][:], ALU.mult, ALU.add,
                              ['xin', 'cw', ('cacc', a)], [('cacc', a)])
                      if ct < 16:
                          dst, dr = xc[:, ct, :], ('xc', ct)
                      elif ct < 20:
                          dst, dr = BTc[:, ct - 16, :], ('BTc', ct - 16)
                      else:
                          dst, dr = CTc[:, ct - 20, :], ('CTc', ct - 20)
                      ACTV(P, dst, cacc[a][:], AF.Silu, [('cacc', a), 'cb'], [dr], bias=cb[:, ct:ct + 1])
              chk(1)
              DMA(P, 'sp', dtr[:], dtd[t0:t0 + 512, :].rearrange("(c p) h -> p c h", p=128), [], ['dtr'], 'd_dtr')
              TT(P, 'dve', dtr[:], dtr[:], dtb[:].unsqueeze(1).to_broadcast([128, 4, NH]), ALU.add, ['dtr', 'dtb'], ['dtr'])
              ACTV(P, dtp[:], dtr[:], AF.Exp, ['dtr'], ['dtp'])
              ACTV(P, dtp[:], dtp[:], AF.Ln, ['dtp', 'ones128'], ['dtp'], bias=ones128[:, 0:1])
              TT(P, 'dve', adt[:], dtp[:], Aneg[:].unsqueeze(1).to_broadcast([128, 4, NH]), ALU.mult, ['dtp', 'Aneg'], ['adt'])
              chk(2)
              for c in range(4):
                  l0 = c * 128
                  MM(P, psmisc[:, 0:NH], U[:], adt[:, c, :], True, True, ['U', 'adt'], ['psmisc'])
                  MM(P, psmisc[:, 32:32 + NH], ones128[:], adt[:, c, :], True, True, ['ones128', 'adt'], ['psmisc'])
                  MM(P, psmisc[0:32, 64:192], adt[:, c, :], U[:], True, True, ['U', 'adt'], ['psmisc'])
                  CP(P, 'dve', acs[:], psmisc[:, 0:NH], ['psmisc'], ['acs'])
                  ACTV(P, ea[:], psmisc[:, 0:NH], AF.Exp, ['psmisc'], ['ea'])
                  ACTV(P, cdec[:], psmisc[:, 32:32 + NH], AF.Exp, ['psmisc'], ['cdec'])
                  TT(P, 'dve', dec[:], psmisc[:, 32:32 + NH], acs[:], ALU.subtract, ['psmisc', 'acs'], ['dec'])
                  ACTV(P, dec[:], dec[:], AF.Exp, ['dec'], ['dec'])
                  CP(P, 'dve', acsT[:], psmisc[0:32, 64:192], ['psmisc'], ['acsT'])
                  TT(P, 'dve', dtdec[:], dtp[:, c, :], dec[:], ALU.mult, ['dtp', 'dec'], ['dtdec'])
                  chk(3)
                  for q4 in range(4):
                      b = nxt('ps', 3)
                      for k in range(4):
                          ct = q4 * 4 + k
                          P.op('pe', 'transpose', dict(out=ps[b][:, k * 128:(k + 1) * 128], in_=xc[:, ct, l0:l0 + 128],
                                                       identity=identf[:]), [('xc', ct), 'identf'], [('ps', b)])
                      CP(P, 'act', x_tm[:, q4 * 8:(q4 + 1) * 8, :].rearrange("p h d -> p (h d)"), ps[b][:, :],
                         [('ps', b)], [('x_tm', q4)])
                  xr = [('x_tm', q) for q in range(4)]
                  TT(P, 'dve', xdt[:], x_tm[:], dtp[:, c, :].unsqueeze(2).to_broadcast([128, NH, 64]), ALU.mult,
                     xr + ['dtp'], ['xdt'])
                  TT(P, 'pool', xdtd[:], x_tm[:], dtdec[:].unsqueeze(2).to_broadcast([128, NH, 64]), ALU.mult,
                     xr + ['dtdec'], ['xdtd'])
                  for g in range(NG):
                      P.op('pe', 'transpose', dict(out=psbt[:, g * 128:(g + 1) * 128], in_=BTc[:, g, l0:l0 + 128],
                                                   identity=identb[:]), [('BTc', g), 'identb'], ['psbt'])
                  CP(P, 'act', B_tm[:].rearrange("p g n -> p (g n)"), psbt[:, 0:512], ['psbt'], ['B_tm'])
                  chk(4)
                  ys = nxt('ystage', 2)
                  for g in range(NG):
                      b = nxt('ps', 3)
                      MM(P, ps[b][:, 0:128], BTc[:, g, l0:l0 + 128], CTc[:, g, l0:l0 + 128], True, True,
                         [('BTc', g), ('CTc', g)], [('ps', b)])
                      cm = nxt('cbTm', 2)
                      TT(P, 'dve', cbTm[cm][:], ps[b][:, 0:128], tri[:], ALU.mult, [('ps', b), 'tri'], [('cbTm', cm)])
                      for hq in range(2):
                          bb = nxt('psbc', 2)
                          tc_ = nxt('tcl', 2)
                          for hh in range(4):
                              h = g * 8 + hq * 4 + hh
                              MM(P, psbc[bb][:, hh * 128:(hh + 1) * 128], sel[:, h, :], acsT[:], True, True,
                                 ['sel', 'acsT'], [('psbc', bb)])
                              TS(P, 'dve', tcl[tc_][:, hh, :], psbc[bb][:, hh * 128:(hh + 1) * 128], acs[:, h:h + 1], 0.0,
                                 ALU.subtract, ALU.min, [('psbc', bb), 'acs'], [('tcl', tc_)])
                          ACTV(P, Lt[tc_][:], tcl[tc_][:], AF.Exp, [('tcl', tc_)], [('Lt', tc_)])
                          TT(P, 'pool', MT[tc_][:], Lt[tc_][:], cbTm[cm][:].unsqueeze(1).to_broadcast([128, 4, 128]), ALU.mult,
                             [('Lt', tc_), ('cbTm', cm)], [('MT', tc_)])
                          for hh in range(4):
                              h = g * 8 + hq * 4 + hh
                              MM(P, psyd[:, (hq * 4 + hh) * 64:(hq * 4 + hh + 1) * 64], MT[tc_][:, hh, :], xdt[:, h, :],
                                 True, True, [('MT', tc_), 'xdt'], ['psyd'])
                      chk(5)
                      b = nxt('ps', 3)
                      MM(P, ps[b][:, :], CTc[:, g, l0:l0 + 128], prevbf[:, g, :], True, True,
                         [('CTc', g), ('prevbf', g)], [('ps', b)])
                      yo = nxt('yos', 2)
                      TT(P, 'dve', yos[yo][:], ps[b][:, :].rearrange("p (h d) -> p h d", d=64),
                         ea[:, g * 8:(g + 1) * 8].unsqueeze(2).to_broadcast([128, 8, 64]), ALU.mult,
                         [('ps', b), 'ea'], [('yos', yo)])
                      TT(P, 'pool', dsk[yo][:], x_tm[:, g * 8:(g + 1) * 8, :],
                         Dv[:, g * 8:(g + 1) * 8].unsqueeze(2).to_broadcast([128, 8, 64]), ALU.mult,
                         [('x_tm', g), 'Dv'], [('dsk', yo)])
                      TT(P, 'pool', dsk[yo][:], dsk[yo][:], yos[yo][:], ALU.add, [('dsk', yo), ('yos', yo)], [('dsk', yo)])
                      TT(P, 'dve', ystage[ys][:, g * 8:(g + 1) * 8, :], psyd[:, :].rearrange("p (h d) -> p h d", d=64),
                         dsk[yo][:], ALU.add, ['psyd', ('dsk', yo)], [('ystage', ys)])
                      chk(6)
                      b = nxt('ps', 3)
                      MM(P, ps[b][:, :], B_tm[:, g, :], xdtd[:, g * 8:(g + 1) * 8, :].rearrange("p h d -> p (h d)"), True, True,
                         ['B_tm', 'xdtd'], [('ps', b)])
                      TT(P, 'pool', prev32[:, g, :].rearrange("p (h d) -> p h d", d=64),
                         prev32[:, g, :].rearrange("p (h d) -> p h d", d=64),
                         cdec[:, g * 8:(g + 1) * 8].unsqueeze(2).to_broadcast([128, 8, 64]), ALU.mult,
                         [('prev32', g), 'cdec'], [('prev32', g)])
                      TT(P, 'dve', prev32[:, g, :], prev32[:, g, :], ps[b][:, :], ALU.add, [('prev32', g), ('ps', b)],
                         [('prev32', g)])
                      CP(P, 'act', prevbf[:, g, :], prev32[:, g, :], [('prev32', g)], [('prevbf', g)])
                  DMA(P, 'sp', yd_out[t0 + l0:t0 + l0 + 128, :], ystage[ys][:].rearrange("p h d -> p (h d)"),
                      [('ystage', ys)], [], f'd_y{ys}')
        except _Stop:
            pass
        P.finish()
        P.emit()
        print("K4 instr counts", P.n_instr)
    return nc


def rmsnorm_stats(P, xt, ones, psk, ps, rstd, sq, xres, nfeat, KCn, eps=EPS):
    for kc in range(KCn):
        ACTV(P, sq[kc % 2][:], xt[:, kc, :], AF.Square, [xres], [('sq', kc % 2)])
        MM(P, ps[:], ones[:], sq[kc % 2][:], kc == 0, kc == KCn - 1, [('sq', kc % 2), 'ones'], [psk])
    TS(P, 'dve', rstd, ps[:], 1.0 / nfeat, eps, ALU.mult, ALU.add, [psk], ['rstd'])
    ACTV(P, rstd, rstd, AF.Sqrt, ['rstd'], ['rstd'])
    P.op('dve', 'reciprocal', dict(out=rstd, in_=rstd), ['rstd'], ['rstd'])


def build_proj(NT, WN, table, outs, stg_dt):
    nc = bass.Bass("TRN2", target_bir_lowering=False)
    xT = nc.dram_tensor("xT", [D, NT], F32, kind="ExternalInput").ap()
    W = nc.dram_tensor("W", [D, WN], F32, kind="ExternalInput").ap()
    nwd = nc.dram_tensor("nw", [128, KC], F32, kind="ExternalInput").ap()
    od = {name: nc.dram_tensor(name, list(shape), dt, kind="ExternalOutput").ap() for name, (shape, dt) in outs.items()}
    NTG = NT // 512
    NTT = NT // 128
    with contextlib.ExitStack() as st:
        sb = lambda name, shape, dt: st.enter_context(nc.sbuf_tensor(name, shape, dt))
        ones = sb("ones", [128, 128], F32)
        nw = sb("nw_sb", [128, KC], F32)
        xt = [sb(f"xt{i}", [128, KC, 512], F32) for i in range(2)]
        sq = [sb(f"sq{i}", [128, 512], F32) for i in range(2)]
        rstd = sb("rstd", [128, 512], F32)
        hT = sb("hT", [128, KC, NT], BF16)
        NW = 3
        wbf = [sb(f"wbf{i}", [128, KC, 512], BF16) for i in range(NW)]
        NS = 4
        stg = [sb(f"stg{i}", [128, 512], stg_dt) for i in range(NS)]
        stgf = sb("stgf", [128, 16], F32)
        psb = [st.enter_context(nc.psum_tensor(f"ps{i}", [128, 512], F32)) for i in range(8)]
        P = Prog(nc, st)
        P.op('dve', 'memset', dict(ap=ones[:], constant=1.0), [], ['ones'])
        DMA(P, 'sp', nw[:], nwd[:, :], [], ['nw'], 'd_nw')
        wt = table

        def load_w(i):
            c0, ncol, kind, dst, d0 = wt[i]
            s = i % NW
            DMA(P, 'pool', wbf[s][:, :, :ncol], W[:, c0:c0 + ncol].rearrange("(kc p) c -> p kc c", p=128),
                [], [('wbf', s)], f'd_w{s}')
        load_w(0)
        load_w(1)
        for tg in range(NTG):
            xr = ('xt', tg % 2)
            DMA(P, 'sp', xt[tg % 2][:], xT[:, tg * 512:(tg + 1) * 512].rearrange("(kc p) t -> p kc t", p=128),
                [], [xr], f'd_x{tg % 2}')
            rmsnorm_stats(P, xt[tg % 2], ones, ('ps', 7), psb[7], rstd[:], sq, xr, D, KC)
            for kc in range(KC):
                STT(P, hT[:, kc, tg * 512:(tg + 1) * 512], xt[tg % 2][:, kc, :], nw[:, kc:kc + 1], rstd[:],
                    ALU.mult, ALU.mult, [xr, 'rstd', 'nw'], [('hT', kc, tg)])
        cnt = {'ps': 0, 'stg': 0, 'ev': 0}

        def nxt(k, n):
            v = cnt[k] % n
            cnt[k] += 1
            return v
        for i in range(len(wt)):
            if i + 2 < len(wt):
                load_w(i + 2)
            c0, ncol, kind, dstn, d0 = wt[i]
            dst = od[dstn]
            s = i % NW
            if kind == 'fm':
                for cb in range((ncol + 127) // 128):
                    m = min(128, ncol - cb * 128)
                    for tg in range(NTG):
                        b = nxt('ps', 6)
                        for kc in range(KC):
                            MM(P, psb[b][:m, :], wbf[s][:, kc, cb * 128:cb * 128 + m], hT[:, kc, tg * 512:(tg + 1) * 512],
                               kc == 0, kc == KC - 1, [('wbf', s), ('hT', kc, tg)], [('ps', b)])
                        ss = nxt('stg', NS)
                        ev = 'act' if nxt('ev', 2) == 0 else 'dve'
                        CP(P, ev, stg[ss][:m, :], psb[b][:m, :], [('ps', b)], [('stg', ss)])
                        r0 = d0 + cb * 128
                        DMA(P, 'sp', dst[r0:r0 + m, tg * 512:(tg + 1) * 512], stg[ss][:m, :], [('stg', ss)], [], f'd_o{ss}')
            else:
                for tt in range(NTT):
                    b = nxt('ps', 6)
                    for kc in range(KC):
                        MM(P, psb[b][:, :ncol], hT[:, kc, tt * 128:(tt + 1) * 128], wbf[s][:, kc, :ncol],
                           kc == 0, kc == KC - 1, [('wbf', s), ('hT', kc, tt // 4)], [('ps', b)])
                    if kind == 'tmw':
                        TS(P, 'dve', stgf[:, :], psb[b][:, :16], 1.0 / 32.0, None, ALU.mult, None, [('ps', b)], ['stgf'])
                        DMA(P, 'sp', dst[tt * 128:(tt + 1) * 128, :], stgf[:, :], ['stgf'], [], 'd_of')
                        continue
                    ss = nxt('stg', NS)
                    if kind == 'tmg':
                        ACTV(P, stg[ss][:, :], psb[b][:, :], AF.Silu, [('ps', b)], [('stg', ss)])
                    else:
                        CP(P, 'dve', stg[ss][:, :], psb[b][:, :], [('ps', b)], [('stg', ss)])
                    DMA(P, 'sp', dst[tt * 128:(tt + 1) * 128, d0:d0 + 512], stg[ss][:, :], [('stg', ss)], [], f'd_o{ss}')
        P.finish()
        P.emit()
    return nc


def table_a():
    wt = []
    for i in range(8):
        wt.append((i * 512, 512, 'fm', 'qT' if i < 4 else 'kT', (i % 4) * 512))
    for i in range(2):
        wt.append((8192 + i * 512, 512, 'fm', 'iqT', i * 512))
    wt.append((9216, 64, 'fm', 'ikT', 0))
    for i in range(4):
        wt.append((4096 + i * 512, 512, 'tm', 'v', i * 512))
    for i in range(4):
        wt.append((6144 + i * 512, 512, 'tmg', 'sg', i * 512))
    wt.append((9280, 16, 'tmw', 'iw', 0))
    return wt


def outs_a(NT):
    return {'qT': ((2048, NT), BF16), 'kT': ((2048, NT), BF16), 'iqT': ((1024, NT), BF16), 'ikT': ((64, NT), BF16),
            'v': ((NT, 2048), BF16), 'sg': ((NT, 2048), BF16), 'iw': ((NT, 16), F32)}


def table_b():
    wt = []
    for i in range(8):
        wt.append((i * 512, 512, 'fm', 'zT', i * 512))
    for i in range(12):
        wt.append((4096 + i * 512, 512, 'fm', 'xbcT', i * 512))
    wt.append((10240, 64, 'fm', 'dtT', 0))
    return wt


def outs_b(NT):
    return {'zT': ((4096, NT), F32), 'xbcT': ((6144, NT), F32), 'dtT': ((64, NT), F32)}


def build_aout(NT):
    nc = bass.Bass("TRN2", target_bir_lowering=False)
    uT = nc.dram_tensor("uT", [D, NT], BF16, kind="ExternalInput").ap()
    xT = nc.dram_tensor("xT", [D, NT], F32, kind="ExternalInput").ap()
    W = nc.dram_tensor("W", [D, D], F32, kind="ExternalInput").ap()
    x1T = nc.dram_tensor("x1T", [D, NT], F32, kind="ExternalOutput").ap()
    NTG = NT // 512
    with contextlib.ExitStack() as st:
        sb = lambda name, shape, dt: st.enter_context(nc.sbuf_tensor(name, shape, dt))
        aT = sb("aT", [128, KC, NT], BF16)
        wbf = [sb(f"wbf{i}", [128, KC, 512], BF16) for i in range(2)]
        rs = [sb(f"rs{i}", [128, 512], F32) for i in range(4)]
        psb = [st.enter_context(nc.psum_tensor(f"ps{i}", [128, 512], F32)) for i in range(6)]
        P = Prog(nc, st)
        cnt = {}

        def nxt(k, n):
            v = cnt.get(k, 0)
            cnt[k] = v + 1
            return v % n
        DMA(P, 'sp', aT[:], uT[:, :].rearrange("(kc p) t -> p kc t", p=128), [], ['aT'], 'd_a')

        def load_w(i):
            DMA(P, 'pool', wbf[i % 2][:], W[:, i * 512:(i + 1) * 512].rearrange("(kc p) c -> p kc c", p=128),
                [], [('wbf', i % 2)], f'd_w{i % 2}')
        load_w(0)
        for i in range(4):
            if i + 1 < 4:
                load_w(i + 1)
            for cb in range(4):
                ob = i * 4 + cb
                for tg in range(NTG):
                    r = nxt('rs', 4)
                    DMA(P, 'sp', rs[r][:], xT[ob * 128:(ob + 1) * 128, tg * 512:(tg + 1) * 512], [], [('rs', r)], f'd_r{r}')
                    b = nxt('ps', 6)
                    for kc in range(KC):
                        MM(P, psb[b][:], wbf[i % 2][:, kc, cb * 128:(cb + 1) * 128], aT[:, kc, tg * 512:(tg + 1) * 512],
                           kc == 0, kc == KC - 1, [('wbf', i % 2), 'aT'], [('ps', b)])
                    TT(P, 'dve', rs[r][:], psb[b][:], rs[r][:], ALU.add, [('ps', b), ('rs', r)], [('rs', r)])
                    DMA(P, 'sp', x1T[ob * 128:(ob + 1) * 128, tg * 512:(tg + 1) * 512], rs[r][:], [('rs', r)], [], f'd_o{r}')
        P.finish()
        P.emit()
    return nc


def build_bout(NT, TP=1024):
    nc = bass.Bass("TRN2", target_bir_lowering=False)
    KI = 32
    yT = nc.dram_tensor("yT", [4096, NT], F32, kind="ExternalInput").ap()
    zT = nc.dram_tensor("zT", [4096, NT], F32, kind="ExternalInput").ap()
    x1T = nc.dram_tensor("x1T", [D, NT], F32, kind="ExternalInput").ap()
    bnwd = nc.dram_tensor("bnw", [128, KI], F32, kind="ExternalInput").ap()
    W = nc.dram_tensor("W", [4096, D], F32, kind="ExternalInput").ap()
    x2T = nc.dram_tensor("x2T", [D, NT], F32, kind="ExternalOutput").ap()
    NPASS = NT // TP
    NTG = TP // 512
    with contextlib.ExitStack() as st:
        sb = lambda name, shape, dt: st.enter_context(nc.sbuf_tensor(name, shape, dt))
        ones = sb("ones", [128, 128], F32)
        bnw = sb("bnws", [128, KI], F32)
        gyb = sb("gyb", [128, KI, TP], BF16)
        rstd = sb("rstd", [128, TP], F32)
        yb = [sb(f"yb{i}", [128, 4, 512], F32) for i in range(2)]
        zb = [sb(f"zb{i}", [128, 4, 512], F32) for i in range(2)]
        sq = [sb(f"sq{i}", [128, 512], F32) for i in range(2)]
        wbf = [sb(f"wbf{i}", [128, KI, 256], BF16) for i in range(3)]
        rs = [sb(f"rs{i}", [128, 512], F32) for i in range(4)]
        t32 = [sb(f"t32{i}", [128, 512], F32) for i in range(2)]
        psb = [st.enter_context(nc.psum_tensor(f"ps{i}", [128, 512], F32)) for i in range(8)]
        P = Prog(nc, st)
        cnt = {}

        def nxt(k, n):
            v = cnt.get(k, 0)
            cnt[k] = v + 1
            return v % n
        P.op('dve', 'memset', dict(ap=ones[:], constant=1.0), [], ['ones'])
        DMA(P, 'sp', bnw[:], bnwd[:, :], [], ['bnw'], 'd_bnw')
        wi = [0]

        def load_w(ci):
            s = wi[0] % 3
            wi[0] += 1
            DMA(P, 'pool', wbf[s][:], W[:, ci * 256:(ci + 1) * 256].rearrange("(kc p) c -> p kc c", p=128),
                [], [('wbf', s)], f'd_w{s}')
            return s
        for ps_ in range(NPASS):
            p0 = ps_ * TP
            for tg in range(NTG):
                c0 = p0 + tg * 512
                for k4 in range(KI // 4):
                    s = nxt('yz', 2)
                    DMA(P, 'sp', yb[s][:], yT[k4 * 512:(k4 + 1) * 512, c0:c0 + 512].rearrange("(k p) t -> p k t", p=128),
                        [], [('yb', s)], f'd_y{s}')
                    DMA(P, 'sp', zb[s][:], zT[k4 * 512:(k4 + 1) * 512, c0:c0 + 512].rearrange("(k p) t -> p k t", p=128),
                        [], [('zb', s)], f'd_z{s}')
                    ACTV(P, zb[s][:], zb[s][:], AF.Silu, [('zb', s)], [('zb', s)])
                    TT(P, 'dve', yb[s][:], yb[s][:], zb[s][:], ALU.mult, [('yb', s), ('zb', s)], [('yb', s)])
                    for k in range(4):
                        kc = k4 * 4 + k
                        q = nxt('sq', 2)
                        ACTV(P, sq[q][:], yb[s][:, k, :], AF.Square, [('yb', s)], [('sq', q)])
                        MM(P, psb[7][:], ones[:], sq[q][:], kc == 0, kc == KI - 1, [('sq', q), 'ones'], [('ps', 7)])
                        TS(P, 'pool', gyb[:, kc, tg * 512:(tg + 1) * 512], yb[s][:, k, :], bnw[:, kc:kc + 1], None,
                           ALU.mult, None, [('yb', s), 'bnw'], [('gyb', kc, tg)])
                rv = rstd[:, tg * 512:(tg + 1) * 512]
                TS(P, 'dve', rv, psb[7][:], 1.0 / 4096.0, EPS, ALU.mult, ALU.add, [('ps', 7)], [('rstd', tg)])
                ACTV(P, rv, rv, AF.Sqrt, [('rstd', tg)], [('rstd', tg)])
                P.op('dve', 'reciprocal', dict(out=rv, in_=rv), [('rstd', tg)], [('rstd', tg)])
            slots = {0: load_w(0), 1: load_w(1)}
            for ci in range(8):
                if ci + 2 < 8:
                    slots[ci + 2] = load_w(ci + 2)
                s = slots[ci]
                for cb in range(2):
                    ob = ci * 2 + cb
                    for tg in range(NTG):
                        c0 = p0 + tg * 512
                        r = nxt('rs', 4)
                        DMA(P, 'sp', rs[r][:], x1T[ob * 128:(ob + 1) * 128, c0:c0 + 512], [], [('rs', r)], f'd_r{r}')
                        b = nxt('ps', 6)
                        for kc in range(KI):
                            MM(P, psb[b][:], wbf[s][:, kc, cb * 128:(cb + 1) * 128], gyb[:, kc, tg * 512:(tg + 1) * 512],
                               kc == 0, kc == KI - 1, [('wbf', s), ('gyb', kc, tg)], [('ps', b)])
                        t = nxt('t32', 2)
                        TT(P, 'dve', t32[t][:], psb[b][:], rstd[:, tg * 512:(tg + 1) * 512], ALU.mult,
                           [('ps', b), ('rstd', tg)], [('t32', t)])
                        TT(P, 'pool', rs[r][:], t32[t][:], rs[r][:], ALU.add, [('t32', t), ('rs', r)], [('rs', r)])
                        DMA(P, 'sp', x2T[ob * 128:(ob + 1) * 128, c0:c0 + 512], rs[r][:], [('rs', r)], [], f'd_o{r}')
        P.finish()
        P.emit()
    return nc


def build_fnorm(NT):
    nc = bass.Bass("TRN2", target_bir_lowering=False)
    xT = nc.dram_tensor("xT", [D, NT], F32, kind="ExternalInput").ap()
    nwd = nc.dram_tensor("nw", [128, KC], F32, kind="ExternalInput").ap()
    oT = nc.dram_tensor("oT", [D, NT], F32, kind="ExternalOutput").ap()
    with contextlib.ExitStack() as st:
        sb = lambda name, shape, dt: st.enter_context(nc.sbuf_tensor(name, shape, dt))
        ones = sb("ones", [128, 128], F32)
        nw = sb("nw_sb", [128, KC], F32)
        xt = [sb(f"xt{i}", [128, KC, 512], F32) for i in range(2)]
        sq = [sb(f"sq{i}", [128, 512], F32) for i in range(2)]
        rstd = sb("rstd", [128, 512], F32)
        ps = st.enter_context(nc.psum_tensor("ps0", [128, 512], F32))
        P = Prog(nc, st)
        P.op('dve', 'memset', dict(ap=ones[:], constant=1.0), [], ['ones'])
        DMA(P, 'sp', nw[:], nwd[:, :], [], ['nw'], 'd_nw')
        for tg in range(NT // 512):
            xr = ('xt', tg % 2)
            DMA(P, 'sp', xt[tg % 2][:], xT[:, tg * 512:(tg + 1) * 512].rearrange("(kc p) t -> p kc t", p=128),
                [], [xr], f'd_x{tg % 2}')
            rmsnorm_stats(P, xt[tg % 2], ones, ('ps', 0), ps, rstd[:], sq, xr, D, KC)
            for kc in range(KC):
                STT(P, xt[tg % 2][:, kc, :], xt[tg % 2][:, kc, :], nw[:, kc:kc + 1], rstd[:],
                    ALU.mult, ALU.mult, [xr, 'rstd', 'nw'], [xr])
            DMA(P, 'sp', oT[:, tg * 512:(tg + 1) * 512].rearrange("(kc p) t -> p kc t", p=128), xt[tg % 2][:],
                [xr], [], f'd_o{tg % 2}')
        P.finish()
        P.emit()
    return nc


NCORES = 8
NT = 2048
_DBG = None


def _run(nc, in_maps):
    res = run_bass_kernel_spmd(nc, in_maps, core_ids=list(range(NCORES)))
    return res.results


def _stash(name, val):
    if _DBG is not None:
        _DBG[name] = val


def _pk(w, n):
    return np.ascontiguousarray(np.asarray(w, np.float32).reshape(n, 128).T)


def _bc(a):
    a = np.asarray(a, np.float32)
    return np.ascontiguousarray(np.broadcast_to(a[None, :], (128, a.shape[0])))


def kernel(x, norm_w, a_w_in, a_w_out, rel_bias, b_w_in, b_conv_w, b_conv_b, b_dt_bias, b_a_log, b_d,
           b_norm_w, b_w_out, final_norm_w):
    x = np.asarray(x, np.float32)
    A = np.ascontiguousarray
    cores = [(c // 2, c % 2) for c in range(NCORES)]
    xT = [A(x[b, hf * NT:(hf + 1) * NT, :].T) for b, hf in cores]
    nc = build_proj(NT, 9296, table_a(), outs_a(NT), BF16)
    Wa = A(np.asarray(a_w_in[0], np.float32))
    r1 = _run(nc, [{"xT": xT[c], "W": Wa, "nw": _pk(norm_w[0], KC)} for c in range(NCORES)])
    nc = build_k2(16, 16)
    rb = np.asarray(rel_bias, np.float32)
    ims = []
    for c, (b, par) in enumerate(cores):
        cat = lambda name, ax: np.concatenate([np.asarray(r1[2 * b][name]), np.asarray(r1[2 * b + 1][name])], axis=ax)
        own = own_blocks(par)
        toks = np.concatenate([np.arange(i * 128, (i + 1) * 128) for i in own])
        im = dict(kT=A(cat('kT', 1)), v=A(cat('v', 0)), ikT=A(cat('ikT', 1)),
                  qT=A(cat('qT', 1)[:, toks]), iqT=A(cat('iqT', 1)[:, toks]),
                  iw=A(cat('iw', 0)[toks]), sg=A(cat('sg', 0)[toks]))
        im.update(k2_consts(rb, par))
        ims.append(im)
    r2 = _run(nc, ims)
    del ims
    _stash('u0', np.asarray(r2[0]['u']))
    _stash('u1', np.asarray(r2[1]['u']))
    uT = []
    for b in range(4):
        ub = np.zeros((4096, 2048), ml_dtypes.bfloat16)
        for par in range(2):
            own = own_blocks(par)
            toks = np.concatenate([np.arange(i * 128, (i + 1) * 128) for i in own])
            ub[toks] = np.asarray(r2[2 * b + par]["u"])
        for hf in range(2):
            uT.append(A(ub[hf * NT:(hf + 1) * NT].T))
    del r1, r2
    nc = build_aout(NT)
    Wo = A(np.asarray(a_w_out[0], np.float32))
    r3 = _run(nc, [{"uT": uT[c], "xT": xT[c], "W": Wo} for c in range(NCORES)])
    x1T = [np.asarray(r3[c]["x1T"]) for c in range(NCORES)]
    _stash('x1T', x1T[:2])
    del r3, uT, xT
    nc = build_proj(NT, 10304, table_b(), outs_b(NT), F32)
    Wb = A(np.asarray(b_w_in[0], np.float32))
    r4 = _run(nc, [{"xT": x1T[c], "W": Wb, "nw": _pk(norm_w[1], KC)} for c in range(NCORES)])
    zT = [np.asarray(r4[c]["zT"]) for c in range(NCORES)]
    nc = build_k4(8)
    kc4 = k4_consts()
    cw = np.asarray(b_conv_w[0], np.float32)
    cbv = np.asarray(b_conv_b[0], np.float32)
    ims = []
    for c, (b, hf) in enumerate(cores):
        chans = np.concatenate([np.arange(hf * 2048, (hf + 1) * 2048), 4096 + np.arange(hf * 512, (hf + 1) * 512),
                                5120 + np.arange(hf * 512, (hf + 1) * 512)])
        xbc = np.concatenate([np.asarray(r4[2 * b]["xbcT"])[chans], np.asarray(r4[2 * b + 1]["xbcT"])[chans]], axis=1)
        dtT = np.concatenate([np.asarray(r4[2 * b]["dtT"]), np.asarray(r4[2 * b + 1]["dtT"])], axis=1)
        hs = slice(hf * 32, (hf + 1) * 32)
        im = dict(xbcT=A(xbc), dt=A(dtT[hs].T),
                  cw=A(cw[:, chans].T.reshape(NCT, 128, 4).transpose(1, 0, 2).reshape(128, NCT * 4)),
                  cb=A(cbv[chans].reshape(NCT, 128).T), dtb=_bc(np.asarray(b_dt_bias[0])[hs]),
                  alog=_bc(np.asarray(b_a_log[0])[hs]), Dv=_bc(np.asarray(b_d[0])[hs]))
        im.update(kc4)
        ims.append(im)
    del r4
    r5 = _run(nc, ims)
    del ims
    yT = []
    for b in range(4):
        yb = np.concatenate([np.asarray(r5[2 * b]["y"]), np.asarray(r5[2 * b + 1]["y"])], axis=1)
        for hf in range(2):
            yT.append(A(yb[hf * NT:(hf + 1) * NT].T))
    _stash('yT', yT[:2])
    del r5
    nc = build_bout(NT)
    Wbo = A(np.asarray(b_w_out[0], np.float32))
    r6 = _run(nc, [{"yT": yT[c], "zT": zT[c], "x1T": x1T[c], "bnw": _pk(b_norm_w[0], 32), "W": Wbo} for c in range(NCORES)])
    _stash('x2T', [np.asarray(r6[c]['x2T']) for c in range(2)])
    del yT, zT, x1T
    nc = build_fnorm(NT)
    r7 = _run(nc, [{"xT": np.asarray(r6[c]["x2T"]), "nw": _pk(final_norm_w, KC)} for c in range(NCORES)])
    out = np.zeros((4, 4096, 2048), np.float32)
    for c, (b, hf) in enumerate(cores):
        out[b, hf * NT:(hf + 1) * NT, :] = np.asarray(r7[c]["oT"]).T
    return out
```

```python
import contextlib
import numpy as np
import ml_dtypes
import concourse.bass as bass
import concourse.mybir as mybir
from concourse.bass_utils import run_bass_kernel_spmd

F32 = mybir.dt.float32
_PH = [0]
BF16 = mybir.dt.bfloat16
ALU = mybir.AluOpType
AF = mybir.ActivationFunctionType
AX = mybir.AxisListType

ENGS = ['pe', 'act', 'dve', 'pool', 'sp']


def _is_psum(r):
    n = r[0] if isinstance(r, tuple) else r
    return isinstance(n, str) and (n.startswith('ps') or n.startswith('acc'))


class Prog:
    def __init__(self, nc, gstack):
        self.nc = nc
        self.gstack = gstack
        self.sems = {}
        self.semval = {}
        self.base = {}
        self.dma_keys = set()
        self.total_instr = {e: 0 for e in ENGS}
        self._reset()

    def _reset(self):
        self.q = {e: [] for e in ENGS}
        self.waited = {e: {} for e in ENGS}
        self.res = {}
        self.touched = set()

    def op(self, eng, meth, kw, reads=(), writes=(), dma=None):
        fn = (meth, kw)
        waits = {}
        own = 'c_' + eng
        xr = [r for r in reads if _is_psum(r)]
        if xr:
            writes = list(writes) + [r for r in xr if r not in writes]

        def need(kv, raw):
            if kv is None:
                return
            k, v = kv
            if k == own and not raw:
                return
            if k == own and eng == 'pe':
                return
            if self.waited[eng].get(k, 0) >= v:
                return
            if waits.get(k, 0) < v:
                waits[k] = v

        for r in reads:
            st = self.res.get(r)
            if st:
                need(st['w'], True)
        for w in writes:
            st = self.res.get(w)
            if st:
                need(st['w'], False)
                for kv in st['r'].items():
                    need(kv, False)
        if dma:
            key, inc = dma, 16
            self.dma_keys.add(key)
        else:
            key, inc = own, 1
        self.semval[key] = self.semval.get(key, 0) + inc
        self.touched.add(key)
        val = self.semval[key]
        for k, v in waits.items():
            self.waited[eng][k] = v
        self.q[eng].append([fn, sorted(waits.items()), key, inc, val])
        for r in reads:
            st = self.res.setdefault(r, {'w': None, 'r': {}})
            st['r'][key] = val
        for w in writes:
            self.res[w] = {'w': (key, val), 'r': {}}
        return (key, val)

    def end_phase(self, pstack):
        nc = self.nc
        fin = [(k, self.semval[k]) for k in sorted(self.touched)]
        for e in ENGS:
            self.q[e].append([None, list(fin), None, 0, 0])
        waited_vals = {}
        for e in ENGS:
            for fn, waits, key, inc, val in self.q[e]:
                for k, v in waits:
                    waited_vals.setdefault(k, set()).add(v)
        remap = {}
        for k, vs in waited_vals.items():
            if k in self.dma_keys:
                continue
            b0 = self.base.get(k, 0)
            remap[k] = {v: b0 + i + 1 for i, v in enumerate(sorted(vs))}
        for k in self.touched:
            if k not in self.sems:
                self.sems[k] = self.gstack.enter_context(nc.semaphore("s_" + k))
        sems = self.sems
        block = pstack.enter_context(nc.Block())
        engmap = {'pe': block.tensor, 'act': block.scalar, 'dve': block.vector,
                  'pool': block.gpsimd, 'sp': block.sync}
        for e in ENGS:
            ops = self.q[e]
            self.total_instr[e] += len(ops)

            def body(engine, ops=ops):
                for fn, waits, key, inc, val in ops:
                    for k, v in waits:
                        if k in self.dma_keys:
                            engine.wait_ge(sems[k], v)
                        else:
                            engine.wait_ge(sems[k], remap[k][v])
                    if fn is None:
                        continue
                    ins = getattr(engine, fn[0])(**fn[1])
                    if key in self.dma_keys:
                        ins.then_inc(sems[key], 16)
                    elif key in remap and val in remap[key]:
                        ins.then_inc(sems[key], 1)
            engmap[e](body)
        for k, m in remap.items():
            self.base[k] = self.base.get(k, 0) + len(m)
        self._reset()


def MM(P, out, lhsT, rhs, start, stop, reads, writes, **kw):
    return P.op('pe', 'matmul', dict(out=out, lhsT=lhsT, rhs=rhs, start=start, stop=stop, **kw), reads, writes)


def ACTV(P, out, in_, func, reads, writes, **kw):
    return P.op('act', 'activation', dict(out=out, in_=in_, func=func, **kw), reads, writes)


def DMA(P, eng, out, in_, reads, writes, key):
    return P.op(eng, 'dma_start', dict(out=out, in_=in_), reads, writes, dma=key)


def TS(P, eng, out, in0, s1, s2, op0, op1, reads, writes, **kw):
    d = dict(out=out, in0=in0, scalar1=s1, scalar2=s2, op0=op0, **kw)
    if op1 is not None:
        d['op1'] = op1
    return P.op(eng, 'tensor_scalar', d, reads, writes)


def TT(P, eng, out, in0, in1, op, reads, writes):
    return P.op(eng, 'tensor_tensor', dict(out=out, in0=in0, in1=in1, op=op), reads, writes)


def STT(P, out, in0, scalar, in1, op0, op1, reads, writes, **kw):
    return P.op('dve', 'scalar_tensor_tensor', dict(out=out, in0=in0, scalar=scalar, in1=in1, op0=op0, op1=op1, **kw),
                reads, writes)


def CP(P, eng, out, in_, reads, writes):
    if eng == 'act':
        return P.op('act', 'activation', dict(out=out, in_=in_, func=AF.Copy), reads, writes)
    return P.op(eng, 'tensor_copy', dict(out=out, in_=in_), reads, writes)


D = 2048
KC = D // 128
EPS = 1e-6


S = 4096
NIT = 26
SCALE = 128 ** -0.5


def own_blocks(par):
    return [i for i in range(32) if (i % 4 in (0, 3)) == (par == 0)]


def k2_consts(rel_bias, par):
    n = np.arange(256)
    nf = np.maximum(n, 1).astype(np.float32)
    large = 16 + (np.log(nf / 16) / np.log(128 / 16) * 16).astype(np.int32)
    large = np.minimum(large, 31)
    bucket = np.where(n < 16, n, large)
    s = np.arange(128)[:, None]
    q = np.arange(128)[None, :]
    biasT = np.zeros((128, 16, 2, 128), np.float32)
    for d in range(2):
        dist = np.clip(d * 128 + q - s, 0, 255)
        biasT[:, :, d, :] = rel_bias[bucket[dist]].transpose(0, 2, 1)
    bfar = np.ascontiguousarray(np.broadcast_to(rel_bias[31][None, :], (128, 16))).astype(np.float32)
    negtri = np.where(np.arange(128)[None, :] <= np.arange(128)[:, None], 0.0, -1e30).astype(np.float32)
    ident = np.eye(128, dtype=np.float32).astype(ml_dtypes.bfloat16)
    dtab = lambda sp, w: (1 - w) if (sp == par) else (2 - w)
    cm = np.zeros((128, 2, 2, 128), np.float32)
    biasN = np.zeros((16, 128, 2, 3, 128), np.float32)
    for sp in range(2):
        for w in range(3):
            d = dtab(sp, w)
            if d >= 2:
                biasN[:, :, sp, w, :] = rel_bias[31][:, None, None]
            elif d >= 0:
                biasN[:, :, sp, w, :] = biasT[:, :, d, :].transpose(1, 0, 2)
            if w >= 1:
                if d == 0:
                    cm[:, sp, w - 1, :] = negtri
                elif d < 0:
                    cm[:, sp, w - 1, :] = -1e30
    return dict(bfar=bfar, ident=ident, cm=cm.reshape(128, 512), biasN=biasN.reshape(16, 128, 768))


def emit_k2(nc, P, st, par, dr, nslots=16, nheads=16, nit=NIT):
    own = own_blocks(par)
    rsel = (0, 3) if par == 0 else (1, 2)
    NQB = nslots
    nkbs = [2 * (qi + 1) for qi in range(nslots)]
    NQ = NQB * 128
    offs = []
    T = 0
    for nkb in nkbs:
        offs.append(T)
        T += nkb
    kT, v, ikT, qT, iqT, iw, sg, uT = dr['kT'], dr['v'], dr['ikT'], dr['qT'], dr['iqT'], dr['iw'], dr['sg'], dr['uT']
    biasNd, bfard, cmd, identd = dr['biasN'], dr['bfar'], dr['cm'], dr['ident']
    if True:
        sb = lambda name, shape, dt: st.enter_context(nc.sbuf_tensor(f"p{_PH[0]}_" + name, shape, dt))
        maskT = sb("maskT", [128, T, 128], BF16)
        score = sb("score", [128, S], F32)
        maskq = sb("maskq", [128, S], BF16)
        relu = [sb(f"relu{i}", [128, 512], F32) for i in range(2)]
        ikTs = sb("ikTs", [64, S], BF16)
        iqb = [sb(f"iqb{i}", [64, 16, 128], BF16) for i in range(2)]
        iwb = [sb(f"iwb{i}", [128, 16], F32) for i in range(2)]
        kTh = [sb(f"kTh{i}", [128, S], BF16) for i in range(2)]
        vh = [sb(f"vh{i}", [128, 32, 129], BF16) for i in range(2)]
        qTh = [sb(f"qTh{i}", [128, NQ], BF16) for i in range(2)]
        sgb = [sb(f"sgb{i}", [128, NQB, 128], BF16) for i in range(2)]
        biasN = [sb(f"biasN{i}", [128, 2, 3, 128], F32) for i in range(2)]
        bfar = sb("bfars", [128, 16], F32)
        cm = sb("cms", [128, 2, 256], F32)
        ident = sb("idents", [128, 128], BF16)
        hi = sb("hi", [128, 1], F32)
        lo = sb("lo", [128, 1], F32)
        w = sb("w", [128, 1], F32)
        mid = sb("mid", [128, 1], F32)
        cnt = sb("cnt", [128, 1], F32)
        ge = sb("ge", [128, 1], F32)
        rinv = sb("rinv", [128, 1], F32)
        NE = 4
        tb = [sb(f"tb{i}", [128, 128], F32) for i in range(2)]
        eb = [sb(f"eb{i}", [128, 128], BF16) for i in range(NE)]
        pt = [sb(f"pt{i}", [128, 128], BF16) for i in range(NE)]
        o32 = [sb(f"o32{i}", [128, 128], F32) for i in range(2)]
        ust = [sb(f"ust{i}", [128, 128], BF16) for i in range(4)]
        usT = [sb(f"usT{i}", [128, 128], BF16) for i in range(4)]
        psb = [st.enter_context(nc.psum_tensor(f"p{_PH[0]}_ps{i}", [128, 512], F32)) for i in range(4)]
        pst = [st.enter_context(nc.psum_tensor(f"p{_PH[0]}_pst{i}", [128, 1024], BF16)) for i in range(2)]
        acc = [st.enter_context(nc.psum_tensor(f"p{_PH[0]}_acc{i}", [128, 512], F32)) for i in range(2)]
        cn = {}

        def nxt(k, n):
            vv = cn.get(k, 0)
            cn[k] = vv + 1
            return vv % n
        DMA(P, 'sp', ikTs[:], ikT[:, :], [], ['ikT'], 'd_c0')
        DMA(P, 'sp', cm[:], cmd[par].rearrange("p (a b) -> p a b", a=2), [], ['cm'], 'd_c1')
        DMA(P, 'sp', ident[:], identd[:, :], [], ['ident'], 'd_c2')
        DMA(P, 'sp', bfar[:], bfard[:, :], [], ['bfar'], 'd_c3')
        for i in range(2):
            P.op('pool', 'memset', dict(ap=vh[i][:, :, 128:129], constant=1.0), [], [('vh1', i)])

        for qi in range(nslots):
            nkb = nkbs[qi]
            nk = nkb * 128
            s2 = qi % 2
            DMA(P, 'sp', iqb[s2][:], iqT[:, own[qi] * 128:(own[qi] + 1) * 128].rearrange("(h d) q -> d h q", d=64),
                [], [('iqb', s2)], f'd_iq{s2}')
            DMA(P, 'sp', iwb[s2][:], iw[own[qi] * 128:(own[qi] + 1) * 128, :], [], [('iwb', s2)], f'd_iw{s2}')
            nch = (nk + 511) // 512
            for c in range(nch):
                kw = min(512, nk - c * 512)
                for hh in range(16):
                    b = nxt('ps', 4)
                    MM(P, psb[b][:, :kw], iqb[s2][:, hh, :], ikTs[:, c * 512:c * 512 + kw], True, True,
                       [('iqb', s2), 'ikT'], [('ps', b)])
                    rb = nxt('relu', 2)
                    ACTV(P, relu[rb][:, :kw], psb[b][:, :kw], AF.Relu, [('ps', b)], [('relu', rb)])
                    sc = score[:, c * 512:c * 512 + kw]
                    if hh == 0:
                        TS(P, 'dve', sc, relu[rb][:, :kw], iwb[s2][:, 0:1], None, ALU.mult, None,
                           [('relu', rb), ('iwb', s2)], [('score', c)])
                    else:
                        STT(P, sc, relu[rb][:, :kw], iwb[s2][:, hh:hh + 1], sc, ALU.mult, ALU.add,
                            [('relu', rb), ('iwb', s2), ('score', c)], [('score', c)])
            scr = [('score', c) for c in range(nch)]
            P.op('dve', 'tensor_reduce', dict(out=hi[:], in_=score[:, :nk], axis=AX.X, op=ALU.max), scr, ['hi'])
            P.op('dve', 'tensor_reduce', dict(out=lo[:], in_=score[:, :nk], axis=AX.X, op=ALU.min), scr, ['lo'])
            TT(P, 'dve', w[:], hi[:], lo[:], ALU.subtract, ['hi', 'lo'], ['w'])
            dg = score[:, (nkb - 2) * 128:nkb * 128]
            TT(P, 'dve', dg, dg, cm[:, qi % 2, :], ALU.add, [('score', (nkb - 2) // 4), 'cm'], [('score', (nkb - 2) // 4)])
            for it in range(nit):
                TS(P, 'dve', w[:], w[:], 0.5, None, ALU.mult, None, ['w'], ['w'])
                TT(P, 'dve', mid[:], lo[:], w[:], ALU.add, ['lo', 'w'], ['mid'])
                TS(P, 'dve', maskq[:, :nk], score[:, :nk], mid[:, 0:1], 0.0, ALU.is_ge, ALU.add,
                   scr + ['mid'], ['maskq', 'cnt'], accum_out=cnt[:, 0:1])
                TS(P, 'dve', ge[:], cnt[:], 255.5, None, ALU.is_ge, None, ['cnt'], ['ge'])
                STT(P, lo[:], ge[:], w[:, 0:1], lo[:], ALU.mult, ALU.add, ['ge', 'w', 'lo'], ['lo'])
            TS(P, 'dve', maskq[:, :nk], score[:, :nk], lo[:, 0:1], None, ALU.is_ge, None, scr + ['lo'], ['maskq'])
            for j in range(nkb):
                b = nxt('pst', 2)
                P.op('pe', 'transpose', dict(out=pst[b][:, :128], in_=maskq[:, j * 128:(j + 1) * 128], identity=ident[:]),
                     ['maskq', 'ident'], [('pst', b)])
                CP(P, 'act', maskT[:, offs[qi] + j, :], pst[b][:, :128], [('pst', b)], [('maskT', qi)])

        for h in range(nheads):
            s2 = h % 2
            DMA(P, 'sp', kTh[s2][:], kT[h * 128:(h + 1) * 128, :], [], [('kTh', s2)], f'd_k{s2}')
            DMA(P, 'sp', vh[s2][:, :, 0:128], v[:, h * 128:(h + 1) * 128].rearrange("(j p) d -> p j d", p=128),
                [], [('vh', s2)], f'd_v{s2}')
            for k in range(2):
                DMA(P, 'sp', qTh[s2][:].rearrange("p (m k t) -> p m k t", k=2, t=128)[:, :, k, :],
                    qT[h * 128:(h + 1) * 128, :].rearrange("p (m r t) -> p m r t", r=4, t=128)[:, :, rsel[k], :],
                    [], [('qTh', s2)], f'd_q{s2}')
            for k in range(2):
                DMA(P, 'sp', sgb[s2][:].rearrange("p (m k) d -> p m k d", k=2)[:, :, k, :],
                    sg[:, h * 128:(h + 1) * 128].rearrange("(m r p) d -> p m r d", r=4, p=128)[:, :, rsel[k], :],
                    [], [('sgb', s2)], f'd_sg{s2}')
            DMA(P, 'sp', biasN[s2][:], biasNd[par, h].rearrange("p (a w q) -> p a w q", a=2, w=3), [], [('biasN', s2)], f'd_bn{s2}')
            for qi in range(nslots):
                nkb = nkbs[qi]
                ab = nxt('acc', 2)
                for j in range(nkb):
                    b = nxt('ps', 4)
                    MM(P, psb[b][:, :128], kTh[s2][:, j * 128:(j + 1) * 128], qTh[s2][:, qi * 128:(qi + 1) * 128],
                       True, True, [('kTh', s2), ('qTh', s2)], [('ps', b)])
                    wv = j - (nkb - 3)
                    e = nxt('eb', NE)
                    if wv < 0:
                        ACTV(P, eb[e][:], psb[b][:, :128], AF.Exp, [('ps', b), 'bfar'], [('eb', e)],
                             scale=SCALE, bias=bfar[:, h:h + 1])
                    else:
                        t = nxt('tb', 2)
                        STT(P, tb[t][:], psb[b][:, :128], SCALE, biasN[s2][:, qi % 2, wv, :], ALU.mult, ALU.add,
                            [('ps', b), ('biasN', s2)], [('tb', t)])
                        ACTV(P, eb[e][:], tb[t][:], AF.Exp, [('tb', t)], [('eb', e)])
                    p = nxt('pt', NE)
                    me = 'dve' if nxt('me', 2) == 0 else 'pool'
                    TT(P, me, pt[p][:], eb[e][:], maskT[:, offs[qi] + j, :], ALU.mult,
                       [('eb', e), ('maskT', qi)], [('pt', p)])
                    MM(P, acc[ab][:, :129], pt[p][:], vh[s2][:, j, :], j == 0, j == nkb - 1,
                       [('pt', p), ('vh', s2), ('vh1', s2)], [('acc', ab)])
                P.op('dve', 'reciprocal', dict(out=rinv[:], in_=acc[ab][:, 128:129]), [('acc', ab)], ['rinv'])
                o = nxt('o32', 2)
                TS(P, 'dve', o32[o][:], acc[ab][:, :128], rinv[:, 0:1], None, ALU.mult, None,
                   [('acc', ab), 'rinv'], [('o32', o)])
                us = nxt('ust', 4)
                TT(P, 'pool', ust[us][:], o32[o][:], sgb[s2][:, qi, :], ALU.mult, [('o32', o), ('sgb', s2)], [('ust', us)])
                tp = nxt('pst', 2)
                P.op('pe', 'transpose', dict(out=pst[tp][:, :128], in_=ust[us][:], identity=ident[:]),
                     [('ust', us), 'ident'], [('pst', tp)])
                CP(P, 'act', usT[us][:], pst[tp][:, :128], [('pst', tp)], [('usT', us)])
                DMA(P, 'sp', uT[h * 128:(h + 1) * 128, own[qi] * 128:(own[qi] + 1) * 128], usT[us][:], [('usT', us)], [], f'd_u{us}')


NH = 32
NG = 4
NCT = 24


def k4_consts():
    U = np.triu(np.ones((128, 128), np.float32))
    sel = np.zeros((32, 32, 128), np.float32)
    for h in range(32):
        sel[h, h, :] = 1.0
    return dict(U=U, ones128=np.ones((128, 128), np.float32), sel=sel.reshape(32, 32 * 128),
                tri=U.copy(), identf=np.eye(128, dtype=np.float32),
                identb=np.eye(128, dtype=np.float32).astype(ml_dtypes.bfloat16))


class _Stop(Exception):
    pass


def emit_k4(nc, P, st, hf, dr, npieces=8, stop=99):
    SS = npieces * 512
    xbcT, dtd, yT = dr['xbcT'], dr['dt'], dr['yT']
    cwd, cbd, dtbd, alogd, Dd = dr['cw'][hf], dr['cb'][hf], dr['dtb'][hf], dr['alog'][hf], dr['Dv'][hf]
    Ud, onesd, seld, trid, identfd, identbd = dr['U'], dr['ones128'], dr['sel'], dr['tri'], dr['identf'], dr['ident']
    if True:
        sb = lambda name, shape, dt: st.enter_context(nc.sbuf_tensor(f"p{_PH[0]}_" + name, shape, dt))
        xin = sb("xin", [128, 12, 515], F32)
        xc = sb("xc", [128, 16, 512], F32)
        BTc = sb("BTc", [128, 4, 512], BF16)
        CTc = sb("CTc", [128, 4, 512], BF16)
        cacc = [sb(f"cacc{i}", [128, 512], F32) for i in range(2)]
        cw = sb("cws", [128, NCT, 4], F32)
        cb = sb("cbs", [128, NCT], F32)
        dtb = sb("dtbs", [128, NH], F32)
        Aneg = sb("Aneg", [128, NH], F32)
        Dv = sb("Dvs", [128, NH], F32)
        U = sb("Us", [128, 128], F32)
        ones128 = sb("ones128s", [128, 128], F32)
        sel = sb("sels", [32, 32, 128], F32)
        tri = sb("tris", [128, 128], F32)
        identf = sb("identfs", [128, 128], F32)
        identb = sb("identbs", [128, 128], BF16)
        dtr = sb("dtr", [128, 4, NH], F32)
        dtp = sb("dtp", [128, 4, NH], F32)
        adt = sb("adt", [128, 4, NH], F32)
        acs = sb("acs", [128, NH], F32)
        ea = sb("ea", [128, NH], F32)
        dec = sb("dec", [128, NH], F32)
        cdec = sb("cdec", [128, NH], F32)
        dtdec = sb("dtdec", [128, NH], F32)
        acsT = sb("acsT", [32, 128], F32)
        x_tm = sb("x_tm", [128, NH, 64], F32)
        xdt = sb("xdt", [128, NH, 64], BF16)
        xdtd = sb("xdtd", [128, NH, 64], BF16)
        B_tm = sb("B_tm", [128, 4, 128], BF16)
        prev32 = sb("prev32", [128, NG, 512], F32)
        prevbf = sb("prevbf", [128, NG, 512], BF16)
        cbTm = [sb(f"cbTm{i}", [128, 128], F32) for i in range(2)]
        tcl = [sb(f"tcl{i}", [128, 4, 128], F32) for i in range(2)]
        Lt = [sb(f"Lt{i}", [128, 4, 128], F32) for i in range(2)]
        MT = [sb(f"MT{i}", [128, 4, 128], BF16) for i in range(2)]
        yos = [sb(f"yos{i}", [128, 8, 64], F32) for i in range(2)]
        dsk = [sb(f"dsk{i}", [128, 8, 64], F32) for i in range(2)]
        ystage = [sb(f"ystage{i}", [128, NH, 64], F32) for i in range(2)]
        yTs = [sb(f"yTs{i}", [128, 16, 128], F32) for i in range(2)]
        ps = [st.enter_context(nc.psum_tensor(f"p{_PH[0]}_ps{i}", [128, 512], F32)) for i in range(3)]
        psmisc = st.enter_context(nc.psum_tensor(f"p{_PH[0]}_psmisc", [128, 512], F32))
        psbt = st.enter_context(nc.psum_tensor(f"p{_PH[0]}_psbt", [128, 1024], BF16))
        psbc = [st.enter_context(nc.psum_tensor(f"p{_PH[0]}_psbc{i}", [128, 512], F32)) for i in range(2)]
        psyd = st.enter_context(nc.psum_tensor(f"p{_PH[0]}_psyd", [128, 512], F32))
        cn = {}

        def nxt(k, n):
            vv = cn.get(k, 0)
            cn[k] = vv + 1
            return vv % n
        for name, t, d in [('cw', cw, cwd.rearrange("p (c k) -> p c k", k=4)), ('cb', cb, cbd),
                           ('dtb', dtb, dtbd), ('Dv', Dv, Dd), ('U', U, Ud[:, :]),
                           ('ones128', ones128, onesd[:, :]), ('sel', sel, seld[:, :].rearrange("p (h m) -> p h m", m=128)),
                           ('tri', tri, trid[:, :]), ('identf', identf, identfd[:, :]), ('identb', identb, identbd[:, :]),
                           ('Aneg', Aneg, alogd)]:
            DMA(P, 'sp', t[:], d, [], [name], 'd_' + name)
        ACTV(P, Aneg[:], Aneg[:], AF.Exp, ['Aneg'], ['Aneg'])
        TS(P, 'dve', Aneg[:], Aneg[:], -1.0, None, ALU.mult, None, ['Aneg'], ['Aneg'])
        P.op('dve', 'memset', dict(ap=prev32[:], constant=0.0), [], [('prev32', g) for g in range(NG)])
        P.op('dve', 'memset', dict(ap=prevbf[:], constant=0.0), [], [('prevbf', g) for g in range(NG)])

        def chk(n):
            if stop <= n:
                raise _Stop()
        try:
          for pc in range(npieces):
              t0 = pc * 512
              for half in range(2):
                  if half == 0:
                      srcs = [(0, 12, hf * 2048)]
                  else:
                      srcs = [(0, 4, hf * 2048 + 1536), (4, 4, 4096 + hf * 512), (8, 4, 5120 + hf * 512)]
                  if pc == 0:
                      P.op('dve', 'memset', dict(ap=xin[:, :, 0:3], constant=0.0), [], ['xin'])
                  for (c0_, n_, r0_) in srcs:
                      src = xbcT[r0_:r0_ + n_ * 128, :]
                      if pc == 0:
                          DMA(P, 'sp', xin[:, c0_:c0_ + n_, 3:515], src[:, 0:512].rearrange("(c p) t -> p c t", p=128), [], ['xin'], 'd_xin')
                      else:
                          DMA(P, 'sp', xin[:, c0_:c0_ + n_, :], src[:, t0 - 3:t0 + 512].rearrange("(c p) t -> p c t", p=128), [], ['xin'], 'd_xin')
                  for cl in range(12):
                      ct = half * 12 + cl
                      a = nxt('cacc', 2)
                      TS(P, 'dve', cacc[a][:], xin[:, cl, 3:515], cw[:, ct, 3:4], None, ALU.mult, None,
                         ['xin', 'cw'], [('cacc', a)])
                      for k in (2, 1, 0):
                          STT(P, cacc[a][:], xin[:, cl, k:k + 512], cw[:, ct, k:k + 1], cacc[a][:], ALU.mult, ALU.add,
                              ['xin', 'cw', ('cacc', a)], [('cacc', a)])
                      if ct < 16:
                          dst, dr = xc[:, ct, :], ('xc', ct)
                      elif ct < 20:
                          dst, dr = BTc[:, ct - 16, :], ('BTc', ct - 16)
                      else:
                          dst, dr = CTc[:, ct - 20, :], ('CTc', ct - 20)
                      ACTV(P, dst, cacc[a][:], AF.Silu, [('cacc', a), 'cb'], [dr], bias=cb[:, ct:ct + 1])
              chk(1)
              DMA(P, 'sp', dtr[:], dtd[t0:t0 + 512, hf * 32:(hf + 1) * 32].rearrange("(c p) h -> p c h", p=128), [], ['dtr'], 'd_dtr')
              TT(P, 'dve', dtr[:], dtr[:], dtb[:].unsqueeze(1).to_broadcast([128, 4, NH]), ALU.add, ['dtr', 'dtb'], ['dtr'])
              ACTV(P, dtp[:], dtr[:], AF.Exp, ['dtr'], ['dtp'])
              ACTV(P, dtp[:], dtp[:], AF.Ln, ['dtp', 'ones128'], ['dtp'], bias=ones128[:, 0:1])
              TT(P, 'dve', adt[:], dtp[:], Aneg[:].unsqueeze(1).to_broadcast([128, 4, NH]), ALU.mult, ['dtp', 'Aneg'], ['adt'])
              chk(2)
              for c in range(4):
                  l0 = c * 128
                  MM(P, psmisc[:, 0:NH], U[:], adt[:, c, :], True, True, ['U', 'adt'], ['psmisc'])
                  MM(P, psmisc[:, 32:32 + NH], ones128[:], adt[:, c, :], True, True, ['ones128', 'adt'], ['psmisc'])
                  MM(P, psmisc[0:32, 64:192], adt[:, c, :], U[:], True, True, ['U', 'adt'], ['psmisc'])
                  CP(P, 'dve', acs[:], psmisc[:, 0:NH], ['psmisc'], ['acs'])
                  ACTV(P, ea[:], psmisc[:, 0:NH], AF.Exp, ['psmisc'], ['ea'])
                  ACTV(P, cdec[:], psmisc[:, 32:32 + NH], AF.Exp, ['psmisc'], ['cdec'])
                  TT(P, 'dve', dec[:], psmisc[:, 32:32 + NH], acs[:], ALU.subtract, ['psmisc', 'acs'], ['dec'])
                  ACTV(P, dec[:], dec[:], AF.Exp, ['dec'], ['dec'])
                  CP(P, 'dve', acsT[:], psmisc[0:32, 64:192], ['psmisc'], ['acsT'])
                  TT(P, 'dve', dtdec[:], dtp[:, c, :], dec[:], ALU.mult, ['dtp', 'dec'], ['dtdec'])
                  chk(3)
                  for q4 in range(4):
                      b = nxt('ps', 3)
                      for k in range(4):
                          ct = q4 * 4 + k
                          P.op('pe', 'transpose', dict(out=ps[b][:, k * 128:(k + 1) * 128], in_=xc[:, ct, l0:l0 + 128],
                                                       identity=identf[:]), [('xc', ct), 'identf'], [('ps', b)])
                      CP(P, 'act', x_tm[:, q4 * 8:(q4 + 1) * 8, :].rearrange("p h d -> p (h d)"), ps[b][:, :],
                         [('ps', b)], [('x_tm', q4)])
                  xr = [('x_tm', q) for q in range(4)]
                  TT(P, 'dve', xdt[:], x_tm[:], dtp[:, c, :].unsqueeze(2).to_broadcast([128, NH, 64]), ALU.mult,
                     xr + ['dtp'], ['xdt'])
                  TT(P, 'pool', xdtd[:], x_tm[:], dtdec[:].unsqueeze(2).to_broadcast([128, NH, 64]), ALU.mult,
                     xr + ['dtdec'], ['xdtd'])
                  for g in range(NG):
                      P.op('pe', 'transpose', dict(out=psbt[:, g * 128:(g + 1) * 128], in_=BTc[:, g, l0:l0 + 128],
                                                   identity=identb[:]), [('BTc', g), 'identb'], ['psbt'])
                  CP(P, 'act', B_tm[:].rearrange("p g n -> p (g n)"), psbt[:, 0:512], ['psbt'], ['B_tm'])
                  chk(4)
                  ys = nxt('ystage', 2)
                  for g in range(NG):
                      b = nxt('ps', 3)
                      MM(P, ps[b][:, 0:128], BTc[:, g, l0:l0 + 128], CTc[:, g, l0:l0 + 128], True, True,
                         [('BTc', g), ('CTc', g)], [('ps', b)])
                      cm = nxt('cbTm', 2)
                      TT(P, 'dve', cbTm[cm][:], ps[b][:, 0:128], tri[:], ALU.mult, [('ps', b), 'tri'], [('cbTm', cm)])
                      for hq in range(2):
                          bb = nxt('psbc', 2)
                          tc_ = nxt('tcl', 2)
                          for hh in range(4):
                              h = g * 8 + hq * 4 + hh
                              MM(P, psbc[bb][:, hh * 128:(hh + 1) * 128], sel[:, h, :], acsT[:], True, True,
                                 ['sel', 'acsT'], [('psbc', bb)])
                              TS(P, 'dve', tcl[tc_][:, hh, :], psbc[bb][:, hh * 128:(hh + 1) * 128], acs[:, h:h + 1], 0.0,
                                 ALU.subtract, ALU.min, [('psbc', bb), 'acs'], [('tcl', tc_)])
                          ACTV(P, Lt[tc_][:], tcl[tc_][:], AF.Exp, [('tcl', tc_)], [('Lt', tc_)])
                          TT(P, 'pool', MT[tc_][:], Lt[tc_][:], cbTm[cm][:].unsqueeze(1).to_broadcast([128, 4, 128]), ALU.mult,
                             [('Lt', tc_), ('cbTm', cm)], [('MT', tc_)])
                          for hh in range(4):
                              h = g * 8 + hq * 4 + hh
                              MM(P, psyd[:, (hq * 4 + hh) * 64:(hq * 4 + hh + 1) * 64], MT[tc_][:, hh, :], xdt[:, h, :],
                                 True, True, [('MT', tc_), 'xdt'], ['psyd'])
                      chk(5)
                      b = nxt('ps', 3)
                      MM(P, ps[b][:, :], CTc[:, g, l0:l0 + 128], prevbf[:, g, :], True, True,
                         [('CTc', g), ('prevbf', g)], [('ps', b)])
                      yo = nxt('yos', 2)
                      TT(P, 'dve', yos[yo][:], ps[b][:, :].rearrange("p (h d) -> p h d", d=64),
                         ea[:, g * 8:(g + 1) * 8].unsqueeze(2).to_broadcast([128, 8, 64]), ALU.mult,
                         [('ps', b), 'ea'], [('yos', yo)])
                      TT(P, 'pool', dsk[yo][:], x_tm[:, g * 8:(g + 1) * 8, :],
                         Dv[:, g * 8:(g + 1) * 8].unsqueeze(2).to_broadcast([128, 8, 64]), ALU.mult,
                         [('x_tm', g), 'Dv'], [('dsk', yo)])
                      TT(P, 'pool', dsk[yo][:], dsk[yo][:], yos[yo][:], ALU.add, [('dsk', yo), ('yos', yo)], [('dsk', yo)])
                      TT(P, 'dve', ystage[ys][:, g * 8:(g + 1) * 8, :], psyd[:, :].rearrange("p (h d) -> p h d", d=64),
                         dsk[yo][:], ALU.add, ['psyd', ('dsk', yo)], [('ystage', ys)])
                      chk(6)
                      b = nxt('ps', 3)
                      MM(P, ps[b][:, :], B_tm[:, g, :], xdtd[:, g * 8:(g + 1) * 8, :].rearrange("p h d -> p (h d)"), True, True,
                         ['B_tm', 'xdtd'], [('ps', b)])
                      TT(P, 'pool', prev32[:, g, :].rearrange("p (h d) -> p h d", d=64),
                         prev32[:, g, :].rearrange("p (h d) -> p h d", d=64),
                         cdec[:, g * 8:(g + 1) * 8].unsqueeze(2).to_broadcast([128, 8, 64]), ALU.mult,
                         [('prev32', g), 'cdec'], [('prev32', g)])
                      TT(P, 'dve', prev32[:, g, :], prev32[:, g, :], ps[b][:, :], ALU.add, [('prev32', g), ('ps', b)],
                         [('prev32', g)])
                      CP(P, 'act', prevbf[:, g, :], prev32[:, g, :], [('prev32', g)], [('prevbf', g)])
                  yt = nxt('yTs', 2)
                  for q4 in range(4):
                      b = nxt('ps', 3)
                      for k in range(4):
                          ct = q4 * 4 + k
                          P.op('pe', 'transpose', dict(out=ps[b][:, k * 128:(k + 1) * 128],
                                                       in_=ystage[ys][:, 2 * ct:2 * ct + 2, :].rearrange("p h d -> p (h d)"),
                                                       identity=identf[:]), [('ystage', ys), 'identf'], [('ps', b)])
                      CP(P, 'act' if q4 % 2 == 0 else 'dve', yTs[yt][:, q4 * 4:(q4 + 1) * 4, :].rearrange("p c t -> p (c t)"),
                         ps[b][:, :], [('ps', b)], [('yTs', yt)])
                  DMA(P, 'sp', yT[hf * 2048:(hf + 1) * 2048, t0 + l0:t0 + l0 + 128].rearrange("(c p) t -> p c t", p=128),
                      yTs[yt][:], [('yTs', yt)], [], f'd_y{yt}')
        except _Stop:
            pass


def rmsnorm_stats(P, xt, ones, psk, ps, rstd, sq, xres, nfeat, KCn, eps=EPS):
    for kc in range(KCn):
        ACTV(P, sq[kc % 2][:], xt[:, kc, :], AF.Square, [xres], [('sq', kc % 2)])
        MM(P, ps[:], ones[:], sq[kc % 2][:], kc == 0, kc == KCn - 1, [('sq', kc % 2), 'ones'], [psk])
    TS(P, 'dve', rstd, ps[:], 1.0 / nfeat, eps, ALU.mult, ALU.add, [psk], ['rstd'])
    ACTV(P, rstd, rstd, AF.Sqrt, ['rstd'], ['rstd'])
    P.op('dve', 'reciprocal', dict(out=rstd, in_=rstd), ['rstd'], ['rstd'])


def emit_proj(nc, P, st, xT, W, nwd, table, od, t0, NT, stg_dt):
    NTG = NT // 512
    NTT = NT // 128
    sb = lambda name, shape, dt: st.enter_context(nc.sbuf_tensor(f"p{_PH[0]}_" + name, shape, dt))
    ones = sb("ones", [128, 128], F32)
    nw = sb("nw_sb", [128, KC], F32)
    xt = [sb(f"xt{i}", [128, KC, 512], F32) for i in range(2)]
    sq = [sb(f"sq{i}", [128, 512], F32) for i in range(2)]
    rstd = sb("rstd", [128, 512], F32)
    hT = sb("hT", [128, KC, NT], BF16)
    NW = 3
    wbf = [sb(f"wbf{i}", [128, KC, 512], BF16) for i in range(NW)]
    NS = 4
    stg = [sb(f"stg{i}", [128, 512], stg_dt) for i in range(NS)]
    stgf = sb("stgf", [128, 16], F32)
    psb = [st.enter_context(nc.psum_tensor(f"p{_PH[0]}_ps{i}", [128, 512], F32)) for i in range(8)]
    P.op('dve', 'memset', dict(ap=ones[:], constant=1.0), [], ['ones'])
    DMA(P, 'sp', nw[:], nwd[:, :], [], ['nw'], 'd_nw')
    wt = table

    def load_w(i):
        c0, ncol, kind, dst, d0 = wt[i]
        s = i % NW
        DMA(P, 'pool', wbf[s][:, :, :ncol], W[:, c0:c0 + ncol].rearrange("(kc p) c -> p kc c", p=128),
            [], [('wbf', s)], f'd_w{s}')
    load_w(0)
    load_w(1)
    for tg in range(NTG):
        xr = ('xt', tg % 2)
        DMA(P, 'sp', xt[tg % 2][:], xT[:, t0 + tg * 512:t0 + (tg + 1) * 512].rearrange("(kc p) t -> p kc t", p=128),
            [], [xr], f'd_x{tg % 2}')
        rmsnorm_stats(P, xt[tg % 2], ones, ('ps', 7), psb[7], rstd[:], sq, xr, D, KC)
        for kc in range(KC):
            STT(P, hT[:, kc, tg * 512:(tg + 1) * 512], xt[tg % 2][:, kc, :], nw[:, kc:kc + 1], rstd[:],
                ALU.mult, ALU.mult, [xr, 'rstd', 'nw'], [('hT', kc, tg)])
    cnt = {'ps': 0, 'stg': 0, 'ev': 0}

    def nxt(k, n):
        v = cnt[k] % n
        cnt[k] += 1
        return v
    for i in range(len(wt)):
        if i + 2 < len(wt):
            load_w(i + 2)
        c0, ncol, kind, dstn, d0 = wt[i]
        dst = od[dstn]
        s = i % NW
        if kind == 'fm':
            for cb in range((ncol + 127) // 128):
                m = min(128, ncol - cb * 128)
                for tg in range(NTG):
                    b = nxt('ps', 6)
                    for kc in range(KC):
                        MM(P, psb[b][:m, :], wbf[s][:, kc, cb * 128:cb * 128 + m], hT[:, kc, tg * 512:(tg + 1) * 512],
                           kc == 0, kc == KC - 1, [('wbf', s), ('hT', kc, tg)], [('ps', b)])
                    ss = nxt('stg', NS)
                    ev = 'act' if nxt('ev', 2) == 0 else 'dve'
                    CP(P, ev, stg[ss][:m, :], psb[b][:m, :], [('ps', b)], [('stg', ss)])
                    r0 = d0 + cb * 128
                    DMA(P, 'sp', dst[r0:r0 + m, t0 + tg * 512:t0 + (tg + 1) * 512], stg[ss][:m, :], [('stg', ss)], [], f'd_o{ss}')
        else:
            for tt in range(NTT):
                b = nxt('ps', 6)
                for kc in range(KC):
                    MM(P, psb[b][:, :ncol], hT[:, kc, tt * 128:(tt + 1) * 128], wbf[s][:, kc, :ncol],
                       kc == 0, kc == KC - 1, [('wbf', s), ('hT', kc, tt // 4)], [('ps', b)])
                r0 = t0 + tt * 128
                if kind == 'tmw':
                    TS(P, 'dve', stgf[:, :], psb[b][:, :16], 1.0 / 32.0, None, ALU.mult, None, [('ps', b)], ['stgf'])
                    DMA(P, 'sp', dst[r0:r0 + 128, :], stgf[:, :], ['stgf'], [], 'd_of')
                    continue
                ss = nxt('stg', NS)
                if kind == 'tmg':
                    ACTV(P, stg[ss][:, :ncol], psb[b][:, :ncol], AF.Silu, [('ps', b)], [('stg', ss)])
                else:
                    CP(P, 'dve', stg[ss][:, :ncol], psb[b][:, :ncol], [('ps', b)], [('stg', ss)])
                DMA(P, 'sp', dst[r0:r0 + 128, d0:d0 + ncol], stg[ss][:, :ncol], [('stg', ss)], [], f'd_o{ss}')


def table_a():
    wt = []
    for i in range(8):
        wt.append((i * 512, 512, 'fm', 'qT' if i < 4 else 'kT', (i % 4) * 512))
    for i in range(2):
        wt.append((8192 + i * 512, 512, 'fm', 'iqT', i * 512))
    wt.append((9216, 64, 'fm', 'ikT', 0))
    for i in range(4):
        wt.append((4096 + i * 512, 512, 'tm', 'v', i * 512))
    for i in range(4):
        wt.append((6144 + i * 512, 512, 'tmg', 'sg', i * 512))
    wt.append((9280, 16, 'tmw', 'iw', 0))
    return wt


def table_b():
    wt = []
    for i in range(8):
        wt.append((i * 512, 512, 'fm', 'zT', i * 512))
    for i in range(12):
        wt.append((4096 + i * 512, 512, 'fm', 'xbcT', i * 512))
    wt.append((10240, 64, 'tm', 'dt', 0))
    return wt


def emit_aout(nc, P, st, uT, xT, W, x1T, t0, NT):
    NTG = NT // 512
    sb = lambda name, shape, dt: st.enter_context(nc.sbuf_tensor(f"p{_PH[0]}_" + name, shape, dt))
    aT = sb("aT", [128, KC, NT], BF16)
    wbf = [sb(f"wbf{i}", [128, KC, 512], BF16) for i in range(2)]
    rs = [sb(f"rs{i}", [128, 512], F32) for i in range(4)]
    psb = [st.enter_context(nc.psum_tensor(f"p{_PH[0]}_ps{i}", [128, 512], F32)) for i in range(6)]
    cnt = {}

    def nxt(k, n):
        v = cnt.get(k, 0)
        cnt[k] = v + 1
        return v % n
    DMA(P, 'sp', aT[:], uT[:, t0:t0 + NT].rearrange("(kc p) t -> p kc t", p=128), [], ['aT'], 'd_a')

    def load_w(i):
        DMA(P, 'pool', wbf[i % 2][:], W[:, i * 512:(i + 1) * 512].rearrange("(kc p) c -> p kc c", p=128),
            [], [('wbf', i % 2)], f'd_w{i % 2}')
    load_w(0)
    for i in range(4):
        if i + 1 < 4:
            load_w(i + 1)
        for cb in range(4):
            ob = i * 4 + cb
            for tg in range(NTG):
                c0 = t0 + tg * 512
                r = nxt('rs', 4)
                DMA(P, 'sp', rs[r][:], xT[ob * 128:(ob + 1) * 128, c0:c0 + 512], [], [('rs', r)], f'd_r{r}')
                b = nxt('ps', 6)
                for kc in range(KC):
                    MM(P, psb[b][:], wbf[i % 2][:, kc, cb * 128:(cb + 1) * 128], aT[:, kc, tg * 512:(tg + 1) * 512],
                       kc == 0, kc == KC - 1, [('wbf', i % 2), 'aT'], [('ps', b)])
                TT(P, 'dve', rs[r][:], psb[b][:], rs[r][:], ALU.add, [('ps', b), ('rs', r)], [('rs', r)])
                DMA(P, 'sp', x1T[ob * 128:(ob + 1) * 128, c0:c0 + 512], rs[r][:], [('rs', r)], [], f'd_o{r}')


def emit_bout(nc, P, st, yT, zT, x1T, bnwd, W, x2T, p0, TP=1024):
    KI = 32
    NTG = TP // 512
    sb = lambda name, shape, dt: st.enter_context(nc.sbuf_tensor(f"p{_PH[0]}_" + name, shape, dt))
    ones = sb("ones", [128, 128], F32)
    bnw = sb("bnws", [128, KI], F32)
    gyb = sb("gyb", [128, KI, TP], BF16)
    rstd = sb("rstd", [128, TP], F32)
    yb = [sb(f"yb{i}", [128, 4, 512], F32) for i in range(2)]
    zb = [sb(f"zb{i}", [128, 4, 512], F32) for i in range(2)]
    sq = [sb(f"sq{i}", [128, 512], F32) for i in range(2)]
    wbf = [sb(f"wbf{i}", [128, KI, 256], BF16) for i in range(3)]
    rs = [sb(f"rs{i}", [128, 512], F32) for i in range(4)]
    t32 = [sb(f"t32{i}", [128, 512], F32) for i in range(2)]
    psb = [st.enter_context(nc.psum_tensor(f"p{_PH[0]}_ps{i}", [128, 512], F32)) for i in range(8)]
    cnt = {}

    def nxt(k, n):
        v = cnt.get(k, 0)
        cnt[k] = v + 1
        return v % n
    P.op('dve', 'memset', dict(ap=ones[:], constant=1.0), [], ['ones'])
    DMA(P, 'sp', bnw[:], bnwd[:, :], [], ['bnw'], 'd_bnw')
    wi = [0]

    def load_w(ci):
        s = wi[0] % 3
        wi[0] += 1
        DMA(P, 'pool', wbf[s][:], W[:, ci * 256:(ci + 1) * 256].rearrange("(kc p) c -> p kc c", p=128),
            [], [('wbf', s)], f'd_w{s}')
        return s
    slots = {0: load_w(0), 1: load_w(1)}
    for tg in range(NTG):
        c0 = p0 + tg * 512
        for k4 in range(KI // 4):
            s = nxt('yz', 2)
            DMA(P, 'sp', yb[s][:], yT[k4 * 512:(k4 + 1) * 512, c0:c0 + 512].rearrange("(k p) t -> p k t", p=128),
                [], [('yb', s)], f'd_y{s}')
            DMA(P, 'sp', zb[s][:], zT[k4 * 512:(k4 + 1) * 512, c0:c0 + 512].rearrange("(k p) t -> p k t", p=128),
                [], [('zb', s)], f'd_z{s}')
            ACTV(P, zb[s][:], zb[s][:], AF.Silu, [('zb', s)], [('zb', s)])
            TT(P, 'dve', yb[s][:], yb[s][:], zb[s][:], ALU.mult, [('yb', s), ('zb', s)], [('yb', s)])
            for k in range(4):
                kc = k4 * 4 + k
                q = nxt('sq', 2)
                ACTV(P, sq[q][:], yb[s][:, k, :], AF.Square, [('yb', s)], [('sq', q)])
                MM(P, psb[7][:], ones[:], sq[q][:], kc == 0, kc == KI - 1, [('sq', q), 'ones'], [('ps', 7)])
                TS(P, 'pool', gyb[:, kc, tg * 512:(tg + 1) * 512], yb[s][:, k, :], bnw[:, kc:kc + 1], None,
                   ALU.mult, None, [('yb', s), 'bnw'], [('gyb', kc, tg)])
        rv = rstd[:, tg * 512:(tg + 1) * 512]
        TS(P, 'dve', rv, psb[7][:], 1.0 / 4096.0, EPS, ALU.mult, ALU.add, [('ps', 7)], [('rstd', tg)])
        ACTV(P, rv, rv, AF.Sqrt, [('rstd', tg)], [('rstd', tg)])
        P.op('dve', 'reciprocal', dict(out=rv, in_=rv), [('rstd', tg)], [('rstd', tg)])
    for ci in range(8):
        if ci + 2 < 8:
            slots[ci + 2] = load_w(ci + 2)
        s = slots[ci]
        for cb in range(2):
            ob = ci * 2 + cb
            for tg in range(NTG):
                c0 = p0 + tg * 512
                r = nxt('rs', 4)
                DMA(P, 'sp', rs[r][:], x1T[ob * 128:(ob + 1) * 128, c0:c0 + 512], [], [('rs', r)], f'd_r{r}')
                b = nxt('ps', 6)
                for kc in range(KI):
                    MM(P, psb[b][:], wbf[s][:, kc, cb * 128:(cb + 1) * 128], gyb[:, kc, tg * 512:(tg + 1) * 512],
                       kc == 0, kc == KI - 1, [('wbf', s), ('gyb', kc, tg)], [('ps', b)])
                t = nxt('t32', 2)
                TT(P, 'dve', t32[t][:], psb[b][:], rstd[:, tg * 512:(tg + 1) * 512], ALU.mult,
                   [('ps', b), ('rstd', tg)], [('t32', t)])
                TT(P, 'pool', rs[r][:], t32[t][:], rs[r][:], ALU.add, [('t32', t), ('rs', r)], [('rs', r)])
                DMA(P, 'sp', x2T[ob * 128:(ob + 1) * 128, c0:c0 + 512], rs[r][:], [('rs', r)], [], f'd_o{r}')


def emit_fnorm(nc, P, st, xT, nwd, oT, NT):
    sb = lambda name, shape, dt: st.enter_context(nc.sbuf_tensor(f"p{_PH[0]}_" + name, shape, dt))
    ones = sb("ones", [128, 128], F32)
    nw = sb("nw_sb", [128, KC], F32)
    xt = [sb(f"xt{i}", [128, KC, 512], F32) for i in range(2)]
    sq = [sb(f"sq{i}", [128, 512], F32) for i in range(2)]
    rstd = sb("rstd", [128, 512], F32)
    ps = st.enter_context(nc.psum_tensor(f"p{_PH[0]}_ps0", [128, 512], F32))
    P.op('dve', 'memset', dict(ap=ones[:], constant=1.0), [], ['ones'])
    DMA(P, 'sp', nw[:], nwd[:, :], [], ['nw'], 'd_nw')
    for tg in range(NT // 512):
        xr = ('xt', tg % 2)
        DMA(P, 'sp', xt[tg % 2][:], xT[:, tg * 512:(tg + 1) * 512].rearrange("(kc p) t -> p kc t", p=128),
            [], [xr], f'd_x{tg % 2}')
        rmsnorm_stats(P, xt[tg % 2], ones, ('ps', 0), ps, rstd[:], sq, xr, D, KC)
        for kc in range(KC):
            STT(P, xt[tg % 2][:, kc, :], xt[tg % 2][:, kc, :], nw[:, kc:kc + 1], rstd[:],
                ALU.mult, ALU.mult, [xr, 'rstd', 'nw'], [xr])
        DMA(P, 'sp', oT[:, tg * 512:(tg + 1) * 512].rearrange("(kc p) t -> p kc t", p=128), xt[tg % 2][:],
            [xr], [], f'd_o{tg % 2}')


SEQ = 4096
_DBG = None


def build_fused(phases=None):
    nc = bass.Bass("TRN2", target_bir_lowering=False)
    ein = lambda name, shape, dt: nc.dram_tensor(name, list(shape), dt, kind="ExternalInput").ap()
    scr = lambda name, shape, dt: nc.dram_tensor(name, list(shape), dt, kind="Internal").ap()
    dr = dict(
        xT=ein("xT", [D, SEQ], F32), nwA=ein("nwA", [128, KC], F32), nwB=ein("nwB", [128, KC], F32),
        nwF=ein("nwF", [128, KC], F32), Wa=ein("Wa", [D, 9296], F32), Wo=ein("Wo", [D, D], F32),
        Wb=ein("Wb", [D, 10304], F32), Wbo=ein("Wbo", [4096, D], F32), bnw=ein("bnw", [128, 32], F32),
        bfar=ein("bfar", [128, 16], F32), ident=ein("ident", [128, 128], BF16), cm=ein("cm", [2, 128, 512], F32),
        biasN=ein("biasN", [2, 16, 128, 768], F32), U=ein("U", [128, 128], F32), ones128=ein("ones128", [128, 128], F32),
        sel=ein("sel", [32, 32 * 128], F32), tri=ein("tri", [128, 128], F32), identf=ein("identf", [128, 128], F32),
        cw=ein("cw", [2, 128, NCT * 4], F32), cb=ein("cb", [2, 128, NCT], F32), dtb=ein("dtb", [2, 128, NH], F32),
        alog=ein("alog", [2, 128, NH], F32), Dv=ein("Dv", [2, 128, NH], F32),
        qT=scr("qT", [2048, SEQ], BF16), kT=scr("kT", [2048, SEQ], BF16), iqT=scr("iqT", [1024, SEQ], BF16),
        ikT=scr("ikT", [64, SEQ], BF16), v=scr("v", [SEQ, 2048], BF16), sg=scr("sg", [SEQ, 2048], BF16),
        iw=scr("iw", [SEQ, 16], F32), uT=scr("uT", [2048, SEQ], BF16), x1T=scr("x1T", [D, SEQ], F32),
        zT=scr("zT", [4096, SEQ], F32), xbcT=scr("xbcT", [6144, SEQ], F32), dt=scr("dt", [SEQ, 64], F32),
        yT=scr("yT", [4096, SEQ], F32), x2T=scr("x2T", [D, SEQ], F32),
    )
    oT = nc.dram_tensor("oT", [D, SEQ], F32, kind="ExternalOutput").ap()
    plist = []
    for hf in range(2):
        plist.append(('projA', lambda P, st, hf=hf: emit_proj(nc, P, st, dr['xT'], dr['Wa'], dr['nwA'], table_a(), dr,
                                                              hf * 2048, 2048, BF16)))
    for par in range(2):
        plist.append(('attn', lambda P, st, par=par: emit_k2(nc, P, st, par, dr)))
    for hf in range(2):
        plist.append(('aout', lambda P, st, hf=hf: emit_aout(nc, P, st, dr['uT'], dr['xT'], dr['Wo'], dr['x1T'], hf * 2048, 2048)))
    for hf in range(2):
        plist.append(('projB', lambda P, st, hf=hf: emit_proj(nc, P, st, dr['x1T'], dr['Wb'], dr['nwB'], table_b(), dr,
                                                              hf * 2048, 2048, F32)))
    for hf in range(2):
        plist.append(('ssd', lambda P, st, hf=hf: emit_k4(nc, P, st, hf, dr)))
    for ps_ in range(4):
        plist.append(('bout', lambda P, st, ps_=ps_: emit_bout(nc, P, st, dr['yT'], dr['zT'], dr['x1T'], dr['bnw'], dr['Wbo'],
                                                               dr['x2T'], ps_ * 1024)))
    plist.append(('fnorm', lambda P, st: emit_fnorm(nc, P, st, dr['x2T'], dr['nwF'], oT, SEQ)))
    with contextlib.ExitStack() as g:
        P = Prog(nc, g)
        for i, (name, fn) in enumerate(plist):
            if phases is not None and i not in phases:
                continue
            _PH[0] = i
            with contextlib.ExitStack() as st:
                fn(P, st)
                P.end_phase(st)
    return nc


def _pk(w, n):
    return np.ascontiguousarray(np.asarray(w, np.float32).reshape(n, 128).T)


def _bc(a):
    a = np.asarray(a, np.float32)
    return np.ascontiguousarray(np.broadcast_to(a[None, :], (128, a.shape[0])))


def host_inputs(b, x, norm_w, a_w_in, a_w_out, rel_bias, b_w_in, b_conv_w, b_conv_b, b_dt_bias, b_a_log, b_d,
                b_norm_w, b_w_out, final_norm_w, shared):
    A = np.ascontiguousarray
    im = dict(shared)
    im["xT"] = A(np.asarray(x[b], np.float32).T)
    return im


def host_shared(norm_w, a_w_in, a_w_out, rel_bias, b_w_in, b_conv_w, b_conv_b, b_dt_bias, b_a_log, b_d,
                b_norm_w, b_w_out, final_norm_w):
    A = np.ascontiguousarray
    f = lambda a: np.asarray(a, np.float32)
    sh = dict(nwA=_pk(norm_w[0], KC), nwB=_pk(norm_w[1], KC), nwF=_pk(final_norm_w, KC), Wa=A(f(a_w_in[0])),
              Wo=A(f(a_w_out[0])), Wb=A(f(b_w_in[0])), Wbo=A(f(b_w_out[0])), bnw=_pk(b_norm_w[0], 32))
    rb = f(rel_bias)
    c0 = k2_consts(rb, 0)
    c1 = k2_consts(rb, 1)
    sh.update(bfar=c0['bfar'], ident=c0['ident'], cm=A(np.stack([c0['cm'], c1['cm']])),
              biasN=A(np.stack([c0['biasN'], c1['biasN']])))
    k4c = k4_consts()
    sh.update(U=k4c['U'], ones128=k4c['ones128'], sel=k4c['sel'], tri=k4c['tri'], identf=k4c['identf'])
    cw = f(b_conv_w[0])
    cbv = f(b_conv_b[0])
    cws, cbs, dtbs, alogs, Dvs = [], [], [], [], []
    for hf in range(2):
        chans = np.concatenate([np.arange(hf * 2048, (hf + 1) * 2048), 4096 + np.arange(hf * 512, (hf + 1) * 512),
                                5120 + np.arange(hf * 512, (hf + 1) * 512)])
        hs = slice(hf * 32, (hf + 1) * 32)
        cws.append(cw[:, chans].T.reshape(NCT, 128, 4).transpose(1, 0, 2).reshape(128, NCT * 4))
        cbs.append(cbv[chans].reshape(NCT, 128).T)
        dtbs.append(_bc(f(b_dt_bias[0])[hs]))
        alogs.append(_bc(f(b_a_log[0])[hs]))
        Dvs.append(_bc(f(b_d[0])[hs]))
    sh.update(cw=A(np.stack(cws)), cb=A(np.stack(cbs)), dtb=A(np.stack(dtbs)), alog=A(np.stack(alogs)), Dv=A(np.stack(Dvs)))
    return sh


NCORES = 8


def kernel(x, norm_w, a_w_in, a_w_out, rel_bias, b_w_in, b_conv_w, b_conv_b, b_dt_bias, b_a_log, b_d,
           b_norm_w, b_w_out, final_norm_w):
    nc = build_fused()
    sh = host_shared(norm_w, a_w_in, a_w_out, rel_bias, b_w_in, b_conv_w, b_conv_b, b_dt_bias, b_a_log, b_d,
                     b_norm_w, b_w_out, final_norm_w)
    ims = []
    for c in range(NCORES):
        im = dict(sh)
        im["xT"] = np.ascontiguousarray(np.asarray(x[c % 4], np.float32).T)
        ims.append(im)
    res = run_bass_kernel_spmd(nc, ims, core_ids=list(range(NCORES)))
    out = np.zeros((4, SEQ, D), np.float32)
    for b in range(4):
        out[b] = np.asarray(res.results[b]["oT"]).T
    return out
```

```python
import contextlib
import numpy as np
import ml_dtypes
import concourse.bass as bass
import concourse.mybir as mybir
from concourse.bass_utils import run_bass_kernel_spmd

F32 = mybir.dt.float32
_PH = [0]
BF16 = mybir.dt.bfloat16
ALU = mybir.AluOpType
AF = mybir.ActivationFunctionType
AX = mybir.AxisListType

ENGS = ['pe', 'act', 'dve', 'pool', 'sp']


def _is_psum(r):
    n = r[0] if isinstance(r, tuple) else r
    return isinstance(n, str) and (n.startswith('ps') or n.startswith('acc'))


class Prog:
    def __init__(self, nc, gstack):
        self.nc = nc
        self.gstack = gstack
        self.sems = {}
        self.semval = {}
        self.base = {}
        self.dma_keys = set()
        self.total_instr = {e: 0 for e in ENGS}
        self._reset()

    def _reset(self):
        self.q = {e: [] for e in ENGS}
        self.waited = {e: {} for e in ENGS}
        self.res = {}
        self.touched = set()

    def op(self, eng, meth, kw, reads=(), writes=(), dma=None):
        fn = (meth, kw)
        waits = {}
        own = 'c_' + eng
        xr = [r for r in reads if _is_psum(r)]
        if xr:
            writes = list(writes) + [r for r in xr if r not in writes]

        def need(kv, raw):
            if kv is None:
                return
            k, v = kv
            if k == own and not raw:
                return
            if k == own and eng == 'pe':
                return
            if self.waited[eng].get(k, 0) >= v:
                return
            if waits.get(k, 0) < v:
                waits[k] = v

        for r in reads:
            st = self.res.get(r)
            if st:
                need(st['w'], True)
        for w in writes:
            st = self.res.get(w)
            if st:
                need(st['w'], False)
                for kv in st['r'].items():
                    need(kv, False)
        if dma:
            key, inc = dma, 16
            self.dma_keys.add(key)
        else:
            key, inc = own, 1
        self.semval[key] = self.semval.get(key, 0) + inc
        self.touched.add(key)
        val = self.semval[key]
        for k, v in waits.items():
            self.waited[eng][k] = v
        self.q[eng].append([fn, sorted(waits.items()), key, inc, val])
        for r in reads:
            st = self.res.setdefault(r, {'w': None, 'r': {}})
            st['r'][key] = val
        for w in writes:
            self.res[w] = {'w': (key, val), 'r': {}}
        return (key, val)

    def end_phase(self, pstack):
        nc = self.nc
        fin = [(k, self.semval[k]) for k in sorted(self.touched)]
        for e in ENGS:
            self.q[e].append([None, list(fin), None, 0, 0])
        waited_vals = {}
        for e in ENGS:
            for fn, waits, key, inc, val in self.q[e]:
                for k, v in waits:
                    waited_vals.setdefault(k, set()).add(v)
        remap = {}
        for k, vs in waited_vals.items():
            if k in self.dma_keys:
                continue
            b0 = self.base.get(k, 0)
            remap[k] = {v: b0 + i + 1 for i, v in enumerate(sorted(vs))}
        for k in self.touched:
            if k not in self.sems:
                self.sems[k] = self.gstack.enter_context(nc.semaphore("s_" + k))
        sems = self.sems
        block = pstack.enter_context(nc.Block())
        engmap = {'pe': block.tensor, 'act': block.scalar, 'dve': block.vector,
                  'pool': block.gpsimd, 'sp': block.sync}
        for e in ENGS:
            ops = self.q[e]
            self.total_instr[e] += len(ops)

            def body(engine, ops=ops):
                for fn, waits, key, inc, val in ops:
                    for k, v in waits:
                        if k in self.dma_keys:
                            engine.wait_ge(sems[k], v)
                        else:
                            engine.wait_ge(sems[k], remap[k][v])
                    if fn is None:
                        continue
                    ins = getattr(engine, fn[0])(**fn[1])
                    if key in self.dma_keys:
                        ins.then_inc(sems[key], 16)
                    elif key in remap and val in remap[key]:
                        ins.then_inc(sems[key], 1)
            engmap[e](body)
        for k, m in remap.items():
            self.base[k] = self.base.get(k, 0) + len(m)
        self._reset()


def MM(P, out, lhsT, rhs, start, stop, reads, writes, **kw):
    return P.op('pe', 'matmul', dict(out=out, lhsT=lhsT, rhs=rhs, start=start, stop=stop, **kw), reads, writes)


def ACTV(P, out, in_, func, reads, writes, **kw):
    return P.op('act', 'activation', dict(out=out, in_=in_, func=func, **kw), reads, writes)


def DMA(P, eng, out, in_, reads, writes, key):
    return P.op(eng, 'dma_start', dict(out=out, in_=in_), reads, writes, dma=key)


def TS(P, eng, out, in0, s1, s2, op0, op1, reads, writes, **kw):
    d = dict(out=out, in0=in0, scalar1=s1, scalar2=s2, op0=op0, **kw)
    if op1 is not None:
        d['op1'] = op1
    return P.op(eng, 'tensor_scalar', d, reads, writes)


def TT(P, eng, out, in0, in1, op, reads, writes):
    return P.op(eng, 'tensor_tensor', dict(out=out, in0=in0, in1=in1, op=op), reads, writes)


def STT(P, out, in0, scalar, in1, op0, op1, reads, writes, **kw):
    return P.op('dve', 'scalar_tensor_tensor', dict(out=out, in0=in0, scalar=scalar, in1=in1, op0=op0, op1=op1, **kw),
                reads, writes)


def CP(P, eng, out, in_, reads, writes):
    if eng == 'act':
        return P.op('act', 'activation', dict(out=out, in_=in_, func=AF.Copy), reads, writes)
    return P.op(eng, 'tensor_copy', dict(out=out, in_=in_), reads, writes)


D = 2048
KC = D // 128
EPS = 1e-6


S = 4096
NIT = 26
SCALE = 128 ** -0.5


def own_blocks(par):
    return [i for i in range(32) if (i % 4 in (0, 3)) == (par == 0)]


def k2_consts(rel_bias, par):
    n = np.arange(256)
    nf = np.maximum(n, 1).astype(np.float32)
    large = 16 + (np.log(nf / 16) / np.log(128 / 16) * 16).astype(np.int32)
    large = np.minimum(large, 31)
    bucket = np.where(n < 16, n, large)
    s = np.arange(128)[:, None]
    q = np.arange(128)[None, :]
    biasT = np.zeros((128, 16, 2, 128), np.float32)
    for d in range(2):
        dist = np.clip(d * 128 + q - s, 0, 255)
        biasT[:, :, d, :] = rel_bias[bucket[dist]].transpose(0, 2, 1)
    bfar = np.ascontiguousarray(np.broadcast_to(rel_bias[31][None, :], (128, 16))).astype(np.float32)
    negtri = np.where(np.arange(128)[None, :] <= np.arange(128)[:, None], 0.0, -1e30).astype(np.float32)
    ident = np.eye(128, dtype=np.float32).astype(ml_dtypes.bfloat16)
    dtab = lambda sp, w: (1 - w) if (sp == par) else (2 - w)
    cm = np.zeros((128, 2, 2, 128), np.float32)
    biasN = np.zeros((16, 128, 2, 3, 128), np.float32)
    for sp in range(2):
        for w in range(3):
            d = dtab(sp, w)
            if d >= 2:
                biasN[:, :, sp, w, :] = rel_bias[31][:, None, None]
            elif d >= 0:
                biasN[:, :, sp, w, :] = biasT[:, :, d, :].transpose(1, 0, 2)
            if w >= 1:
                if d == 0:
                    cm[:, sp, w - 1, :] = negtri
                elif d < 0:
                    cm[:, sp, w - 1, :] = -1e30
    return dict(bfar=bfar, ident=ident, cm=cm.reshape(128, 512), biasN=biasN.reshape(16, 128, 768))


def emit_k2(nc, P, st, par, dr, nslots=16, nheads=16, nit=NIT):
    own = own_blocks(par)
    rsel = (0, 3) if par == 0 else (1, 2)
    NQB = nslots
    nkbs = [2 * (qi + 1) for qi in range(nslots)]
    NQ = NQB * 128
    offs = []
    T = 0
    for nkb in nkbs:
        offs.append(T)
        T += nkb
    kT, v, ikT, qT, iqT, iw, sg, uT = dr['kT'], dr['v'], dr['ikT'], dr['qT'], dr['iqT'], dr['iw'], dr['sg'], dr['uT']
    biasNd, bfard, cmd, identd = dr['biasN'], dr['bfar'], dr['cm'], dr['ident']
    if True:
        sb = lambda name, shape, dt: st.enter_context(nc.sbuf_tensor(f"p{_PH[0]}_" + name, shape, dt))
        maskT = sb("maskT", [128, T, 128], BF16)
        score = sb("score", [128, S], F32)
        maskq = sb("maskq", [128, S], BF16)
        relu = [sb(f"relu{i}", [128, 512], F32) for i in range(2)]
        ikTs = sb("ikTs", [64, S], BF16)
        iqb = [sb(f"iqb{i}", [64, 16, 128], BF16) for i in range(2)]
        iwb = [sb(f"iwb{i}", [128, 16], F32) for i in range(2)]
        kTh = [sb(f"kTh{i}", [128, S], BF16) for i in range(2)]
        vh = [sb(f"vh{i}", [128, 32, 136], BF16) for i in range(2)]
        qTh = [sb(f"qTh{i}", [128, NQ], BF16) for i in range(2)]
        sgb = [sb(f"sgb{i}", [128, NQB, 128], BF16) for i in range(2)]
        biasN = [sb(f"biasN{i}", [128, 2, 3, 128], F32) for i in range(2)]
        bfar = sb("bfars", [128, 16], F32)
        cm = sb("cms", [128, 2, 256], F32)
        ident = sb("idents", [128, 128], BF16)
        hi = sb("hi", [128, 1], F32)
        lo = sb("lo", [128, 1], F32)
        w = sb("w", [128, 1], F32)
        mid = sb("mid", [128, 1], F32)
        cnt = sb("cnt", [128, 1], F32)
        ge = sb("ge", [128, 1], F32)
        rinv = sb("rinv", [128, 1], F32)
        NE = 4
        tb = [sb(f"tb{i}", [128, 128], F32) for i in range(2)]
        eb = [sb(f"eb{i}", [128, 512], BF16) for i in range(NE)]
        pt = [sb(f"pt{i}", [128, 512], BF16) for i in range(NE)]
        o32 = [sb(f"o32{i}", [128, 128], F32) for i in range(2)]
        ust = [sb(f"ust{i}", [128, 128], BF16) for i in range(4)]
        usT = [sb(f"usT{i}", [128, 128], BF16) for i in range(4)]
        psb = [st.enter_context(nc.psum_tensor(f"p{_PH[0]}_ps{i}", [128, 512], F32)) for i in range(4)]
        pst = [st.enter_context(nc.psum_tensor(f"p{_PH[0]}_pst{i}", [128, 1024], BF16)) for i in range(2)]
        acc = [st.enter_context(nc.psum_tensor(f"p{_PH[0]}_acc{i}", [128, 512], F32)) for i in range(2)]
        cn = {}

        def nxt(k, n):
            vv = cn.get(k, 0)
            cn[k] = vv + 1
            return vv % n
        DMA(P, 'sp', ikTs[:], ikT[:, :], [], ['ikT'], 'd_c0')
        DMA(P, 'sp', cm[:], cmd[par].rearrange("p (a b) -> p a b", a=2), [], ['cm'], 'd_c1')
        DMA(P, 'sp', ident[:], identd[:, :], [], ['ident'], 'd_c2')
        DMA(P, 'sp', bfar[:], bfard[:, :], [], ['bfar'], 'd_c3')
        for i in range(2):
            P.op('pool', 'memset', dict(ap=vh[i][:, :, 128:129], constant=1.0), [], [('vh1', i)])

        for qi in range(nslots):
            nkb = nkbs[qi]
            nk = nkb * 128
            s2 = qi % 2
            DMA(P, 'sp', iqb[s2][:], iqT[:, own[qi] * 128:(own[qi] + 1) * 128].rearrange("(h d) q -> d h q", d=64),
                [], [('iqb', s2)], f'd_iq{s2}')
            DMA(P, 'sp', iwb[s2][:], iw[own[qi] * 128:(own[qi] + 1) * 128, :], [], [('iwb', s2)], f'd_iw{s2}')
            nch = (nk + 511) // 512
            for c in range(nch):
                kw = min(512, nk - c * 512)
                for hh in range(16):
                    b = nxt('ps', 4)
                    MM(P, psb[b][:, :kw], iqb[s2][:, hh, :], ikTs[:, c * 512:c * 512 + kw], True, True,
                       [('iqb', s2), 'ikT'], [('ps', b)])
                    rb = nxt('relu', 2)
                    ACTV(P, relu[rb][:, :kw], psb[b][:, :kw], AF.Relu, [('ps', b)], [('relu', rb)])
                    sc = score[:, c * 512:c * 512 + kw]
                    if hh == 0:
                        TS(P, 'dve', sc, relu[rb][:, :kw], iwb[s2][:, 0:1], None, ALU.mult, None,
                           [('relu', rb), ('iwb', s2)], [('score', c)])
                    else:
                        STT(P, sc, relu[rb][:, :kw], iwb[s2][:, hh:hh + 1], sc, ALU.mult, ALU.add,
                            [('relu', rb), ('iwb', s2), ('score', c)], [('score', c)])
            scr = [('score', c) for c in range(nch)]
            P.op('dve', 'tensor_reduce', dict(out=hi[:], in_=score[:, :nk], axis=AX.X, op=ALU.max), scr, ['hi'])
            P.op('dve', 'tensor_reduce', dict(out=lo[:], in_=score[:, :nk], axis=AX.X, op=ALU.min), scr, ['lo'])
            TT(P, 'dve', w[:], hi[:], lo[:], ALU.subtract, ['hi', 'lo'], ['w'])
            dg = score[:, (nkb - 2) * 128:nkb * 128]
            TT(P, 'dve', dg, dg, cm[:, qi % 2, :], ALU.add, [('score', (nkb - 2) // 4), 'cm'], [('score', (nkb - 2) // 4)])
            for it in range(nit):
                TS(P, 'dve', w[:], w[:], 0.5, None, ALU.mult, None, ['w'], ['w'])
                TT(P, 'dve', mid[:], lo[:], w[:], ALU.add, ['lo', 'w'], ['mid'])
                TS(P, 'dve', maskq[:, :nk], score[:, :nk], mid[:, 0:1], 0.0, ALU.is_ge, ALU.add,
                   scr + ['mid'], ['maskq', 'cnt'], accum_out=cnt[:, 0:1])
                TS(P, 'dve', ge[:], cnt[:], 255.5, None, ALU.is_ge, None, ['cnt'], ['ge'])
                STT(P, lo[:], ge[:], w[:, 0:1], lo[:], ALU.mult, ALU.add, ['ge', 'w', 'lo'], ['lo'])
            TS(P, 'dve', maskq[:, :nk], score[:, :nk], lo[:, 0:1], None, ALU.is_ge, None, scr + ['lo'], ['maskq'])
            for j in range(nkb):
                b = nxt('pst', 2)
                P.op('pe', 'transpose', dict(out=pst[b][:, :128], in_=maskq[:, j * 128:(j + 1) * 128], identity=ident[:]),
                     ['maskq', 'ident'], [('pst', b)])
                CP(P, 'act', maskT[:, offs[qi] + j, :], pst[b][:, :128], [('pst', b)], [('maskT', qi)])

        def head_loads(h):
            s2 = h % 2
            DMA(P, 'sp', kTh[s2][:], kT[h * 128:(h + 1) * 128, :], [], [('kTh', s2)], f'd_k{s2}')
            DMA(P, 'sp', vh[s2][:, :, 0:128], v[:, h * 128:(h + 1) * 128].rearrange("(j p) d -> p j d", p=128),
                [], [('vh', s2)], f'd_v{s2}')
            for k in range(2):
                DMA(P, 'sp', qTh[s2][:].rearrange("p (m k t) -> p m k t", k=2, t=128)[:, :, k, :],
                    qT[h * 128:(h + 1) * 128, :].rearrange("p (m r t) -> p m r t", r=4, t=128)[:, :, rsel[k], :],
                    [], [('qTh', s2)], f'd_q{s2}')
            for k in range(2):
                DMA(P, 'sp', sgb[s2][:].rearrange("p (m k) d -> p m k d", k=2)[:, :, k, :],
                    sg[:, h * 128:(h + 1) * 128].rearrange("(m r p) d -> p m r d", r=4, p=128)[:, :, rsel[k], :],
                    [], [('sgb', s2)], f'd_sg{s2}')
            DMA(P, 'sp', biasN[s2][:], biasNd[par, h].rearrange("p (a w q) -> p a w q", a=2, w=3), [], [('biasN', s2)], f'd_bn{s2}')

        units = []
        for h in range(nheads):
            for qi in range(nslots):
                nkb = nkbs[qi]
                nfar = max(0, nkb - 3)
                j = 0
                while j < nfar:
                    n = min(4, nfar - j)
                    units.append((h, qi, j, n, True))
                    j += n
                for j in range(nfar, nkb):
                    units.append((h, qi, j, 1, False))
        ubuf = {}

        def stage_a(t):
            h, qi, j0, n, far = units[t]
            s2 = h % 2
            nkb = nkbs[qi]
            b = nxt('ps', 4)
            for k in range(n):
                MM(P, psb[b][:, k * 128:(k + 1) * 128], kTh[s2][:, (j0 + k) * 128:(j0 + k + 1) * 128],
                   qTh[s2][:, qi * 128:(qi + 1) * 128], True, True, [('kTh', s2), ('qTh', s2)], [('ps', b)])
            e = nxt('eb', NE)
            if far:
                ACTV(P, eb[e][:, :n * 128], psb[b][:, :n * 128], AF.Exp, [('ps', b), 'bfar'], [('eb', e)],
                     scale=SCALE, bias=bfar[:, h:h + 1])
            else:
                wv = j0 - (nkb - 3)
                tt_ = nxt('tb', 2)
                STT(P, tb[tt_][:], psb[b][:, :128], SCALE, biasN[s2][:, qi % 2, wv, :], ALU.mult, ALU.add,
                    [('ps', b), ('biasN', s2)], [('tb', tt_)])
                ACTV(P, eb[e][:, :128], tb[tt_][:], AF.Exp, [('tb', tt_)], [('eb', e)])
            p = nxt('pt', NE)
            me = 'dve' if nxt('me', 2) == 0 else 'pool'
            TT(P, me, pt[p][:, :n * 128], eb[e][:, :n * 128],
               maskT[:, offs[qi] + j0:offs[qi] + j0 + n, :].rearrange("p a b -> p (a b)"), ALU.mult,
               [('eb', e), ('maskT', qi)], [('pt', p)])
            ubuf[t] = p

        def stage_b(t):
            h, qi, j0, n, far = units[t]
            s2 = h % 2
            nkb = nkbs[qi]
            ab = (h * nslots + qi) % 2
            p = ubuf.pop(t)
            for k in range(n):
                j = j0 + k
                MM(P, acc[ab][:, :129], pt[p][:, k * 128:(k + 1) * 128], vh[s2][:, j, 0:129], j == 0, j == nkb - 1,
                   [('pt', p), ('vh', s2), ('vh1', s2)], [('acc', ab)])
            if j0 + n != nkb:
                return
            P.op('dve', 'reciprocal', dict(out=rinv[:], in_=acc[ab][:, 128:129]), [('acc', ab)], ['rinv'])
            o = nxt('o32', 2)
            TS(P, 'dve', o32[o][:], acc[ab][:, :128], rinv[:, 0:1], None, ALU.mult, None,
               [('acc', ab), 'rinv'], [('o32', o)])
            us = nxt('ust', 4)
            TT(P, 'pool', ust[us][:], o32[o][:], sgb[s2][:, qi, :], ALU.mult, [('o32', o), ('sgb', s2)], [('ust', us)])
            tp = nxt('pst', 2)
            P.op('pe', 'transpose', dict(out=pst[tp][:, :128], in_=ust[us][:], identity=ident[:]),
                 [('ust', us), 'ident'], [('pst', tp)])
            CP(P, 'act', usT[us][:], pst[tp][:, :128], [('pst', tp)], [('usT', us)])
            DMA(P, 'sp', uT[h * 128:(h + 1) * 128, own[qi] * 128:(own[qi] + 1) * 128], usT[us][:], [('usT', us)], [], f'd_u{us}')
            if qi == nslots - 1 and h + 2 < nheads:
                head_loads(h + 2)

        LA = 2
        head_loads(0)
        if nheads > 1:
            head_loads(1)
        for t in range(len(units) + LA):
            if t < len(units):
                stage_a(t)
            if t - LA >= 0:
                stage_b(t - LA)


NH = 32
NG = 4
NCT = 24


def k4_consts():
    U = np.triu(np.ones((128, 128), np.float32))
    sel = np.zeros((32, 32, 128), np.float32)
    for h in range(32):
        sel[h, h, :] = 1.0
    return dict(U=U, ones128=np.ones((128, 128), np.float32), sel=sel.reshape(32, 32 * 128),
                tri=U.copy(), identf=np.eye(128, dtype=np.float32),
                identb=np.eye(128, dtype=np.float32).astype(ml_dtypes.bfloat16))


class _Stop(Exception):
    pass


def emit_k4(nc, P, st, hf, dr, npieces=8, stop=99):
    SS = npieces * 512
    xbcT, dtd, yT = dr['xbcT'], dr['dt'], dr['yT']
    cwd, cbd, dtbd, alogd, Dd = dr['cw'][hf], dr['cb'][hf], dr['dtb'][hf], dr['alog'][hf], dr['Dv'][hf]
    Ud, onesd, seld, trid, identfd, identbd = dr['U'], dr['ones128'], dr['sel'], dr['tri'], dr['identf'], dr['ident']
    if True:
        sb = lambda name, shape, dt: st.enter_context(nc.sbuf_tensor(f"p{_PH[0]}_" + name, shape, dt))
        xin = sb("xin", [128, 12, 515], F32)
        xc = sb("xc", [128, 16, 512], F32)
        BTc = sb("BTc", [128, 4, 512], BF16)
        CTc = sb("CTc", [128, 4, 512], BF16)
        cacc = [sb(f"cacc{i}", [128, 512], F32) for i in range(2)]
        cw = sb("cws", [128, NCT, 4], F32)
        cb = sb("cbs", [128, NCT], F32)
        dtb = sb("dtbs", [128, NH], F32)
        Aneg = sb("Aneg", [128, NH], F32)
        Dv = sb("Dvs", [128, NH], F32)
        U = sb("Us", [128, 128], F32)
        ones128 = sb("ones128s", [128, 128], F32)
        sel = sb("sels", [32, 32, 128], F32)
        tri = sb("tris", [128, 128], F32)
        identf = sb("identfs", [128, 128], F32)
        identb = sb("identbs", [128, 128], BF16)
        dtr = sb("dtr", [128, 4, NH], F32)
        dtp = sb("dtp", [128, 4, NH], F32)
        adt = sb("adt", [128, 4, NH], F32)
        acs = sb("acs", [128, NH], F32)
        ea = sb("ea", [128, NH], F32)
        dec = sb("dec", [128, NH], F32)
        cdec = sb("cdec", [128, NH], F32)
        dtdec = sb("dtdec", [128, NH], F32)
        acsT = sb("acsT", [32, 128], F32)
        x_tm = sb("x_tm", [128, NH, 64], F32)
        xdt = sb("xdt", [128, NH, 64], BF16)
        xdtd = sb("xdtd", [128, NH, 64], BF16)
        B_tm = sb("B_tm", [128, 4, 128], BF16)
        prev32 = sb("prev32", [128, NG, 512], F32)
        prevbf = sb("prevbf", [128, NG, 512], BF16)
        cbTm = [sb(f"cbTm{i}", [128, 128], F32) for i in range(2)]
        tcl = [sb(f"tcl{i}", [128, 4, 128], F32) for i in range(2)]
        Lt = [sb(f"Lt{i}", [128, 4, 128], F32) for i in range(2)]
        MT = [sb(f"MT{i}", [128, 4, 128], BF16) for i in range(2)]
        yos = [sb(f"yos{i}", [128, 8, 64], F32) for i in range(2)]
        dsk = [sb(f"dsk{i}", [128, 8, 64], F32) for i in range(4)]
        ystage = [sb(f"ystage{i}", [128, NH, 64], F32) for i in range(2)]
        yTs = [sb(f"yTs{i}", [128, 16, 128], F32) for i in range(2)]
        ps = [st.enter_context(nc.psum_tensor(f"p{_PH[0]}_ps{i}", [128, 512], F32)) for i in range(3)]
        psmisc = st.enter_context(nc.psum_tensor(f"p{_PH[0]}_psmisc", [128, 512], F32))
        psbt = st.enter_context(nc.psum_tensor(f"p{_PH[0]}_psbt", [128, 1024], BF16))
        psbc = [st.enter_context(nc.psum_tensor(f"p{_PH[0]}_psbc{i}", [128, 512], F32)) for i in range(2)]
        psyd = st.enter_context(nc.psum_tensor(f"p{_PH[0]}_psyd", [128, 512], F32))
        cn = {}

        def nxt(k, n):
            vv = cn.get(k, 0)
            cn[k] = vv + 1
            return vv % n
        for name, t, d in [('cw', cw, cwd.rearrange("p (c k) -> p c k", k=4)), ('cb', cb, cbd),
                           ('dtb', dtb, dtbd), ('Dv', Dv, Dd), ('U', U, Ud[:, :]),
                           ('ones128', ones128, onesd[:, :]), ('sel', sel, seld[:, :].rearrange("p (h m) -> p h m", m=128)),
                           ('tri', tri, trid[:, :]), ('identf', identf, identfd[:, :]), ('identb', identb, identbd[:, :]),
                           ('Aneg', Aneg, alogd)]:
            DMA(P, 'sp', t[:], d, [], [name], 'd_' + name)
        ACTV(P, Aneg[:], Aneg[:], AF.Exp, ['Aneg'], ['Aneg'])
        TS(P, 'dve', Aneg[:], Aneg[:], -1.0, None, ALU.mult, None, ['Aneg'], ['Aneg'])
        P.op('dve', 'memset', dict(ap=prev32[:], constant=0.0), [], [('prev32', g) for g in range(NG)])
        P.op('dve', 'memset', dict(ap=prevbf[:], constant=0.0), [], [('prevbf', g) for g in range(NG)])

        def chk(n):
            if stop <= n:
                raise _Stop()
        try:
          for pc in range(npieces):
              t0 = pc * 512
              for half in range(2):
                  if half == 0:
                      srcs = [(0, 12, hf * 2048)]
                  else:
                      srcs = [(0, 4, hf * 2048 + 1536), (4, 4, 4096 + hf * 512), (8, 4, 5120 + hf * 512)]
                  if pc == 0:
                      P.op('dve', 'memset', dict(ap=xin[:, :, 0:3], constant=0.0), [], ['xin'])
                  for (c0_, n_, r0_) in srcs:
                      src = xbcT[r0_:r0_ + n_ * 128, :]
                      if pc == 0:
                          DMA(P, 'sp', xin[:, c0_:c0_ + n_, 3:515], src[:, 0:512].rearrange("(c p) t -> p c t", p=128), [], ['xin'], 'd_xin')
                      else:
                          DMA(P, 'sp', xin[:, c0_:c0_ + n_, :], src[:, t0 - 3:t0 + 512].rearrange("(c p) t -> p c t", p=128), [], ['xin'], 'd_xin')
                  for cl in range(12):
                      ct = half * 12 + cl
                      a = nxt('cacc', 2)
                      TS(P, 'dve', cacc[a][:], xin[:, cl, 3:515], cw[:, ct, 3:4], None, ALU.mult, None,
                         ['xin', 'cw'], [('cacc', a)])
                      for k in (2, 1, 0):
                          STT(P, cacc[a][:], xin[:, cl, k:k + 512], cw[:, ct, k:k + 1], cacc[a][:], ALU.mult, ALU.add,
                              ['xin', 'cw', ('cacc', a)], [('cacc', a)])
                      if ct < 16:
                          dst, dr = xc[:, ct, :], ('xc', ct)
                      elif ct < 20:
                          dst, dr = BTc[:, ct - 16, :], ('BTc', ct - 16)
                      else:
                          dst, dr = CTc[:, ct - 20, :], ('CTc', ct - 20)
                      ACTV(P, dst, cacc[a][:], AF.Silu, [('cacc', a), 'cb'], [dr], bias=cb[:, ct:ct + 1])
              chk(1)
              DMA(P, 'sp', dtr[:], dtd[t0:t0 + 512, hf * 32:(hf + 1) * 32].rearrange("(c p) h -> p c h", p=128), [], ['dtr'], 'd_dtr')
              TT(P, 'dve', dtr[:], dtr[:], dtb[:].unsqueeze(1).to_broadcast([128, 4, NH]), ALU.add, ['dtr', 'dtb'], ['dtr'])
              ACTV(P, dtp[:], dtr[:], AF.Exp, ['dtr'], ['dtp'])
              ACTV(P, dtp[:], dtp[:], AF.Ln, ['dtp', 'ones128'], ['dtp'], bias=ones128[:, 0:1])
              TT(P, 'dve', adt[:], dtp[:], Aneg[:].unsqueeze(1).to_broadcast([128, 4, NH]), ALU.mult, ['dtp', 'Aneg'], ['adt'])
              chk(2)
              for c in range(4):
                  l0 = c * 128
                  MM(P, psmisc[:, 0:NH], U[:], adt[:, c, :], True, True, ['U', 'adt'], ['psmisc'])
                  MM(P, psmisc[:, 32:32 + NH], ones128[:], adt[:, c, :], True, True, ['ones128', 'adt'], ['psmisc'])
                  MM(P, psmisc[0:32, 64:192], adt[:, c, :], U[:], True, True, ['U', 'adt'], ['psmisc'])
                  CP(P, 'dve', acs[:], psmisc[:, 0:NH], ['psmisc'], ['acs'])
                  ACTV(P, ea[:], psmisc[:, 0:NH], AF.Exp, ['psmisc'], ['ea'])
                  ACTV(P, cdec[:], psmisc[:, 32:32 + NH], AF.Exp, ['psmisc'], ['cdec'])
                  TT(P, 'dve', dec[:], psmisc[:, 32:32 + NH], acs[:], ALU.subtract, ['psmisc', 'acs'], ['dec'])
                  ACTV(P, dec[:], dec[:], AF.Exp, ['dec'], ['dec'])
                  CP(P, 'dve', acsT[:], psmisc[0:32, 64:192], ['psmisc'], ['acsT'])
                  TT(P, 'dve', dtdec[:], dtp[:, c, :], dec[:], ALU.mult, ['dtp', 'dec'], ['dtdec'])
                  chk(3)
                  for q4 in range(4):
                      b = nxt('ps', 3)
                      for k in range(4):
                          ct = q4 * 4 + k
                          P.op('pe', 'transpose', dict(out=ps[b][:, k * 128:(k + 1) * 128], in_=xc[:, ct, l0:l0 + 128],
                                                       identity=identf[:]), [('xc', ct), 'identf'], [('ps', b)])
                      CP(P, 'act', x_tm[:, q4 * 8:(q4 + 1) * 8, :].rearrange("p h d -> p (h d)"), ps[b][:, :],
                         [('ps', b)], [('x_tm', q4)])
                  xr = [('x_tm', q) for q in range(4)]
                  TT(P, 'dve', xdt[:], x_tm[:], dtp[:, c, :].unsqueeze(2).to_broadcast([128, NH, 64]), ALU.mult,
                     xr + ['dtp'], ['xdt'])
                  TT(P, 'pool', xdtd[:], x_tm[:], dtdec[:].unsqueeze(2).to_broadcast([128, NH, 64]), ALU.mult,
                     xr + ['dtdec'], ['xdtd'])
                  for g in range(NG):
                      P.op('pe', 'transpose', dict(out=psbt[:, g * 128:(g + 1) * 128], in_=BTc[:, g, l0:l0 + 128],
                                                   identity=identb[:]), [('BTc', g), 'identb'], ['psbt'])
                  CP(P, 'act', B_tm[:].rearrange("p g n -> p (g n)"), psbt[:, 0:512], ['psbt'], ['B_tm'])
                  chk(4)
                  ys = nxt('ystage', 2)
                  for g in range(NG):
                      b = nxt('ps', 3)
                      MM(P, ps[b][:, :], CTc[:, g, l0:l0 + 128], prevbf[:, g, :], True, True,
                         [('CTc', g), ('prevbf', g)], [('ps', b)])
                      yo = nxt('yos', 2)
                      TT(P, 'dve', yos[yo][:], ps[b][:, :].rearrange("p (h d) -> p h d", d=64),
                         ea[:, g * 8:(g + 1) * 8].unsqueeze(2).to_broadcast([128, 8, 64]), ALU.mult,
                         [('ps', b), 'ea'], [('yos', yo)])
                      TT(P, 'pool', dsk[g][:], x_tm[:, g * 8:(g + 1) * 8, :],
                         Dv[:, g * 8:(g + 1) * 8].unsqueeze(2).to_broadcast([128, 8, 64]), ALU.mult,
                         [('x_tm', g), 'Dv'], [('dsk', g)])
                      TT(P, 'pool', dsk[g][:], dsk[g][:], yos[yo][:], ALU.add, [('dsk', g), ('yos', yo)], [('dsk', g)])
                      b = nxt('ps', 3)
                      MM(P, ps[b][:, :], B_tm[:, g, :], xdtd[:, g * 8:(g + 1) * 8, :].rearrange("p h d -> p (h d)"), True, True,
                         ['B_tm', 'xdtd'], [('ps', b)])
                      TT(P, 'pool', prev32[:, g, :].rearrange("p (h d) -> p h d", d=64),
                         prev32[:, g, :].rearrange("p (h d) -> p h d", d=64),
                         cdec[:, g * 8:(g + 1) * 8].unsqueeze(2).to_broadcast([128, 8, 64]), ALU.mult,
                         [('prev32', g), 'cdec'], [('prev32', g)])
                      TT(P, 'dve', prev32[:, g, :], prev32[:, g, :], ps[b][:, :], ALU.add, [('prev32', g), ('ps', b)],
                         [('prev32', g)])
                      CP(P, 'act', prevbf[:, g, :], prev32[:, g, :], [('prev32', g)], [('prevbf', g)])
                  quads = [(g, hq) for g in range(NG) for hq in range(2)]
                  qbuf = {}

                  def quad_a(i):
                      g, hq = quads[i]
                      if hq == 0:
                          b = nxt('ps', 3)
                          MM(P, ps[b][:, 0:128], BTc[:, g, l0:l0 + 128], CTc[:, g, l0:l0 + 128], True, True,
                             [('BTc', g), ('CTc', g)], [('ps', b)])
                          TT(P, 'dve', cbTm[g % 2][:], ps[b][:, 0:128], tri[:], ALU.mult, [('ps', b), 'tri'], [('cbTm', g % 2)])
                      cm = g % 2
                      bb = nxt('psbc', 2)
                      tc_ = nxt('tcl', 2)
                      for hh in range(4):
                          h = g * 8 + hq * 4 + hh
                          MM(P, psbc[bb][:, hh * 128:(hh + 1) * 128], sel[:, h, :], acsT[:], True, True,
                             ['sel', 'acsT'], [('psbc', bb)])
                          TS(P, 'dve', tcl[tc_][:, hh, :], psbc[bb][:, hh * 128:(hh + 1) * 128], acs[:, h:h + 1], 0.0,
                             ALU.subtract, ALU.min, [('psbc', bb), 'acs'], [('tcl', tc_)])
                      ACTV(P, Lt[tc_][:], tcl[tc_][:], AF.Exp, [('tcl', tc_)], [('Lt', tc_)])
                      TT(P, 'pool', MT[tc_][:], Lt[tc_][:], cbTm[cm][:].unsqueeze(1).to_broadcast([128, 4, 128]), ALU.mult,
                         [('Lt', tc_), ('cbTm', cm)], [('MT', tc_)])
                      qbuf[i] = tc_

                  def quad_b(i):
                      g, hq = quads[i]
                      tc_ = qbuf.pop(i)
                      for hh in range(4):
                          h = g * 8 + hq * 4 + hh
                          MM(P, psyd[:, (hq * 4 + hh) * 64:(hq * 4 + hh + 1) * 64], MT[tc_][:, hh, :], xdt[:, h, :],
                             True, True, [('MT', tc_), 'xdt'], ['psyd'])
                      if hq == 1:
                          TT(P, 'dve', ystage[ys][:, g * 8:(g + 1) * 8, :], psyd[:, :].rearrange("p (h d) -> p h d", d=64),
                             dsk[g][:], ALU.add, ['psyd', ('dsk', g)], [('ystage', ys)])

                  quad_a(0)
                  for i in range(len(quads)):
                      if i + 1 < len(quads):
                          quad_a(i + 1)
                      quad_b(i)
                  yt = nxt('yTs', 2)
                  for q4 in range(4):
                      b = nxt('ps', 3)
                      for k in range(4):
                          ct = q4 * 4 + k
                          P.op('pe', 'transpose', dict(out=ps[b][:, k * 128:(k + 1) * 128],
                                                       in_=ystage[ys][:, 2 * ct:2 * ct + 2, :].rearrange("p h d -> p (h d)"),
                                                       identity=identf[:]), [('ystage', ys), 'identf'], [('ps', b)])
                      CP(P, 'act' if q4 % 2 == 0 else 'dve', yTs[yt][:, q4 * 4:(q4 + 1) * 4, :].rearrange("p c t -> p (c t)"),
                         ps[b][:, :], [('ps', b)], [('yTs', yt)])
                  DMA(P, 'sp', yT[hf * 2048:(hf + 1) * 2048, t0 + l0:t0 + l0 + 128].rearrange("(c p) t -> p c t", p=128),
                      yTs[yt][:], [('yTs', yt)], [], f'd_y{yt}')
        except _Stop:
            pass


def rmsnorm_stats(P, xt, ones, psk, ps, rstd, sq, xres, nfeat, KCn, eps=EPS):
    for kc in range(KCn):
        ACTV(P, sq[kc % 2][:], xt[:, kc, :], AF.Square, [xres], [('sq', kc % 2)])
        MM(P, ps[:], ones[:], sq[kc % 2][:], kc == 0, kc == KCn - 1, [('sq', kc % 2), 'ones'], [psk])
    TS(P, 'dve', rstd, ps[:], 1.0 / nfeat, eps, ALU.mult, ALU.add, [psk], ['rstd'])
    ACTV(P, rstd, rstd, AF.Sqrt, ['rstd'], ['rstd'])
    P.op('dve', 'reciprocal', dict(out=rstd, in_=rstd), ['rstd'], ['rstd'])


def emit_proj(nc, P, st, xT, W, nwd, table, od, t0, NT, stg_dt):
    NTG = NT // 512
    NTT = NT // 128
    sb = lambda name, shape, dt: st.enter_context(nc.sbuf_tensor(f"p{_PH[0]}_" + name, shape, dt))
    ones = sb("ones", [128, 128], F32)
    nw = sb("nw_sb", [128, KC], F32)
    xt = [sb(f"xt{i}", [128, KC, 512], F32) for i in range(2)]
    sq = [sb(f"sq{i}", [128, 512], F32) for i in range(2)]
    rstd = sb("rstd", [128, 512], F32)
    hT = sb("hT", [128, KC, NT], BF16)
    NW = 3
    wbf = [sb(f"wbf{i}", [128, KC, 512], BF16) for i in range(NW)]
    NS = 4
    stg = [sb(f"stg{i}", [128, 512], stg_dt) for i in range(NS)]
    stgf = sb("stgf", [128, 16], F32)
    psb = [st.enter_context(nc.psum_tensor(f"p{_PH[0]}_ps{i}", [128, 512], F32)) for i in range(8)]
    P.op('dve', 'memset', dict(ap=ones[:], constant=1.0), [], ['ones'])
    DMA(P, 'sp', nw[:], nwd[:, :], [], ['nw'], 'd_nw')
    wt = table

    def load_w(i):
        c0, ncol, kind, dst, d0 = wt[i]
        s = i % NW
        DMA(P, 'pool', wbf[s][:, :, :ncol], W[:, c0:c0 + ncol].rearrange("(kc p) c -> p kc c", p=128),
            [], [('wbf', s)], f'd_w{s}')
    load_w(0)
    load_w(1)
    for tg in range(NTG):
        xr = ('xt', tg % 2)
        DMA(P, 'sp', xt[tg % 2][:], xT[:, t0 + tg * 512:t0 + (tg + 1) * 512].rearrange("(kc p) t -> p kc t", p=128),
            [], [xr], f'd_x{tg % 2}')
        rmsnorm_stats(P, xt[tg % 2], ones, ('ps', 7), psb[7], rstd[:], sq, xr, D, KC)
        for kc in range(KC):
            STT(P, hT[:, kc, tg * 512:(tg + 1) * 512], xt[tg % 2][:, kc, :], nw[:, kc:kc + 1], rstd[:],
                ALU.mult, ALU.mult, [xr, 'rstd', 'nw'], [('hT', kc, tg)])
    cnt = {'ps': 0, 'stg': 0, 'ev': 0}

    def nxt(k, n):
        v = cnt[k] % n
        cnt[k] += 1
        return v
    for i in range(len(wt)):
        if i + 2 < len(wt):
            load_w(i + 2)
        c0, ncol, kind, dstn, d0 = wt[i]
        dst = od[dstn]
        s = i % NW
        if kind == 'fm':
            for cb in range((ncol + 127) // 128):
                m = min(128, ncol - cb * 128)
                for tg in range(NTG):
                    b = nxt('ps', 6)
                    for kc in range(KC):
                        MM(P, psb[b][:m, :], wbf[s][:, kc, cb * 128:cb * 128 + m], hT[:, kc, tg * 512:(tg + 1) * 512],
                           kc == 0, kc == KC - 1, [('wbf', s), ('hT', kc, tg)], [('ps', b)])
                    ss = nxt('stg', NS)
                    ev = 'act' if nxt('ev', 2) == 0 else 'dve'
                    CP(P, ev, stg[ss][:m, :], psb[b][:m, :], [('ps', b)], [('stg', ss)])
                    r0 = d0 + cb * 128
                    DMA(P, 'sp', dst[r0:r0 + m, t0 + tg * 512:t0 + (tg + 1) * 512], stg[ss][:m, :], [('stg', ss)], [], f'd_o{ss}')
        else:
            for tt in range(NTT):
                b = nxt('ps', 6)
                for kc in range(KC):
                    MM(P, psb[b][:, :ncol], hT[:, kc, tt * 128:(tt + 1) * 128], wbf[s][:, kc, :ncol],
                       kc == 0, kc == KC - 1, [('wbf', s), ('hT', kc, tt // 4)], [('ps', b)])
                r0 = t0 + tt * 128
                if kind == 'tmw':
                    TS(P, 'dve', stgf[:, :], psb[b][:, :16], 1.0 / 32.0, None, ALU.mult, None, [('ps', b)], ['stgf'])
                    DMA(P, 'sp', dst[r0:r0 + 128, :], stgf[:, :], ['stgf'], [], 'd_of')
                    continue
                ss = nxt('stg', NS)
                if kind == 'tmg':
                    ACTV(P, stg[ss][:, :ncol], psb[b][:, :ncol], AF.Silu, [('ps', b)], [('stg', ss)])
                else:
                    CP(P, 'dve', stg[ss][:, :ncol], psb[b][:, :ncol], [('ps', b)], [('stg', ss)])
                DMA(P, 'sp', dst[r0:r0 + 128, d0:d0 + ncol], stg[ss][:, :ncol], [('stg', ss)], [], f'd_o{ss}')


def table_a():
    wt = []
    for i in range(8):
        wt.append((i * 512, 512, 'fm', 'qT' if i < 4 else 'kT', (i % 4) * 512))
    for i in range(2):
        wt.append((8192 + i * 512, 512, 'fm', 'iqT', i * 512))
    wt.append((9216, 64, 'fm', 'ikT', 0))
    for i in range(4):
        wt.append((4096 + i * 512, 512, 'tm', 'v', i * 512))
    for i in range(4):
        wt.append((6144 + i * 512, 512, 'tmg', 'sg', i * 512))
    wt.append((9280, 16, 'tmw', 'iw', 0))
    return wt


def table_b():
    wt = []
    for i in range(8):
        wt.append((i * 512, 512, 'fm', 'zT', i * 512))
    for i in range(12):
        wt.append((4096 + i * 512, 512, 'fm', 'xbcT', i * 512))
    wt.append((10240, 64, 'tm', 'dt', 0))
    return wt


def emit_aout(nc, P, st, uT, xT, W, x1T, t0, NT):
    NTG = NT // 512
    sb = lambda name, shape, dt: st.enter_context(nc.sbuf_tensor(f"p{_PH[0]}_" + name, shape, dt))
    aT = sb("aT", [128, KC, NT], BF16)
    wbf = [sb(f"wbf{i}", [128, KC, 512], BF16) for i in range(2)]
    rs = [sb(f"rs{i}", [128, 512], F32) for i in range(4)]
    psb = [st.enter_context(nc.psum_tensor(f"p{_PH[0]}_ps{i}", [128, 512], F32)) for i in range(6)]
    cnt = {}

    def nxt(k, n):
        v = cnt.get(k, 0)
        cnt[k] = v + 1
        return v % n
    DMA(P, 'sp', aT[:], uT[:, t0:t0 + NT].rearrange("(kc p) t -> p kc t", p=128), [], ['aT'], 'd_a')

    def load_w(i):
        DMA(P, 'pool', wbf[i % 2][:], W[:, i * 512:(i + 1) * 512].rearrange("(kc p) c -> p kc c", p=128),
            [], [('wbf', i % 2)], f'd_w{i % 2}')
    load_w(0)
    for i in range(4):
        if i + 1 < 4:
            load_w(i + 1)
        for cb in range(4):
            ob = i * 4 + cb
            for tg in range(NTG):
                c0 = t0 + tg * 512
                r = nxt('rs', 4)
                DMA(P, 'sp', rs[r][:], xT[ob * 128:(ob + 1) * 128, c0:c0 + 512], [], [('rs', r)], f'd_r{r}')
                b = nxt('ps', 6)
                for kc in range(KC):
                    MM(P, psb[b][:], wbf[i % 2][:, kc, cb * 128:(cb + 1) * 128], aT[:, kc, tg * 512:(tg + 1) * 512],
                       kc == 0, kc == KC - 1, [('wbf', i % 2), 'aT'], [('ps', b)])
                TT(P, 'dve', rs[r][:], psb[b][:], rs[r][:], ALU.add, [('ps', b), ('rs', r)], [('rs', r)])
                DMA(P, 'sp', x1T[ob * 128:(ob + 1) * 128, c0:c0 + 512], rs[r][:], [('rs', r)], [], f'd_o{r}')


def emit_bout(nc, P, st, yT, zT, x1T, bnwd, W, x2T, p0, TP=1024):
    KI = 32
    NTG = TP // 512
    sb = lambda name, shape, dt: st.enter_context(nc.sbuf_tensor(f"p{_PH[0]}_" + name, shape, dt))
    ones = sb("ones", [128, 128], F32)
    bnw = sb("bnws", [128, KI], F32)
    gyb = sb("gyb", [128, KI, TP], BF16)
    rstd = sb("rstd", [128, TP], F32)
    yb = [sb(f"yb{i}", [128, 4, 512], F32) for i in range(2)]
    zb = [sb(f"zb{i}", [128, 4, 512], F32) for i in range(2)]
    sq = [sb(f"sq{i}", [128, 512], F32) for i in range(2)]
    wbf = [sb(f"wbf{i}", [128, KI, 256], BF16) for i in range(3)]
    rs = [sb(f"rs{i}", [128, 512], F32) for i in range(4)]
    t32 = [sb(f"t32{i}", [128, 512], F32) for i in range(2)]
    psb = [st.enter_context(nc.psum_tensor(f"p{_PH[0]}_ps{i}", [128, 512], F32)) for i in range(8)]
    cnt = {}

    def nxt(k, n):
        v = cnt.get(k, 0)
        cnt[k] = v + 1
        return v % n
    P.op('dve', 'memset', dict(ap=ones[:], constant=1.0), [], ['ones'])
    DMA(P, 'sp', bnw[:], bnwd[:, :], [], ['bnw'], 'd_bnw')
    wi = [0]

    def load_w(ci):
        s = wi[0] % 3
        wi[0] += 1
        DMA(P, 'pool', wbf[s][:], W[:, ci * 256:(ci + 1) * 256].rearrange("(kc p) c -> p kc c", p=128),
            [], [('wbf', s)], f'd_w{s}')
        return s
    slots = {0: load_w(0), 1: load_w(1)}
    for tg in range(NTG):
        c0 = p0 + tg * 512
        for k4 in range(KI // 4):
            s = nxt('yz', 2)
            DMA(P, 'sp', yb[s][:], yT[k4 * 512:(k4 + 1) * 512, c0:c0 + 512].rearrange("(k p) t -> p k t", p=128),
                [], [('yb', s)], f'd_y{s}')
            DMA(P, 'sp', zb[s][:], zT[k4 * 512:(k4 + 1) * 512, c0:c0 + 512].rearrange("(k p) t -> p k t", p=128),
                [], [('zb', s)], f'd_z{s}')
            ACTV(P, zb[s][:], zb[s][:], AF.Silu, [('zb', s)], [('zb', s)])
            TT(P, 'dve', yb[s][:], yb[s][:], zb[s][:], ALU.mult, [('yb', s), ('zb', s)], [('yb', s)])
            for k in range(4):
                kc = k4 * 4 + k
                q = nxt('sq', 2)
                ACTV(P, sq[q][:], yb[s][:, k, :], AF.Square, [('yb', s)], [('sq', q)])
                MM(P, psb[7][:], ones[:], sq[q][:], kc == 0, kc == KI - 1, [('sq', q), 'ones'], [('ps', 7)])
                TS(P, 'pool', gyb[:, kc, tg * 512:(tg + 1) * 512], yb[s][:, k, :], bnw[:, kc:kc + 1], 1.0,
                   ALU.mult, ALU.mult, [('yb', s), 'bnw'], [('gyb', kc, tg)])
        rv = rstd[:, tg * 512:(tg + 1) * 512]
        TS(P, 'dve', rv, psb[7][:], 1.0 / 4096.0, EPS, ALU.mult, ALU.add, [('ps', 7)], [('rstd', tg)])
        ACTV(P, rv, rv, AF.Sqrt, [('rstd', tg)], [('rstd', tg)])
        P.op('dve', 'reciprocal', dict(out=rv, in_=rv), [('rstd', tg)], [('rstd', tg)])
    for ci in range(8):
        if ci + 2 < 8:
            slots[ci + 2] = load_w(ci + 2)
        s = slots[ci]
        for cb in range(2):
            ob = ci * 2 + cb
            for tg in range(NTG):
                c0 = p0 + tg * 512
                r = nxt('rs', 4)
                DMA(P, 'sp', rs[r][:], x1T[ob * 128:(ob + 1) * 128, c0:c0 + 512], [], [('rs', r)], f'd_r{r}')
                b = nxt('ps', 6)
                for kc in range(KI):
                    MM(P, psb[b][:], wbf[s][:, kc, cb * 128:(cb + 1) * 128], gyb[:, kc, tg * 512:(tg + 1) * 512],
                       kc == 0, kc == KI - 1, [('wbf', s), ('gyb', kc, tg)], [('ps', b)])
                t = nxt('t32', 2)
                TT(P, 'dve', t32[t][:], psb[b][:], rstd[:, tg * 512:(tg + 1) * 512], ALU.mult,
                   [('ps', b), ('rstd', tg)], [('t32', t)])
                TT(P, 'pool', rs[r][:], t32[t][:], rs[r][:], ALU.add, [('t32', t), ('rs', r)], [('rs', r)])
                DMA(P, 'sp', x2T[ob * 128:(ob + 1) * 128, c0:c0 + 512], rs[r][:], [('rs', r)], [], f'd_o{r}')


def emit_fnorm(nc, P, st, xT, nwd, oT, NT):
    sb = lambda name, shape, dt: st.enter_context(nc.sbuf_tensor(f"p{_PH[0]}_" + name, shape, dt))
    ones = sb("ones", [128, 128], F32)
    nw = sb("nw_sb", [128, KC], F32)
    xt = [sb(f"xt{i}", [128, KC, 512], F32) for i in range(2)]
    sq = [sb(f"sq{i}", [128, 512], F32) for i in range(2)]
    rstd = sb("rstd", [128, 512], F32)
    ps = st.enter_context(nc.psum_tensor(f"p{_PH[0]}_ps0", [128, 512], F32))
    P.op('dve', 'memset', dict(ap=ones[:], constant=1.0), [], ['ones'])
    DMA(P, 'sp', nw[:], nwd[:, :], [], ['nw'], 'd_nw')
    for tg in range(NT // 512):
        xr = ('xt', tg % 2)
        DMA(P, 'sp', xt[tg % 2][:], xT[:, tg * 512:(tg + 1) * 512].rearrange("(kc p) t -> p kc t", p=128),
            [], [xr], f'd_x{tg % 2}')
        rmsnorm_stats(P, xt[tg % 2], ones, ('ps', 0), ps, rstd[:], sq, xr, D, KC)
        for kc in range(KC):
            STT(P, xt[tg % 2][:, kc, :], xt[tg % 2][:, kc, :], nw[:, kc:kc + 1], rstd[:],
                ALU.mult, ALU.mult, [xr, 'rstd', 'nw'], [xr])
        DMA(P, 'sp', oT[:, tg * 512:(tg + 1) * 512].rearrange("(kc p) t -> p kc t", p=128), xt[tg % 2][:],
            [xr], [], f'd_o{tg % 2}')


SEQ = 4096
_DBG = None


def build_fused(phases=None):
    nc = bass.Bass("TRN2", target_bir_lowering=False)
    ein = lambda name, shape, dt: nc.dram_tensor(name, list(shape), dt, kind="ExternalInput").ap()
    scr = lambda name, shape, dt: nc.dram_tensor(name, list(shape), dt, kind="Internal").ap()
    dr = dict(
        xT=ein("xT", [D, SEQ], F32), nwA=ein("nwA", [128, KC], F32), nwB=ein("nwB", [128, KC], F32),
        nwF=ein("nwF", [128, KC], F32), Wa=ein("Wa", [D, 9296], F32), Wo=ein("Wo", [D, D], F32),
        Wb=ein("Wb", [D, 10304], F32), Wbo=ein("Wbo", [4096, D], F32), bnw=ein("bnw", [128, 32], F32),
        bfar=ein("bfar", [128, 16], F32), ident=ein("ident", [128, 128], BF16), cm=ein("cm", [2, 128, 512], F32),
        biasN=ein("biasN", [2, 16, 128, 768], F32), U=ein("U", [128, 128], F32), ones128=ein("ones128", [128, 128], F32),
        sel=ein("sel", [32, 32 * 128], F32), tri=ein("tri", [128, 128], F32), identf=ein("identf", [128, 128], F32),
        cw=ein("cw", [2, 128, NCT * 4], F32), cb=ein("cb", [2, 128, NCT], F32), dtb=ein("dtb", [2, 128, NH], F32),
        alog=ein("alog", [2, 128, NH], F32), Dv=ein("Dv", [2, 128, NH], F32),
        qT=scr("qT", [2048, SEQ], BF16), kT=scr("kT", [2048, SEQ], BF16), iqT=scr("iqT", [1024, SEQ], BF16),
        ikT=scr("ikT", [64, SEQ], BF16), v=scr("v", [SEQ, 2048], BF16), sg=scr("sg", [SEQ, 2048], BF16),
        iw=scr("iw", [SEQ, 16], F32), uT=scr("uT", [2048, SEQ], BF16), x1T=scr("x1T", [D, SEQ], F32),
        zT=scr("zT", [4096, SEQ], F32), xbcT=scr("xbcT", [6144, SEQ], F32), dt=scr("dt", [SEQ, 64], F32),
        yT=scr("yT", [4096, SEQ], F32), x2T=scr("x2T", [D, SEQ], F32),
    )
    oT = nc.dram_tensor("oT", [D, SEQ], F32, kind="ExternalOutput").ap()
    plist = []
    for hf in range(2):
        plist.append(('projA', lambda P, st, hf=hf: emit_proj(nc, P, st, dr['xT'], dr['Wa'], dr['nwA'], table_a(), dr,
                                                              hf * 2048, 2048, BF16)))
    for par in range(2):
        plist.append(('attn', lambda P, st, par=par: emit_k2(nc, P, st, par, dr)))
    for hf in range(2):
        plist.append(('aout', lambda P, st, hf=hf: emit_aout(nc, P, st, dr['uT'], dr['xT'], dr['Wo'], dr['x1T'], hf * 2048, 2048)))
    for hf in range(2):
        plist.append(('projB', lambda P, st, hf=hf: emit_proj(nc, P, st, dr['x1T'], dr['Wb'], dr['nwB'], table_b(), dr,
                                                              hf * 2048, 2048, F32)))
    for hf in range(2):
        plist.append(('ssd', lambda P, st, hf=hf: emit_k4(nc, P, st, hf, dr)))
    for ps_ in range(4):
        plist.append(('bout', lambda P, st, ps_=ps_: emit_bout(nc, P, st, dr['yT'], dr['zT'], dr['x1T'], dr['bnw'], dr['Wbo'],
                                                               dr['x2T'], ps_ * 1024)))
    plist.append(('fnorm', lambda P, st: emit_fnorm(nc, P, st, dr['x2T'], dr['nwF'], oT, SEQ)))
    with contextlib.ExitStack() as g:
        P = Prog(nc, g)
        for i, (name, fn) in enumerate(plist):
            if phases is not None and i not in phases:
                continue
            _PH[0] = i
            with contextlib.ExitStack() as st:
                fn(P, st)
                P.end_phase(st)
    return nc


def _pk(w, n):
    return np.ascontiguousarray(np.asarray(w, np.float32).reshape(n, 128).T)


def _bc(a):
    a = np.asarray(a, np.float32)
    return np.ascontiguousarray(np.broadcast_to(a[None, :], (128, a.shape[0])))


def host_inputs(b, x, norm_w, a_w_in, a_w_out, rel_bias, b_w_in, b_conv_w, b_conv_b, b_dt_bias, b_a_log, b_d,
                b_norm_w, b_w_out, final_norm_w, shared):
    A = np.ascontiguousarray
    im = dict(shared)
    im["xT"] = A(np.asarray(x[b], np.float32).T)
    return im


def host_shared(norm_w, a_w_in, a_w_out, rel_bias, b_w_in, b_conv_w, b_conv_b, b_dt_bias, b_a_log, b_d,
                b_norm_w, b_w_out, final_norm_w):
    A = np.ascontiguousarray
    f = lambda a: np.asarray(a, np.float32)
    sh = dict(nwA=_pk(norm_w[0], KC), nwB=_pk(norm_w[1], KC), nwF=_pk(final_norm_w, KC), Wa=A(f(a_w_in[0])),
              Wo=A(f(a_w_out[0])), Wb=A(f(b_w_in[0])), Wbo=A(f(b_w_out[0])), bnw=_pk(b_norm_w[0], 32))
    rb = f(rel_bias)
    c0 = k2_consts(rb, 0)
    c1 = k2_consts(rb, 1)
    sh.update(bfar=c0['bfar'], ident=c0['ident'], cm=A(np.stack([c0['cm'], c1['cm']])),
              biasN=A(np.stack([c0['biasN'], c1['biasN']])))
    k4c = k4_consts()
    sh.update(U=k4c['U'], ones128=k4c['ones128'], sel=k4c['sel'], tri=k4c['tri'], identf=k4c['identf'])
    cw = f(b_conv_w[0])
    cbv = f(b_conv_b[0])
    cws, cbs, dtbs, alogs, Dvs = [], [], [], [], []
    for hf in range(2):
        chans = np.concatenate([np.arange(hf * 2048, (hf + 1) * 2048), 4096 + np.arange(hf * 512, (hf + 1) * 512),
                                5120 + np.arange(hf * 512, (hf + 1) * 512)])
        hs = slice(hf * 32, (hf + 1) * 32)
        cws.append(cw[:, chans].T.reshape(NCT, 128, 4).transpose(1, 0, 2).reshape(128, NCT * 4))
        cbs.append(cbv[chans].reshape(NCT, 128).T)
        dtbs.append(_bc(f(b_dt_bias[0])[hs]))
        alogs.append(_bc(f(b_a_log[0])[hs]))
        Dvs.append(_bc(f(b_d[0])[hs]))
    sh.update(cw=A(np.stack(cws)), cb=A(np.stack(cbs)), dtb=A(np.stack(dtbs)), alog=A(np.stack(alogs)), Dv=A(np.stack(Dvs)))
    return sh


NCORES = 8


def kernel(x, norm_w, a_w_in, a_w_out, rel_bias, b_w_in, b_conv_w, b_conv_b, b_dt_bias, b_a_log, b_d,
           b_norm_w, b_w_out, final_norm_w):
    nc = build_fused()
    sh = host_shared(norm_w, a_w_in, a_w_out, rel_bias, b_w_in, b_conv_w, b_conv_b, b_dt_bias, b_a_log, b_d,
                     b_norm_w, b_w_out, final_norm_w)
    ims = []
    for c in range(NCORES):
        im = dict(sh)
        im["xT"] = np.ascontiguousarray(np.asarray(x[c % 4], np.float32).T)
        ims.append(im)
    res = run_bass_kernel_spmd(nc, ims, core_ids=list(range(NCORES)))
    out = np.zeros((4, SEQ, D), np.float32)
    for b in range(4):
        out[b] = np.asarray(res.results[b]["oT"]).T
    return out
```

```python
import contextlib
import numpy as np
import ml_dtypes
import concourse.bass as bass
import concourse.mybir as mybir
from concourse.bass_utils import run_bass_kernel_spmd

F32 = mybir.dt.float32
_PH = [0]
BF16 = mybir.dt.bfloat16
ALU = mybir.AluOpType
AF = mybir.ActivationFunctionType
AX = mybir.AxisListType

ENGS = ['pe', 'act', 'dve', 'pool', 'sp']


def _is_psum(r):
    n = r[0] if isinstance(r, tuple) else r
    return isinstance(n, str) and (n.startswith('ps') or n.startswith('acc'))


class Prog:
    def __init__(self, nc, gstack):
        self.nc = nc
        self.gstack = gstack
        self.sems = {}
        self.semval = {}
        self.base = {}
        self.dma_keys = set()
        self.total_instr = {e: 0 for e in ENGS}
        self._reset()

    def _reset(self):
        self.q = {e: [] for e in ENGS}
        self.waited = {e: {} for e in ENGS}
        self.res = {}
        self.touched = set()

    def op(self, eng, meth, kw, reads=(), writes=(), dma=None):
        fn = (meth, kw)
        waits = {}
        own = 'c_' + eng
        xr = [r for r in reads if _is_psum(r)]
        if xr:
            writes = list(writes) + [r for r in xr if r not in writes]

        def need(kv, raw):
            if kv is None:
                return
            k, v = kv
            if k == own and not raw:
                return
            if k == own and eng == 'pe':
                return
            if self.waited[eng].get(k, 0) >= v:
                return
            if waits.get(k, 0) < v:
                waits[k] = v

        for r in reads:
            st = self.res.get(r)
            if st:
                need(st['w'], True)
        for w in writes:
            st = self.res.get(w)
            if st:
                need(st['w'], False)
                for kv in st['r'].items():
                    need(kv, False)
        if dma:
            key, inc = dma, 16
            self.dma_keys.add(key)
        else:
            key, inc = own, 1
        self.semval[key] = self.semval.get(key, 0) + inc
        self.touched.add(key)
        val = self.semval[key]
        for k, v in waits.items():
            self.waited[eng][k] = v
        self.q[eng].append([fn, sorted(waits.items()), key, inc, val])
        for r in reads:
            st = self.res.setdefault(r, {'w': None, 'r': {}})
            st['r'][key] = val
        for w in writes:
            self.res[w] = {'w': (key, val), 'r': {}}
        return (key, val)

    def end_phase(self, pstack):
        nc = self.nc
        fin = [(k, self.semval[k]) for k in sorted(self.touched)]
        for e in ENGS:
            self.q[e].append([None, list(fin), None, 0, 0])
        waited_vals = {}
        for e in ENGS:
            for fn, waits, key, inc, val in self.q[e]:
                for k, v in waits:
                    waited_vals.setdefault(k, set()).add(v)
        remap = {}
        for k, vs in waited_vals.items():
            if k in self.dma_keys:
                continue
            b0 = self.base.get(k, 0)
            remap[k] = {v: b0 + i + 1 for i, v in enumerate(sorted(vs))}
        for k in self.touched:
            if k not in self.sems:
                self.sems[k] = self.gstack.enter_context(nc.semaphore("s_" + k))
        sems = self.sems
        block = pstack.enter_context(nc.Block())
        engmap = {'pe': block.tensor, 'act': block.scalar, 'dve': block.vector,
                  'pool': block.gpsimd, 'sp': block.sync}
        for e in ENGS:
            ops = self.q[e]
            self.total_instr[e] += len(ops)

            def body(engine, ops=ops):
                for fn, waits, key, inc, val in ops:
                    for k, v in waits:
                        if k in self.dma_keys:
                            engine.wait_ge(sems[k], v)
                        else:
                            engine.wait_ge(sems[k], remap[k][v])
                    if fn is None:
                        continue
                    ins = getattr(engine, fn[0])(**fn[1])
                    if key in self.dma_keys:
                        ins.then_inc(sems[key], 16)
                    elif key in remap and val in remap[key]:
                        ins.then_inc(sems[key], 1)
            engmap[e](body)
        for k, m in remap.items():
            self.base[k] = self.base.get(k, 0) + len(m)
        self._reset()


def MM(P, out, lhsT, rhs, start, stop, reads, writes, **kw):
    return P.op('pe', 'matmul', dict(out=out, lhsT=lhsT, rhs=rhs, start=start, stop=stop, **kw), reads, writes)


def ACTV(P, out, in_, func, reads, writes, **kw):
    return P.op('act', 'activation', dict(out=out, in_=in_, func=func, **kw), reads, writes)


def DMA(P, eng, out, in_, reads, writes, key):
    return P.op(eng, 'dma_start', dict(out=out, in_=in_), reads, writes, dma=key)


def TS(P, eng, out, in0, s1, s2, op0, op1, reads, writes, **kw):
    d = dict(out=out, in0=in0, scalar1=s1, scalar2=s2, op0=op0, **kw)
    if op1 is not None:
        d['op1'] = op1
    return P.op(eng, 'tensor_scalar', d, reads, writes)


def TT(P, eng, out, in0, in1, op, reads, writes):
    return P.op(eng, 'tensor_tensor', dict(out=out, in0=in0, in1=in1, op=op), reads, writes)


def STT(P, out, in0, scalar, in1, op0, op1, reads, writes, **kw):
    return P.op('dve', 'scalar_tensor_tensor', dict(out=out, in0=in0, scalar=scalar, in1=in1, op0=op0, op1=op1, **kw),
                reads, writes)


def CP(P, eng, out, in_, reads, writes):
    if eng == 'act':
        return P.op('act', 'activation', dict(out=out, in_=in_, func=AF.Copy), reads, writes)
    return P.op(eng, 'tensor_copy', dict(out=out, in_=in_), reads, writes)


D = 2048
KC = D // 128
EPS = 1e-6


S = 4096
NIT = 26
SCALE = 128 ** -0.5


def own_blocks(par):
    return [i for i in range(32) if (i % 4 in (0, 3)) == (par == 0)]


def k2_consts(rel_bias, par):
    n = np.arange(256)
    nf = np.maximum(n, 1).astype(np.float32)
    large = 16 + (np.log(nf / 16) / np.log(128 / 16) * 16).astype(np.int32)
    large = np.minimum(large, 31)
    bucket = np.where(n < 16, n, large)
    s = np.arange(128)[:, None]
    q = np.arange(128)[None, :]
    biasT = np.zeros((128, 16, 2, 128), np.float32)
    for d in range(2):
        dist = np.clip(d * 128 + q - s, 0, 255)
        biasT[:, :, d, :] = rel_bias[bucket[dist]].transpose(0, 2, 1)
    bfar = np.ascontiguousarray(np.broadcast_to(rel_bias[31][None, :], (128, 16))).astype(np.float32)
    negtri = np.where(np.arange(128)[None, :] <= np.arange(128)[:, None], 0.0, -1e30).astype(np.float32)
    ident = np.eye(128, dtype=np.float32).astype(ml_dtypes.bfloat16)
    dtab = lambda sp, w: (1 - w) if (sp == par) else (2 - w)
    cm = np.zeros((128, 2, 2, 128), np.float32)
    biasN = np.zeros((16, 128, 2, 3, 128), np.float32)
    for sp in range(2):
        for w in range(3):
            d = dtab(sp, w)
            if d >= 2:
                biasN[:, :, sp, w, :] = rel_bias[31][:, None, None]
            elif d >= 0:
                biasN[:, :, sp, w, :] = biasT[:, :, d, :].transpose(1, 0, 2)
            if w >= 1:
                if d == 0:
                    cm[:, sp, w - 1, :] = negtri
                elif d < 0:
                    cm[:, sp, w - 1, :] = -1e30
    return dict(bfar=bfar, ident=ident, cm=cm.reshape(128, 512), biasN=biasN.reshape(16, 128, 768))


def emit_k2(nc, P, st, par, dr, nslots=16, nheads=16, nit=NIT):
    own = own_blocks(par)
    rsel = (0, 3) if par == 0 else (1, 2)
    NQB = nslots
    nkbs = [2 * (qi + 1) for qi in range(nslots)]
    NQ = NQB * 128
    offs = []
    T = 0
    for nkb in nkbs:
        offs.append(T)
        T += nkb
    kT, v, ikT, qT, iqT, iw, sg, uT = dr['kT'], dr['v'], dr['ikT'], dr['qT'], dr['iqT'], dr['iw'], dr['sg'], dr['uT']
    biasNd, bfard, cmd, identd = dr['biasN'], dr['bfar'], dr['cm'], dr['ident']
    if True:
        sb = lambda name, shape, dt: st.enter_context(nc.sbuf_tensor(f"p{_PH[0]}_" + name, shape, dt))
        maskT = sb("maskT", [128, T, 128], BF16)
        score = sb("score", [128, S], F32)
        maskq = sb("maskq", [128, S], BF16)
        relu = [sb(f"relu{i}", [128, 512], F32) for i in range(2)]
        ikTs = sb("ikTs", [64, S], BF16)
        iqb = [sb(f"iqb{i}", [64, 16, 128], BF16) for i in range(2)]
        iwb = [sb(f"iwb{i}", [128, 16], F32) for i in range(2)]
        kTh = [sb(f"kTh{i}", [128, S], BF16) for i in range(2)]
        vh = [sb(f"vh{i}", [128, 32, 136], BF16) for i in range(2)]
        qTh = [sb(f"qTh{i}", [128, NQ], BF16) for i in range(2)]
        sgb = [sb(f"sgb{i}", [128, NQB, 128], BF16) for i in range(2)]
        biasN = [sb(f"biasN{i}", [128, 2, 3, 128], F32) for i in range(2)]
        bfar = sb("bfars", [128, 16], F32)
        cm = sb("cms", [128, 2, 256], F32)
        ident = sb("idents", [128, 128], BF16)
        hi = sb("hi", [128, 1], F32)
        lo = sb("lo", [128, 1], F32)
        w = sb("w", [128, 1], F32)
        mid = sb("mid", [128, 1], F32)
        cnt = sb("cnt", [128, 1], F32)
        ge = sb("ge", [128, 1], F32)
        rinv = sb("rinv", [128, 1], F32)
        NE = 6
        tb = [sb(f"tb{i}", [128, 128], F32) for i in range(2)]
        eb = [sb(f"eb{i}", [128, 512], BF16) for i in range(NE)]
        pt = [sb(f"pt{i}", [128, 512], BF16) for i in range(NE)]
        o32 = [sb(f"o32{i}", [128, 128], F32) for i in range(2)]
        ust = [sb(f"ust{i}", [128, 128], BF16) for i in range(4)]
        usT = [sb(f"usT{i}", [128, 128], BF16) for i in range(4)]
        psb = [st.enter_context(nc.psum_tensor(f"p{_PH[0]}_ps{i}", [128, 512], F32)) for i in range(4)]
        pst = [st.enter_context(nc.psum_tensor(f"p{_PH[0]}_pst{i}", [128, 1024], BF16)) for i in range(2)]
        acc = [st.enter_context(nc.psum_tensor(f"p{_PH[0]}_acc{i}", [128, 512], F32)) for i in range(2)]
        cn = {}

        def nxt(k, n):
            vv = cn.get(k, 0)
            cn[k] = vv + 1
            return vv % n
        DMA(P, 'sp', ikTs[:], ikT[:, :], [], ['ikT'], 'd_c0')
        DMA(P, 'sp', cm[:], cmd[par].rearrange("p (a b) -> p a b", a=2), [], ['cm'], 'd_c1')
        DMA(P, 'sp', ident[:], identd[:, :], [], ['ident'], 'd_c2')
        DMA(P, 'sp', bfar[:], bfard[:, :], [], ['bfar'], 'd_c3')
        for i in range(2):
            P.op('pool', 'memset', dict(ap=vh[i][:, :, 128:129], constant=1.0), [], [('vh1', i)])

        for qi in range(nslots):
            nkb = nkbs[qi]
            nk = nkb * 128
            s2 = qi % 2
            DMA(P, 'sp', iqb[s2][:], iqT[:, own[qi] * 128:(own[qi] + 1) * 128].rearrange("(h d) q -> d h q", d=64),
                [], [('iqb', s2)], f'd_iq{s2}')
            DMA(P, 'sp', iwb[s2][:], iw[own[qi] * 128:(own[qi] + 1) * 128, :], [], [('iwb', s2)], f'd_iw{s2}')
            nch = (nk + 511) // 512
            for c in range(nch):
                kw = min(512, nk - c * 512)
                for hh in range(16):
                    b = nxt('ps', 4)
                    MM(P, psb[b][:, :kw], iqb[s2][:, hh, :], ikTs[:, c * 512:c * 512 + kw], True, True,
                       [('iqb', s2), 'ikT'], [('ps', b)])
                    rb = nxt('relu', 2)
                    ACTV(P, relu[rb][:, :kw], psb[b][:, :kw], AF.Relu, [('ps', b)], [('relu', rb)])
                    sc = score[:, c * 512:c * 512 + kw]
                    if hh == 0:
                        TS(P, 'dve', sc, relu[rb][:, :kw], iwb[s2][:, 0:1], None, ALU.mult, None,
                           [('relu', rb), ('iwb', s2)], [('score', c)])
                    else:
                        STT(P, sc, relu[rb][:, :kw], iwb[s2][:, hh:hh + 1], sc, ALU.mult, ALU.add,
                            [('relu', rb), ('iwb', s2), ('score', c)], [('score', c)])
            scr = [('score', c) for c in range(nch)]
            P.op('dve', 'tensor_reduce', dict(out=hi[:], in_=score[:, :nk], axis=AX.X, op=ALU.max), scr, ['hi'])
            P.op('dve', 'tensor_reduce', dict(out=lo[:], in_=score[:, :nk], axis=AX.X, op=ALU.min), scr, ['lo'])
            TT(P, 'dve', w[:], hi[:], lo[:], ALU.subtract, ['hi', 'lo'], ['w'])
            dg = score[:, (nkb - 2) * 128:nkb * 128]
            TT(P, 'dve', dg, dg, cm[:, qi % 2, :], ALU.add, [('score', (nkb - 2) // 4), 'cm'], [('score', (nkb - 2) // 4)])
            for it in range(nit):
                TS(P, 'dve', w[:], w[:], 0.5, None, ALU.mult, None, ['w'], ['w'])
                TT(P, 'dve', mid[:], lo[:], w[:], ALU.add, ['lo', 'w'], ['mid'])
                TS(P, 'dve', maskq[:, :nk], score[:, :nk], mid[:, 0:1], 0.0, ALU.is_ge, ALU.add,
                   scr + ['mid'], ['maskq', 'cnt'], accum_out=cnt[:, 0:1])
                TS(P, 'dve', ge[:], cnt[:], 255.5, None, ALU.is_ge, None, ['cnt'], ['ge'])
                STT(P, lo[:], ge[:], w[:, 0:1], lo[:], ALU.mult, ALU.add, ['ge', 'w', 'lo'], ['lo'])
            TS(P, 'dve', maskq[:, :nk], score[:, :nk], lo[:, 0:1], None, ALU.is_ge, None, scr + ['lo'], ['maskq'])
            for j in range(nkb):
                b = nxt('pst', 2)
                P.op('pe', 'transpose', dict(out=pst[b][:, :128], in_=maskq[:, j * 128:(j + 1) * 128], identity=ident[:]),
                     ['maskq', 'ident'], [('pst', b)])
                CP(P, 'act', maskT[:, offs[qi] + j, :], pst[b][:, :128], [('pst', b)], [('maskT', qi)])

        def head_loads(h):
            s2 = h % 2
            DMA(P, 'sp', kTh[s2][:], kT[h * 128:(h + 1) * 128, :], [], [('kTh', s2)], f'd_k{s2}')
            DMA(P, 'sp', vh[s2][:, :, 0:128], v[:, h * 128:(h + 1) * 128].rearrange("(j p) d -> p j d", p=128),
                [], [('vh', s2)], f'd_v{s2}')
            for k in range(2):
                DMA(P, 'sp', qTh[s2][:].rearrange("p (m k t) -> p m k t", k=2, t=128)[:, :, k, :],
                    qT[h * 128:(h + 1) * 128, :].rearrange("p (m r t) -> p m r t", r=4, t=128)[:, :, rsel[k], :],
                    [], [('qTh', s2)], f'd_q{s2}')
            for k in range(2):
                DMA(P, 'sp', sgb[s2][:].rearrange("p (m k) d -> p m k d", k=2)[:, :, k, :],
                    sg[:, h * 128:(h + 1) * 128].rearrange("(m r p) d -> p m r d", r=4, p=128)[:, :, rsel[k], :],
                    [], [('sgb', s2)], f'd_sg{s2}')
            DMA(P, 'sp', biasN[s2][:], biasNd[par, h].rearrange("p (a w q) -> p a w q", a=2, w=3), [], [('biasN', s2)], f'd_bn{s2}')

        units = []
        for h in range(nheads):
            for qi in range(nslots):
                nkb = nkbs[qi]
                nfar = max(0, nkb - 3)
                j = 0
                while j < nfar:
                    n = min(4, nfar - j)
                    units.append((h, qi, j, n, True))
                    j += n
                for j in range(nfar, nkb):
                    units.append((h, qi, j, 1, False))
        ubuf = {}

        def stage_a(t):
            h, qi, j0, n, far = units[t]
            s2 = h % 2
            nkb = nkbs[qi]
            b = nxt('ps', 4)
            for k in range(n):
                MM(P, psb[b][:, k * 128:(k + 1) * 128], kTh[s2][:, (j0 + k) * 128:(j0 + k + 1) * 128],
                   qTh[s2][:, qi * 128:(qi + 1) * 128], True, True, [('kTh', s2), ('qTh', s2)], [('ps', b)])
            e = nxt('eb', NE)
            if far:
                ACTV(P, eb[e][:, :n * 128], psb[b][:, :n * 128], AF.Exp, [('ps', b), 'bfar'], [('eb', e)],
                     scale=SCALE, bias=bfar[:, h:h + 1])
            else:
                wv = j0 - (nkb - 3)
                tt_ = nxt('tb', 2)
                STT(P, tb[tt_][:], psb[b][:, :128], SCALE, biasN[s2][:, qi % 2, wv, :], ALU.mult, ALU.add,
                    [('ps', b), ('biasN', s2)], [('tb', tt_)])
                ACTV(P, eb[e][:, :128], tb[tt_][:], AF.Exp, [('tb', tt_)], [('eb', e)])
            p = nxt('pt', NE)
            me = 'dve' if nxt('me', 2) == 0 else 'pool'
            TT(P, me, pt[p][:, :n * 128], eb[e][:, :n * 128],
               maskT[:, offs[qi] + j0:offs[qi] + j0 + n, :].rearrange("p a b -> p (a b)"), ALU.mult,
               [('eb', e), ('maskT', qi)], [('pt', p)])
            ubuf[t] = p

        def stage_b(t):
            h, qi, j0, n, far = units[t]
            s2 = h % 2
            nkb = nkbs[qi]
            ab = (h * nslots + qi) % 2
            p = ubuf.pop(t)
            for k in range(n):
                j = j0 + k
                MM(P, acc[ab][:, :129], pt[p][:, k * 128:(k + 1) * 128], vh[s2][:, j, 0:129], j == 0, j == nkb - 1,
                   [('pt', p), ('vh', s2), ('vh1', s2)], [('acc', ab)])
            if j0 + n != nkb:
                return
            P.op('dve', 'reciprocal', dict(out=rinv[:], in_=acc[ab][:, 128:129]), [('acc', ab)], ['rinv'])
            o = nxt('o32', 2)
            TS(P, 'dve', o32[o][:], acc[ab][:, :128], rinv[:, 0:1], None, ALU.mult, None,
               [('acc', ab), 'rinv'], [('o32', o)])
            us = nxt('ust', 4)
            TT(P, 'pool', ust[us][:], o32[o][:], sgb[s2][:, qi, :], ALU.mult, [('o32', o), ('sgb', s2)], [('ust', us)])
            tp = nxt('pst', 2)
            P.op('pe', 'transpose', dict(out=pst[tp][:, :128], in_=ust[us][:], identity=ident[:]),
                 [('ust', us), 'ident'], [('pst', tp)])
            CP(P, 'act', usT[us][:], pst[tp][:, :128], [('pst', tp)], [('usT', us)])
            DMA(P, 'sp', uT[h * 128:(h + 1) * 128, own[qi] * 128:(own[qi] + 1) * 128], usT[us][:], [('usT', us)], [], f'd_u{us}')
            if qi == nslots - 1 and h + 2 < nheads:
                head_loads(h + 2)

        LA = 3
        head_loads(0)
        if nheads > 1:
            head_loads(1)
        for t in range(len(units) + LA):
            if t < len(units):
                stage_a(t)
            if t - LA >= 0:
                stage_b(t - LA)


NH = 32
NG = 4
NCT = 24


def k4_consts():
    U = np.triu(np.ones((128, 128), np.float32))
    sel = np.zeros((32, 32, 128), np.float32)
    for h in range(32):
        sel[h, h, :] = 1.0
    return dict(U=U, ones128=np.ones((128, 128), np.float32), sel=sel.reshape(32, 32 * 128),
                tri=U.copy(), identf=np.eye(128, dtype=np.float32),
                identb=np.eye(128, dtype=np.float32).astype(ml_dtypes.bfloat16))


class _Stop(Exception):
    pass


def emit_k4(nc, P, st, hf, dr, npieces=8, stop=99):
    SS = npieces * 512
    xbcT, dtd, yT = dr['xbcT'], dr['dt'], dr['yT']
    cwd, cbd, dtbd, alogd, Dd = dr['cw'][hf], dr['cb'][hf], dr['dtb'][hf], dr['alog'][hf], dr['Dv'][hf]
    Ud, onesd, seld, trid, identfd, identbd = dr['U'], dr['ones128'], dr['sel'], dr['tri'], dr['identf'], dr['ident']
    if True:
        sb = lambda name, shape, dt: st.enter_context(nc.sbuf_tensor(f"p{_PH[0]}_" + name, shape, dt))
        xin = sb("xin", [128, 12, 515], F32)
        xc = sb("xc", [128, 16, 512], F32)
        BTc = sb("BTc", [128, 4, 512], BF16)
        CTc = sb("CTc", [128, 4, 512], BF16)
        cacc = [sb(f"cacc{i}", [128, 512], F32) for i in range(2)]
        cw = sb("cws", [128, NCT, 4], F32)
        cb = sb("cbs", [128, NCT], F32)
        dtb = sb("dtbs", [128, NH], F32)
        Aneg = sb("Aneg", [128, NH], F32)
        Dv = sb("Dvs", [128, NH], F32)
        U = sb("Us", [128, 128], F32)
        ones128 = sb("ones128s", [128, 128], F32)
        sel = sb("sels", [32, 32, 128], F32)
        tri = sb("tris", [128, 128], F32)
        identf = sb("identfs", [128, 128], F32)
        identb = sb("identbs", [128, 128], BF16)
        dtr = sb("dtr", [128, 4, NH], F32)
        dtp = sb("dtp", [128, 4, NH], F32)
        adt = sb("adt", [128, 4, NH], F32)
        acs = sb("acs", [128, NH], F32)
        ea = sb("ea", [128, NH], F32)
        dec = sb("dec", [128, NH], F32)
        cdec = sb("cdec", [128, NH], F32)
        dtdec = sb("dtdec", [128, NH], F32)
        acsT = sb("acsT", [32, 128], F32)
        x_tm = sb("x_tm", [128, NH, 64], F32)
        xdt = sb("xdt", [128, NH, 64], BF16)
        xdtd = sb("xdtd", [128, NH, 64], BF16)
        B_tm = sb("B_tm", [128, 4, 128], BF16)
        prev32 = sb("prev32", [128, NG, 512], F32)
        prevbf = sb("prevbf", [128, NG, 512], BF16)
        cbTm = [sb(f"cbTm{i}", [128, 128], F32) for i in range(2)]
        tcl = [sb(f"tcl{i}", [128, 4, 128], F32) for i in range(2)]
        Lt = [sb(f"Lt{i}", [128, 4, 128], F32) for i in range(2)]
        MT = [sb(f"MT{i}", [128, 4, 128], BF16) for i in range(2)]
        yos = [sb(f"yos{i}", [128, 8, 64], F32) for i in range(2)]
        dsk = [sb(f"dsk{i}", [128, 8, 64], F32) for i in range(4)]
        ystage = [sb(f"ystage{i}", [128, NH, 64], F32) for i in range(2)]
        yTs = [sb(f"yTs{i}", [128, 16, 128], F32) for i in range(2)]
        ps = [st.enter_context(nc.psum_tensor(f"p{_PH[0]}_ps{i}", [128, 512], F32)) for i in range(3)]
        psmisc = st.enter_context(nc.psum_tensor(f"p{_PH[0]}_psmisc", [128, 512], F32))
        psbt = st.enter_context(nc.psum_tensor(f"p{_PH[0]}_psbt", [128, 1024], BF16))
        psbc = [st.enter_context(nc.psum_tensor(f"p{_PH[0]}_psbc{i}", [128, 512], F32)) for i in range(2)]
        psyd = st.enter_context(nc.psum_tensor(f"p{_PH[0]}_psyd", [128, 512], F32))
        cn = {}

        def nxt(k, n):
            vv = cn.get(k, 0)
            cn[k] = vv + 1
            return vv % n
        for name, t, d in [('cw', cw, cwd.rearrange("p (c k) -> p c k", k=4)), ('cb', cb, cbd),
                           ('dtb', dtb, dtbd), ('Dv', Dv, Dd), ('U', U, Ud[:, :]),
                           ('ones128', ones128, onesd[:, :]), ('sel', sel, seld[:, :].rearrange("p (h m) -> p h m", m=128)),
                           ('tri', tri, trid[:, :]), ('identf', identf, identfd[:, :]), ('identb', identb, identbd[:, :]),
                           ('Aneg', Aneg, alogd)]:
            DMA(P, 'sp', t[:], d, [], [name], 'd_' + name)
        ACTV(P, Aneg[:], Aneg[:], AF.Exp, ['Aneg'], ['Aneg'])
        TS(P, 'dve', Aneg[:], Aneg[:], -1.0, None, ALU.mult, None, ['Aneg'], ['Aneg'])
        P.op('dve', 'memset', dict(ap=prev32[:], constant=0.0), [], [('prev32', g) for g in range(NG)])
        P.op('dve', 'memset', dict(ap=prevbf[:], constant=0.0), [], [('prevbf', g) for g in range(NG)])

        def chk(n):
            if stop <= n:
                raise _Stop()
        try:
          for pc in range(npieces):
              t0 = pc * 512
              for half in range(2):
                  if half == 0:
                      srcs = [(0, 12, hf * 2048)]
                  else:
                      srcs = [(0, 4, hf * 2048 + 1536), (4, 4, 4096 + hf * 512), (8, 4, 5120 + hf * 512)]
                  if pc == 0:
                      P.op('dve', 'memset', dict(ap=xin[:, :, 0:3], constant=0.0), [], ['xin'])
                  for (c0_, n_, r0_) in srcs:
                      src = xbcT[r0_:r0_ + n_ * 128, :]
                      if pc == 0:
                          DMA(P, 'sp', xin[:, c0_:c0_ + n_, 3:515], src[:, 0:512].rearrange("(c p) t -> p c t", p=128), [], ['xin'], 'd_xin')
                      else:
                          DMA(P, 'sp', xin[:, c0_:c0_ + n_, :], src[:, t0 - 3:t0 + 512].rearrange("(c p) t -> p c t", p=128), [], ['xin'], 'd_xin')
                  for cl in range(12):
                      ct = half * 12 + cl
                      a = nxt('cacc', 2)
                      TS(P, 'dve', cacc[a][:], xin[:, cl, 3:515], cw[:, ct, 3:4], None, ALU.mult, None,
                         ['xin', 'cw'], [('cacc', a)])
                      for k in (2, 1, 0):
                          STT(P, cacc[a][:], xin[:, cl, k:k + 512], cw[:, ct, k:k + 1], cacc[a][:], ALU.mult, ALU.add,
                              ['xin', 'cw', ('cacc', a)], [('cacc', a)])
                      if ct < 16:
                          dst, dr = xc[:, ct, :], ('xc', ct)
                      elif ct < 20:
                          dst, dr = BTc[:, ct - 16, :], ('BTc', ct - 16)
                      else:
                          dst, dr = CTc[:, ct - 20, :], ('CTc', ct - 20)
                      ACTV(P, dst, cacc[a][:], AF.Silu, [('cacc', a), 'cb'], [dr], bias=cb[:, ct:ct + 1])
              chk(1)
              DMA(P, 'sp', dtr[:], dtd[t0:t0 + 512, hf * 32:(hf + 1) * 32].rearrange("(c p) h -> p c h", p=128), [], ['dtr'], 'd_dtr')
              TT(P, 'dve', dtr[:], dtr[:], dtb[:].unsqueeze(1).to_broadcast([128, 4, NH]), ALU.add, ['dtr', 'dtb'], ['dtr'])
              ACTV(P, dtp[:], dtr[:], AF.Exp, ['dtr'], ['dtp'])
              ACTV(P, dtp[:], dtp[:], AF.Ln, ['dtp', 'ones128'], ['dtp'], bias=ones128[:, 0:1])
              TT(P, 'dve', adt[:], dtp[:], Aneg[:].unsqueeze(1).to_broadcast([128, 4, NH]), ALU.mult, ['dtp', 'Aneg'], ['adt'])
              chk(2)
              for c in range(4):
                  l0 = c * 128
                  MM(P, psmisc[:, 0:NH], U[:], adt[:, c, :], True, True, ['U', 'adt'], ['psmisc'])
                  MM(P, psmisc[:, 32:32 + NH], ones128[:], adt[:, c, :], True, True, ['ones128', 'adt'], ['psmisc'])
                  MM(P, psmisc[0:32, 64:192], adt[:, c, :], U[:], True, True, ['U', 'adt'], ['psmisc'])
                  CP(P, 'dve', acs[:], psmisc[:, 0:NH], ['psmisc'], ['acs'])
                  ACTV(P, ea[:], psmisc[:, 0:NH], AF.Exp, ['psmisc'], ['ea'])
                  ACTV(P, cdec[:], psmisc[:, 32:32 + NH], AF.Exp, ['psmisc'], ['cdec'])
                  TT(P, 'dve', dec[:], psmisc[:, 32:32 + NH], acs[:], ALU.subtract, ['psmisc', 'acs'], ['dec'])
                  ACTV(P, dec[:], dec[:], AF.Exp, ['dec'], ['dec'])
                  CP(P, 'dve', acsT[:], psmisc[0:32, 64:192], ['psmisc'], ['acsT'])
                  TT(P, 'dve', dtdec[:], dtp[:, c, :], dec[:], ALU.mult, ['dtp', 'dec'], ['dtdec'])
                  chk(3)
                  for q4 in range(4):
                      b = nxt('ps', 3)
                      for k in range(4):
                          ct = q4 * 4 + k
                          P.op('pe', 'transpose', dict(out=ps[b][:, k * 128:(k + 1) * 128], in_=xc[:, ct, l0:l0 + 128],
                                                       identity=identf[:]), [('xc', ct), 'identf'], [('ps', b)])
                      CP(P, 'act', x_tm[:, q4 * 8:(q4 + 1) * 8, :].rearrange("p h d -> p (h d)"), ps[b][:, :],
                         [('ps', b)], [('x_tm', q4)])
                  xr = [('x_tm', q) for q in range(4)]
                  TT(P, 'dve', xdt[:], x_tm[:], dtp[:, c, :].unsqueeze(2).to_broadcast([128, NH, 64]), ALU.mult,
                     xr + ['dtp'], ['xdt'])
                  TT(P, 'pool', xdtd[:], x_tm[:], dtdec[:].unsqueeze(2).to_broadcast([128, NH, 64]), ALU.mult,
                     xr + ['dtdec'], ['xdtd'])
                  for g in range(NG):
                      P.op('pe', 'transpose', dict(out=psbt[:, g * 128:(g + 1) * 128], in_=BTc[:, g, l0:l0 + 128],
                                                   identity=identb[:]), [('BTc', g), 'identb'], ['psbt'])
                  CP(P, 'act', B_tm[:].rearrange("p g n -> p (g n)"), psbt[:, 0:512], ['psbt'], ['B_tm'])
                  chk(4)
                  ys = nxt('ystage', 2)
                  for g in range(NG):
                      b = nxt('ps', 3)
                      MM(P, ps[b][:, :], CTc[:, g, l0:l0 + 128], prevbf[:, g, :], True, True,
                         [('CTc', g), ('prevbf', g)], [('ps', b)])
                      yo = nxt('yos', 2)
                      TT(P, 'dve', yos[yo][:], ps[b][:, :].rearrange("p (h d) -> p h d", d=64),
                         ea[:, g * 8:(g + 1) * 8].unsqueeze(2).to_broadcast([128, 8, 64]), ALU.mult,
                         [('ps', b), 'ea'], [('yos', yo)])
                      TT(P, 'pool', dsk[g][:], x_tm[:, g * 8:(g + 1) * 8, :],
                         Dv[:, g * 8:(g + 1) * 8].unsqueeze(2).to_broadcast([128, 8, 64]), ALU.mult,
                         [('x_tm', g), 'Dv'], [('dsk', g)])
                      TT(P, 'pool', dsk[g][:], dsk[g][:], yos[yo][:], ALU.add, [('dsk', g), ('yos', yo)], [('dsk', g)])
                      b = nxt('ps', 3)
                      MM(P, ps[b][:, :], B_tm[:, g, :], xdtd[:, g * 8:(g + 1) * 8, :].rearrange("p h d -> p (h d)"), True, True,
                         ['B_tm', 'xdtd'], [('ps', b)])
                      TT(P, 'pool', prev32[:, g, :].rearrange("p (h d) -> p h d", d=64),
                         prev32[:, g, :].rearrange("p (h d) -> p h d", d=64),
                         cdec[:, g * 8:(g + 1) * 8].unsqueeze(2).to_broadcast([128, 8, 64]), ALU.mult,
                         [('prev32', g), 'cdec'], [('prev32', g)])
                      TT(P, 'dve', prev32[:, g, :], prev32[:, g, :], ps[b][:, :], ALU.add, [('prev32', g), ('ps', b)],
                         [('prev32', g)])
                      CP(P, 'act', prevbf[:, g, :], prev32[:, g, :], [('prev32', g)], [('prevbf', g)])
                  quads = [(g, hq) for g in range(NG) for hq in range(2)]
                  qbuf = {}

                  def quad_a(i):
                      g, hq = quads[i]
                      if hq == 0:
                          b = nxt('ps', 3)
                          MM(P, ps[b][:, 0:128], BTc[:, g, l0:l0 + 128], CTc[:, g, l0:l0 + 128], True, True,
                             [('BTc', g), ('CTc', g)], [('ps', b)])
                          TT(P, 'dve', cbTm[g % 2][:], ps[b][:, 0:128], tri[:], ALU.mult, [('ps', b), 'tri'], [('cbTm', g % 2)])
                      cm = g % 2
                      bb = nxt('psbc', 2)
                      tc_ = nxt('tcl', 2)
                      for hh in range(4):
                          h = g * 8 + hq * 4 + hh
                          MM(P, psbc[bb][:, hh * 128:(hh + 1) * 128], sel[:, h, :], acsT[:], True, True,
                             ['sel', 'acsT'], [('psbc', bb)])
                          TS(P, 'dve', tcl[tc_][:, hh, :], psbc[bb][:, hh * 128:(hh + 1) * 128], acs[:, h:h + 1], 0.0,
                             ALU.subtract, ALU.min, [('psbc', bb), 'acs'], [('tcl', tc_)])
                      ACTV(P, Lt[tc_][:], tcl[tc_][:], AF.Exp, [('tcl', tc_)], [('Lt', tc_)])
                      TT(P, 'pool', MT[tc_][:], Lt[tc_][:], cbTm[cm][:].unsqueeze(1).to_broadcast([128, 4, 128]), ALU.mult,
                         [('Lt', tc_), ('cbTm', cm)], [('MT', tc_)])
                      qbuf[i] = tc_

                  def quad_b(i):
                      g, hq = quads[i]
                      tc_ = qbuf.pop(i)
                      for hh in range(4):
                          h = g * 8 + hq * 4 + hh
                          MM(P, psyd[:, (hq * 4 + hh) * 64:(hq * 4 + hh + 1) * 64], MT[tc_][:, hh, :], xdt[:, h, :],
                             True, True, [('MT', tc_), 'xdt'], ['psyd'])
                      if hq == 1:
                          TT(P, 'dve', ystage[ys][:, g * 8:(g + 1) * 8, :], psyd[:, :].rearrange("p (h d) -> p h d", d=64),
                             dsk[g][:], ALU.add, ['psyd', ('dsk', g)], [('ystage', ys)])

                  quad_a(0)
                  for i in range(len(quads)):
                      if i + 1 < len(quads):
                          quad_a(i + 1)
                      quad_b(i)
                  yt = nxt('yTs', 2)
                  for q4 in range(4):
                      b = nxt('ps', 3)
                      for k in range(4):
                          ct = q4 * 4 + k
                          P.op('pe', 'transpose', dict(out=ps[b][:, k * 128:(k + 1) * 128],
                                                       in_=ystage[ys][:, 2 * ct:2 * ct + 2, :].rearrange("p h d -> p (h d)"),
                                                       identity=identf[:]), [('ystage', ys), 'identf'], [('ps', b)])
                      CP(P, 'act' if q4 % 2 == 0 else 'dve', yTs[yt][:, q4 * 4:(q4 + 1) * 4, :].rearrange("p c t -> p (c t)"),
                         ps[b][:, :], [('ps', b)], [('yTs', yt)])
                  DMA(P, 'sp', yT[hf * 2048:(hf + 1) * 2048, t0 + l0:t0 + l0 + 128].rearrange("(c p) t -> p c t", p=128),
                      yTs[yt][:], [('yTs', yt)], [], f'd_y{yt}')
        except _Stop:
            pass


def rmsnorm_stats(P, xt, ones, psk, ps, rstd, sq, xres, nfeat, KCn, eps=EPS):
    for kc in range(KCn):
        ACTV(P, sq[kc % 2][:], xt[:, kc, :], AF.Square, [xres], [('sq', kc % 2)])
        MM(P, ps[:], ones[:], sq[kc % 2][:], kc == 0, kc == KCn - 1, [('sq', kc % 2), 'ones'], [psk])
    TS(P, 'dve', rstd, ps[:], 1.0 / nfeat, eps, ALU.mult, ALU.add, [psk], ['rstd'])
    ACTV(P, rstd, rstd, AF.Sqrt, ['rstd'], ['rstd'])
    P.op('dve', 'reciprocal', dict(out=rstd, in_=rstd), ['rstd'], ['rstd'])


def emit_proj(nc, P, st, xT, W, nwd, table, od, t0, NT, stg_dt):
    NTG = NT // 512
    NTT = NT // 128
    sb = lambda name, shape, dt: st.enter_context(nc.sbuf_tensor(f"p{_PH[0]}_" + name, shape, dt))
    ones = sb("ones", [128, 128], F32)
    nw = sb("nw_sb", [128, KC], F32)
    xt = [sb(f"xt{i}", [128, KC, 512], F32) for i in range(2)]
    sq = [sb(f"sq{i}", [128, 512], F32) for i in range(2)]
    rstd = sb("rstd", [128, 512], F32)
    hT = sb("hT", [128, KC, NT], BF16)
    NW = 3
    wbf = [sb(f"wbf{i}", [128, KC, 512], BF16) for i in range(NW)]
    NS = 4
    stg = [sb(f"stg{i}", [128, 512], stg_dt) for i in range(NS)]
    stgf = sb("stgf", [128, 16], F32)
    psb = [st.enter_context(nc.psum_tensor(f"p{_PH[0]}_ps{i}", [128, 512], F32)) for i in range(8)]
    P.op('dve', 'memset', dict(ap=ones[:], constant=1.0), [], ['ones'])
    DMA(P, 'sp', nw[:], nwd[:, :], [], ['nw'], 'd_nw')
    wt = table

    def load_w(i):
        c0, ncol, kind, dst, d0 = wt[i]
        s = i % NW
        DMA(P, 'pool', wbf[s][:, :, :ncol], W[:, c0:c0 + ncol].rearrange("(kc p) c -> p kc c", p=128),
            [], [('wbf', s)], f'd_w{s}')
    load_w(0)
    load_w(1)
    for tg in range(NTG):
        xr = ('xt', tg % 2)
        DMA(P, 'sp', xt[tg % 2][:], xT[:, t0 + tg * 512:t0 + (tg + 1) * 512].rearrange("(kc p) t -> p kc t", p=128),
            [], [xr], f'd_x{tg % 2}')
        rmsnorm_stats(P, xt[tg % 2], ones, ('ps', 7), psb[7], rstd[:], sq, xr, D, KC)
        for kc in range(KC):
            STT(P, hT[:, kc, tg * 512:(tg + 1) * 512], xt[tg % 2][:, kc, :], nw[:, kc:kc + 1], rstd[:],
                ALU.mult, ALU.mult, [xr, 'rstd', 'nw'], [('hT', kc, tg)])
    cnt = {'ps': 0, 'stg': 0, 'ev': 0}

    def nxt(k, n):
        v = cnt[k] % n
        cnt[k] += 1
        return v
    for i in range(len(wt)):
        if i + 2 < len(wt):
            load_w(i + 2)
        c0, ncol, kind, dstn, d0 = wt[i]
        dst = od[dstn]
        s = i % NW
        if kind == 'fm':
            for cb in range((ncol + 127) // 128):
                m = min(128, ncol - cb * 128)
                for tg in range(NTG):
                    b = nxt('ps', 6)
                    for kc in range(KC):
                        MM(P, psb[b][:m, :], wbf[s][:, kc, cb * 128:cb * 128 + m], hT[:, kc, tg * 512:(tg + 1) * 512],
                           kc == 0, kc == KC - 1, [('wbf', s), ('hT', kc, tg)], [('ps', b)])
                    ss = nxt('stg', NS)
                    ev = 'act' if nxt('ev', 2) == 0 else 'dve'
                    CP(P, ev, stg[ss][:m, :], psb[b][:m, :], [('ps', b)], [('stg', ss)])
                    r0 = d0 + cb * 128
                    DMA(P, 'sp', dst[r0:r0 + m, t0 + tg * 512:t0 + (tg + 1) * 512], stg[ss][:m, :], [('stg', ss)], [], f'd_o{ss}')
        else:
            for tt in range(NTT):
                b = nxt('ps', 6)
                for kc in range(KC):
                    MM(P, psb[b][:, :ncol], hT[:, kc, tt * 128:(tt + 1) * 128], wbf[s][:, kc, :ncol],
                       kc == 0, kc == KC - 1, [('wbf', s), ('hT', kc, tt // 4)], [('ps', b)])
                r0 = t0 + tt * 128
                if kind == 'tmw':
                    TS(P, 'dve', stgf[:, :], psb[b][:, :16], 1.0 / 32.0, None, ALU.mult, None, [('ps', b)], ['stgf'])
                    DMA(P, 'sp', dst[r0:r0 + 128, :], stgf[:, :], ['stgf'], [], 'd_of')
                    continue
                ss = nxt('stg', NS)
                if kind == 'tmg':
                    ACTV(P, stg[ss][:, :ncol], psb[b][:, :ncol], AF.Silu, [('ps', b)], [('stg', ss)])
                else:
                    CP(P, 'dve', stg[ss][:, :ncol], psb[b][:, :ncol], [('ps', b)], [('stg', ss)])
                DMA(P, 'sp', dst[r0:r0 + 128, d0:d0 + ncol], stg[ss][:, :ncol], [('stg', ss)], [], f'd_o{ss}')


def table_a():
    wt = []
    for i in range(8):
        wt.append((i * 512, 512, 'fm', 'qT' if i < 4 else 'kT', (i % 4) * 512))
    for i in range(2):
        wt.append((8192 + i * 512, 512, 'fm', 'iqT', i * 512))
    wt.append((9216, 64, 'fm', 'ikT', 0))
    for i in range(4):
        wt.append((4096 + i * 512, 512, 'tm', 'v', i * 512))
    for i in range(4):
        wt.append((6144 + i * 512, 512, 'tmg', 'sg', i * 512))
    wt.append((9280, 16, 'tmw', 'iw', 0))
    return wt


def table_b():
    wt = []
    for i in range(8):
        wt.append((i * 512, 512, 'fm', 'zT', i * 512))
    for i in range(12):
        wt.append((4096 + i * 512, 512, 'fm', 'xbcT', i * 512))
    wt.append((10240, 64, 'tm', 'dt', 0))
    return wt


def emit_aout(nc, P, st, uT, xT, W, x1T, t0, NT):
    NTG = NT // 512
    sb = lambda name, shape, dt: st.enter_context(nc.sbuf_tensor(f"p{_PH[0]}_" + name, shape, dt))
    aT = sb("aT", [128, KC, NT], BF16)
    wbf = [sb(f"wbf{i}", [128, KC, 512], BF16) for i in range(2)]
    rs = [sb(f"rs{i}", [128, 512], F32) for i in range(4)]
    psb = [st.enter_context(nc.psum_tensor(f"p{_PH[0]}_ps{i}", [128, 512], F32)) for i in range(6)]
    cnt = {}

    def nxt(k, n):
        v = cnt.get(k, 0)
        cnt[k] = v + 1
        return v % n
    DMA(P, 'sp', aT[:], uT[:, t0:t0 + NT].rearrange("(kc p) t -> p kc t", p=128), [], ['aT'], 'd_a')

    def load_w(i):
        DMA(P, 'pool', wbf[i % 2][:], W[:, i * 512:(i + 1) * 512].rearrange("(kc p) c -> p kc c", p=128),
            [], [('wbf', i % 2)], f'd_w{i % 2}')
    load_w(0)
    for i in range(4):
        if i + 1 < 4:
            load_w(i + 1)
        for cb in range(4):
            ob = i * 4 + cb
            for tg in range(NTG):
                c0 = t0 + tg * 512
                r = nxt('rs', 4)
                DMA(P, 'sp', rs[r][:], xT[ob * 128:(ob + 1) * 128, c0:c0 + 512], [], [('rs', r)], f'd_r{r}')
                b = nxt('ps', 6)
                for kc in range(KC):
                    MM(P, psb[b][:], wbf[i % 2][:, kc, cb * 128:(cb + 1) * 128], aT[:, kc, tg * 512:(tg + 1) * 512],
                       kc == 0, kc == KC - 1, [('wbf', i % 2), 'aT'], [('ps', b)])
                TT(P, 'dve', rs[r][:], psb[b][:], rs[r][:], ALU.add, [('ps', b), ('rs', r)], [('rs', r)])
                DMA(P, 'sp', x1T[ob * 128:(ob + 1) * 128, c0:c0 + 512], rs[r][:], [('rs', r)], [], f'd_o{r}')


def emit_bout(nc, P, st, yT, zT, x1T, bnwd, W, x2T, p0, TP=1024):
    KI = 32
    NTG = TP // 512
    sb = lambda name, shape, dt: st.enter_context(nc.sbuf_tensor(f"p{_PH[0]}_" + name, shape, dt))
    ones = sb("ones", [128, 128], F32)
    bnw = sb("bnws", [128, KI], F32)
    gyb = sb("gyb", [128, KI, TP], BF16)
    rstd = sb("rstd", [128, TP], F32)
    yb = [sb(f"yb{i}", [128, 4, 512], F32) for i in range(2)]
    zb = [sb(f"zb{i}", [128, 4, 512], F32) for i in range(2)]
    sq = [sb(f"sq{i}", [128, 512], F32) for i in range(2)]
    wbf = [sb(f"wbf{i}", [128, KI, 256], BF16) for i in range(3)]
    rs = [sb(f"rs{i}", [128, 512], F32) for i in range(4)]
    t32 = [sb(f"t32{i}", [128, 512], F32) for i in range(2)]
    psb = [st.enter_context(nc.psum_tensor(f"p{_PH[0]}_ps{i}", [128, 512], F32)) for i in range(8)]
    cnt = {}

    def nxt(k, n):
        v = cnt.get(k, 0)
        cnt[k] = v + 1
        return v % n
    P.op('dve', 'memset', dict(ap=ones[:], constant=1.0), [], ['ones'])
    DMA(P, 'sp', bnw[:], bnwd[:, :], [], ['bnw'], 'd_bnw')
    wi = [0]

    def load_w(ci):
        s = wi[0] % 3
        wi[0] += 1
        DMA(P, 'pool', wbf[s][:], W[:, ci * 256:(ci + 1) * 256].rearrange("(kc p) c -> p kc c", p=128),
            [], [('wbf', s)], f'd_w{s}')
        return s
    slots = {0: load_w(0), 1: load_w(1)}
    for tg in range(NTG):
        c0 = p0 + tg * 512
        for k4 in range(KI // 4):
            s = nxt('yz', 2)
            DMA(P, 'sp', yb[s][:], yT[k4 * 512:(k4 + 1) * 512, c0:c0 + 512].rearrange("(k p) t -> p k t", p=128),
                [], [('yb', s)], f'd_y{s}')
            DMA(P, 'sp', zb[s][:], zT[k4 * 512:(k4 + 1) * 512, c0:c0 + 512].rearrange("(k p) t -> p k t", p=128),
                [], [('zb', s)], f'd_z{s}')
            ACTV(P, zb[s][:], zb[s][:], AF.Silu, [('zb', s)], [('zb', s)])
            TT(P, 'dve', yb[s][:], yb[s][:], zb[s][:], ALU.mult, [('yb', s), ('zb', s)], [('yb', s)])
            for k in range(4):
                kc = k4 * 4 + k
                q = nxt('sq', 2)
                ACTV(P, sq[q][:], yb[s][:, k, :], AF.Square, [('yb', s)], [('sq', q)])
                MM(P, psb[7][:], ones[:], sq[q][:], kc == 0, kc == KI - 1, [('sq', q), 'ones'], [('ps', 7)])
                TS(P, 'pool', gyb[:, kc, tg * 512:(tg + 1) * 512], yb[s][:, k, :], bnw[:, kc:kc + 1], 1.0,
                   ALU.mult, ALU.mult, [('yb', s), 'bnw'], [('gyb', kc, tg)])
        rv = rstd[:, tg * 512:(tg + 1) * 512]
        TS(P, 'dve', rv, psb[7][:], 1.0 / 4096.0, EPS, ALU.mult, ALU.add, [('ps', 7)], [('rstd', tg)])
        ACTV(P, rv, rv, AF.Sqrt, [('rstd', tg)], [('rstd', tg)])
        P.op('dve', 'reciprocal', dict(out=rv, in_=rv), [('rstd', tg)], [('rstd', tg)])
    for ci in range(8):
        if ci + 2 < 8:
            slots[ci + 2] = load_w(ci + 2)
        s = slots[ci]
        for cb in range(2):
            ob = ci * 2 + cb
            for tg in range(NTG):
                c0 = p0 + tg * 512
                r = nxt('rs', 4)
                DMA(P, 'sp', rs[r][:], x1T[ob * 128:(ob + 1) * 128, c0:c0 + 512], [], [('rs', r)], f'd_r{r}')
                b = nxt('ps', 6)
                for kc in range(KI):
                    MM(P, psb[b][:], wbf[s][:, kc, cb * 128:(cb + 1) * 128], gyb[:, kc, tg * 512:(tg + 1) * 512],
                       kc == 0, kc == KI - 1, [('wbf', s), ('gyb', kc, tg)], [('ps', b)])
                t = nxt('t32', 2)
                TT(P, 'dve', t32[t][:], psb[b][:], rstd[:, tg * 512:(tg + 1) * 512], ALU.mult,
                   [('ps', b), ('rstd', tg)], [('t32', t)])
                TT(P, 'pool', rs[r][:], t32[t][:], rs[r][:], ALU.add, [('t32', t), ('rs', r)], [('rs', r)])
                DMA(P, 'sp', x2T[ob * 128:(ob + 1) * 128, c0:c0 + 512], rs[r][:], [('rs', r)], [], f'd_o{r}')


def emit_fnorm(nc, P, st, xT, nwd, oT, NT):
    sb = lambda name, shape, dt: st.enter_context(nc.sbuf_tensor(f"p{_PH[0]}_" + name, shape, dt))
    ones = sb("ones", [128, 128], F32)
    nw = sb("nw_sb", [128, KC], F32)
    xt = [sb(f"xt{i}", [128, KC, 512], F32) for i in range(2)]
    sq = [sb(f"sq{i}", [128, 512], F32) for i in range(2)]
    rstd = sb("rstd", [128, 512], F32)
    ps = st.enter_context(nc.psum_tensor(f"p{_PH[0]}_ps0", [128, 512], F32))
    P.op('dve', 'memset', dict(ap=ones[:], constant=1.0), [], ['ones'])
    DMA(P, 'sp', nw[:], nwd[:, :], [], ['nw'], 'd_nw')
    for tg in range(NT // 512):
        xr = ('xt', tg % 2)
        DMA(P, 'sp', xt[tg % 2][:], xT[:, tg * 512:(tg + 1) * 512].rearrange("(kc p) t -> p kc t", p=128),
            [], [xr], f'd_x{tg % 2}')
        rmsnorm_stats(P, xt[tg % 2], ones, ('ps', 0), ps, rstd[:], sq, xr, D, KC)
        for kc in range(KC):
            STT(P, xt[tg % 2][:, kc, :], xt[tg % 2][:, kc, :], nw[:, kc:kc + 1], rstd[:],
                ALU.mult, ALU.mult, [xr, 'rstd', 'nw'], [xr])
        DMA(P, 'sp', oT[:, tg * 512:(tg + 1) * 512].rearrange("(kc p) t -> p kc t", p=128), xt[tg % 2][:],
            [xr], [], f'd_o{tg % 2}')


SEQ = 4096
_DBG = None


def build_fused(phases=None):
    nc = bass.Bass("TRN2", target_bir_lowering=False)
    ein = lambda name, shape, dt: nc.dram_tensor(name, list(shape), dt, kind="ExternalInput").ap()
    scr = lambda name, shape, dt: nc.dram_tensor(name, list(shape), dt, kind="Internal").ap()
    dr = dict(
        xT=ein("xT", [D, SEQ], F32), nwA=ein("nwA", [128, KC], F32), nwB=ein("nwB", [128, KC], F32),
        nwF=ein("nwF", [128, KC], F32), Wa=ein("Wa", [D, 9296], F32), Wo=ein("Wo", [D, D], F32),
        Wb=ein("Wb", [D, 10304], F32), Wbo=ein("Wbo", [4096, D], F32), bnw=ein("bnw", [128, 32], F32),
        bfar=ein("bfar", [128, 16], F32), ident=ein("ident", [128, 128], BF16), cm=ein("cm", [2, 128, 512], F32),
        biasN=ein("biasN", [2, 16, 128, 768], F32), U=ein("U", [128, 128], F32), ones128=ein("ones128", [128, 128], F32),
        sel=ein("sel", [32, 32 * 128], F32), tri=ein("tri", [128, 128], F32), identf=ein("identf", [128, 128], F32),
        cw=ein("cw", [2, 128, NCT * 4], F32), cb=ein("cb", [2, 128, NCT], F32), dtb=ein("dtb", [2, 128, NH], F32),
        alog=ein("alog", [2, 128, NH], F32), Dv=ein("Dv", [2, 128, NH], F32),
        qT=scr("qT", [2048, SEQ], BF16), kT=scr("kT", [2048, SEQ], BF16), iqT=scr("iqT", [1024, SEQ], BF16),
        ikT=scr("ikT", [64, SEQ], BF16), v=scr("v", [SEQ, 2048], BF16), sg=scr("sg", [SEQ, 2048], BF16),
        iw=scr("iw", [SEQ, 16], F32), uT=scr("uT", [2048, SEQ], BF16), x1T=scr("x1T", [D, SEQ], F32),
        zT=scr("zT", [4096, SEQ], F32), xbcT=scr("xbcT", [6144, SEQ], F32), dt=scr("dt", [SEQ, 64], F32),
        yT=scr("yT", [4096, SEQ], F32), x2T=scr("x2T", [D, SEQ], F32),
    )
    oT = nc.dram_tensor("oT", [D, SEQ], F32, kind="ExternalOutput").ap()
    plist = []
    for hf in range(2):
        plist.append(('projA', lambda P, st, hf=hf: emit_proj(nc, P, st, dr['xT'], dr['Wa'], dr['nwA'], table_a(), dr,
                                                              hf * 2048, 2048, BF16)))
    for par in range(2):
        plist.append(('attn', lambda P, st, par=par: emit_k2(nc, P, st, par, dr)))
    for hf in range(2):
        plist.append(('aout', lambda P, st, hf=hf: emit_aout(nc, P, st, dr['uT'], dr['xT'], dr['Wo'], dr['x1T'], hf * 2048, 2048)))
    for hf in range(2):
        plist.append(('projB', lambda P, st, hf=hf: emit_proj(nc, P, st, dr['x1T'], dr['Wb'], dr['nwB'], table_b(), dr,
                                                              hf * 2048, 2048, F32)))
    for hf in range(2):
        plist.append(('ssd', lambda P, st, hf=hf: emit_k4(nc, P, st, hf, dr)))
    for ps_ in range(4):
        plist.append(('bout', lambda P, st, ps_=ps_: emit_bout(nc, P, st, dr['yT'], dr['zT'], dr['x1T'], dr['bnw'], dr['Wbo'],
                                                               dr['x2T'], ps_ * 1024)))
    plist.append(('fnorm', lambda P, st: emit_fnorm(nc, P, st, dr['x2T'], dr['nwF'], oT, SEQ)))
    with contextlib.ExitStack() as g:
        P = Prog(nc, g)
        for i, (name, fn) in enumerate(plist):
            if phases is not None and i not in phases:
                continue
            _PH[0] = i
            with contextlib.ExitStack() as st:
                fn(P, st)
                P.end_phase(st)
    return nc


def _pk(w, n):
    return np.ascontiguousarray(np.asarray(w, np.float32).reshape(n, 128).T)


def _bc(a):
    a = np.asarray(a, np.float32)
    return np.ascontiguousarray(np.broadcast_to(a[None, :], (128, a.shape[0])))


def host_inputs(b, x, norm_w, a_w_in, a_w_out, rel_bias, b_w_in, b_conv_w, b_conv_b, b_dt_bias, b_a_log, b_d,
                b_norm_w, b_w_out, final_norm_w, shared):
    A = np.ascontiguousarray
    im = dict(shared)
    im["xT"] = A(np.asarray(x[b], np.float32).T)
    return im


def host_shared(norm_w, a_w_in, a_w_out, rel_bias, b_w_in, b_conv_w, b_conv_b, b_dt_bias, b_a_log, b_d,
                b_norm_w, b_w_out, final_norm_w):
    A = np.ascontiguousarray
    f = lambda a: np.asarray(a, np.float32)
    sh = dict(nwA=_pk(norm_w[0], KC), nwB=_pk(norm_w[1], KC), nwF=_pk(final_norm_w, KC), Wa=A(f(a_w_in[0])),
              Wo=A(f(a_w_out[0])), Wb=A(f(b_w_in[0])), Wbo=A(f(b_w_out[0])), bnw=_pk(b_norm_w[0], 32))
    rb = f(rel_bias)
    c0 = k2_consts(rb, 0)
    c1 = k2_consts(rb, 1)
    sh.update(bfar=c0['bfar'], ident=c0['ident'], cm=A(np.stack([c0['cm'], c1['cm']])),
              biasN=A(np.stack([c0['biasN'], c1['biasN']])))
    k4c = k4_consts()
    sh.update(U=k4c['U'], ones128=k4c['ones128'], sel=k4c['sel'], tri=k4c['tri'], identf=k4c['identf'])
    cw = f(b_conv_w[0])
    cbv = f(b_conv_b[0])
    cws, cbs, dtbs, alogs, Dvs = [], [], [], [], []
    for hf in range(2):
        chans = np.concatenate([np.arange(hf * 2048, (hf + 1) * 2048), 4096 + np.arange(hf * 512, (hf + 1) * 512),
                                5120 + np.arange(hf * 512, (hf + 1) * 512)])
        hs = slice(hf * 32, (hf + 1) * 32)
        cws.append(cw[:, chans].T.reshape(NCT, 128, 4).transpose(1, 0, 2).reshape(128, NCT * 4))
        cbs.append(cbv[chans].reshape(NCT, 128).T)
        dtbs.append(_bc(f(b_dt_bias[0])[hs]))
        alogs.append(_bc(f(b_a_log[0])[hs]))
        Dvs.append(_bc(f(b_d[0])[hs]))
    sh.update(cw=A(np.stack(cws)), cb=A(np.stack(cbs)), dtb=A(np.stack(dtbs)), alog=A(np.stack(alogs)), Dv=A(np.stack(Dvs)))
    return sh


NCORES = 8


def kernel(x, norm_w, a_w_in, a_w_out, rel_bias, b_w_in, b_conv_w, b_conv_b, b_dt_bias, b_a_log, b_d,
           b_norm_w, b_w_out, final_norm_w):
    nc = build_fused()
    sh = host_shared(norm_w, a_w_in, a_w_out, rel_bias, b_w_in, b_conv_w, b_conv_b, b_dt_bias, b_a_log, b_d,
                     b_norm_w, b_w_out, final_norm_w)
    ims = []
    for c in range(NCORES):
        im = dict(sh)
        im["xT"] = np.ascontiguousarray(np.asarray(x[c % 4], np.float32).T)
        ims.append(im)
    res = run_bass_kernel_spmd(nc, ims, core_ids=list(range(NCORES)))
    out = np.zeros((4, SEQ, D), np.float32)
    for b in range(4):
        out[b] = np.asarray(res.results[b]["oT"]).T
    return out
```

```python
import contextlib
import numpy as np
import ml_dtypes
import concourse.bass as bass
import concourse.mybir as mybir
from concourse.bass_utils import run_bass_kernel_spmd

F32 = mybir.dt.float32
_PH = [0]
BF16 = mybir.dt.bfloat16
ALU = mybir.AluOpType
AF = mybir.ActivationFunctionType
AX = mybir.AxisListType

ENGS = ['pe', 'act', 'dve', 'pool', 'sp']


def _is_psum(r):
    n = r[0] if isinstance(r, tuple) else r
    return isinstance(n, str) and (n.startswith('ps') or n.startswith('acc'))


class Prog:
    def __init__(self, nc, gstack):
        self.nc = nc
        self.gstack = gstack
        self.sems = {}
        self.semval = {}
        self.base = {}
        self.dma_keys = set()
        self.total_instr = {e: 0 for e in ENGS}
        self._reset()

    def _reset(self):
        self.q = {e: [] for e in ENGS}
        self.waited = {e: {} for e in ENGS}
        self.res = {}
        self.touched = set()

    def op(self, eng, meth, kw, reads=(), writes=(), dma=None):
        fn = (meth, kw)
        waits = {}
        own = 'c_' + eng
        xr = [r for r in reads if _is_psum(r)]
        if xr:
            writes = list(writes) + [r for r in xr if r not in writes]

        def need(kv, raw):
            if kv is None:
                return
            k, v = kv
            if k == own and not raw:
                return
            if k == own and eng == 'pe':
                return
            if self.waited[eng].get(k, 0) >= v:
                return
            if waits.get(k, 0) < v:
                waits[k] = v

        for r in reads:
            st = self.res.get(r)
            if st:
                need(st['w'], True)
        for w in writes:
            st = self.res.get(w)
            if st:
                need(st['w'], False)
                for kv in st['r'].items():
                    need(kv, False)
        if dma:
            key, inc = dma, 16
            self.dma_keys.add(key)
        else:
            key, inc = own, 1
        self.semval[key] = self.semval.get(key, 0) + inc
        self.touched.add(key)
        val = self.semval[key]
        for k, v in waits.items():
            self.waited[eng][k] = v
        self.q[eng].append([fn, sorted(waits.items()), key, inc, val])
        for r in reads:
            st = self.res.setdefault(r, {'w': None, 'r': {}})
            st['r'][key] = val
        for w in writes:
            self.res[w] = {'w': (key, val), 'r': {}}
        return (key, val)

    def end_phase(self, pstack):
        nc = self.nc
        fin = [(k, self.semval[k]) for k in sorted(self.touched)]
        for e in ENGS:
            self.q[e].append([None, list(fin), None, 0, 0])
        waited_vals = {}
        for e in ENGS:
            for fn, waits, key, inc, val in self.q[e]:
                for k, v in waits:
                    waited_vals.setdefault(k, set()).add(v)
        remap = {}
        for k, vs in waited_vals.items():
            if k in self.dma_keys:
                continue
            b0 = self.base.get(k, 0)
            remap[k] = {v: b0 + i + 1 for i, v in enumerate(sorted(vs))}
        for k in self.touched:
            if k not in self.sems:
                self.sems[k] = self.gstack.enter_context(nc.semaphore("s_" + k))
        sems = self.sems
        block = pstack.enter_context(nc.Block())
        engmap = {'pe': block.tensor, 'act': block.scalar, 'dve': block.vector,
                  'pool': block.gpsimd, 'sp': block.sync}
        for e in ENGS:
            ops = self.q[e]
            self.total_instr[e] += len(ops)

            def body(engine, ops=ops):
                for fn, waits, key, inc, val in ops:
                    for k, v in waits:
                        if k in self.dma_keys:
                            engine.wait_ge(sems[k], v)
                        else:
                            engine.wait_ge(sems[k], remap[k][v])
                    if fn is None:
                        continue
                    ins = getattr(engine, fn[0])(**fn[1])
                    if key in self.dma_keys:
                        ins.then_inc(sems[key], 16)
                    elif key in remap and val in remap[key]:
                        ins.then_inc(sems[key], 1)
            engmap[e](body)
        for k, m in remap.items():
            self.base[k] = self.base.get(k, 0) + len(m)
        self._reset()


def MM(P, out, lhsT, rhs, start, stop, reads, writes, **kw):
    return P.op('pe', 'matmul', dict(out=out, lhsT=lhsT, rhs=rhs, start=start, stop=stop, **kw), reads, writes)


def ACTV(P, out, in_, func, reads, writes, **kw):
    return P.op('act', 'activation', dict(out=out, in_=in_, func=func, **kw), reads, writes)


def DMA(P, eng, out, in_, reads, writes, key):
    return P.op(eng, 'dma_start', dict(out=out, in_=in_), reads, writes, dma=key)


def TS(P, eng, out, in0, s1, s2, op0, op1, reads, writes, **kw):
    d = dict(out=out, in0=in0, scalar1=s1, scalar2=s2, op0=op0, **kw)
    if op1 is not None:
        d['op1'] = op1
    return P.op(eng, 'tensor_scalar', d, reads, writes)


def TT(P, eng, out, in0, in1, op, reads, writes):
    return P.op(eng, 'tensor_tensor', dict(out=out, in0=in0, in1=in1, op=op), reads, writes)


def STT(P, out, in0, scalar, in1, op0, op1, reads, writes, **kw):
    return P.op('dve', 'scalar_tensor_tensor', dict(out=out, in0=in0, scalar=scalar, in1=in1, op0=op0, op1=op1, **kw),
                reads, writes)


def CP(P, eng, out, in_, reads, writes):
    if eng == 'act':
        return P.op('act', 'activation', dict(out=out, in_=in_, func=AF.Copy), reads, writes)
    return P.op(eng, 'tensor_copy', dict(out=out, in_=in_), reads, writes)


D = 2048
KC = D // 128
EPS = 1e-6


S = 4096
NIT = 26
SCALE = 128 ** -0.5


def own_blocks(par):
    return [i for i in range(32) if (i % 4 in (0, 3)) == (par == 0)]


def k2_consts(rel_bias, par):
    n = np.arange(256)
    nf = np.maximum(n, 1).astype(np.float32)
    large = 16 + (np.log(nf / 16) / np.log(128 / 16) * 16).astype(np.int32)
    large = np.minimum(large, 31)
    bucket = np.where(n < 16, n, large)
    s = np.arange(128)[:, None]
    q = np.arange(128)[None, :]
    biasT = np.zeros((128, 16, 2, 128), np.float32)
    for d in range(2):
        dist = np.clip(d * 128 + q - s, 0, 255)
        biasT[:, :, d, :] = rel_bias[bucket[dist]].transpose(0, 2, 1)
    bfar = np.ascontiguousarray(np.broadcast_to(rel_bias[31][None, :], (128, 16))).astype(np.float32)
    negtri = np.where(np.arange(128)[None, :] <= np.arange(128)[:, None], 0.0, -1e30).astype(np.float32)
    ident = np.eye(128, dtype=np.float32).astype(ml_dtypes.bfloat16)
    dtab = lambda sp, w: (1 - w) if (sp == par) else (2 - w)
    cm = np.zeros((128, 2, 2, 128), np.float32)
    biasN = np.zeros((16, 128, 2, 3, 128), np.float32)
    for sp in range(2):
        for w in range(3):
            d = dtab(sp, w)
            if d >= 2:
                biasN[:, :, sp, w, :] = rel_bias[31][:, None, None]
            elif d >= 0:
                biasN[:, :, sp, w, :] = biasT[:, :, d, :].transpose(1, 0, 2)
            if w >= 1:
                if d == 0:
                    cm[:, sp, w - 1, :] = negtri
                elif d < 0:
                    cm[:, sp, w - 1, :] = -1e30
    return dict(bfar=bfar, ident=ident, cm=cm.reshape(128, 512), biasN=biasN.reshape(16, 128, 768))


def emit_k2(nc, P, st, par, dr, nslots=16, nheads=16, nit=NIT):
    own = own_blocks(par)
    rsel = (0, 3) if par == 0 else (1, 2)
    NQB = nslots
    nkbs = [2 * (qi + 1) for qi in range(nslots)]
    NQ = NQB * 128
    offs = []
    T = 0
    for nkb in nkbs:
        offs.append(T)
        T += nkb
    kT, v, ikT, qT, iqT, iw, sg, uT = dr['kT'], dr['v'], dr['ikT'], dr['qT'], dr['iqT'], dr['iw'], dr['sg'], dr['uT']
    biasNd, bfard, cmd, identd = dr['biasN'], dr['bfar'], dr['cm'], dr['ident']
    if True:
        sb = lambda name, shape, dt: st.enter_context(nc.sbuf_tensor(f"p{_PH[0]}_" + name, shape, dt))
        maskT = sb("maskT", [128, T, 128], BF16)
        score = sb("score", [128, S], F32)
        maskq = sb("maskq", [128, S], BF16)
        relu = [sb(f"relu{i}", [128, 512], F32) for i in range(2)]
        ikTs = sb("ikTs", [64, S], BF16)
        iqb = [sb(f"iqb{i}", [64, 16, 128], BF16) for i in range(2)]
        iwb = [sb(f"iwb{i}", [128, 16], F32) for i in range(2)]
        kTh = [sb(f"kTh{i}", [128, S], BF16) for i in range(2)]
        vh = [sb(f"vh{i}", [128, 32, 136], BF16) for i in range(2)]
        qTh = [sb(f"qTh{i}", [128, NQ], BF16) for i in range(2)]
        sgb = [sb(f"sgb{i}", [128, NQB, 128], BF16) for i in range(2)]
        biasN = [sb(f"biasN{i}", [128, 2, 3, 128], F32) for i in range(2)]
        bfar = sb("bfars", [128, 16], F32)
        cm = sb("cms", [128, 2, 256], F32)
        ident = sb("idents", [128, 128], BF16)
        hi = sb("hi", [128, 1], F32)
        lo = sb("lo", [128, 1], F32)
        w = sb("w", [128, 1], F32)
        mid = sb("mid", [128, 1], F32)
        cnt = sb("cnt", [128, 1], F32)
        ge = sb("ge", [128, 1], F32)
        rinv = sb("rinv", [128, 1], F32)
        NE = 6
        tb = [sb(f"tb{i}", [128, 128], F32) for i in range(2)]
        eb = [sb(f"eb{i}", [128, 512], BF16) for i in range(NE)]
        pt = [sb(f"pt{i}", [128, 512], BF16) for i in range(NE)]
        o32 = [sb(f"o32{i}", [128, 128], F32) for i in range(2)]
        ust = [sb(f"ust{i}", [128, 128], BF16) for i in range(4)]
        usT = [sb(f"usT{i}", [128, 128], BF16) for i in range(4)]
        psb = [st.enter_context(nc.psum_tensor(f"p{_PH[0]}_ps{i}", [128, 512], F32)) for i in range(4)]
        pst = [st.enter_context(nc.psum_tensor(f"p{_PH[0]}_pst{i}", [128, 1024], BF16)) for i in range(2)]
        acc = [st.enter_context(nc.psum_tensor(f"p{_PH[0]}_acc{i}", [128, 512], F32)) for i in range(2)]
        cn = {}

        def nxt(k, n):
            vv = cn.get(k, 0)
            cn[k] = vv + 1
            return vv % n
        DMA(P, 'sp', ikTs[:], ikT[:, :], [], ['ikT'], 'd_c0')
        DMA(P, 'sp', cm[:], cmd[par].rearrange("p (a b) -> p a b", a=2), [], ['cm'], 'd_c1')
        DMA(P, 'sp', ident[:], identd[:, :], [], ['ident'], 'd_c2')
        DMA(P, 'sp', bfar[:], bfard[:, :], [], ['bfar'], 'd_c3')
        for i in range(2):
            P.op('pool', 'memset', dict(ap=vh[i][:, :, 128:129], constant=1.0), [], [('vh1', i)])

        for qi in range(nslots):
            nkb = nkbs[qi]
            nk = nkb * 128
            s2 = qi % 2
            DMA(P, 'sp', iqb[s2][:], iqT[:, own[qi] * 128:(own[qi] + 1) * 128].rearrange("(h d) q -> d h q", d=64),
                [], [('iqb', s2)], f'd_iq{s2}')
            DMA(P, 'sp', iwb[s2][:], iw[own[qi] * 128:(own[qi] + 1) * 128, :], [], [('iwb', s2)], f'd_iw{s2}')
            nch = (nk + 511) // 512
            for c in range(nch):
                kw = min(512, nk - c * 512)
                for hh in range(16):
                    b = nxt('ps', 4)
                    MM(P, psb[b][:, :kw], iqb[s2][:, hh, :], ikTs[:, c * 512:c * 512 + kw], True, True,
                       [('iqb', s2), 'ikT'], [('ps', b)])
                    rb = nxt('relu', 2)
                    ACTV(P, relu[rb][:, :kw], psb[b][:, :kw], AF.Relu, [('ps', b)], [('relu', rb)])
                    sc = score[:, c * 512:c * 512 + kw]
                    if hh == 0:
                        TS(P, 'dve', sc, relu[rb][:, :kw], iwb[s2][:, 0:1], None, ALU.mult, None,
                           [('relu', rb), ('iwb', s2)], [('score', c)])
                    else:
                        STT(P, sc, relu[rb][:, :kw], iwb[s2][:, hh:hh + 1], sc, ALU.mult, ALU.add,
                            [('relu', rb), ('iwb', s2), ('score', c)], [('score', c)])
            scr = [('score', c) for c in range(nch)]
            P.op('dve', 'tensor_reduce', dict(out=hi[:], in_=score[:, :nk], axis=AX.X, op=ALU.max), scr, ['hi'])
            P.op('dve', 'tensor_reduce', dict(out=lo[:], in_=score[:, :nk], axis=AX.X, op=ALU.min), scr, ['lo'])
            TT(P, 'dve', w[:], hi[:], lo[:], ALU.subtract, ['hi', 'lo'], ['w'])
            dg = score[:, (nkb - 2) * 128:nkb * 128]
            TT(P, 'dve', dg, dg, cm[:, qi % 2, :], ALU.add, [('score', (nkb - 2) // 4), 'cm'], [('score', (nkb - 2) // 4)])
            for it in range(nit):
                TS(P, 'dve', w[:], w[:], 0.5, None, ALU.mult, None, ['w'], ['w'])
                TT(P, 'dve', mid[:], lo[:], w[:], ALU.add, ['lo', 'w'], ['mid'])
                TS(P, 'dve', maskq[:, :nk], score[:, :nk], mid[:, 0:1], 0.0, ALU.is_ge, ALU.add,
                   scr + ['mid'], ['maskq', 'cnt'], accum_out=cnt[:, 0:1])
                TS(P, 'dve', ge[:], cnt[:], 255.5, None, ALU.is_ge, None, ['cnt'], ['ge'])
                STT(P, lo[:], ge[:], w[:, 0:1], lo[:], ALU.mult, ALU.add, ['ge', 'w', 'lo'], ['lo'])
            TS(P, 'dve', maskq[:, :nk], score[:, :nk], lo[:, 0:1], None, ALU.is_ge, None, scr + ['lo'], ['maskq'])
            for j in range(nkb):
                b = nxt('pst', 2)
                P.op('pe', 'transpose', dict(out=pst[b][:, :128], in_=maskq[:, j * 128:(j + 1) * 128], identity=ident[:]),
                     ['maskq', 'ident'], [('pst', b)])
                CP(P, 'act', maskT[:, offs[qi] + j, :], pst[b][:, :128], [('pst', b)], [('maskT', qi)])

        def head_loads(h):
            s2 = h % 2
            DMA(P, 'sp', kTh[s2][:], kT[h * 128:(h + 1) * 128, :], [], [('kTh', s2)], f'd_k{s2}')
            DMA(P, 'sp', vh[s2][:, :, 0:128], v[:, h * 128:(h + 1) * 128].rearrange("(j p) d -> p j d", p=128),
                [], [('vh', s2)], f'd_v{s2}')
            for k in range(2):
                DMA(P, 'sp', qTh[s2][:].rearrange("p (m k t) -> p m k t", k=2, t=128)[:, :, k, :],
                    qT[h * 128:(h + 1) * 128, :].rearrange("p (m r t) -> p m r t", r=4, t=128)[:, :, rsel[k], :],
                    [], [('qTh', s2)], f'd_q{s2}')
            for k in range(2):
                DMA(P, 'sp', sgb[s2][:].rearrange("p (m k) d -> p m k d", k=2)[:, :, k, :],
                    sg[:, h * 128:(h + 1) * 128].rearrange("(m r p) d -> p m r d", r=4, p=128)[:, :, rsel[k], :],
                    [], [('sgb', s2)], f'd_sg{s2}')
            DMA(P, 'sp', biasN[s2][:], biasNd[par, h].rearrange("p (a w q) -> p a w q", a=2, w=3), [], [('biasN', s2)], f'd_bn{s2}')

        units = []
        for h in range(nheads):
            for qi in range(nslots):
                nkb = nkbs[qi]
                nfar = max(0, nkb - 3)
                j = 0
                while j < nfar:
                    n = min(4, nfar - j)
                    units.append((h, qi, j, n, True))
                    j += n
                for j in range(nfar, nkb):
                    units.append((h, qi, j, 1, False))
        ubuf = {}

        def stage_a(t):
            h, qi, j0, n, far = units[t]
            s2 = h % 2
            nkb = nkbs[qi]
            b = nxt('ps', 4)
            for k in range(n):
                MM(P, psb[b][:, k * 128:(k + 1) * 128], kTh[s2][:, (j0 + k) * 128:(j0 + k + 1) * 128],
                   qTh[s2][:, qi * 128:(qi + 1) * 128], True, True, [('kTh', s2), ('qTh', s2)], [('ps', b)])
            e = nxt('eb', NE)
            if far:
                ACTV(P, eb[e][:, :n * 128], psb[b][:, :n * 128], AF.Exp, [('ps', b), 'bfar'], [('eb', e)],
                     scale=SCALE, bias=bfar[:, h:h + 1])
            else:
                wv = j0 - (nkb - 3)
                tt_ = nxt('tb', 2)
                STT(P, tb[tt_][:], psb[b][:, :128], SCALE, biasN[s2][:, qi % 2, wv, :], ALU.mult, ALU.add,
                    [('ps', b), ('biasN', s2)], [('tb', tt_)])
                ACTV(P, eb[e][:, :128], tb[tt_][:], AF.Exp, [('tb', tt_)], [('eb', e)])
            p = nxt('pt', NE)
            me = 'dve' if nxt('me', 2) == 0 else 'pool'
            TT(P, me, pt[p][:, :n * 128], eb[e][:, :n * 128],
               maskT[:, offs[qi] + j0:offs[qi] + j0 + n, :].rearrange("p a b -> p (a b)"), ALU.mult,
               [('eb', e), ('maskT', qi)], [('pt', p)])
            ubuf[t] = p

        def stage_b(t):
            h, qi, j0, n, far = units[t]
            s2 = h % 2
            nkb = nkbs[qi]
            ab = (h * nslots + qi) % 2
            p = ubuf.pop(t)
            for k in range(n):
                j = j0 + k
                MM(P, acc[ab][:, :129], pt[p][:, k * 128:(k + 1) * 128], vh[s2][:, j, 0:129], j == 0, j == nkb - 1,
                   [('pt', p), ('vh', s2), ('vh1', s2)], [('acc', ab)])
            if j0 + n != nkb:
                return
            P.op('dve', 'reciprocal', dict(out=rinv[:], in_=acc[ab][:, 128:129]), [('acc', ab)], ['rinv'])
            o = nxt('o32', 2)
            TS(P, 'dve', o32[o][:], acc[ab][:, :128], rinv[:, 0:1], None, ALU.mult, None,
               [('acc', ab), 'rinv'], [('o32', o)])
            us = nxt('ust', 4)
            TT(P, 'pool', ust[us][:], o32[o][:], sgb[s2][:, qi, :], ALU.mult, [('o32', o), ('sgb', s2)], [('ust', us)])
            tp = nxt('pst', 2)
            P.op('pe', 'transpose', dict(out=pst[tp][:, :128], in_=ust[us][:], identity=ident[:]),
                 [('ust', us), 'ident'], [('pst', tp)])
            CP(P, 'act', usT[us][:], pst[tp][:, :128], [('pst', tp)], [('usT', us)])
            DMA(P, 'sp', uT[h * 128:(h + 1) * 128, own[qi] * 128:(own[qi] + 1) * 128], usT[us][:], [('usT', us)], [], f'd_u{us}')
            if qi == nslots - 1 and h + 2 < nheads:
                head_loads(h + 2)

        LA = 3
        head_loads(0)
        if nheads > 1:
            head_loads(1)
        for t in range(len(units) + LA):
            if t < len(units):
                stage_a(t)
            if t - LA >= 0:
                stage_b(t - LA)


NH = 32
NG = 4
NCT = 24


def k4_consts():
    U = np.triu(np.ones((128, 128), np.float32))
    sel = np.zeros((32, 32, 128), np.float32)
    for h in range(32):
        sel[h, h, :] = 1.0
    return dict(U=U, ones128=np.ones((128, 128), np.float32), sel=sel.reshape(32, 32 * 128),
                tri=U.copy(), identf=np.eye(128, dtype=np.float32),
                identb=np.eye(128, dtype=np.float32).astype(ml_dtypes.bfloat16))


class _Stop(Exception):
    pass


def emit_k4(nc, P, st, hf, dr, npieces=8, stop=99):
    SS = npieces * 512
    xbcT, dtd, yT = dr['xbcT'], dr['dt'], dr['yT']
    cwd, cbd, dtbd, alogd, Dd = dr['cw'][hf], dr['cb'][hf], dr['dtb'][hf], dr['alog'][hf], dr['Dv'][hf]
    Ud, onesd, seld, trid, identfd, identbd = dr['U'], dr['ones128'], dr['sel'], dr['tri'], dr['identf'], dr['ident']
    if True:
        sb = lambda name, shape, dt: st.enter_context(nc.sbuf_tensor(f"p{_PH[0]}_" + name, shape, dt))
        xin = sb("xin", [128, 12, 515], F32)
        xc = sb("xc", [128, 16, 512], F32)
        BTc = sb("BTc", [128, 4, 512], BF16)
        CTc = sb("CTc", [128, 4, 512], BF16)
        cacc = [sb(f"cacc{i}", [128, 512], F32) for i in range(2)]
        cw = sb("cws", [128, NCT, 4], F32)
        cb = sb("cbs", [128, NCT], F32)
        dtb = sb("dtbs", [128, NH], F32)
        Aneg = sb("Aneg", [128, NH], F32)
        Dv = sb("Dvs", [128, NH], F32)
        U = sb("Us", [128, 128], F32)
        ones128 = sb("ones128s", [128, 128], F32)
        sel = sb("sels", [32, 32, 128], F32)
        tri = sb("tris", [128, 128], F32)
        identf = sb("identfs", [128, 128], F32)
        identb = sb("identbs", [128, 128], BF16)
        dtr = sb("dtr", [128, 4, NH], F32)
        dtp = sb("dtp", [128, 4, NH], F32)
        adt = sb("adt", [128, 4, NH], F32)
        acs = sb("acs", [128, NH], F32)
        ea = sb("ea", [128, NH], F32)
        dec = sb("dec", [128, NH], F32)
        cdec = sb("cdec", [128, NH], F32)
        dtdec = sb("dtdec", [128, NH], F32)
        acsT = sb("acsT", [32, 128], F32)
        x_tm = sb("x_tm", [128, NH, 64], F32)
        xdt = sb("xdt", [128, NH, 64], BF16)
        xdtd = sb("xdtd", [128, NH, 64], BF16)
        B_tm = sb("B_tm", [128, 4, 128], BF16)
        prev32 = sb("prev32", [128, NG, 512], F32)
        prevbf = sb("prevbf", [128, NG, 512], BF16)
        cbTm = [sb(f"cbTm{i}", [128, 128], F32) for i in range(2)]
        tcl = [sb(f"tcl{i}", [128, 4, 128], F32) for i in range(2)]
        Lt = [sb(f"Lt{i}", [128, 4, 128], F32) for i in range(2)]
        MT = [sb(f"MT{i}", [128, 4, 128], BF16) for i in range(2)]
        yos = [sb(f"yos{i}", [128, 8, 64], F32) for i in range(4)]
        Dcol = sb("Dcols", [128, 16], F32)
        ystage = [sb(f"ystage{i}", [128, NH, 64], F32) for i in range(2)]
        yTs = [sb(f"yTs{i}", [128, 16, 128], F32) for i in range(2)]
        ps = [st.enter_context(nc.psum_tensor(f"p{_PH[0]}_ps{i}", [128, 512], F32)) for i in range(3)]
        psmisc = st.enter_context(nc.psum_tensor(f"p{_PH[0]}_psmisc", [128, 512], F32))
        psbt = st.enter_context(nc.psum_tensor(f"p{_PH[0]}_psbt", [128, 1024], BF16))
        psbc = [st.enter_context(nc.psum_tensor(f"p{_PH[0]}_psbc{i}", [128, 512], F32)) for i in range(2)]
        psyd = st.enter_context(nc.psum_tensor(f"p{_PH[0]}_psyd", [128, 512], F32))
        cn = {}

        def nxt(k, n):
            vv = cn.get(k, 0)
            cn[k] = vv + 1
            return vv % n
        for name, t, d in [('cw', cw, cwd.rearrange("p (c k) -> p c k", k=4)), ('cb', cb, cbd),
                           ('dtb', dtb, dtbd), ('Dv', Dv, Dd), ('U', U, Ud[:, :]),
                           ('ones128', ones128, onesd[:, :]), ('sel', sel, seld[:, :].rearrange("p (h m) -> p h m", m=128)),
                           ('tri', tri, trid[:, :]), ('identf', identf, identfd[:, :]), ('identb', identb, identbd[:, :]),
                           ('Aneg', Aneg, alogd), ('Dcol', Dcol, dr['Dcol'][hf])]:
            DMA(P, 'sp', t[:], d, [], [name], 'd_' + name)
        ACTV(P, Aneg[:], Aneg[:], AF.Exp, ['Aneg'], ['Aneg'])
        TS(P, 'dve', Aneg[:], Aneg[:], -1.0, None, ALU.mult, None, ['Aneg'], ['Aneg'])
        P.op('dve', 'memset', dict(ap=prev32[:], constant=0.0), [], [('prev32', g) for g in range(NG)])
        P.op('dve', 'memset', dict(ap=prevbf[:], constant=0.0), [], [('prevbf', g) for g in range(NG)])

        def chk(n):
            if stop <= n:
                raise _Stop()
        try:
          for pc in range(npieces):
              t0 = pc * 512
              for half in range(2):
                  if half == 0:
                      srcs = [(0, 12, hf * 2048)]
                  else:
                      srcs = [(0, 4, hf * 2048 + 1536), (4, 4, 4096 + hf * 512), (8, 4, 5120 + hf * 512)]
                  if pc == 0:
                      P.op('dve', 'memset', dict(ap=xin[:, :, 0:3], constant=0.0), [], ['xin'])
                  for (c0_, n_, r0_) in srcs:
                      src = xbcT[r0_:r0_ + n_ * 128, :]
                      if pc == 0:
                          DMA(P, 'sp', xin[:, c0_:c0_ + n_, 3:515], src[:, 0:512].rearrange("(c p) t -> p c t", p=128), [], ['xin'], 'd_xin')
                      else:
                          DMA(P, 'sp', xin[:, c0_:c0_ + n_, :], src[:, t0 - 3:t0 + 512].rearrange("(c p) t -> p c t", p=128), [], ['xin'], 'd_xin')
                  for cl in range(12):
                      ct = half * 12 + cl
                      a = nxt('cacc', 2)
                      TS(P, 'dve', cacc[a][:], xin[:, cl, 3:515], cw[:, ct, 3:4], None, ALU.mult, None,
                         ['xin', 'cw'], [('cacc', a)])
                      for k in (2, 1, 0):
                          STT(P, cacc[a][:], xin[:, cl, k:k + 512], cw[:, ct, k:k + 1], cacc[a][:], ALU.mult, ALU.add,
                              ['xin', 'cw', ('cacc', a)], [('cacc', a)])
                      if ct < 16:
                          dst, dr = xc[:, ct, :], ('xc', ct)
                      elif ct < 20:
                          dst, dr = BTc[:, ct - 16, :], ('BTc', ct - 16)
                      else:
                          dst, dr = CTc[:, ct - 20, :], ('CTc', ct - 20)
                      ACTV(P, dst, cacc[a][:], AF.Silu, [('cacc', a), 'cb'], [dr], bias=cb[:, ct:ct + 1])
              chk(1)
              DMA(P, 'sp', dtr[:], dtd[t0:t0 + 512, hf * 32:(hf + 1) * 32].rearrange("(c p) h -> p c h", p=128), [], ['dtr'], 'd_dtr')
              TT(P, 'dve', dtr[:], dtr[:], dtb[:].unsqueeze(1).to_broadcast([128, 4, NH]), ALU.add, ['dtr', 'dtb'], ['dtr'])
              ACTV(P, dtp[:], dtr[:], AF.Exp, ['dtr'], ['dtp'])
              ACTV(P, dtp[:], dtp[:], AF.Ln, ['dtp', 'ones128'], ['dtp'], bias=ones128[:, 0:1])
              TT(P, 'dve', adt[:], dtp[:], Aneg[:].unsqueeze(1).to_broadcast([128, 4, NH]), ALU.mult, ['dtp', 'Aneg'], ['adt'])
              chk(2)
              for c in range(4):
                  l0 = c * 128
                  MM(P, psmisc[:, 0:NH], U[:], adt[:, c, :], True, True, ['U', 'adt'], ['psmisc'])
                  MM(P, psmisc[:, 32:32 + NH], ones128[:], adt[:, c, :], True, True, ['ones128', 'adt'], ['psmisc'])
                  MM(P, psmisc[0:32, 64:192], adt[:, c, :], U[:], True, True, ['U', 'adt'], ['psmisc'])
                  CP(P, 'dve', acs[:], psmisc[:, 0:NH], ['psmisc'], ['acs'])
                  ACTV(P, ea[:], psmisc[:, 0:NH], AF.Exp, ['psmisc'], ['ea'])
                  ACTV(P, cdec[:], psmisc[:, 32:32 + NH], AF.Exp, ['psmisc'], ['cdec'])
                  TT(P, 'dve', dec[:], psmisc[:, 32:32 + NH], acs[:], ALU.subtract, ['psmisc', 'acs'], ['dec'])
                  ACTV(P, dec[:], dec[:], AF.Exp, ['dec'], ['dec'])
                  CP(P, 'dve', acsT[:], psmisc[0:32, 64:192], ['psmisc'], ['acsT'])
                  TT(P, 'dve', dtdec[:], dtp[:, c, :], dec[:], ALU.mult, ['dtp', 'dec'], ['dtdec'])
                  chk(3)
                  for q4 in range(4):
                      b = nxt('ps', 3)
                      for k in range(4):
                          ct = q4 * 4 + k
                          P.op('pe', 'transpose', dict(out=ps[b][:, k * 128:(k + 1) * 128], in_=xc[:, ct, l0:l0 + 128],
                                                       identity=identf[:]), [('xc', ct), 'identf'], [('ps', b)])
                      CP(P, 'act', x_tm[:, q4 * 8:(q4 + 1) * 8, :].rearrange("p h d -> p (h d)"), ps[b][:, :],
                         [('ps', b)], [('x_tm', q4)])
                  xr = [('x_tm', q) for q in range(4)]
                  TT(P, 'dve', xdt[:], x_tm[:], dtp[:, c, :].unsqueeze(2).to_broadcast([128, NH, 64]), ALU.mult,
                     xr + ['dtp'], ['xdt'])
                  TT(P, 'dve', xdtd[:], x_tm[:], dtdec[:].unsqueeze(2).to_broadcast([128, NH, 64]), ALU.mult,
                     xr + ['dtdec'], ['xdtd'])
                  for g in range(NG):
                      P.op('pe', 'transpose', dict(out=psbt[:, g * 128:(g + 1) * 128], in_=BTc[:, g, l0:l0 + 128],
                                                   identity=identb[:]), [('BTc', g), 'identb'], ['psbt'])
                  CP(P, 'act', B_tm[:].rearrange("p g n -> p (g n)"), psbt[:, 0:512], ['psbt'], ['B_tm'])
                  chk(4)
                  ys = nxt('ystage', 2)
                  for g in range(NG):
                      b = nxt('ps', 3)
                      MM(P, ps[b][:, :], CTc[:, g, l0:l0 + 128], prevbf[:, g, :], True, True,
                         [('CTc', g), ('prevbf', g)], [('ps', b)])
                      yo = g
                      TT(P, 'dve', yos[yo][:], ps[b][:, :].rearrange("p (h d) -> p h d", d=64),
                         ea[:, g * 8:(g + 1) * 8].unsqueeze(2).to_broadcast([128, 8, 64]), ALU.mult,
                         [('ps', b), 'ea'], [('yos', yo)])
                      b = nxt('ps', 3)
                      MM(P, ps[b][:, :], B_tm[:, g, :], xdtd[:, g * 8:(g + 1) * 8, :].rearrange("p h d -> p (h d)"), True, True,
                         ['B_tm', 'xdtd'], [('ps', b)])
                      TT(P, 'pool', prev32[:, g, :].rearrange("p (h d) -> p h d", d=64),
                         prev32[:, g, :].rearrange("p (h d) -> p h d", d=64),
                         cdec[:, g * 8:(g + 1) * 8].unsqueeze(2).to_broadcast([128, 8, 64]), ALU.mult,
                         [('prev32', g), 'cdec'], [('prev32', g)])
                      TT(P, 'dve', prev32[:, g, :], prev32[:, g, :], ps[b][:, :], ALU.add, [('prev32', g), ('ps', b)],
                         [('prev32', g)])
                      CP(P, 'act', prevbf[:, g, :], prev32[:, g, :], [('prev32', g)], [('prevbf', g)])
                  quads = [(g, hq) for g in range(NG) for hq in range(2)]
                  qbuf = {}

                  def quad_a(i):
                      g, hq = quads[i]
                      if hq == 0:
                          b = nxt('ps', 3)
                          MM(P, ps[b][:, 0:128], BTc[:, g, l0:l0 + 128], CTc[:, g, l0:l0 + 128], True, True,
                             [('BTc', g), ('CTc', g)], [('ps', b)])
                          TT(P, 'dve', cbTm[g % 2][:], ps[b][:, 0:128], tri[:], ALU.mult, [('ps', b), 'tri'], [('cbTm', g % 2)])
                      cm = g % 2
                      bb = nxt('psbc', 2)
                      tc_ = nxt('tcl', 2)
                      for hh in range(4):
                          h = g * 8 + hq * 4 + hh
                          MM(P, psbc[bb][:, hh * 128:(hh + 1) * 128], sel[:, h, :], acsT[:], True, True,
                             ['sel', 'acsT'], [('psbc', bb)])
                          ACTV(P, tcl[tc_][:, hh, :], psbc[bb][:, hh * 128:(hh + 1) * 128], AF.Relu, [('psbc', bb), 'acs'],
                               [('tcl', tc_)], scale=-1.0, bias=acs[:, h:h + 1])
                      ACTV(P, Lt[tc_][:], tcl[tc_][:], AF.Exp, [('tcl', tc_)], [('Lt', tc_)], scale=-1.0)
                      TT(P, 'pool', MT[tc_][:], Lt[tc_][:], cbTm[cm][:].unsqueeze(1).to_broadcast([128, 4, 128]), ALU.mult,
                         [('Lt', tc_), ('cbTm', cm)], [('MT', tc_)])
                      qbuf[i] = tc_

                  def quad_b(i):
                      g, hq = quads[i]
                      tc_ = qbuf.pop(i)
                      for hh in range(4):
                          h = g * 8 + hq * 4 + hh
                          MM(P, psyd[:, (hq * 4 + hh) * 64:(hq * 4 + hh + 1) * 64], MT[tc_][:, hh, :], xdt[:, h, :],
                             True, True, [('MT', tc_), 'xdt'], ['psyd'])
                      if hq == 1:
                          TT(P, 'dve', ystage[ys][:, g * 8:(g + 1) * 8, :], psyd[:, :].rearrange("p (h d) -> p h d", d=64),
                             yos[g][:], ALU.add, ['psyd', ('yos', g)], [('ystage', ys)])

                  quad_a(0)
                  for i in range(len(quads)):
                      if i + 1 < len(quads):
                          quad_a(i + 1)
                      quad_b(i)
                  yt = nxt('yTs', 2)
                  for q4 in range(4):
                      b = nxt('ps', 3)
                      for k in range(4):
                          ct = q4 * 4 + k
                          P.op('pe', 'transpose', dict(out=ps[b][:, k * 128:(k + 1) * 128],
                                                       in_=ystage[ys][:, 2 * ct:2 * ct + 2, :].rearrange("p h d -> p (h d)"),
                                                       identity=identf[:]), [('ystage', ys), 'identf'], [('ps', b)])
                      for k in range(4):
                          ct = q4 * 4 + k
                          STT(P, yTs[yt][:, ct, :], xc[:, ct, l0:l0 + 128], Dcol[:, ct:ct + 1], ps[b][:, k * 128:(k + 1) * 128],
                              ALU.mult, ALU.add, [('xc', ct), 'Dcol', ('ps', b)], [('yTs', yt)])
                  DMA(P, 'sp', yT[hf * 2048:(hf + 1) * 2048, t0 + l0:t0 + l0 + 128].rearrange("(c p) t -> p c t", p=128),
                      yTs[yt][:], [('yTs', yt)], [], f'd_y{yt}')
        except _Stop:
            pass


def rmsnorm_stats(P, xt, ones, psk, ps, rstd, sq, xres, nfeat, KCn, eps=EPS):
    for kc in range(KCn):
        ACTV(P, sq[kc % 2][:], xt[:, kc, :], AF.Square, [xres], [('sq', kc % 2)])
        MM(P, ps[:], ones[:], sq[kc % 2][:], kc == 0, kc == KCn - 1, [('sq', kc % 2), 'ones'], [psk])
    TS(P, 'dve', rstd, ps[:], 1.0 / nfeat, eps, ALU.mult, ALU.add, [psk], ['rstd'])
    ACTV(P, rstd, rstd, AF.Sqrt, ['rstd'], ['rstd'])
    P.op('dve', 'reciprocal', dict(out=rstd, in_=rstd), ['rstd'], ['rstd'])


def emit_proj(nc, P, st, xT, W, nwd, table, od, t0, NT, stg_dt):
    NTG = NT // 512
    NTT = NT // 128
    sb = lambda name, shape, dt: st.enter_context(nc.sbuf_tensor(f"p{_PH[0]}_" + name, shape, dt))
    ones = sb("ones", [128, 128], F32)
    nw = sb("nw_sb", [128, KC], F32)
    xt = [sb(f"xt{i}", [128, KC, 512], F32) for i in range(2)]
    sq = [sb(f"sq{i}", [128, 512], F32) for i in range(2)]
    rstd = sb("rstd", [128, 512], F32)
    hT = sb("hT", [128, KC, NT], BF16)
    NW = 3
    wbf = [sb(f"wbf{i}", [128, KC, 512], BF16) for i in range(NW)]
    NS = 4
    stg = [sb(f"stg{i}", [128, 512], stg_dt) for i in range(NS)]
    stgf = sb("stgf", [128, 16], F32)
    psb = [st.enter_context(nc.psum_tensor(f"p{_PH[0]}_ps{i}", [128, 512], F32)) for i in range(8)]
    P.op('dve', 'memset', dict(ap=ones[:], constant=1.0), [], ['ones'])
    DMA(P, 'sp', nw[:], nwd[:, :], [], ['nw'], 'd_nw')
    wt = table

    def load_w(i):
        c0, ncol, kind, dst, d0 = wt[i]
        s = i % NW
        DMA(P, 'pool', wbf[s][:, :, :ncol], W[:, c0:c0 + ncol].rearrange("(kc p) c -> p kc c", p=128),
            [], [('wbf', s)], f'd_w{s}')
    load_w(0)
    load_w(1)
    for tg in range(NTG):
        xr = ('xt', tg % 2)
        DMA(P, 'sp', xt[tg % 2][:], xT[:, t0 + tg * 512:t0 + (tg + 1) * 512].rearrange("(kc p) t -> p kc t", p=128),
            [], [xr], f'd_x{tg % 2}')
        rmsnorm_stats(P, xt[tg % 2], ones, ('ps', 7), psb[7], rstd[:], sq, xr, D, KC)
        for kc in range(KC):
            STT(P, hT[:, kc, tg * 512:(tg + 1) * 512], xt[tg % 2][:, kc, :], nw[:, kc:kc + 1], rstd[:],
                ALU.mult, ALU.mult, [xr, 'rstd', 'nw'], [('hT', kc, tg)])
    cnt = {'ps': 0, 'stg': 0, 'ev': 0}

    def nxt(k, n):
        v = cnt[k] % n
        cnt[k] += 1
        return v
    for i in range(len(wt)):
        if i + 2 < len(wt):
            load_w(i + 2)
        c0, ncol, kind, dstn, d0 = wt[i]
        dst = od[dstn]
        s = i % NW
        if kind == 'fm':
            for cb in range((ncol + 127) // 128):
                m = min(128, ncol - cb * 128)
                for tg in range(NTG):
                    b = nxt('ps', 6)
                    for kc in range(KC):
                        MM(P, psb[b][:m, :], wbf[s][:, kc, cb * 128:cb * 128 + m], hT[:, kc, tg * 512:(tg + 1) * 512],
                           kc == 0, kc == KC - 1, [('wbf', s), ('hT', kc, tg)], [('ps', b)])
                    ss = nxt('stg', NS)
                    ev = 'act' if nxt('ev', 2) == 0 else 'dve'
                    CP(P, ev, stg[ss][:m, :], psb[b][:m, :], [('ps', b)], [('stg', ss)])
                    r0 = d0 + cb * 128
                    DMA(P, 'sp', dst[r0:r0 + m, t0 + tg * 512:t0 + (tg + 1) * 512], stg[ss][:m, :], [('stg', ss)], [], f'd_o{ss}')
        else:
            for tt in range(NTT):
                b = nxt('ps', 6)
                for kc in range(KC):
                    MM(P, psb[b][:, :ncol], hT[:, kc, tt * 128:(tt + 1) * 128], wbf[s][:, kc, :ncol],
                       kc == 0, kc == KC - 1, [('wbf', s), ('hT', kc, tt // 4)], [('ps', b)])
                r0 = t0 + tt * 128
                if kind == 'tmw':
                    TS(P, 'dve', stgf[:, :], psb[b][:, :16], 1.0 / 32.0, None, ALU.mult, None, [('ps', b)], ['stgf'])
                    DMA(P, 'sp', dst[r0:r0 + 128, :], stgf[:, :], ['stgf'], [], 'd_of')
                    continue
                ss = nxt('stg', NS)
                if kind == 'tmg':
                    ACTV(P, stg[ss][:, :ncol], psb[b][:, :ncol], AF.Silu, [('ps', b)], [('stg', ss)])
                else:
                    CP(P, 'dve', stg[ss][:, :ncol], psb[b][:, :ncol], [('ps', b)], [('stg', ss)])
                DMA(P, 'sp', dst[r0:r0 + 128, d0:d0 + ncol], stg[ss][:, :ncol], [('stg', ss)], [], f'd_o{ss}')


def table_a():
    wt = []
    for i in range(8):
        wt.append((i * 512, 512, 'fm', 'qT' if i < 4 else 'kT', (i % 4) * 512))
    for i in range(2):
        wt.append((8192 + i * 512, 512, 'fm', 'iqT', i * 512))
    wt.append((9216, 64, 'fm', 'ikT', 0))
    for i in range(4):
        wt.append((4096 + i * 512, 512, 'tm', 'v', i * 512))
    for i in range(4):
        wt.append((6144 + i * 512, 512, 'tmg', 'sg', i * 512))
    wt.append((9280, 16, 'tmw', 'iw', 0))
    return wt


def table_b():
    wt = []
    for i in range(8):
        wt.append((i * 512, 512, 'fm', 'zT', i * 512))
    for i in range(12):
        wt.append((4096 + i * 512, 512, 'fm', 'xbcT', i * 512))
    wt.append((10240, 64, 'tm', 'dt', 0))
    return wt


def emit_aout(nc, P, st, uT, xT, W, x1T, t0, NT):
    NTG = NT // 512
    sb = lambda name, shape, dt: st.enter_context(nc.sbuf_tensor(f"p{_PH[0]}_" + name, shape, dt))
    aT = sb("aT", [128, KC, NT], BF16)
    wbf = [sb(f"wbf{i}", [128, KC, 512], BF16) for i in range(2)]
    rs = [sb(f"rs{i}", [128, 512], F32) for i in range(4)]
    psb = [st.enter_context(nc.psum_tensor(f"p{_PH[0]}_ps{i}", [128, 512], F32)) for i in range(6)]
    cnt = {}

    def nxt(k, n):
        v = cnt.get(k, 0)
        cnt[k] = v + 1
        return v % n
    DMA(P, 'sp', aT[:], uT[:, t0:t0 + NT].rearrange("(kc p) t -> p kc t", p=128), [], ['aT'], 'd_a')

    def load_w(i):
        DMA(P, 'pool', wbf[i % 2][:], W[:, i * 512:(i + 1) * 512].rearrange("(kc p) c -> p kc c", p=128),
            [], [('wbf', i % 2)], f'd_w{i % 2}')
    load_w(0)
    for i in range(4):
        if i + 1 < 4:
            load_w(i + 1)
        for cb in range(4):
            ob = i * 4 + cb
            for tg in range(NTG):
                c0 = t0 + tg * 512
                r = nxt('rs', 4)
                DMA(P, 'sp', rs[r][:], xT[ob * 128:(ob + 1) * 128, c0:c0 + 512], [], [('rs', r)], f'd_r{r}')
                b = nxt('ps', 6)
                for kc in range(KC):
                    MM(P, psb[b][:], wbf[i % 2][:, kc, cb * 128:(cb + 1) * 128], aT[:, kc, tg * 512:(tg + 1) * 512],
                       kc == 0, kc == KC - 1, [('wbf', i % 2), 'aT'], [('ps', b)])
                TT(P, 'dve', rs[r][:], psb[b][:], rs[r][:], ALU.add, [('ps', b), ('rs', r)], [('rs', r)])
                DMA(P, 'sp', x1T[ob * 128:(ob + 1) * 128, c0:c0 + 512], rs[r][:], [('rs', r)], [], f'd_o{r}')


def emit_bout(nc, P, st, yT, zT, x1T, bnwd, W, x2T, p0, TP=1024):
    KI = 32
    NTG = TP // 512
    sb = lambda name, shape, dt: st.enter_context(nc.sbuf_tensor(f"p{_PH[0]}_" + name, shape, dt))
    ones = sb("ones", [128, 128], F32)
    bnw = sb("bnws", [128, KI], F32)
    gyb = sb("gyb", [128, KI, TP], BF16)
    rstd = sb("rstd", [128, TP], F32)
    yb = [sb(f"yb{i}", [128, 4, 512], F32) for i in range(2)]
    zb = [sb(f"zb{i}", [128, 4, 512], F32) for i in range(2)]
    sq = [sb(f"sq{i}", [128, 512], F32) for i in range(2)]
    wbf = [sb(f"wbf{i}", [128, KI, 256], BF16) for i in range(3)]
    rs = [sb(f"rs{i}", [128, 512], F32) for i in range(4)]
    t32 = [sb(f"t32{i}", [128, 512], F32) for i in range(2)]
    psb = [st.enter_context(nc.psum_tensor(f"p{_PH[0]}_ps{i}", [128, 512], F32)) for i in range(8)]
    cnt = {}

    def nxt(k, n):
        v = cnt.get(k, 0)
        cnt[k] = v + 1
        return v % n
    P.op('dve', 'memset', dict(ap=ones[:], constant=1.0), [], ['ones'])
    DMA(P, 'sp', bnw[:], bnwd[:, :], [], ['bnw'], 'd_bnw')
    wi = [0]

    def load_w(ci):
        s = wi[0] % 3
        wi[0] += 1
        DMA(P, 'pool', wbf[s][:], W[:, ci * 256:(ci + 1) * 256].rearrange("(kc p) c -> p kc c", p=128),
            [], [('wbf', s)], f'd_w{s}')
        return s
    slots = {0: load_w(0), 1: load_w(1)}
    for tg in range(NTG):
        c0 = p0 + tg * 512
        for k4 in range(KI // 4):
            s = nxt('yz', 2)
            DMA(P, 'sp', yb[s][:], yT[k4 * 512:(k4 + 1) * 512, c0:c0 + 512].rearrange("(k p) t -> p k t", p=128),
                [], [('yb', s)], f'd_y{s}')
            DMA(P, 'sp', zb[s][:], zT[k4 * 512:(k4 + 1) * 512, c0:c0 + 512].rearrange("(k p) t -> p k t", p=128),
                [], [('zb', s)], f'd_z{s}')
            ACTV(P, zb[s][:], zb[s][:], AF.Silu, [('zb', s)], [('zb', s)])
            TT(P, 'dve', yb[s][:], yb[s][:], zb[s][:], ALU.mult, [('yb', s), ('zb', s)], [('yb', s)])
            for k in range(4):
                kc = k4 * 4 + k
                q = nxt('sq', 2)
                ACTV(P, sq[q][:], yb[s][:, k, :], AF.Square, [('yb', s)], [('sq', q)])
                MM(P, psb[7][:], ones[:], sq[q][:], kc == 0, kc == KI - 1, [('sq', q), 'ones'], [('ps', 7)])
                TS(P, 'pool', gyb[:, kc, tg * 512:(tg + 1) * 512], yb[s][:, k, :], bnw[:, kc:kc + 1], 1.0,
                   ALU.mult, ALU.mult, [('yb', s), 'bnw'], [('gyb', kc, tg)])
        rv = rstd[:, tg * 512:(tg + 1) * 512]
        TS(P, 'dve', rv, psb[7][:], 1.0 / 4096.0, EPS, ALU.mult, ALU.add, [('ps', 7)], [('rstd', tg)])
        ACTV(P, rv, rv, AF.Sqrt, [('rstd', tg)], [('rstd', tg)])
        P.op('dve', 'reciprocal', dict(out=rv, in_=rv), [('rstd', tg)], [('rstd', tg)])
    for ci in range(8):
        if ci + 2 < 8:
            slots[ci + 2] = load_w(ci + 2)
        s = slots[ci]
        for cb in range(2):
            ob = ci * 2 + cb
            for tg in range(NTG):
                c0 = p0 + tg * 512
                r = nxt('rs', 4)
                DMA(P, 'sp', rs[r][:], x1T[ob * 128:(ob + 1) * 128, c0:c0 + 512], [], [('rs', r)], f'd_r{r}')
                b = nxt('ps', 6)
                for kc in range(KI):
                    MM(P, psb[b][:], wbf[s][:, kc, cb * 128:(cb + 1) * 128], gyb[:, kc, tg * 512:(tg + 1) * 512],
                       kc == 0, kc == KI - 1, [('wbf', s), ('gyb', kc, tg)], [('ps', b)])
                t = nxt('t32', 2)
                TT(P, 'dve', t32[t][:], psb[b][:], rstd[:, tg * 512:(tg + 1) * 512], ALU.mult,
                   [('ps', b), ('rstd', tg)], [('t32', t)])
                TT(P, 'pool', rs[r][:], t32[t][:], rs[r][:], ALU.add, [('t32', t), ('rs', r)], [('rs', r)])
                DMA(P, 'sp', x2T[ob * 128:(ob + 1) * 128, c0:c0 + 512], rs[r][:], [('rs', r)], [], f'd_o{r}')


def emit_fnorm(nc, P, st, xT, nwd, oT, NT):
    sb = lambda name, shape, dt: st.enter_context(nc.sbuf_tensor(f"p{_PH[0]}_" + name, shape, dt))
    ones = sb("ones", [128, 128], F32)
    nw = sb("nw_sb", [128, KC], F32)
    xt = [sb(f"xt{i}", [128, KC, 512], F32) for i in range(2)]
    sq = [sb(f"sq{i}", [128, 512], F32) for i in range(2)]
    rstd = sb("rstd", [128, 512], F32)
    ps = st.enter_context(nc.psum_tensor(f"p{_PH[0]}_ps0", [128, 512], F32))
    P.op('dve', 'memset', dict(ap=ones[:], constant=1.0), [], ['ones'])
    DMA(P, 'sp', nw[:], nwd[:, :], [], ['nw'], 'd_nw')
    for tg in range(NT // 512):
        xr = ('xt', tg % 2)
        DMA(P, 'sp', xt[tg % 2][:], xT[:, tg * 512:(tg + 1) * 512].rearrange("(kc p) t -> p kc t", p=128),
            [], [xr], f'd_x{tg % 2}')
        rmsnorm_stats(P, xt[tg % 2], ones, ('ps', 0), ps, rstd[:], sq, xr, D, KC)
        for kc in range(KC):
            STT(P, xt[tg % 2][:, kc, :], xt[tg % 2][:, kc, :], nw[:, kc:kc + 1], rstd[:],
                ALU.mult, ALU.mult, [xr, 'rstd', 'nw'], [xr])
        DMA(P, 'sp', oT[:, tg * 512:(tg + 1) * 512].rearrange("(kc p) t -> p kc t", p=128), xt[tg % 2][:],
            [xr], [], f'd_o{tg % 2}')


SEQ = 4096
_DBG = None


def build_fused(phases=None):
    nc = bass.Bass("TRN2", target_bir_lowering=False)
    ein = lambda name, shape, dt: nc.dram_tensor(name, list(shape), dt, kind="ExternalInput").ap()
    scr = lambda name, shape, dt: nc.dram_tensor(name, list(shape), dt, kind="Internal").ap()
    dr = dict(
        xT=ein("xT", [D, SEQ], F32), nwA=ein("nwA", [128, KC], F32), nwB=ein("nwB", [128, KC], F32),
        nwF=ein("nwF", [128, KC], F32), Wa=ein("Wa", [D, 9296], F32), Wo=ein("Wo", [D, D], F32),
        Wb=ein("Wb", [D, 10304], F32), Wbo=ein("Wbo", [4096, D], F32), bnw=ein("bnw", [128, 32], F32),
        bfar=ein("bfar", [128, 16], F32), ident=ein("ident", [128, 128], BF16), cm=ein("cm", [2, 128, 512], F32),
        biasN=ein("biasN", [2, 16, 128, 768], F32), U=ein("U", [128, 128], F32), ones128=ein("ones128", [128, 128], F32),
        sel=ein("sel", [32, 32 * 128], F32), tri=ein("tri", [128, 128], F32), identf=ein("identf", [128, 128], F32),
        cw=ein("cw", [2, 128, NCT * 4], F32), cb=ein("cb", [2, 128, NCT], F32), dtb=ein("dtb", [2, 128, NH], F32),
        alog=ein("alog", [2, 128, NH], F32), Dv=ein("Dv", [2, 128, NH], F32), Dcol=ein("Dcol", [2, 128, 16], F32),
        qT=scr("qT", [2048, SEQ], BF16), kT=scr("kT", [2048, SEQ], BF16), iqT=scr("iqT", [1024, SEQ], BF16),
        ikT=scr("ikT", [64, SEQ], BF16), v=scr("v", [SEQ, 2048], BF16), sg=scr("sg", [SEQ, 2048], BF16),
        iw=scr("iw", [SEQ, 16], F32), uT=scr("uT", [2048, SEQ], BF16), x1T=scr("x1T", [D, SEQ], F32),
        zT=scr("zT", [4096, SEQ], F32), xbcT=scr("xbcT", [6144, SEQ], F32), dt=scr("dt", [SEQ, 64], F32),
        yT=scr("yT", [4096, SEQ], F32), x2T=scr("x2T", [D, SEQ], F32),
    )
    oT = nc.dram_tensor("oT", [D, SEQ], F32, kind="ExternalOutput").ap()
    plist = []
    for hf in range(2):
        plist.append(('projA', lambda P, st, hf=hf: emit_proj(nc, P, st, dr['xT'], dr['Wa'], dr['nwA'], table_a(), dr,
                                                              hf * 2048, 2048, BF16)))
    for par in range(2):
        plist.append(('attn', lambda P, st, par=par: emit_k2(nc, P, st, par, dr)))
    for hf in range(2):
        plist.append(('aout', lambda P, st, hf=hf: emit_aout(nc, P, st, dr['uT'], dr['xT'], dr['Wo'], dr['x1T'], hf * 2048, 2048)))
    for hf in range(2):
        plist.append(('projB', lambda P, st, hf=hf: emit_proj(nc, P, st, dr['x1T'], dr['Wb'], dr['nwB'], table_b(), dr,
                                                              hf * 2048, 2048, F32)))
    for hf in range(2):
        plist.append(('ssd', lambda P, st, hf=hf: emit_k4(nc, P, st, hf, dr)))
    for ps_ in range(4):
        plist.append(('bout', lambda P, st, ps_=ps_: emit_bout(nc, P, st, dr['yT'], dr['zT'], dr['x1T'], dr['bnw'], dr['Wbo'],
                                                               dr['x2T'], ps_ * 1024)))
    plist.append(('fnorm', lambda P, st: emit_fnorm(nc, P, st, dr['x2T'], dr['nwF'], oT, SEQ)))
    with contextlib.ExitStack() as g:
        P = Prog(nc, g)
        for i, (name, fn) in enumerate(plist):
            if phases is not None and i not in phases:
                continue
            _PH[0] = i
            with contextlib.ExitStack() as st:
                fn(P, st)
                P.end_phase(st)
    return nc


def _pk(w, n):
    return np.ascontiguousarray(np.asarray(w, np.float32).reshape(n, 128).T)


def _bc(a):
    a = np.asarray(a, np.float32)
    return np.ascontiguousarray(np.broadcast_to(a[None, :], (128, a.shape[0])))


def host_inputs(b, x, norm_w, a_w_in, a_w_out, rel_bias, b_w_in, b_conv_w, b_conv_b, b_dt_bias, b_a_log, b_d,
                b_norm_w, b_w_out, final_norm_w, shared):
    A = np.ascontiguousarray
    im = dict(shared)
    im["xT"] = A(np.asarray(x[b], np.float32).T)
    return im


def host_shared(norm_w, a_w_in, a_w_out, rel_bias, b_w_in, b_conv_w, b_conv_b, b_dt_bias, b_a_log, b_d,
                b_norm_w, b_w_out, final_norm_w):
    A = np.ascontiguousarray
    f = lambda a: np.asarray(a, np.float32)
    sh = dict(nwA=_pk(norm_w[0], KC), nwB=_pk(norm_w[1], KC), nwF=_pk(final_norm_w, KC), Wa=A(f(a_w_in[0])),
              Wo=A(f(a_w_out[0])), Wb=A(f(b_w_in[0])), Wbo=A(f(b_w_out[0])), bnw=_pk(b_norm_w[0], 32))
    rb = f(rel_bias)
    c0 = k2_consts(rb, 0)
    c1 = k2_consts(rb, 1)
    sh.update(bfar=c0['bfar'], ident=c0['ident'], cm=A(np.stack([c0['cm'], c1['cm']])),
              biasN=A(np.stack([c0['biasN'], c1['biasN']])))
    k4c = k4_consts()
    sh.update(U=k4c['U'], ones128=k4c['ones128'], sel=k4c['sel'], tri=k4c['tri'], identf=k4c['identf'])
    cw = f(b_conv_w[0])
    cbv = f(b_conv_b[0])
    cws, cbs, dtbs, alogs, Dvs = [], [], [], [], []
    for hf in range(2):
        chans = np.concatenate([np.arange(hf * 2048, (hf + 1) * 2048), 4096 + np.arange(hf * 512, (hf + 1) * 512),
                                5120 + np.arange(hf * 512, (hf + 1) * 512)])
        hs = slice(hf * 32, (hf + 1) * 32)
        cws.append(cw[:, chans].T.reshape(NCT, 128, 4).transpose(1, 0, 2).reshape(128, NCT * 4))
        cbs.append(cbv[chans].reshape(NCT, 128).T)
        dtbs.append(_bc(f(b_dt_bias[0])[hs]))
        alogs.append(_bc(f(b_a_log[0])[hs]))
        Dvs.append(_bc(f(b_d[0])[hs]))
    sh.update(cw=A(np.stack(cws)), cb=A(np.stack(cbs)), dtb=A(np.stack(dtbs)), alog=A(np.stack(alogs)), Dv=A(np.stack(Dvs)))
    dcol = [np.repeat(f(b_d[0])[hf * 32:(hf + 1) * 32], 64).reshape(16, 128).T for hf in range(2)]
    sh['Dcol'] = A(np.stack(dcol))
    return sh


NCORES = 8


def kernel(x, norm_w, a_w_in, a_w_out, rel_bias, b_w_in, b_conv_w, b_conv_b, b_dt_bias, b_a_log, b_d,
           b_norm_w, b_w_out, final_norm_w):
    nc = build_fused()
    sh = host_shared(norm_w, a_w_in, a_w_out, rel_bias, b_w_in, b_conv_w, b_conv_b, b_dt_bias, b_a_log, b_d,
                     b_norm_w, b_w_out, final_norm_w)
    ims = []
    for c in range(NCORES):
        im = dict(sh)
        im["xT"] = np.ascontiguousarray(np.asarray(x[c % 4], np.float32).T)
        ims.append(im)
    res = run_bass_kernel_spmd(nc, ims, core_ids=list(range(NCORES)))
    out = np.zeros((4, SEQ, D), np.float32)
    for b in range(4):
        out[b] = np.asarray(res.results[b]["oT"]).T
    return out
```

```python
import contextlib
import numpy as np
import ml_dtypes
import concourse.bass as bass
import concourse.mybir as mybir
from concourse.bass_utils import run_bass_kernel_spmd

F32 = mybir.dt.float32
_PH = [0]
BF16 = mybir.dt.bfloat16
ALU = mybir.AluOpType
AF = mybir.ActivationFunctionType
AX = mybir.AxisListType

ENGS = ['pe', 'act', 'dve', 'pool', 'sp']


def _is_psum(r):
    n = r[0] if isinstance(r, tuple) else r
    return isinstance(n, str) and (n.startswith('ps') or n.startswith('acc'))


class Prog:
    def __init__(self, nc, gstack):
        self.nc = nc
        self.gstack = gstack
        self.sems = {}
        self.semval = {}
        self.base = {}
        self.dma_keys = set()
        self.total_instr = {e: 0 for e in ENGS}
        self._reset()

    def _reset(self):
        self.q = {e: [] for e in ENGS}
        self.waited = {e: {} for e in ENGS}
        self.res = {}
        self.touched = set()

    def op(self, eng, meth, kw, reads=(), writes=(), dma=None):
        fn = (meth, kw)
        waits = {}
        own = 'c_' + eng
        xr = [r for r in reads if _is_psum(r)]
        if xr:
            writes = list(writes) + [r for r in xr if r not in writes]

        def need(kv, raw):
            if kv is None:
                return
            k, v = kv
            if k == own and not raw:
                return
            if k == own and eng == 'pe':
                return
            if self.waited[eng].get(k, 0) >= v:
                return
            if waits.get(k, 0) < v:
                waits[k] = v

        for r in reads:
            st = self.res.get(r)
            if st:
                need(st['w'], True)
        for w in writes:
            st = self.res.get(w)
            if st:
                need(st['w'], False)
                for kv in st['r'].items():
                    need(kv, False)
        if dma:
            key, inc = dma, 16
            self.dma_keys.add(key)
        else:
            key, inc = own, 1
        self.semval[key] = self.semval.get(key, 0) + inc
        self.touched.add(key)
        val = self.semval[key]
        for k, v in waits.items():
            self.waited[eng][k] = v
        self.q[eng].append([fn, sorted(waits.items()), key, inc, val])
        for r in reads:
            st = self.res.setdefault(r, {'w': None, 'r': {}})
            st['r'][key] = val
        for w in writes:
            self.res[w] = {'w': (key, val), 'r': {}}
        return (key, val)

    def end_phase(self, pstack):
        nc = self.nc
        fin = [(k, self.semval[k]) for k in sorted(self.touched)]
        for e in ENGS:
            self.q[e].append([None, list(fin), None, 0, 0])
        waited_vals = {}
        for e in ENGS:
            for fn, waits, key, inc, val in self.q[e]:
                for k, v in waits:
                    waited_vals.setdefault(k, set()).add(v)
        remap = {}
        for k, vs in waited_vals.items():
            if k in self.dma_keys:
                continue
            b0 = self.base.get(k, 0)
            remap[k] = {v: b0 + i + 1 for i, v in enumerate(sorted(vs))}
        for k in self.touched:
            if k not in self.sems:
                self.sems[k] = self.gstack.enter_context(nc.semaphore("s_" + k))
        sems = self.sems
        block = pstack.enter_context(nc.Block())
        engmap = {'pe': block.tensor, 'act': block.scalar, 'dve': block.vector,
                  'pool': block.gpsimd, 'sp': block.sync}
        for e in ENGS:
            ops = self.q[e]
            self.total_instr[e] += len(ops)

            def body(engine, ops=ops):
                for fn, waits, key, inc, val in ops:
                    for k, v in waits:
                        if k in self.dma_keys:
                            engine.wait_ge(sems[k], v)
                        else:
                            engine.wait_ge(sems[k], remap[k][v])
                    if fn is None:
                        continue
                    ins = getattr(engine, fn[0])(**fn[1])
                    if key in self.dma_keys:
                        ins.then_inc(sems[key], 16)
                    elif key in remap and val in remap[key]:
                        ins.then_inc(sems[key], 1)
            engmap[e](body)
        for k, m in remap.items():
            self.base[k] = self.base.get(k, 0) + len(m)
        self._reset()


def MM(P, out, lhsT, rhs, start, stop, reads, writes, **kw):
    return P.op('pe', 'matmul', dict(out=out, lhsT=lhsT, rhs=rhs, start=start, stop=stop, **kw), reads, writes)


def ACTV(P, out, in_, func, reads, writes, **kw):
    return P.op('act', 'activation', dict(out=out, in_=in_, func=func, **kw), reads, writes)


def DMA(P, eng, out, in_, reads, writes, key):
    return P.op(eng, 'dma_start', dict(out=out, in_=in_), reads, writes, dma=key)


def TS(P, eng, out, in0, s1, s2, op0, op1, reads, writes, **kw):
    d = dict(out=out, in0=in0, scalar1=s1, scalar2=s2, op0=op0, **kw)
    if op1 is not None:
        d['op1'] = op1
    return P.op(eng, 'tensor_scalar', d, reads, writes)


def TT(P, eng, out, in0, in1, op, reads, writes):
    return P.op(eng, 'tensor_tensor', dict(out=out, in0=in0, in1=in1, op=op), reads, writes)


def STT(P, out, in0, scalar, in1, op0, op1, reads, writes, **kw):
    return P.op('dve', 'scalar_tensor_tensor', dict(out=out, in0=in0, scalar=scalar, in1=in1, op0=op0, op1=op1, **kw),
                reads, writes)


def CP(P, eng, out, in_, reads, writes):
    if eng == 'act':
        return P.op('act', 'activation', dict(out=out, in_=in_, func=AF.Copy), reads, writes)
    return P.op(eng, 'tensor_copy', dict(out=out, in_=in_), reads, writes)


D = 2048
KC = D // 128
EPS = 1e-6


S = 4096
NIT = 26
SCALE = 128 ** -0.5


def own_blocks(par):
    return [i for i in range(32) if (i % 4 in (0, 3)) == (par == 0)]


def k2_consts(rel_bias, par):
    n = np.arange(256)
    nf = np.maximum(n, 1).astype(np.float32)
    large = 16 + (np.log(nf / 16) / np.log(128 / 16) * 16).astype(np.int32)
    large = np.minimum(large, 31)
    bucket = np.where(n < 16, n, large)
    s = np.arange(128)[:, None]
    q = np.arange(128)[None, :]
    biasT = np.zeros((128, 16, 2, 128), np.float32)
    for d in range(2):
        dist = np.clip(d * 128 + q - s, 0, 255)
        biasT[:, :, d, :] = rel_bias[bucket[dist]].transpose(0, 2, 1)
    bfar = np.ascontiguousarray(np.broadcast_to(rel_bias[31][None, :], (128, 16))).astype(np.float32)
    negtri = np.where(np.arange(128)[None, :] <= np.arange(128)[:, None], 0.0, -1e30).astype(np.float32)
    ident = np.eye(128, dtype=np.float32).astype(ml_dtypes.bfloat16)
    dtab = lambda sp, w: (1 - w) if (sp == par) else (2 - w)
    cm = np.zeros((128, 2, 2, 128), np.float32)
    biasN = np.zeros((16, 128, 2, 3, 128), np.float32)
    for sp in range(2):
        for w in range(3):
            d = dtab(sp, w)
            if d >= 2:
                biasN[:, :, sp, w, :] = rel_bias[31][:, None, None]
            elif d >= 0:
                biasN[:, :, sp, w, :] = biasT[:, :, d, :].transpose(1, 0, 2)
            if w >= 1:
                if d == 0:
                    cm[:, sp, w - 1, :] = negtri
                elif d < 0:
                    cm[:, sp, w - 1, :] = -1e30
    return dict(bfar=bfar, ident=ident, cm=cm.reshape(128, 512), biasN=biasN.reshape(16, 128, 768))


def emit_k2(nc, P, st, par, dr, nslots=16, nheads=16, nit=NIT):
    own = own_blocks(par)
    rsel = (0, 3) if par == 0 else (1, 2)
    NQB = nslots
    nkbs = [2 * (qi + 1) for qi in range(nslots)]
    NQ = NQB * 128
    offs = []
    T = 0
    for nkb in nkbs:
        offs.append(T)
        T += nkb
    kT, v, ikT, qT, iqT, iw, sg, uT = dr['kT'], dr['v'], dr['ikT'], dr['qT'], dr['iqT'], dr['iw'], dr['sg'], dr['uT']
    biasNd, bfard, cmd, identd = dr['biasN'], dr['bfar'], dr['cm'], dr['ident']
    if True:
        sb = lambda name, shape, dt: st.enter_context(nc.sbuf_tensor(f"p{_PH[0]}_" + name, shape, dt))
        maskT = sb("maskT", [128, T, 128], BF16)
        score = sb("score", [128, S], F32)
        maskq = sb("maskq", [128, S], BF16)
        relu = [sb(f"relu{i}", [128, 512], F32) for i in range(2)]
        ikTs = sb("ikTs", [64, S], BF16)
        iqb = [sb(f"iqb{i}", [64, 16, 128], BF16) for i in range(2)]
        iwb = [sb(f"iwb{i}", [128, 16], F32) for i in range(2)]
        kTh = [sb(f"kTh{i}", [128, S], BF16) for i in range(2)]
        vh = [sb(f"vh{i}", [128, 32, 136], BF16) for i in range(2)]
        qTh = [sb(f"qTh{i}", [128, NQ], BF16) for i in range(2)]
        sgb = [sb(f"sgb{i}", [128, NQB, 128], BF16) for i in range(2)]
        biasN = [sb(f"biasN{i}", [128, 2, 3, 128], F32) for i in range(2)]
        bfar = sb("bfars", [128, 16], F32)
        cm = sb("cms", [128, 2, 256], F32)
        ident = sb("idents", [128, 128], BF16)
        hi = sb("hi", [128, 1], F32)
        lo = sb("lo", [128, 1], F32)
        w = sb("w", [128, 1], F32)
        mid = sb("mid", [128, 1], F32)
        cnt = sb("cnt", [128, 1], F32)
        ge = sb("ge", [128, 1], F32)
        rinv = sb("rinv", [128, 1], F32)
        NE = 6
        tb = [sb(f"tb{i}", [128, 128], F32) for i in range(2)]
        eb = [sb(f"eb{i}", [128, 512], BF16) for i in range(NE)]
        pt = [sb(f"pt{i}", [128, 512], BF16) for i in range(NE)]
        o32 = [sb(f"o32{i}", [128, 128], F32) for i in range(2)]
        ust = [sb(f"ust{i}", [128, 128], BF16) for i in range(4)]
        usT = [sb(f"usT{i}", [128, 128], BF16) for i in range(4)]
        psb = [st.enter_context(nc.psum_tensor(f"p{_PH[0]}_ps{i}", [128, 512], F32)) for i in range(4)]
        pst = [st.enter_context(nc.psum_tensor(f"p{_PH[0]}_pst{i}", [128, 1024], BF16)) for i in range(2)]
        acc = [st.enter_context(nc.psum_tensor(f"p{_PH[0]}_acc{i}", [128, 512], F32)) for i in range(2)]
        cn = {}

        def nxt(k, n):
            vv = cn.get(k, 0)
            cn[k] = vv + 1
            return vv % n
        DMA(P, 'sp', ikTs[:], ikT[:, :], [], ['ikT'], 'd_c0')
        DMA(P, 'sp', cm[:], cmd[par].rearrange("p (a b) -> p a b", a=2), [], ['cm'], 'd_c1')
        DMA(P, 'sp', ident[:], identd[:, :], [], ['ident'], 'd_c2')
        DMA(P, 'sp', bfar[:], bfard[:, :], [], ['bfar'], 'd_c3')
        for i in range(2):
            P.op('pool', 'memset', dict(ap=vh[i][:, :, 128:129], constant=1.0), [], [('vh1', i)])

        for qi in range(nslots):
            nkb = nkbs[qi]
            nk = nkb * 128
            s2 = qi % 2
            DMA(P, 'sp', iqb[s2][:], iqT[:, own[qi] * 128:(own[qi] + 1) * 128].rearrange("(h d) q -> d h q", d=64),
                [], [('iqb', s2)], f'd_iq{s2}')
            DMA(P, 'sp', iwb[s2][:], iw[own[qi] * 128:(own[qi] + 1) * 128, :], [], [('iwb', s2)], f'd_iw{s2}')
            nch = (nk + 511) // 512
            for c in range(nch):
                kw = min(512, nk - c * 512)
                for hh in range(16):
                    b = nxt('ps', 4)
                    MM(P, psb[b][:, :kw], iqb[s2][:, hh, :], ikTs[:, c * 512:c * 512 + kw], True, True,
                       [('iqb', s2), 'ikT'], [('ps', b)])
                    rb = nxt('relu', 2)
                    ACTV(P, relu[rb][:, :kw], psb[b][:, :kw], AF.Relu, [('ps', b)], [('relu', rb)])
                    sc = score[:, c * 512:c * 512 + kw]
                    if hh == 0:
                        TS(P, 'dve', sc, relu[rb][:, :kw], iwb[s2][:, 0:1], None, ALU.mult, None,
                           [('relu', rb), ('iwb', s2)], [('score', c)])
                    else:
                        STT(P, sc, relu[rb][:, :kw], iwb[s2][:, hh:hh + 1], sc, ALU.mult, ALU.add,
                            [('relu', rb), ('iwb', s2), ('score', c)], [('score', c)])
            scr = [('score', c) for c in range(nch)]
            P.op('dve', 'tensor_reduce', dict(out=hi[:], in_=score[:, :nk], axis=AX.X, op=ALU.max), scr, ['hi'])
            P.op('dve', 'tensor_reduce', dict(out=lo[:], in_=score[:, :nk], axis=AX.X, op=ALU.min), scr, ['lo'])
            TT(P, 'dve', w[:], hi[:], lo[:], ALU.subtract, ['hi', 'lo'], ['w'])
            dg = score[:, (nkb - 2) * 128:nkb * 128]
            TT(P, 'dve', dg, dg, cm[:, qi % 2, :], ALU.add, [('score', (nkb - 2) // 4), 'cm'], [('score', (nkb - 2) // 4)])
            for it in range(nit):
                TS(P, 'dve', w[:], w[:], 0.5, None, ALU.mult, None, ['w'], ['w'])
                TT(P, 'dve', mid[:], lo[:], w[:], ALU.add, ['lo', 'w'], ['mid'])
                TS(P, 'dve', maskq[:, :nk], score[:, :nk], mid[:, 0:1], 0.0, ALU.is_ge, ALU.add,
                   scr + ['mid'], ['maskq', 'cnt'], accum_out=cnt[:, 0:1])
                TS(P, 'dve', ge[:], cnt[:], 255.5, None, ALU.is_ge, None, ['cnt'], ['ge'])
                STT(P, lo[:], ge[:], w[:, 0:1], lo[:], ALU.mult, ALU.add, ['ge', 'w', 'lo'], ['lo'])
            TS(P, 'dve', maskq[:, :nk], score[:, :nk], lo[:, 0:1], None, ALU.is_ge, None, scr + ['lo'], ['maskq'])
            for j in range(nkb):
                b = nxt('pst', 2)
                P.op('pe', 'transpose', dict(out=pst[b][:, :128], in_=maskq[:, j * 128:(j + 1) * 128], identity=ident[:]),
                     ['maskq', 'ident'], [('pst', b)])
                CP(P, 'act', maskT[:, offs[qi] + j, :], pst[b][:, :128], [('pst', b)], [('maskT', qi)])

        def head_loads(h):
            s2 = h % 2
            DMA(P, 'sp', kTh[s2][:], kT[h * 128:(h + 1) * 128, :], [], [('kTh', s2)], f'd_k{s2}')
            DMA(P, 'sp', vh[s2][:, :, 0:128], v[:, h * 128:(h + 1) * 128].rearrange("(j p) d -> p j d", p=128),
                [], [('vh', s2)], f'd_v{s2}')
            for k in range(2):
                DMA(P, 'sp', qTh[s2][:].rearrange("p (m k t) -> p m k t", k=2, t=128)[:, :, k, :],
                    qT[h * 128:(h + 1) * 128, :].rearrange("p (m r t) -> p m r t", r=4, t=128)[:, :, rsel[k], :],
                    [], [('qTh', s2)], f'd_q{s2}')
            for k in range(2):
                DMA(P, 'sp', sgb[s2][:].rearrange("p (m k) d -> p m k d", k=2)[:, :, k, :],
                    sg[:, h * 128:(h + 1) * 128].rearrange("(m r p) d -> p m r d", r=4, p=128)[:, :, rsel[k], :],
                    [], [('sgb', s2)], f'd_sg{s2}')
            DMA(P, 'sp', biasN[s2][:], biasNd[par, h].rearrange("p (a w q) -> p a w q", a=2, w=3), [], [('biasN', s2)], f'd_bn{s2}')

        units = []
        for h in range(nheads):
            for qi in range(nslots):
                nkb = nkbs[qi]
                nfar = max(0, nkb - 3)
                j = 0
                while j < nfar:
                    n = min(4, nfar - j)
                    units.append((h, qi, j, n, True))
                    j += n
                for j in range(nfar, nkb):
                    units.append((h, qi, j, 1, False))
        ubuf = {}

        def stage_a(t):
            h, qi, j0, n, far = units[t]
            s2 = h % 2
            nkb = nkbs[qi]
            b = nxt('ps', 4)
            for k in range(n):
                MM(P, psb[b][:, k * 128:(k + 1) * 128], kTh[s2][:, (j0 + k) * 128:(j0 + k + 1) * 128],
                   qTh[s2][:, qi * 128:(qi + 1) * 128], True, True, [('kTh', s2), ('qTh', s2)], [('ps', b)])
            e = nxt('eb', NE)
            if far:
                ACTV(P, eb[e][:, :n * 128], psb[b][:, :n * 128], AF.Exp, [('ps', b), 'bfar'], [('eb', e)],
                     scale=SCALE, bias=bfar[:, h:h + 1])
            else:
                wv = j0 - (nkb - 3)
                tt_ = nxt('tb', 2)
                STT(P, tb[tt_][:], psb[b][:, :128], SCALE, biasN[s2][:, qi % 2, wv, :], ALU.mult, ALU.add,
                    [('ps', b), ('biasN', s2)], [('tb', tt_)])
                ACTV(P, eb[e][:, :128], tb[tt_][:], AF.Exp, [('tb', tt_)], [('eb', e)])
            p = nxt('pt', NE)
            me = 'dve' if nxt('me', 2) == 0 else 'pool'
            TT(P, me, pt[p][:, :n * 128], eb[e][:, :n * 128],
               maskT[:, offs[qi] + j0:offs[qi] + j0 + n, :].rearrange("p a b -> p (a b)"), ALU.mult,
               [('eb', e), ('maskT', qi)], [('pt', p)])
            ubuf[t] = p

        def stage_b(t):
            h, qi, j0, n, far = units[t]
            s2 = h % 2
            nkb = nkbs[qi]
            ab = (h * nslots + qi) % 2
            p = ubuf.pop(t)
            for k in range(n):
                j = j0 + k
                MM(P, acc[ab][:, :129], pt[p][:, k * 128:(k + 1) * 128], vh[s2][:, j, 0:129], j == 0, j == nkb - 1,
                   [('pt', p), ('vh', s2), ('vh1', s2)], [('acc', ab)])
            if j0 + n != nkb:
                return
            P.op('dve', 'reciprocal', dict(out=rinv[:], in_=acc[ab][:, 128:129]), [('acc', ab)], ['rinv'])
            o = nxt('o32', 2)
            TS(P, 'dve', o32[o][:], acc[ab][:, :128], rinv[:, 0:1], None, ALU.mult, None,
               [('acc', ab), 'rinv'], [('o32', o)])
            us = nxt('ust', 4)
            TT(P, 'pool', ust[us][:], o32[o][:], sgb[s2][:, qi, :], ALU.mult, [('o32', o), ('sgb', s2)], [('ust', us)])
            tp = nxt('pst', 2)
            P.op('pe', 'transpose', dict(out=pst[tp][:, :128], in_=ust[us][:], identity=ident[:]),
                 [('ust', us), 'ident'], [('pst', tp)])
            CP(P, 'act', usT[us][:], pst[tp][:, :128], [('pst', tp)], [('usT', us)])
            DMA(P, 'sp', uT[h * 128:(h + 1) * 128, own[qi] * 128:(own[qi] + 1) * 128], usT[us][:], [('usT', us)], [], f'd_u{us}')
            if qi == nslots - 1 and h + 2 < nheads:
                head_loads(h + 2)

        LA = 3
        head_loads(0)
        if nheads > 1:
            head_loads(1)
        for t in range(len(units) + LA):
            if t < len(units):
                stage_a(t)
            if t - LA >= 0:
                stage_b(t - LA)


NH = 32
NG = 4
NCT = 24


def k4_consts():
    U = np.triu(np.ones((128, 128), np.float32))
    sel = np.zeros((32, 32, 128), np.float32)
    for h in range(32):
        sel[h, h, :] = 1.0
    return dict(U=U, ones128=np.ones((128, 128), np.float32), sel=sel.reshape(32, 32 * 128),
                tri=U.copy(), identf=np.eye(128, dtype=np.float32),
                identb=np.eye(128, dtype=np.float32).astype(ml_dtypes.bfloat16))


class _Stop(Exception):
    pass


def emit_k4(nc, P, st, hf, dr, npieces=8, stop=99):
    SS = npieces * 512
    xbcT, dtd, yT = dr['xbcT'], dr['dt'], dr['yT']
    cwd, cbd, dtbd, alogd, Dd = dr['cw'][hf], dr['cb'][hf], dr['dtb'][hf], dr['alog'][hf], dr['Dv'][hf]
    Ud, onesd, seld, trid, identfd, identbd = dr['U'], dr['ones128'], dr['sel'], dr['tri'], dr['identf'], dr['ident']
    if True:
        sb = lambda name, shape, dt: st.enter_context(nc.sbuf_tensor(f"p{_PH[0]}_" + name, shape, dt))
        xin = sb("xin", [128, 12, 515], F32)
        xc = sb("xc", [128, 16, 512], F32)
        BTc = sb("BTc", [128, 4, 512], BF16)
        CTc = sb("CTc", [128, 4, 512], BF16)
        cacc = [sb(f"cacc{i}", [128, 512], F32) for i in range(2)]
        cw = sb("cws", [128, NCT, 4], F32)
        cb = sb("cbs", [128, NCT], F32)
        dtb = sb("dtbs", [128, NH], F32)
        Aneg = sb("Aneg", [128, NH], F32)
        Dv = sb("Dvs", [128, NH], F32)
        U = sb("Us", [128, 128], F32)
        ones128 = sb("ones128s", [128, 128], F32)
        sel = sb("sels", [32, 32, 128], F32)
        tri = sb("tris", [128, 128], F32)
        identf = sb("identfs", [128, 128], F32)
        identb = sb("identbs", [128, 128], BF16)
        dtr = sb("dtr", [128, 4, NH], F32)
        dtp = sb("dtp", [128, 4, NH], F32)
        adt = sb("adt", [128, 4, NH], F32)
        acs2 = [sb(f"acs{i}", [128, NH], F32) for i in range(2)]
        ea2 = [sb(f"ea{i}", [128, NH], F32) for i in range(2)]
        dec2 = [sb(f"dec{i}", [128, NH], F32) for i in range(2)]
        cdec2 = [sb(f"cdec{i}", [128, NH], F32) for i in range(2)]
        dtdec2 = [sb(f"dtdec{i}", [128, NH], F32) for i in range(2)]
        acsT2 = [sb(f"acsT{i}", [32, 128], F32) for i in range(2)]
        x_tm2 = [sb(f"x_tm{i}", [128, NH, 64], F32) for i in range(2)]
        xdt2 = [sb(f"xdt{i}", [128, NH, 64], BF16) for i in range(2)]
        xdtd2 = [sb(f"xdtd{i}", [128, NH, 64], BF16) for i in range(2)]
        B_tm2 = [sb(f"B_tm{i}", [128, 4, 128], BF16) for i in range(2)]
        prev32 = sb("prev32", [128, NG, 512], F32)
        prevbf = sb("prevbf", [128, NG, 512], BF16)
        cbTm = [sb(f"cbTm{i}", [128, 128], F32) for i in range(2)]
        tcl = [sb(f"tcl{i}", [128, 4, 128], F32) for i in range(2)]
        Lt = [sb(f"Lt{i}", [128, 4, 128], F32) for i in range(2)]
        MT = [sb(f"MT{i}", [128, 4, 128], BF16) for i in range(2)]
        yos = [sb(f"yos{i}", [128, 8, 64], F32) for i in range(4)]
        Dcol = sb("Dcols", [128, 16], F32)
        ystage = [sb(f"ystage{i}", [128, NH, 64], F32) for i in range(2)]
        yTs = [sb(f"yTs{i}", [128, 16, 128], F32) for i in range(2)]
        ps = [st.enter_context(nc.psum_tensor(f"p{_PH[0]}_ps{i}", [128, 512], F32)) for i in range(3)]
        psmisc = st.enter_context(nc.psum_tensor(f"p{_PH[0]}_psmisc", [128, 512], F32))
        psbt = st.enter_context(nc.psum_tensor(f"p{_PH[0]}_psbt", [128, 1024], BF16))
        psbc = [st.enter_context(nc.psum_tensor(f"p{_PH[0]}_psbc{i}", [128, 512], F32)) for i in range(2)]
        psyd = st.enter_context(nc.psum_tensor(f"p{_PH[0]}_psyd", [128, 512], F32))
        cn = {}

        def nxt(k, n):
            vv = cn.get(k, 0)
            cn[k] = vv + 1
            return vv % n
        for name, t, d in [('cw', cw, cwd.rearrange("p (c k) -> p c k", k=4)), ('cb', cb, cbd),
                           ('dtb', dtb, dtbd), ('Dv', Dv, Dd), ('U', U, Ud[:, :]),
                           ('ones128', ones128, onesd[:, :]), ('sel', sel, seld[:, :].rearrange("p (h m) -> p h m", m=128)),
                           ('tri', tri, trid[:, :]), ('identf', identf, identfd[:, :]), ('identb', identb, identbd[:, :]),
                           ('Aneg', Aneg, alogd), ('Dcol', Dcol, dr['Dcol'][hf])]:
            DMA(P, 'sp', t[:], d, [], [name], 'd_' + name)
        ACTV(P, Aneg[:], Aneg[:], AF.Exp, ['Aneg'], ['Aneg'])
        TS(P, 'dve', Aneg[:], Aneg[:], -1.0, None, ALU.mult, None, ['Aneg'], ['Aneg'])
        P.op('dve', 'memset', dict(ap=prev32[:], constant=0.0), [], [('prev32', g) for g in range(NG)])
        P.op('dve', 'memset', dict(ap=prevbf[:], constant=0.0), [], [('prevbf', g) for g in range(NG)])

        def chk(n):
            if stop <= n:
                raise _Stop()
        try:
          for pc in range(npieces):
              t0 = pc * 512
              for half in range(2):
                  if half == 0:
                      srcs = [(0, 12, hf * 2048)]
                  else:
                      srcs = [(0, 4, hf * 2048 + 1536), (4, 4, 4096 + hf * 512), (8, 4, 5120 + hf * 512)]
                  if pc == 0:
                      P.op('dve', 'memset', dict(ap=xin[:, :, 0:3], constant=0.0), [], ['xin'])
                  for (c0_, n_, r0_) in srcs:
                      src = xbcT[r0_:r0_ + n_ * 128, :]
                      if pc == 0:
                          DMA(P, 'sp', xin[:, c0_:c0_ + n_, 3:515], src[:, 0:512].rearrange("(c p) t -> p c t", p=128), [], ['xin'], 'd_xin')
                      else:
                          DMA(P, 'sp', xin[:, c0_:c0_ + n_, :], src[:, t0 - 3:t0 + 512].rearrange("(c p) t -> p c t", p=128), [], ['xin'], 'd_xin')
                  for cl in range(12):
                      ct = half * 12 + cl
                      a = nxt('cacc', 2)
                      TS(P, 'dve', cacc[a][:], xin[:, cl, 3:515], cw[:, ct, 3:4], None, ALU.mult, None,
                         ['xin', 'cw'], [('cacc', a)])
                      for k in (2, 1, 0):
                          STT(P, cacc[a][:], xin[:, cl, k:k + 512], cw[:, ct, k:k + 1], cacc[a][:], ALU.mult, ALU.add,
                              ['xin', 'cw', ('cacc', a)], [('cacc', a)])
                      if ct < 16:
                          dst, dr = xc[:, ct, :], ('xc', ct)
                      elif ct < 20:
                          dst, dr = BTc[:, ct - 16, :], ('BTc', ct - 16)
                      else:
                          dst, dr = CTc[:, ct - 20, :], ('CTc', ct - 20)
                      ACTV(P, dst, cacc[a][:], AF.Silu, [('cacc', a), 'cb'], [dr], bias=cb[:, ct:ct + 1])
              chk(1)
              DMA(P, 'sp', dtr[:], dtd[t0:t0 + 512, hf * 32:(hf + 1) * 32].rearrange("(c p) h -> p c h", p=128), [], ['dtr'], 'd_dtr')
              TT(P, 'dve', dtr[:], dtr[:], dtb[:].unsqueeze(1).to_broadcast([128, 4, NH]), ALU.add, ['dtr', 'dtb'], ['dtr'])
              ACTV(P, dtp[:], dtr[:], AF.Exp, ['dtr'], ['dtp'])
              ACTV(P, dtp[:], dtp[:], AF.Ln, ['dtp', 'ones128'], ['dtp'], bias=ones128[:, 0:1])
              TT(P, 'dve', adt[:], dtp[:], Aneg[:].unsqueeze(1).to_broadcast([128, 4, NH]), ALU.mult, ['dtp', 'Aneg'], ['adt'])
              chk(2)
              def preamble(c):
                  cbi = (pc * 4 + c) % 2
                  l0 = c * 128
                  acs, ea, dec, cdec, dtdec, acsT, x_tm, xdt, xdtd, B_tm = (acs2[cbi], ea2[cbi], dec2[cbi], cdec2[cbi], dtdec2[cbi], acsT2[cbi], x_tm2[cbi], xdt2[cbi], xdtd2[cbi], B_tm2[cbi])
                  MM(P, psmisc[:, 0:NH], U[:], adt[:, c, :], True, True, ['U', 'adt'], ['psmisc'])
                  MM(P, psmisc[:, 32:32 + NH], ones128[:], adt[:, c, :], True, True, ['ones128', 'adt'], ['psmisc'])
                  MM(P, psmisc[0:32, 64:192], adt[:, c, :], U[:], True, True, ['U', 'adt'], ['psmisc'])
                  CP(P, 'dve', acs[:], psmisc[:, 0:NH], ['psmisc'], [('acs', cbi)])
                  ACTV(P, ea[:], psmisc[:, 0:NH], AF.Exp, ['psmisc'], [('ea', cbi)])
                  ACTV(P, cdec[:], psmisc[:, 32:32 + NH], AF.Exp, ['psmisc'], [('cdec', cbi)])
                  TT(P, 'dve', dec[:], psmisc[:, 32:32 + NH], acs[:], ALU.subtract, ['psmisc', ('acs', cbi)], [('dec', cbi)])
                  ACTV(P, dec[:], dec[:], AF.Exp, [('dec', cbi)], [('dec', cbi)])
                  CP(P, 'dve', acsT[:], psmisc[0:32, 64:192], ['psmisc'], [('acsT', cbi)])
                  TT(P, 'dve', dtdec[:], dtp[:, c, :], dec[:], ALU.mult, ['dtp', ('dec', cbi)], [('dtdec', cbi)])
                  for q4 in range(4):
                      b = nxt('ps', 3)
                      for k in range(4):
                          ct = q4 * 4 + k
                          P.op('pe', 'transpose', dict(out=ps[b][:, k * 128:(k + 1) * 128], in_=xc[:, ct, l0:l0 + 128],
                                                       identity=identf[:]), [('xc', ct), 'identf'], [('ps', b)])
                      CP(P, 'act', x_tm[:, q4 * 8:(q4 + 1) * 8, :].rearrange("p h d -> p (h d)"), ps[b][:, :],
                         [('ps', b)], [('x_tm', cbi, q4)])
                  xr = [('x_tm', cbi, q) for q in range(4)]
                  TT(P, 'dve', xdt[:], x_tm[:], dtp[:, c, :].unsqueeze(2).to_broadcast([128, NH, 64]), ALU.mult,
                     xr + ['dtp'], [('xdt', cbi)])
                  TT(P, 'dve', xdtd[:], x_tm[:], dtdec[:].unsqueeze(2).to_broadcast([128, NH, 64]), ALU.mult,
                     xr + [('dtdec', cbi)], [('xdtd', cbi)])
                  for g in range(NG):
                      P.op('pe', 'transpose', dict(out=psbt[:, g * 128:(g + 1) * 128], in_=BTc[:, g, l0:l0 + 128],
                                                   identity=identb[:]), [('BTc', g), 'identb'], ['psbt'])
                  CP(P, 'act', B_tm[:].rearrange("p g n -> p (g n)"), psbt[:, 0:512], ['psbt'], [('B_tm', cbi)])

              preamble(0)
              for c in range(4):
                  cbi = (pc * 4 + c) % 2
                  l0 = c * 128
                  acs, ea, dec, cdec, dtdec, acsT, x_tm, xdt, xdtd, B_tm = (acs2[cbi], ea2[cbi], dec2[cbi], cdec2[cbi], dtdec2[cbi], acsT2[cbi], x_tm2[cbi], xdt2[cbi], xdtd2[cbi], B_tm2[cbi])
                  ys = nxt('ystage', 2)
                  for g in range(NG):
                      b = nxt('ps', 3)
                      MM(P, ps[b][:, :], CTc[:, g, l0:l0 + 128], prevbf[:, g, :], True, True,
                         [('CTc', g), ('prevbf', g)], [('ps', b)])
                      yo = g
                      TT(P, 'dve', yos[yo][:], ps[b][:, :].rearrange("p (h d) -> p h d", d=64),
                         ea[:, g * 8:(g + 1) * 8].unsqueeze(2).to_broadcast([128, 8, 64]), ALU.mult,
                         [('ps', b), ('ea', cbi)], [('yos', yo)])
                      b = nxt('ps', 3)
                      MM(P, ps[b][:, :], B_tm[:, g, :], xdtd[:, g * 8:(g + 1) * 8, :].rearrange("p h d -> p (h d)"), True, True,
                         [('B_tm', cbi), ('xdtd', cbi)], [('ps', b)])
                      TT(P, 'pool', prev32[:, g, :].rearrange("p (h d) -> p h d", d=64),
                         prev32[:, g, :].rearrange("p (h d) -> p h d", d=64),
                         cdec[:, g * 8:(g + 1) * 8].unsqueeze(2).to_broadcast([128, 8, 64]), ALU.mult,
                         [('prev32', g), ('cdec', cbi)], [('prev32', g)])
                      TT(P, 'dve', prev32[:, g, :], prev32[:, g, :], ps[b][:, :], ALU.add, [('prev32', g), ('ps', b)],
                         [('prev32', g)])
                      CP(P, 'act', prevbf[:, g, :], prev32[:, g, :], [('prev32', g)], [('prevbf', g)])
                  if c + 1 < 4:
                      preamble(c + 1)
                  quads = [(g, hq) for g in range(NG) for hq in range(2)]
                  qbuf = {}

                  def quad_a(i):
                      g, hq = quads[i]
                      if hq == 0:
                          b = nxt('ps', 3)
                          MM(P, ps[b][:, 0:128], BTc[:, g, l0:l0 + 128], CTc[:, g, l0:l0 + 128], True, True,
                             [('BTc', g), ('CTc', g)], [('ps', b)])
                          TT(P, 'dve', cbTm[g % 2][:], ps[b][:, 0:128], tri[:], ALU.mult, [('ps', b), 'tri'], [('cbTm', g % 2)])
                      cm = g % 2
                      bb = nxt('psbc', 2)
                      tc_ = nxt('tcl', 2)
                      for hh in range(4):
                          h = g * 8 + hq * 4 + hh
                          MM(P, psbc[bb][:, hh * 128:(hh + 1) * 128], sel[:, h, :], acsT[:], True, True,
                             ['sel', ('acsT', cbi)], [('psbc', bb)])
                          ACTV(P, tcl[tc_][:, hh, :], psbc[bb][:, hh * 128:(hh + 1) * 128], AF.Relu, [('psbc', bb), ('acs', cbi)],
                               [('tcl', tc_)], scale=-1.0, bias=acs[:, h:h + 1])
                      ACTV(P, Lt[tc_][:], tcl[tc_][:], AF.Exp, [('tcl', tc_)], [('Lt', tc_)], scale=-1.0)
                      TT(P, 'pool', MT[tc_][:], Lt[tc_][:], cbTm[cm][:].unsqueeze(1).to_broadcast([128, 4, 128]), ALU.mult,
                         [('Lt', tc_), ('cbTm', cm)], [('MT', tc_)])
                      qbuf[i] = tc_

                  def quad_b(i):
                      g, hq = quads[i]
                      tc_ = qbuf.pop(i)
                      for hh in range(4):
                          h = g * 8 + hq * 4 + hh
                          MM(P, psyd[:, (hq * 4 + hh) * 64:(hq * 4 + hh + 1) * 64], MT[tc_][:, hh, :], xdt[:, h, :],
                             True, True, [('MT', tc_), ('xdt', cbi)], ['psyd'])
                      if hq == 1:
                          TT(P, 'dve', ystage[ys][:, g * 8:(g + 1) * 8, :], psyd[:, :].rearrange("p (h d) -> p h d", d=64),
                             yos[g][:], ALU.add, ['psyd', ('yos', g)], [('ystage', ys)])

                  quad_a(0)
                  for i in range(len(quads)):
                      if i + 1 < len(quads):
                          quad_a(i + 1)
                      quad_b(i)
                  yt = nxt('yTs', 2)
                  for q4 in range(4):
                      b = nxt('ps', 3)
                      for k in range(4):
                          ct = q4 * 4 + k
                          P.op('pe', 'transpose', dict(out=ps[b][:, k * 128:(k + 1) * 128],
                                                       in_=ystage[ys][:, 2 * ct:2 * ct + 2, :].rearrange("p h d -> p (h d)"),
                                                       identity=identf[:]), [('ystage', ys), 'identf'], [('ps', b)])
                      for k in range(4):
                          ct = q4 * 4 + k
                          STT(P, yTs[yt][:, ct, :], xc[:, ct, l0:l0 + 128], Dcol[:, ct:ct + 1], ps[b][:, k * 128:(k + 1) * 128],
                              ALU.mult, ALU.add, [('xc', ct), 'Dcol', ('ps', b)], [('yTs', yt)])
                  DMA(P, 'sp', yT[hf * 2048:(hf + 1) * 2048, t0 + l0:t0 + l0 + 128].rearrange("(c p) t -> p c t", p=128),
                      yTs[yt][:], [('yTs', yt)], [], f'd_y{yt}')
        except _Stop:
            pass


def rmsnorm_stats(P, xt, ones, psk, ps, rstd, sq, xres, nfeat, KCn, eps=EPS):
    for kc in range(KCn):
        ACTV(P, sq[kc % 2][:], xt[:, kc, :], AF.Square, [xres], [('sq', kc % 2)])
        MM(P, ps[:], ones[:], sq[kc % 2][:], kc == 0, kc == KCn - 1, [('sq', kc % 2), 'ones'], [psk])
    TS(P, 'dve', rstd, ps[:], 1.0 / nfeat, eps, ALU.mult, ALU.add, [psk], ['rstd'])
    ACTV(P, rstd, rstd, AF.Sqrt, ['rstd'], ['rstd'])
    P.op('dve', 'reciprocal', dict(out=rstd, in_=rstd), ['rstd'], ['rstd'])


def emit_proj(nc, P, st, xT, W, nwd, table, od, t0, NT, stg_dt):
    NTG = NT // 512
    NTT = NT // 128
    sb = lambda name, shape, dt: st.enter_context(nc.sbuf_tensor(f"p{_PH[0]}_" + name, shape, dt))
    ones = sb("ones", [128, 128], F32)
    nw = sb("nw_sb", [128, KC], F32)
    xt = [sb(f"xt{i}", [128, KC, 512], F32) for i in range(2)]
    sq = [sb(f"sq{i}", [128, 512], F32) for i in range(2)]
    rstd = sb("rstd", [128, 512], F32)
    hT = sb("hT", [128, KC, NT], BF16)
    NW = 3
    wbf = [sb(f"wbf{i}", [128, KC, 512], BF16) for i in range(NW)]
    NS = 4
    stg = [sb(f"stg{i}", [128, 512], stg_dt) for i in range(NS)]
    stgf = sb("stgf", [128, 16], F32)
    psb = [st.enter_context(nc.psum_tensor(f"p{_PH[0]}_ps{i}", [128, 512], F32)) for i in range(8)]
    P.op('dve', 'memset', dict(ap=ones[:], constant=1.0), [], ['ones'])
    DMA(P, 'sp', nw[:], nwd[:, :], [], ['nw'], 'd_nw')
    wt = table

    def load_w(i):
        c0, ncol, kind, dst, d0 = wt[i]
        s = i % NW
        DMA(P, 'pool', wbf[s][:, :, :ncol], W[:, c0:c0 + ncol].rearrange("(kc p) c -> p kc c", p=128),
            [], [('wbf', s)], f'd_w{s}')
    load_w(0)
    load_w(1)
    for tg in range(NTG):
        xr = ('xt', tg % 2)
        DMA(P, 'sp', xt[tg % 2][:], xT[:, t0 + tg * 512:t0 + (tg + 1) * 512].rearrange("(kc p) t -> p kc t", p=128),
            [], [xr], f'd_x{tg % 2}')
        rmsnorm_stats(P, xt[tg % 2], ones, ('ps', 7), psb[7], rstd[:], sq, xr, D, KC)
        for kc in range(KC):
            STT(P, hT[:, kc, tg * 512:(tg + 1) * 512], xt[tg % 2][:, kc, :], nw[:, kc:kc + 1], rstd[:],
                ALU.mult, ALU.mult, [xr, 'rstd', 'nw'], [('hT', kc, tg)])
    cnt = {'ps': 0, 'stg': 0, 'ev': 0}

    def nxt(k, n):
        v = cnt[k] % n
        cnt[k] += 1
        return v
    for i in range(len(wt)):
        if i + 2 < len(wt):
            load_w(i + 2)
        c0, ncol, kind, dstn, d0 = wt[i]
        dst = od[dstn]
        s = i % NW
        if kind == 'fm':
            for cb in range((ncol + 127) // 128):
                m = min(128, ncol - cb * 128)
                for tg in range(NTG):
                    b = nxt('ps', 6)
                    for kc in range(KC):
                        MM(P, psb[b][:m, :], wbf[s][:, kc, cb * 128:cb * 128 + m], hT[:, kc, tg * 512:(tg + 1) * 512],
                           kc == 0, kc == KC - 1, [('wbf', s), ('hT', kc, tg)], [('ps', b)])
                    ss = nxt('stg', NS)
                    ev = 'act' if nxt('ev', 2) == 0 else 'dve'
                    CP(P, ev, stg[ss][:m, :], psb[b][:m, :], [('ps', b)], [('stg', ss)])
                    r0 = d0 + cb * 128
                    DMA(P, 'sp', dst[r0:r0 + m, t0 + tg * 512:t0 + (tg + 1) * 512], stg[ss][:m, :], [('stg', ss)], [], f'd_o{ss}')
        else:
            for tt in range(NTT):
                b = nxt('ps', 6)
                for kc in range(KC):
                    MM(P, psb[b][:, :ncol], hT[:, kc, tt * 128:(tt + 1) * 128], wbf[s][:, kc, :ncol],
                       kc == 0, kc == KC - 1, [('wbf', s), ('hT', kc, tt // 4)], [('ps', b)])
                r0 = t0 + tt * 128
                if kind == 'tmw':
                    TS(P, 'dve', stgf[:, :], psb[b][:, :16], 1.0 / 32.0, None, ALU.mult, None, [('ps', b)], ['stgf'])
                    DMA(P, 'sp', dst[r0:r0 + 128, :], stgf[:, :], ['stgf'], [], 'd_of')
                    continue
                ss = nxt('stg', NS)
                if kind == 'tmg':
                    ACTV(P, stg[ss][:, :ncol], psb[b][:, :ncol], AF.Silu, [('ps', b)], [('stg', ss)])
                else:
                    CP(P, 'dve', stg[ss][:, :ncol], psb[b][:, :ncol], [('ps', b)], [('stg', ss)])
                DMA(P, 'sp', dst[r0:r0 + 128, d0:d0 + ncol], stg[ss][:, :ncol], [('stg', ss)], [], f'd_o{ss}')


def table_a():
    wt = []
    for i in range(8):
        wt.append((i * 512, 512, 'fm', 'qT' if i < 4 else 'kT', (i % 4) * 512))
    for i in range(2):
        wt.append((8192 + i * 512, 512, 'fm', 'iqT', i * 512))
    wt.append((9216, 64, 'fm', 'ikT', 0))
    for i in range(4):
        wt.append((4096 + i * 512, 512, 'tm', 'v', i * 512))
    for i in range(4):
        wt.append((6144 + i * 512, 512, 'tmg', 'sg', i * 512))
    wt.append((9280, 16, 'tmw', 'iw', 0))
    return wt


def table_b():
    wt = []
    for i in range(8):
        wt.append((i * 512, 512, 'fm', 'zT', i * 512))
    for i in range(12):
        wt.append((4096 + i * 512, 512, 'fm', 'xbcT', i * 512))
    wt.append((10240, 64, 'tm', 'dt', 0))
    return wt


def emit_aout(nc, P, st, uT, xT, W, x1T, t0, NT):
    NTG = NT // 512
    sb = lambda name, shape, dt: st.enter_context(nc.sbuf_tensor(f"p{_PH[0]}_" + name, shape, dt))
    aT = sb("aT", [128, KC, NT], BF16)
    wbf = [sb(f"wbf{i}", [128, KC, 512], BF16) for i in range(2)]
    rs = [sb(f"rs{i}", [128, 512], F32) for i in range(4)]
    psb = [st.enter_context(nc.psum_tensor(f"p{_PH[0]}_ps{i}", [128, 512], F32)) for i in range(6)]
    cnt = {}

    def nxt(k, n):
        v = cnt.get(k, 0)
        cnt[k] = v + 1
        return v % n
    DMA(P, 'sp', aT[:], uT[:, t0:t0 + NT].rearrange("(kc p) t -> p kc t", p=128), [], ['aT'], 'd_a')

    def load_w(i):
        DMA(P, 'pool', wbf[i % 2][:], W[:, i * 512:(i + 1) * 512].rearrange("(kc p) c -> p kc c", p=128),
            [], [('wbf', i % 2)], f'd_w{i % 2}')
    load_w(0)
    for i in range(4):
        if i + 1 < 4:
            load_w(i + 1)
        for cb in range(4):
            ob = i * 4 + cb
            for tg in range(NTG):
                c0 = t0 + tg * 512
                r = nxt('rs', 4)
                DMA(P, 'sp', rs[r][:], xT[ob * 128:(ob + 1) * 128, c0:c0 + 512], [], [('rs', r)], f'd_r{r}')
                b = nxt('ps', 6)
                for kc in range(KC):
                    MM(P, psb[b][:], wbf[i % 2][:, kc, cb * 128:(cb + 1) * 128], aT[:, kc, tg * 512:(tg + 1) * 512],
                       kc == 0, kc == KC - 1, [('wbf', i % 2), 'aT'], [('ps', b)])
                TT(P, 'dve', rs[r][:], psb[b][:], rs[r][:], ALU.add, [('ps', b), ('rs', r)], [('rs', r)])
                DMA(P, 'sp', x1T[ob * 128:(ob + 1) * 128, c0:c0 + 512], rs[r][:], [('rs', r)], [], f'd_o{r}')


def emit_bout(nc, P, st, yT, zT, x1T, bnwd, W, x2T, p0, TP=1024):
    KI = 32
    NTG = TP // 512
    sb = lambda name, shape, dt: st.enter_context(nc.sbuf_tensor(f"p{_PH[0]}_" + name, shape, dt))
    ones = sb("ones", [128, 128], F32)
    bnw = sb("bnws", [128, KI], F32)
    gyb = sb("gyb", [128, KI, TP], BF16)
    rstd = sb("rstd", [128, TP], F32)
    yb = [sb(f"yb{i}", [128, 4, 512], F32) for i in range(2)]
    zb = [sb(f"zb{i}", [128, 4, 512], F32) for i in range(2)]
    sq = [sb(f"sq{i}", [128, 512], F32) for i in range(2)]
    wbf = [sb(f"wbf{i}", [128, KI, 256], BF16) for i in range(3)]
    rs = [sb(f"rs{i}", [128, 512], F32) for i in range(4)]
    t32 = [sb(f"t32{i}", [128, 512], F32) for i in range(2)]
    psb = [st.enter_context(nc.psum_tensor(f"p{_PH[0]}_ps{i}", [128, 512], F32)) for i in range(8)]
    cnt = {}

    def nxt(k, n):
        v = cnt.get(k, 0)
        cnt[k] = v + 1
        return v % n
    P.op('dve', 'memset', dict(ap=ones[:], constant=1.0), [], ['ones'])
    DMA(P, 'sp', bnw[:], bnwd[:, :], [], ['bnw'], 'd_bnw')
    wi = [0]

    def load_w(ci):
        s = wi[0] % 3
        wi[0] += 1
        DMA(P, 'pool', wbf[s][:], W[:, ci * 256:(ci + 1) * 256].rearrange("(kc p) c -> p kc c", p=128),
            [], [('wbf', s)], f'd_w{s}')
        return s
    slots = {0: load_w(0), 1: load_w(1)}
    for tg in range(NTG):
        c0 = p0 + tg * 512
        for k4 in range(KI // 4):
            s = nxt('yz', 2)
            DMA(P, 'sp', yb[s][:], yT[k4 * 512:(k4 + 1) * 512, c0:c0 + 512].rearrange("(k p) t -> p k t", p=128),
                [], [('yb', s)], f'd_y{s}')
            DMA(P, 'sp', zb[s][:], zT[k4 * 512:(k4 + 1) * 512, c0:c0 + 512].rearrange("(k p) t -> p k t", p=128),
                [], [('zb', s)], f'd_z{s}')
            ACTV(P, zb[s][:], zb[s][:], AF.Silu, [('zb', s)], [('zb', s)])
            TT(P, 'dve', yb[s][:], yb[s][:], zb[s][:], ALU.mult, [('yb', s), ('zb', s)], [('yb', s)])
            for k in range(4):
                kc = k4 * 4 + k
                q = nxt('sq', 2)
                ACTV(P, sq[q][:], yb[s][:, k, :], AF.Square, [('yb', s)], [('sq', q)])
                MM(P, psb[7][:], ones[:], sq[q][:], kc == 0, kc == KI - 1, [('sq', q), 'ones'], [('ps', 7)])
                TS(P, 'pool', gyb[:, kc, tg * 512:(tg + 1) * 512], yb[s][:, k, :], bnw[:, kc:kc + 1], 1.0,
                   ALU.mult, ALU.mult, [('yb', s), 'bnw'], [('gyb', kc, tg)])
        rv = rstd[:, tg * 512:(tg + 1) * 512]
        TS(P, 'dve', rv, psb[7][:], 1.0 / 4096.0, EPS, ALU.mult, ALU.add, [('ps', 7)], [('rstd', tg)])
        ACTV(P, rv, rv, AF.Sqrt, [('rstd', tg)], [('rstd', tg)])
        P.op('dve', 'reciprocal', dict(out=rv, in_=rv), [('rstd', tg)], [('rstd', tg)])
    for ci in range(8):
        if ci + 2 < 8:
            slots[ci + 2] = load_w(ci + 2)
        s = slots[ci]
        for cb in range(2):
            ob = ci * 2 + cb
            for tg in range(NTG):
                c0 = p0 + tg * 512
                r = nxt('rs', 4)
                DMA(P, 'sp', rs[r][:], x1T[ob * 128:(ob + 1) * 128, c0:c0 + 512], [], [('rs', r)], f'd_r{r}')
                b = nxt('ps', 6)
                for kc in range(KI):
                    MM(P, psb[b][:], wbf[s][:, kc, cb * 128:(cb + 1) * 128], gyb[:, kc, tg * 512:(tg + 1) * 512],
                       kc == 0, kc == KI - 1, [('wbf', s), ('gyb', kc, tg)], [('ps', b)])
                t = nxt('t32', 2)
                TT(P, 'dve', t32[t][:], psb[b][:], rstd[:, tg * 512:(tg + 1) * 512], ALU.mult,
                   [('ps', b), ('rstd', tg)], [('t32', t)])
                TT(P, 'pool', rs[r][:], t32[t][:], rs[r][:], ALU.add, [('t32', t), ('rs', r)], [('rs', r)])
                DMA(P, 'sp', x2T[ob * 128:(ob + 1) * 128, c0:c0 + 512], rs[r][:], [('rs', r)], [], f'd_o{r}')


def emit_fnorm(nc, P, st, xT, nwd, oT, NT):
    sb = lambda name, shape, dt: st.enter_context(nc.sbuf_tensor(f"p{_PH[0]}_" + name, shape, dt))
    ones = sb("ones", [128, 128], F32)
    nw = sb("nw_sb", [128, KC], F32)
    xt = [sb(f"xt{i}", [128, KC, 512], F32) for i in range(2)]
    sq = [sb(f"sq{i}", [128, 512], F32) for i in range(2)]
    rstd = sb("rstd", [128, 512], F32)
    ps = st.enter_context(nc.psum_tensor(f"p{_PH[0]}_ps0", [128, 512], F32))
    P.op('dve', 'memset', dict(ap=ones[:], constant=1.0), [], ['ones'])
    DMA(P, 'sp', nw[:], nwd[:, :], [], ['nw'], 'd_nw')
    for tg in range(NT // 512):
        xr = ('xt', tg % 2)
        DMA(P, 'sp', xt[tg % 2][:], xT[:, tg * 512:(tg + 1) * 512].rearrange("(kc p) t -> p kc t", p=128),
            [], [xr], f'd_x{tg % 2}')
        rmsnorm_stats(P, xt[tg % 2], ones, ('ps', 0), ps, rstd[:], sq, xr, D, KC)
        for kc in range(KC):
            STT(P, xt[tg % 2][:, kc, :], xt[tg % 2][:, kc, :], nw[:, kc:kc + 1], rstd[:],
                ALU.mult, ALU.mult, [xr, 'rstd', 'nw'], [xr])
        DMA(P, 'sp', oT[:, tg * 512:(tg + 1) * 512].rearrange("(kc p) t -> p kc t", p=128), xt[tg % 2][:],
            [xr], [], f'd_o{tg % 2}')


SEQ = 4096
_DBG = None


def build_fused(phases=None):
    nc = bass.Bass("TRN2", target_bir_lowering=False)
    ein = lambda name, shape, dt: nc.dram_tensor(name, list(shape), dt, kind="ExternalInput").ap()
    scr = lambda name, shape, dt: nc.dram_tensor(name, list(shape), dt, kind="Internal").ap()
    dr = dict(
        xT=ein("xT", [D, SEQ], F32), nwA=ein("nwA", [128, KC], F32), nwB=ein("nwB", [128, KC], F32),
        nwF=ein("nwF", [128, KC], F32), Wa=ein("Wa", [D, 9296], F32), Wo=ein("Wo", [D, D], F32),
        Wb=ein("Wb", [D, 10304], F32), Wbo=ein("Wbo", [4096, D], F32), bnw=ein("bnw", [128, 32], F32),
        bfar=ein("bfar", [128, 16], F32), ident=ein("ident", [128, 128], BF16), cm=ein("cm", [2, 128, 512], F32),
        biasN=ein("biasN", [2, 16, 128, 768], F32), U=ein("U", [128, 128], F32), ones128=ein("ones128", [128, 128], F32),
        sel=ein("sel", [32, 32 * 128], F32), tri=ein("tri", [128, 128], F32), identf=ein("identf", [128, 128], F32),
        cw=ein("cw", [2, 128, NCT * 4], F32), cb=ein("cb", [2, 128, NCT], F32), dtb=ein("dtb", [2, 128, NH], F32),
        alog=ein("alog", [2, 128, NH], F32), Dv=ein("Dv", [2, 128, NH], F32), Dcol=ein("Dcol", [2, 128, 16], F32),
        qT=scr("qT", [2048, SEQ], BF16), kT=scr("kT", [2048, SEQ], BF16), iqT=scr("iqT", [1024, SEQ], BF16),
        ikT=scr("ikT", [64, SEQ], BF16), v=scr("v", [SEQ, 2048], BF16), sg=scr("sg", [SEQ, 2048], BF16),
        iw=scr("iw", [SEQ, 16], F32), uT=scr("uT", [2048, SEQ], BF16), x1T=scr("x1T", [D, SEQ], F32),
        zT=scr("zT", [4096, SEQ], F32), xbcT=scr("xbcT", [6144, SEQ], F32), dt=scr("dt", [SEQ, 64], F32),
        yT=scr("yT", [4096, SEQ], F32), x2T=scr("x2T", [D, SEQ], F32),
    )
    oT = nc.dram_tensor("oT", [D, SEQ], F32, kind="ExternalOutput").ap()
    plist = []
    for hf in range(2):
        plist.append(('projA', lambda P, st, hf=hf: emit_proj(nc, P, st, dr['xT'], dr['Wa'], dr['nwA'], table_a(), dr,
                                                              hf * 2048, 2048, BF16)))
    for par in range(2):
        plist.append(('attn', lambda P, st, par=par: emit_k2(nc, P, st, par, dr)))
    for hf in range(2):
        plist.append(('aout', lambda P, st, hf=hf: emit_aout(nc, P, st, dr['uT'], dr['xT'], dr['Wo'], dr['x1T'], hf * 2048, 2048)))
    for hf in range(2):
        plist.append(('projB', lambda P, st, hf=hf: emit_proj(nc, P, st, dr['x1T'], dr['Wb'], dr['nwB'], table_b(), dr,
                                                              hf * 2048, 2048, F32)))
    for hf in range(2):
        plist.append(('ssd', lambda P, st, hf=hf: emit_k4(nc, P, st, hf, dr)))
    for ps_ in range(4):
        plist.append(('bout', lambda P, st, ps_=ps_: emit_bout(nc, P, st, dr['yT'], dr['zT'], dr['x1T'], dr['bnw'], dr['Wbo'],
                                                               dr['x2T'], ps_ * 1024)))
    plist.append(('fnorm', lambda P, st: emit_fnorm(nc, P, st, dr['x2T'], dr['nwF'], oT, SEQ)))
    with contextlib.ExitStack() as g:
        P = Prog(nc, g)
        for i, (name, fn) in enumerate(plist):
            if phases is not None and i not in phases:
                continue
            _PH[0] = i
            with contextlib.ExitStack() as st:
                fn(P, st)
                P.end_phase(st)
    return nc


def _pk(w, n):
    return np.ascontiguousarray(np.asarray(w, np.float32).reshape(n, 128).T)


def _bc(a):
    a = np.asarray(a, np.float32)
    return np.ascontiguousarray(np.broadcast_to(a[None, :], (128, a.shape[0])))


def host_inputs(b, x, norm_w, a_w_in, a_w_out, rel_bias, b_w_in, b_conv_w, b_conv_b, b_dt_bias, b_a_log, b_d,
                b_norm_w, b_w_out, final_norm_w, shared):
    A = np.ascontiguousarray
    im = dict(shared)
    im["xT"] = A(np.asarray(x[b], np.float32).T)
    return im


def host_shared(norm_w, a_w_in, a_w_out, rel_bias, b_w_in, b_conv_w, b_conv_b, b_dt_bias, b_a_log, b_d,
                b_norm_w, b_w_out, final_norm_w):
    A = np.ascontiguousarray
    f = lambda a: np.asarray(a, np.float32)
    sh = dict(nwA=_pk(norm_w[0], KC), nwB=_pk(norm_w[1], KC), nwF=_pk(final_norm_w, KC), Wa=A(f(a_w_in[0])),
              Wo=A(f(a_w_out[0])), Wb=A(f(b_w_in[0])), Wbo=A(f(b_w_out[0])), bnw=_pk(b_norm_w[0], 32))
    rb = f(rel_bias)
    c0 = k2_consts(rb, 0)
    c1 = k2_consts(rb, 1)
    sh.update(bfar=c0['bfar'], ident=c0['ident'], cm=A(np.stack([c0['cm'], c1['cm']])),
              biasN=A(np.stack([c0['biasN'], c1['biasN']])))
    k4c = k4_consts()
    sh.update(U=k4c['U'], ones128=k4c['ones128'], sel=k4c['sel'], tri=k4c['tri'], identf=k4c['identf'])
    cw = f(b_conv_w[0])
    cbv = f(b_conv_b[0])
    cws, cbs, dtbs, alogs, Dvs = [], [], [], [], []
    for hf in range(2):
        chans = np.concatenate([np.arange(hf * 2048, (hf + 1) * 2048), 4096 + np.arange(hf * 512, (hf + 1) * 512),
                                5120 + np.arange(hf * 512, (hf + 1) * 512)])
        hs = slice(hf * 32, (hf + 1) * 32)
        cws.append(cw[:, chans].T.reshape(NCT, 128, 4).transpose(1, 0, 2).reshape(128, NCT * 4))
        cbs.append(cbv[chans].reshape(NCT, 128).T)
        dtbs.append(_bc(f(b_dt_bias[0])[hs]))
        alogs.append(_bc(f(b_a_log[0])[hs]))
        Dvs.append(_bc(f(b_d[0])[hs]))
    sh.update(cw=A(np.stack(cws)), cb=A(np.stack(cbs)), dtb=A(np.stack(dtbs)), alog=A(np.stack(alogs)), Dv=A(np.stack(Dvs)))
    dcol = [np.repeat(f(b_d[0])[hf * 32:(hf + 1) * 32], 64).reshape(16, 128).T for hf in range(2)]
    sh['Dcol'] = A(np.stack(dcol))
    return sh


NCORES = 8


def kernel(x, norm_w, a_w_in, a_w_out, rel_bias, b_w_in, b_conv_w, b_conv_b, b_dt_bias, b_a_log, b_d,
           b_norm_w, b_w_out, final_norm_w):
    nc = build_fused()
    sh = host_shared(norm_w, a_w_in, a_w_out, rel_bias, b_w_in, b_conv_w, b_conv_b, b_dt_bias, b_a_log, b_d,
                     b_norm_w, b_w_out, final_norm_w)
    ims = []
    for c in range(NCORES):
        im = dict(sh)
        im["xT"] = np.ascontiguousarray(np.asarray(x[c % 4], np.float32).T)
        ims.append(im)
    res = run_bass_kernel_spmd(nc, ims, core_ids=list(range(NCORES)))
    out = np.zeros((4, SEQ, D), np.float32)
    for b in range(4):
        out[b] = np.asarray(res.results[b]["oT"]).T
    return out
```

```python
import contextlib
import numpy as np
import ml_dtypes
import concourse.bass as bass
import concourse.mybir as mybir
from concourse.bass_utils import run_bass_kernel_spmd

F32 = mybir.dt.float32
_PH = [0]
BF16 = mybir.dt.bfloat16
ALU = mybir.AluOpType
AF = mybir.ActivationFunctionType
AX = mybir.AxisListType

ENGS = ['pe', 'act', 'dve', 'pool', 'sp']


def _is_psum(r):
    n = r[0] if isinstance(r, tuple) else r
    return isinstance(n, str) and (n.startswith('ps') or n.startswith('acc'))


class Prog:
    def __init__(self, nc, gstack):
        self.nc = nc
        self.gstack = gstack
        self.sems = {}
        self.semval = {}
        self.base = {}
        self.dma_keys = set()
        self.total_instr = {e: 0 for e in ENGS}
        self._reset()

    def _reset(self):
        self.q = {e: [] for e in ENGS}
        self.waited = {e: {} for e in ENGS}
        self.res = {}
        self.touched = set()

    def op(self, eng, meth, kw, reads=(), writes=(), dma=None):
        fn = (meth, kw)
        waits = {}
        own = 'c_' + eng
        xr = [r for r in reads if _is_psum(r)]
        if xr:
            writes = list(writes) + [r for r in xr if r not in writes]

        def need(kv, raw):
            if kv is None:
                return
            k, v = kv
            if k == own and not raw:
                return
            if k == own and eng == 'pe':
                return
            if self.waited[eng].get(k, 0) >= v:
                return
            if waits.get(k, 0) < v:
                waits[k] = v

        for r in reads:
            st = self.res.get(r)
            if st:
                need(st['w'], True)
        for w in writes:
            st = self.res.get(w)
            if st:
                need(st['w'], False)
                for kv in st['r'].items():
                    need(kv, False)
        if dma:
            key, inc = dma, 16
            self.dma_keys.add(key)
        else:
            key, inc = own, 1
        self.semval[key] = self.semval.get(key, 0) + inc
        self.touched.add(key)
        val = self.semval[key]
        for k, v in waits.items():
            self.waited[eng][k] = v
        self.q[eng].append([fn, sorted(waits.items()), key, inc, val])
        for r in reads:
            st = self.res.setdefault(r, {'w': None, 'r': {}})
            st['r'][key] = val
        for w in writes:
            self.res[w] = {'w': (key, val), 'r': {}}
        return (key, val)

    def end_phase(self, pstack):
        nc = self.nc
        fin = [(k, self.semval[k]) for k in sorted(self.touched)]
        for e in ENGS:
            self.q[e].append([None, list(fin), None, 0, 0])
        waited_vals = {}
        for e in ENGS:
            for fn, waits, key, inc, val in self.q[e]:
                for k, v in waits:
                    waited_vals.setdefault(k, set()).add(v)
        remap = {}
        for k, vs in waited_vals.items():
            if k in self.dma_keys:
                continue
            b0 = self.base.get(k, 0)
            remap[k] = {v: b0 + i + 1 for i, v in enumerate(sorted(vs))}
        for k in self.touched:
            if k not in self.sems:
                self.sems[k] = self.gstack.enter_context(nc.semaphore("s_" + k))
        sems = self.sems
        block = pstack.enter_context(nc.Block())
        engmap = {'pe': block.tensor, 'act': block.scalar, 'dve': block.vector,
                  'pool': block.gpsimd, 'sp': block.sync}
        for e in ENGS:
            ops = self.q[e]
            self.total_instr[e] += len(ops)

            def body(engine, ops=ops):
                for fn, waits, key, inc, val in ops:
                    for k, v in waits:
                        if k in self.dma_keys:
                            engine.wait_ge(sems[k], v)
                        else:
                            engine.wait_ge(sems[k], remap[k][v])
                    if fn is None:
                        continue
                    ins = getattr(engine, fn[0])(**fn[1])
                    if key in self.dma_keys:
                        ins.then_inc(sems[key], 16)
                    elif key in remap and val in remap[key]:
                        ins.then_inc(sems[key], 1)
            engmap[e](body)
        for k, m in remap.items():
            self.base[k] = self.base.get(k, 0) + len(m)
        self._reset()


def MM(P, out, lhsT, rhs, start, stop, reads, writes, **kw):
    return P.op('pe', 'matmul', dict(out=out, lhsT=lhsT, rhs=rhs, start=start, stop=stop, **kw), reads, writes)


def ACTV(P, out, in_, func, reads, writes, **kw):
    return P.op('act', 'activation', dict(out=out, in_=in_, func=func, **kw), reads, writes)


def DMA(P, eng, out, in_, reads, writes, key):
    return P.op(eng, 'dma_start', dict(out=out, in_=in_), reads, writes, dma=key)


def TS(P, eng, out, in0, s1, s2, op0, op1, reads, writes, **kw):
    d = dict(out=out, in0=in0, scalar1=s1, scalar2=s2, op0=op0, **kw)
    if op1 is not None:
        d['op1'] = op1
    return P.op(eng, 'tensor_scalar', d, reads, writes)


def TT(P, eng, out, in0, in1, op, reads, writes):
    return P.op(eng, 'tensor_tensor', dict(out=out, in0=in0, in1=in1, op=op), reads, writes)


def STT(P, out, in0, scalar, in1, op0, op1, reads, writes, **kw):
    return P.op('dve', 'scalar_tensor_tensor', dict(out=out, in0=in0, scalar=scalar, in1=in1, op0=op0, op1=op1, **kw),
                reads, writes)


def CP(P, eng, out, in_, reads, writes):
    if eng == 'act':
        return P.op('act', 'activation', dict(out=out, in_=in_, func=AF.Copy), reads, writes)
    return P.op(eng, 'tensor_copy', dict(out=out, in_=in_), reads, writes)


D = 2048
KC = D // 128
EPS = 1e-6


S = 4096
NIT = 26
SCALE = 128 ** -0.5


def own_blocks(par):
    return [i for i in range(32) if (i % 4 in (0, 3)) == (par == 0)]


def k2_consts(rel_bias, par):
    n = np.arange(256)
    nf = np.maximum(n, 1).astype(np.float32)
    large = 16 + (np.log(nf / 16) / np.log(128 / 16) * 16).astype(np.int32)
    large = np.minimum(large, 31)
    bucket = np.where(n < 16, n, large)
    s = np.arange(128)[:, None]
    q = np.arange(128)[None, :]
    biasT = np.zeros((128, 16, 2, 128), np.float32)
    for d in range(2):
        dist = np.clip(d * 128 + q - s, 0, 255)
        biasT[:, :, d, :] = rel_bias[bucket[dist]].transpose(0, 2, 1)
    bfar = np.ascontiguousarray(np.broadcast_to(rel_bias[31][None, :], (128, 16))).astype(np.float32)
    negtri = np.where(np.arange(128)[None, :] <= np.arange(128)[:, None], 0.0, -1e30).astype(np.float32)
    ident = np.eye(128, dtype=np.float32).astype(ml_dtypes.bfloat16)
    dtab = lambda sp, w: (1 - w) if (sp == par) else (2 - w)
    cm = np.zeros((128, 2, 2, 128), np.float32)
    biasN = np.zeros((16, 128, 2, 3, 128), np.float32)
    for sp in range(2):
        for w in range(3):
            d = dtab(sp, w)
            if d >= 2:
                biasN[:, :, sp, w, :] = rel_bias[31][:, None, None]
            elif d >= 0:
                biasN[:, :, sp, w, :] = biasT[:, :, d, :].transpose(1, 0, 2)
            if w >= 1:
                if d == 0:
                    cm[:, sp, w - 1, :] = negtri
                elif d < 0:
                    cm[:, sp, w - 1, :] = -1e30
    return dict(bfar=bfar, ident=ident, cm=cm.reshape(128, 512), biasN=biasN.reshape(16, 128, 768))


def emit_k2(nc, P, st, par, dr, nslots=16, nheads=16, nit=NIT):
    own = own_blocks(par)
    rsel = (0, 3) if par == 0 else (1, 2)
    NQB = nslots
    nkbs = [2 * (qi + 1) for qi in range(nslots)]
    NQ = NQB * 128
    offs = []
    T = 0
    for nkb in nkbs:
        offs.append(T)
        T += nkb
    kT, v, ikT, qT, iqT, iw, sg, uT = dr['kT'], dr['v'], dr['ikT'], dr['qT'], dr['iqT'], dr['iw'], dr['sg'], dr['uT']
    biasNd, bfard, cmd, identd = dr['biasN'], dr['bfar'], dr['cm'], dr['ident']
    if True:
        sb = lambda name, shape, dt: st.enter_context(nc.sbuf_tensor(f"p{_PH[0]}_" + name, shape, dt))
        maskT = sb("maskT", [128, T, 128], BF16)
        score = sb("score", [128, S], F32)
        maskq = sb("maskq", [128, S], BF16)
        relu = [sb(f"relu{i}", [128, 2048], F32) for i in range(2)]
        ikTs = sb("ikTs", [64, S], BF16)
        iqb = [sb(f"iqb{i}", [64, 16, 128], BF16) for i in range(2)]
        iwb = [sb(f"iwb{i}", [128, 16], F32) for i in range(2)]
        kTh = [sb(f"kTh{i}", [128, S], BF16) for i in range(2)]
        vh = [sb(f"vh{i}", [128, 32, 136], BF16) for i in range(2)]
        qTh = [sb(f"qTh{i}", [128, NQ], BF16) for i in range(2)]
        sgb = [sb(f"sgb{i}", [128, NQB, 128], BF16) for i in range(2)]
        biasN = [sb(f"biasN{i}", [128, 2, 3, 128], F32) for i in range(2)]
        bfar = sb("bfars", [128, 16], F32)
        cm = sb("cms", [128, 2, 256], F32)
        ident = sb("idents", [128, 128], BF16)
        hi = sb("hi", [128, 1], F32)
        lo = sb("lo", [128, 1], F32)
        w = sb("w", [128, 1], F32)
        mid = sb("mid", [128, 1], F32)
        cnt = sb("cnt", [128, 1], F32)
        ge = sb("ge", [128, 1], F32)
        rinv = sb("rinv", [128, 1], F32)
        NE = 6
        tb = [sb(f"tb{i}", [128, 128], F32) for i in range(2)]
        eb = [sb(f"eb{i}", [128, 512], BF16) for i in range(NE)]
        pt = [sb(f"pt{i}", [128, 512], BF16) for i in range(NE)]
        o32 = [sb(f"o32{i}", [128, 128], F32) for i in range(2)]
        ust = [sb(f"ust{i}", [128, 128], BF16) for i in range(4)]
        usT = [sb(f"usT{i}", [128, 128], BF16) for i in range(4)]
        psb = [st.enter_context(nc.psum_tensor(f"p{_PH[0]}_ps{i}", [128, 512], F32)) for i in range(4)]
        pst = [st.enter_context(nc.psum_tensor(f"p{_PH[0]}_pst{i}", [128, 1024], BF16)) for i in range(2)]
        acc = [st.enter_context(nc.psum_tensor(f"p{_PH[0]}_acc{i}", [128, 512], F32)) for i in range(2)]
        cn = {}

        def nxt(k, n):
            vv = cn.get(k, 0)
            cn[k] = vv + 1
            return vv % n
        DMA(P, 'sp', ikTs[:], ikT[:, :], [], ['ikT'], 'd_c0')
        DMA(P, 'sp', cm[:], cmd[par].rearrange("p (a b) -> p a b", a=2), [], ['cm'], 'd_c1')
        DMA(P, 'sp', ident[:], identd[:, :], [], ['ident'], 'd_c2')
        DMA(P, 'sp', bfar[:], bfard[:, :], [], ['bfar'], 'd_c3')
        for i in range(2):
            P.op('pool', 'memset', dict(ap=vh[i][:, :, 128:129], constant=1.0), [], [('vh1', i)])

        for qi in range(nslots):
            nkb = nkbs[qi]
            nk = nkb * 128
            s2 = qi % 2
            DMA(P, 'sp', iqb[s2][:], iqT[:, own[qi] * 128:(own[qi] + 1) * 128].rearrange("(h d) q -> d h q", d=64),
                [], [('iqb', s2)], f'd_iq{s2}')
            DMA(P, 'sp', iwb[s2][:], iw[own[qi] * 128:(own[qi] + 1) * 128, :], [], [('iwb', s2)], f'd_iw{s2}')
            nch = (nk + 511) // 512
            for hh in range(16):
                for w0 in range(0, nk, 2048):
                    ww = min(2048, nk - w0)
                    rb = nxt('relu', 2)
                    for c0 in range(0, ww, 512):
                        kw = min(512, ww - c0)
                        b = nxt('ps', 4)
                        MM(P, psb[b][:, :kw], iqb[s2][:, hh, :], ikTs[:, w0 + c0:w0 + c0 + kw], True, True,
                           [('iqb', s2), 'ikT'], [('ps', b)])
                        ACTV(P, relu[rb][:, c0:c0 + kw], psb[b][:, :kw], AF.Relu, [('ps', b)], [('relu', rb)])
                    sc = score[:, w0:w0 + ww]
                    skeys = [('score', c) for c in range(w0 // 512, (w0 + ww + 511) // 512)]
                    if hh == 0:
                        TS(P, 'dve', sc, relu[rb][:, :ww], iwb[s2][:, 0:1], None, ALU.mult, None,
                           [('relu', rb), ('iwb', s2)], skeys)
                    else:
                        STT(P, sc, relu[rb][:, :ww], iwb[s2][:, hh:hh + 1], sc, ALU.mult, ALU.add,
                            [('relu', rb), ('iwb', s2)] + skeys, skeys)
            scr = [('score', c) for c in range(nch)]
            P.op('dve', 'tensor_reduce', dict(out=hi[:], in_=score[:, :nk], axis=AX.X, op=ALU.max), scr, ['hi'])
            P.op('dve', 'tensor_reduce', dict(out=lo[:], in_=score[:, :nk], axis=AX.X, op=ALU.min), scr, ['lo'])
            TT(P, 'dve', w[:], hi[:], lo[:], ALU.subtract, ['hi', 'lo'], ['w'])
            dg = score[:, (nkb - 2) * 128:nkb * 128]
            TT(P, 'dve', dg, dg, cm[:, qi % 2, :], ALU.add, [('score', (nkb - 2) // 4), 'cm'], [('score', (nkb - 2) // 4)])
            for it in range(nit):
                TS(P, 'dve', w[:], w[:], 0.5, None, ALU.mult, None, ['w'], ['w'])
                TT(P, 'dve', mid[:], lo[:], w[:], ALU.add, ['lo', 'w'], ['mid'])
                TS(P, 'dve', maskq[:, :nk], score[:, :nk], mid[:, 0:1], 0.0, ALU.is_ge, ALU.add,
                   scr + ['mid'], ['maskq', 'cnt'], accum_out=cnt[:, 0:1])
                TS(P, 'dve', ge[:], cnt[:], 255.5, None, ALU.is_ge, None, ['cnt'], ['ge'])
                STT(P, lo[:], ge[:], w[:, 0:1], lo[:], ALU.mult, ALU.add, ['ge', 'w', 'lo'], ['lo'])
            TS(P, 'dve', maskq[:, :nk], score[:, :nk], lo[:, 0:1], None, ALU.is_ge, None, scr + ['lo'], ['maskq'])
            for j in range(nkb):
                b = nxt('pst', 2)
                P.op('pe', 'transpose', dict(out=pst[b][:, :128], in_=maskq[:, j * 128:(j + 1) * 128], identity=ident[:]),
                     ['maskq', 'ident'], [('pst', b)])
                CP(P, 'act', maskT[:, offs[qi] + j, :], pst[b][:, :128], [('pst', b)], [('maskT', qi)])

        def head_loads(h):
            s2 = h % 2
            DMA(P, 'sp', kTh[s2][:], kT[h * 128:(h + 1) * 128, :], [], [('kTh', s2)], f'd_k{s2}')
            DMA(P, 'sp', vh[s2][:, :, 0:128], v[:, h * 128:(h + 1) * 128].rearrange("(j p) d -> p j d", p=128),
                [], [('vh', s2)], f'd_v{s2}')
            for k in range(2):
                DMA(P, 'sp', qTh[s2][:].rearrange("p (m k t) -> p m k t", k=2, t=128)[:, :, k, :],
                    qT[h * 128:(h + 1) * 128, :].rearrange("p (m r t) -> p m r t", r=4, t=128)[:, :, rsel[k], :],
                    [], [('qTh', s2)], f'd_q{s2}')
            for k in range(2):
                DMA(P, 'sp', sgb[s2][:].rearrange("p (m k) d -> p m k d", k=2)[:, :, k, :],
                    sg[:, h * 128:(h + 1) * 128].rearrange("(m r p) d -> p m r d", r=4, p=128)[:, :, rsel[k], :],
                    [], [('sgb', s2)], f'd_sg{s2}')
            DMA(P, 'sp', biasN[s2][:], biasNd[par, h].rearrange("p (a w q) -> p a w q", a=2, w=3), [], [('biasN', s2)], f'd_bn{s2}')

        units = []
        for h in range(nheads):
            for qi in range(nslots):
                nkb = nkbs[qi]
                nfar = max(0, nkb - 3)
                j = 0
                while j < nfar:
                    n = min(4, nfar - j)
                    units.append((h, qi, j, n, True))
                    j += n
                for j in range(nfar, nkb):
                    units.append((h, qi, j, 1, False))
        ubuf = {}

        def stage_a(t):
            h, qi, j0, n, far = units[t]
            s2 = h % 2
            nkb = nkbs[qi]
            b = nxt('ps', 4)
            for k in range(n):
                MM(P, psb[b][:, k * 128:(k + 1) * 128], kTh[s2][:, (j0 + k) * 128:(j0 + k + 1) * 128],
                   qTh[s2][:, qi * 128:(qi + 1) * 128], True, True, [('kTh', s2), ('qTh', s2)], [('ps', b)])
            e = nxt('eb', NE)
            if far:
                ACTV(P, eb[e][:, :n * 128], psb[b][:, :n * 128], AF.Exp, [('ps', b), 'bfar'], [('eb', e)],
                     scale=SCALE, bias=bfar[:, h:h + 1])
            else:
                wv = j0 - (nkb - 3)
                tt_ = nxt('tb', 2)
                STT(P, tb[tt_][:], psb[b][:, :128], SCALE, biasN[s2][:, qi % 2, wv, :], ALU.mult, ALU.add,
                    [('ps', b), ('biasN', s2)], [('tb', tt_)])
                ACTV(P, eb[e][:, :128], tb[tt_][:], AF.Exp, [('tb', tt_)], [('eb', e)])
            p = nxt('pt', NE)
            me = 'dve' if nxt('me', 2) == 0 else 'pool'
            TT(P, me, pt[p][:, :n * 128], eb[e][:, :n * 128],
               maskT[:, offs[qi] + j0:offs[qi] + j0 + n, :].rearrange("p a b -> p (a b)"), ALU.mult,
               [('eb', e), ('maskT', qi)], [('pt', p)])
            ubuf[t] = p

        def stage_b(t):
            h, qi, j0, n, far = units[t]
            s2 = h % 2
            nkb = nkbs[qi]
            ab = (h * nslots + qi) % 2
            p = ubuf.pop(t)
            for k in range(n):
                j = j0 + k
                MM(P, acc[ab][:, :129], pt[p][:, k * 128:(k + 1) * 128], vh[s2][:, j, 0:129], j == 0, j == nkb - 1,
                   [('pt', p), ('vh', s2), ('vh1', s2)], [('acc', ab)])
            if j0 + n != nkb:
                return
            P.op('dve', 'reciprocal', dict(out=rinv[:], in_=acc[ab][:, 128:129]), [('acc', ab)], ['rinv'])
            o = nxt('o32', 2)
            TS(P, 'dve', o32[o][:], acc[ab][:, :128], rinv[:, 0:1], None, ALU.mult, None,
               [('acc', ab), 'rinv'], [('o32', o)])
            us = nxt('ust', 4)
            TT(P, 'pool', ust[us][:], o32[o][:], sgb[s2][:, qi, :], ALU.mult, [('o32', o), ('sgb', s2)], [('ust', us)])
            tp = nxt('pst', 2)
            P.op('pe', 'transpose', dict(out=pst[tp][:, :128], in_=ust[us][:], identity=ident[:]),
                 [('ust', us), 'ident'], [('pst', tp)])
            CP(P, 'act', usT[us][:], pst[tp][:, :128], [('pst', tp)], [('usT', us)])
            DMA(P, 'sp', uT[h * 128:(h + 1) * 128, own[qi] * 128:(own[qi] + 1) * 128], usT[us][:], [('usT', us)], [], f'd_u{us}')
            if qi == nslots - 1 and h + 2 < nheads:
                head_loads(h + 2)

        LA = 3
        head_loads(0)
        if nheads > 1:
            head_loads(1)
        for t in range(len(units) + LA):
            if t < len(units):
                stage_a(t)
            if t - LA >= 0:
                stage_b(t - LA)


NH = 32
NG = 4
NCT = 24


def k4_consts():
    U = np.triu(np.ones((128, 128), np.float32))
    sel = np.zeros((32, 32, 128), np.float32)
    for h in range(32):
        sel[h, h, :] = 1.0
    return dict(U=U, ones128=np.ones((128, 128), np.float32), sel=sel.reshape(32, 32 * 128),
                tri=U.copy(), identf=np.eye(128, dtype=np.float32),
                identb=np.eye(128, dtype=np.float32).astype(ml_dtypes.bfloat16))


class _Stop(Exception):
    pass


def emit_k4(nc, P, st, hf, dr, npieces=8, stop=99):
    SS = npieces * 512
    xbcT, dtd, yT = dr['xbcT'], dr['dt'], dr['yT']
    cwd, cbd, dtbd, alogd, Dd = dr['cw'][hf], dr['cb'][hf], dr['dtb'][hf], dr['alog'][hf], dr['Dv'][hf]
    Ud, onesd, seld, trid, identfd, identbd = dr['U'], dr['ones128'], dr['sel'], dr['tri'], dr['identf'], dr['ident']
    if True:
        sb = lambda name, shape, dt: st.enter_context(nc.sbuf_tensor(f"p{_PH[0]}_" + name, shape, dt))
        xin = sb("xin", [128, 12, 515], F32)
        xc = sb("xc", [128, 16, 512], F32)
        BTc = sb("BTc", [128, 4, 512], BF16)
        CTc = sb("CTc", [128, 4, 512], BF16)
        cacc = [sb(f"cacc{i}", [128, 512], F32) for i in range(2)]
        cw = sb("cws", [128, NCT, 4], F32)
        cb = sb("cbs", [128, NCT], F32)
        dtb = sb("dtbs", [128, NH], F32)
        Aneg = sb("Aneg", [128, NH], F32)
        Dv = sb("Dvs", [128, NH], F32)
        U = sb("Us", [128, 128], F32)
        ones128 = sb("ones128s", [128, 128], F32)
        sel = sb("sels", [32, 32, 128], F32)
        tri = sb("tris", [128, 128], F32)
        identf = sb("identfs", [128, 128], F32)
        identb = sb("identbs", [128, 128], BF16)
        dtr = sb("dtr", [128, 4, NH], F32)
        dtp = sb("dtp", [128, 4, NH], F32)
        adt = sb("adt", [128, 4, NH], F32)
        acs2 = [sb(f"acs{i}", [128, NH], F32) for i in range(2)]
        ea2 = [sb(f"ea{i}", [128, NH], F32) for i in range(2)]
        dec2 = [sb(f"dec{i}", [128, NH], F32) for i in range(2)]
        cdec2 = [sb(f"cdec{i}", [128, NH], F32) for i in range(2)]
        dtdec2 = [sb(f"dtdec{i}", [128, NH], F32) for i in range(2)]
        acsT2 = [sb(f"acsT{i}", [32, 128], F32) for i in range(2)]
        x_tm2 = [sb(f"x_tm{i}", [128, NH, 64], F32) for i in range(2)]
        xdt2 = [sb(f"xdt{i}", [128, NH, 64], BF16) for i in range(2)]
        xdtd2 = [sb(f"xdtd{i}", [128, NH, 64], BF16) for i in range(2)]
        B_tm2 = [sb(f"B_tm{i}", [128, 4, 128], BF16) for i in range(2)]
        prev32 = sb("prev32", [128, NG, 512], F32)
        prevbf = sb("prevbf", [128, NG, 512], BF16)
        cbTm = [sb(f"cbTm{i}", [128, 128], F32) for i in range(2)]
        tcl = [sb(f"tcl{i}", [128, 4, 128], F32) for i in range(2)]
        Lt = [sb(f"Lt{i}", [128, 4, 128], F32) for i in range(2)]
        MT = [sb(f"MT{i}", [128, 4, 128], BF16) for i in range(2)]
        yos = [sb(f"yos{i}", [128, 8, 64], F32) for i in range(4)]
        Dcol = sb("Dcols", [128, 16], F32)
        ystage = [sb(f"ystage{i}", [128, NH, 64], F32) for i in range(2)]
        yTs = [sb(f"yTs{i}", [128, 16, 128], F32) for i in range(2)]
        ps = [st.enter_context(nc.psum_tensor(f"p{_PH[0]}_ps{i}", [128, 512], F32)) for i in range(3)]
        psmisc = st.enter_context(nc.psum_tensor(f"p{_PH[0]}_psmisc", [128, 512], F32))
        psbt = st.enter_context(nc.psum_tensor(f"p{_PH[0]}_psbt", [128, 1024], BF16))
        psbc = [st.enter_context(nc.psum_tensor(f"p{_PH[0]}_psbc{i}", [128, 512], F32)) for i in range(2)]
        psyd = st.enter_context(nc.psum_tensor(f"p{_PH[0]}_psyd", [128, 512], F32))
        cn = {}

        def nxt(k, n):
            vv = cn.get(k, 0)
            cn[k] = vv + 1
            return vv % n
        for name, t, d in [('cw', cw, cwd.rearrange("p (c k) -> p c k", k=4)), ('cb', cb, cbd),
                           ('dtb', dtb, dtbd), ('Dv', Dv, Dd), ('U', U, Ud[:, :]),
                           ('ones128', ones128, onesd[:, :]), ('sel', sel, seld[:, :].rearrange("p (h m) -> p h m", m=128)),
                           ('tri', tri, trid[:, :]), ('identf', identf, identfd[:, :]), ('identb', identb, identbd[:, :]),
                           ('Aneg', Aneg, alogd), ('Dcol', Dcol, dr['Dcol'][hf])]:
            DMA(P, 'sp', t[:], d, [], [name], 'd_' + name)
        ACTV(P, Aneg[:], Aneg[:], AF.Exp, ['Aneg'], ['Aneg'])
        TS(P, 'dve', Aneg[:], Aneg[:], -1.0, None, ALU.mult, None, ['Aneg'], ['Aneg'])
        P.op('dve', 'memset', dict(ap=prev32[:], constant=0.0), [], [('prev32', g) for g in range(NG)])
        P.op('dve', 'memset', dict(ap=prevbf[:], constant=0.0), [], [('prevbf', g) for g in range(NG)])

        def chk(n):
            if stop <= n:
                raise _Stop()
        try:
          for pc in range(npieces):
              t0 = pc * 512
              for half in range(2):
                  if half == 0:
                      srcs = [(0, 12, hf * 2048)]
                  else:
                      srcs = [(0, 4, hf * 2048 + 1536), (4, 4, 4096 + hf * 512), (8, 4, 5120 + hf * 512)]
                  if pc == 0:
                      P.op('dve', 'memset', dict(ap=xin[:, :, 0:3], constant=0.0), [], ['xin'])
                  for (c0_, n_, r0_) in srcs:
                      src = xbcT[r0_:r0_ + n_ * 128, :]
                      if pc == 0:
                          DMA(P, 'sp', xin[:, c0_:c0_ + n_, 3:515], src[:, 0:512].rearrange("(c p) t -> p c t", p=128), [], ['xin'], 'd_xin')
                      else:
                          DMA(P, 'sp', xin[:, c0_:c0_ + n_, :], src[:, t0 - 3:t0 + 512].rearrange("(c p) t -> p c t", p=128), [], ['xin'], 'd_xin')
                  for cl in range(12):
                      ct = half * 12 + cl
                      a = nxt('cacc', 2)
                      TS(P, 'dve', cacc[a][:], xin[:, cl, 3:515], cw[:, ct, 3:4], None, ALU.mult, None,
                         ['xin', 'cw'], [('cacc', a)])
                      for k in (2, 1, 0):
                          STT(P, cacc[a][:], xin[:, cl, k:k + 512], cw[:, ct, k:k + 1], cacc[a][:], ALU.mult, ALU.add,
                              ['xin', 'cw', ('cacc', a)], [('cacc', a)])
                      if ct < 16:
                          dst, dr = xc[:, ct, :], ('xc', ct)
                      elif ct < 20:
                          dst, dr = BTc[:, ct - 16, :], ('BTc', ct - 16)
                      else:
                          dst, dr = CTc[:, ct - 20, :], ('CTc', ct - 20)
                      ACTV(P, dst, cacc[a][:], AF.Silu, [('cacc', a), 'cb'], [dr], bias=cb[:, ct:ct + 1])
              chk(1)
              DMA(P, 'sp', dtr[:], dtd[t0:t0 + 512, hf * 32:(hf + 1) * 32].rearrange("(c p) h -> p c h", p=128), [], ['dtr'], 'd_dtr')
              TT(P, 'dve', dtr[:], dtr[:], dtb[:].unsqueeze(1).to_broadcast([128, 4, NH]), ALU.add, ['dtr', 'dtb'], ['dtr'])
              ACTV(P, dtp[:], dtr[:], AF.Exp, ['dtr'], ['dtp'])
              ACTV(P, dtp[:], dtp[:], AF.Ln, ['dtp', 'ones128'], ['dtp'], bias=ones128[:, 0:1])
              TT(P, 'dve', adt[:], dtp[:], Aneg[:].unsqueeze(1).to_broadcast([128, 4, NH]), ALU.mult, ['dtp', 'Aneg'], ['adt'])
              chk(2)
              def preamble(c):
                  cbi = (pc * 4 + c) % 2
                  l0 = c * 128
                  acs, ea, dec, cdec, dtdec, acsT, x_tm, xdt, xdtd, B_tm = (acs2[cbi], ea2[cbi], dec2[cbi], cdec2[cbi], dtdec2[cbi], acsT2[cbi], x_tm2[cbi], xdt2[cbi], xdtd2[cbi], B_tm2[cbi])
                  MM(P, psmisc[:, 0:NH], U[:], adt[:, c, :], True, True, ['U', 'adt'], ['psmisc'])
                  MM(P, psmisc[:, 32:32 + NH], ones128[:], adt[:, c, :], True, True, ['ones128', 'adt'], ['psmisc'])
                  MM(P, psmisc[0:32, 64:192], adt[:, c, :], U[:], True, True, ['U', 'adt'], ['psmisc'])
                  CP(P, 'dve', acs[:], psmisc[:, 0:NH], ['psmisc'], [('acs', cbi)])
                  ACTV(P, ea[:], psmisc[:, 0:NH], AF.Exp, ['psmisc'], [('ea', cbi)])
                  ACTV(P, cdec[:], psmisc[:, 32:32 + NH], AF.Exp, ['psmisc'], [('cdec', cbi)])
                  TT(P, 'dve', dec[:], psmisc[:, 32:32 + NH], acs[:], ALU.subtract, ['psmisc', ('acs', cbi)], [('dec', cbi)])
                  ACTV(P, dec[:], dec[:], AF.Exp, [('dec', cbi)], [('dec', cbi)])
                  CP(P, 'dve', acsT[:], psmisc[0:32, 64:192], ['psmisc'], [('acsT', cbi)])
                  TT(P, 'dve', dtdec[:], dtp[:, c, :], dec[:], ALU.mult, ['dtp', ('dec', cbi)], [('dtdec', cbi)])
                  for q4 in range(4):
                      b = nxt('ps', 3)
                      for k in range(4):
                          ct = q4 * 4 + k
                          P.op('pe', 'transpose', dict(out=ps[b][:, k * 128:(k + 1) * 128], in_=xc[:, ct, l0:l0 + 128],
                                                       identity=identf[:]), [('xc', ct), 'identf'], [('ps', b)])
                      CP(P, 'act', x_tm[:, q4 * 8:(q4 + 1) * 8, :].rearrange("p h d -> p (h d)"), ps[b][:, :],
                         [('ps', b)], [('x_tm', cbi, q4)])
                  xr = [('x_tm', cbi, q) for q in range(4)]
                  TT(P, 'dve', xdt[:], x_tm[:], dtp[:, c, :].unsqueeze(2).to_broadcast([128, NH, 64]), ALU.mult,
                     xr + ['dtp'], [('xdt', cbi)])
                  TT(P, 'dve', xdtd[:], x_tm[:], dtdec[:].unsqueeze(2).to_broadcast([128, NH, 64]), ALU.mult,
                     xr + [('dtdec', cbi)], [('xdtd', cbi)])
                  for g in range(NG):
                      P.op('pe', 'transpose', dict(out=psbt[:, g * 128:(g + 1) * 128], in_=BTc[:, g, l0:l0 + 128],
                                                   identity=identb[:]), [('BTc', g), 'identb'], ['psbt'])
                  CP(P, 'act', B_tm[:].rearrange("p g n -> p (g n)"), psbt[:, 0:512], ['psbt'], [('B_tm', cbi)])

              preamble(0)
              for c in range(4):
                  cbi = (pc * 4 + c) % 2
                  l0 = c * 128
                  acs, ea, dec, cdec, dtdec, acsT, x_tm, xdt, xdtd, B_tm = (acs2[cbi], ea2[cbi], dec2[cbi], cdec2[cbi], dtdec2[cbi], acsT2[cbi], x_tm2[cbi], xdt2[cbi], xdtd2[cbi], B_tm2[cbi])
                  ys = nxt('ystage', 2)
                  for g in range(NG):
                      b = nxt('ps', 3)
                      MM(P, ps[b][:, :], CTc[:, g, l0:l0 + 128], prevbf[:, g, :], True, True,
                         [('CTc', g), ('prevbf', g)], [('ps', b)])
                      yo = g
                      TT(P, 'dve', yos[yo][:], ps[b][:, :].rearrange("p (h d) -> p h d", d=64),
                         ea[:, g * 8:(g + 1) * 8].unsqueeze(2).to_broadcast([128, 8, 64]), ALU.mult,
                         [('ps', b), ('ea', cbi)], [('yos', yo)])
                      b = nxt('ps', 3)
                      MM(P, ps[b][:, :], B_tm[:, g, :], xdtd[:, g * 8:(g + 1) * 8, :].rearrange("p h d -> p (h d)"), True, True,
                         [('B_tm', cbi), ('xdtd', cbi)], [('ps', b)])
                      TT(P, 'pool', prev32[:, g, :].rearrange("p (h d) -> p h d", d=64),
                         prev32[:, g, :].rearrange("p (h d) -> p h d", d=64),
                         cdec[:, g * 8:(g + 1) * 8].unsqueeze(2).to_broadcast([128, 8, 64]), ALU.mult,
                         [('prev32', g), ('cdec', cbi)], [('prev32', g)])
                      TT(P, 'dve', prev32[:, g, :], prev32[:, g, :], ps[b][:, :], ALU.add, [('prev32', g), ('ps', b)],
                         [('prev32', g)])
                      CP(P, 'act', prevbf[:, g, :], prev32[:, g, :], [('prev32', g)], [('prevbf', g)])
                  if c + 1 < 4:
                      preamble(c + 1)
                  quads = [(g, hq) for g in range(NG) for hq in range(2)]
                  qbuf = {}

                  def quad_a(i):
                      g, hq = quads[i]
                      if hq == 0:
                          b = nxt('ps', 3)
                          MM(P, ps[b][:, 0:128], BTc[:, g, l0:l0 + 128], CTc[:, g, l0:l0 + 128], True, True,
                             [('BTc', g), ('CTc', g)], [('ps', b)])
                          TT(P, 'dve', cbTm[g % 2][:], ps[b][:, 0:128], tri[:], ALU.mult, [('ps', b), 'tri'], [('cbTm', g % 2)])
                      cm = g % 2
                      bb = nxt('psbc', 2)
                      tc_ = nxt('tcl', 2)
                      for hh in range(4):
                          h = g * 8 + hq * 4 + hh
                          MM(P, psbc[bb][:, hh * 128:(hh + 1) * 128], sel[:, h, :], acsT[:], True, True,
                             ['sel', ('acsT', cbi)], [('psbc', bb)])
                          ACTV(P, tcl[tc_][:, hh, :], psbc[bb][:, hh * 128:(hh + 1) * 128], AF.Relu, [('psbc', bb), ('acs', cbi)],
                               [('tcl', tc_)], scale=-1.0, bias=acs[:, h:h + 1])
                      ACTV(P, Lt[tc_][:], tcl[tc_][:], AF.Exp, [('tcl', tc_)], [('Lt', tc_)], scale=-1.0)
                      TT(P, 'pool', MT[tc_][:], Lt[tc_][:], cbTm[cm][:].unsqueeze(1).to_broadcast([128, 4, 128]), ALU.mult,
                         [('Lt', tc_), ('cbTm', cm)], [('MT', tc_)])
                      qbuf[i] = tc_

                  def quad_b(i):
                      g, hq = quads[i]
                      tc_ = qbuf.pop(i)
                      for hh in range(4):
                          h = g * 8 + hq * 4 + hh
                          MM(P, psyd[:, (hq * 4 + hh) * 64:(hq * 4 + hh + 1) * 64], MT[tc_][:, hh, :], xdt[:, h, :],
                             True, True, [('MT', tc_), ('xdt', cbi)], ['psyd'])
                      if hq == 1:
                          TT(P, 'dve', ystage[ys][:, g * 8:(g + 1) * 8, :], psyd[:, :].rearrange("p (h d) -> p h d", d=64),
                             yos[g][:], ALU.add, ['psyd', ('yos', g)], [('ystage', ys)])

                  quad_a(0)
                  for i in range(len(quads)):
                      if i + 1 < len(quads):
                          quad_a(i + 1)
                      quad_b(i)
                  yt = nxt('yTs', 2)
                  for q4 in range(4):
                      b = nxt('ps', 3)
                      for k in range(4):
                          ct = q4 * 4 + k
                          P.op('pe', 'transpose', dict(out=ps[b][:, k * 128:(k + 1) * 128],
                                                       in_=ystage[ys][:, 2 * ct:2 * ct + 2, :].rearrange("p h d -> p (h d)"),
                                                       identity=identf[:]), [('ystage', ys), 'identf'], [('ps', b)])
                      for k in range(4):
                          ct = q4 * 4 + k
                          STT(P, yTs[yt][:, ct, :], xc[:, ct, l0:l0 + 128], Dcol[:, ct:ct + 1], ps[b][:, k * 128:(k + 1) * 128],
                              ALU.mult, ALU.add, [('xc', ct), 'Dcol', ('ps', b)], [('yTs', yt)])
                  DMA(P, 'sp', yT[hf * 2048:(hf + 1) * 2048, t0 + l0:t0 + l0 + 128].rearrange("(c p) t -> p c t", p=128),
                      yTs[yt][:], [('yTs', yt)], [], f'd_y{yt}')
        except _Stop:
            pass


def rmsnorm_stats(P, xt, ones, psk, ps, rstd, sq, xres, nfeat, KCn, eps=EPS):
    for kc in range(KCn):
        ACTV(P, sq[kc % 2][:], xt[:, kc, :], AF.Square, [xres], [('sq', kc % 2)])
        MM(P, ps[:], ones[:], sq[kc % 2][:], kc == 0, kc == KCn - 1, [('sq', kc % 2), 'ones'], [psk])
    TS(P, 'dve', rstd, ps[:], 1.0 / nfeat, eps, ALU.mult, ALU.add, [psk], ['rstd'])
    ACTV(P, rstd, rstd, AF.Sqrt, ['rstd'], ['rstd'])
    P.op('dve', 'reciprocal', dict(out=rstd, in_=rstd), ['rstd'], ['rstd'])


def emit_proj(nc, P, st, xT, W, nwd, table, od, t0, NT, stg_dt):
    NTG = NT // 512
    NTT = NT // 128
    sb = lambda name, shape, dt: st.enter_context(nc.sbuf_tensor(f"p{_PH[0]}_" + name, shape, dt))
    ones = sb("ones", [128, 128], F32)
    nw = sb("nw_sb", [128, KC], F32)
    xt = [sb(f"xt{i}", [128, KC, 512], F32) for i in range(2)]
    sq = [sb(f"sq{i}", [128, 512], F32) for i in range(2)]
    rstd = sb("rstd", [128, 512], F32)
    hT = sb("hT", [128, KC, NT], BF16)
    NW = 3
    wbf = [sb(f"wbf{i}", [128, KC, 512], BF16) for i in range(NW)]
    NS = 4
    stg = [sb(f"stg{i}", [128, 512], stg_dt) for i in range(NS)]
    stgf = sb("stgf", [128, 16], F32)
    psb = [st.enter_context(nc.psum_tensor(f"p{_PH[0]}_ps{i}", [128, 512], F32)) for i in range(8)]
    P.op('dve', 'memset', dict(ap=ones[:], constant=1.0), [], ['ones'])
    DMA(P, 'sp', nw[:], nwd[:, :], [], ['nw'], 'd_nw')
    wt = table

    def load_w(i):
        c0, ncol, kind, dst, d0 = wt[i]
        s = i % NW
        DMA(P, 'pool', wbf[s][:, :, :ncol], W[:, c0:c0 + ncol].rearrange("(kc p) c -> p kc c", p=128),
            [], [('wbf', s)], f'd_w{s}')
    load_w(0)
    load_w(1)
    for tg in range(NTG):
        xr = ('xt', tg % 2)
        DMA(P, 'sp', xt[tg % 2][:], xT[:, t0 + tg * 512:t0 + (tg + 1) * 512].rearrange("(kc p) t -> p kc t", p=128),
            [], [xr], f'd_x{tg % 2}')
        rmsnorm_stats(P, xt[tg % 2], ones, ('ps', 7), psb[7], rstd[:], sq, xr, D, KC)
        for kc in range(KC):
            STT(P, hT[:, kc, tg * 512:(tg + 1) * 512], xt[tg % 2][:, kc, :], nw[:, kc:kc + 1], rstd[:],
                ALU.mult, ALU.mult, [xr, 'rstd', 'nw'], [('hT', kc, tg)])
    cnt = {'ps': 0, 'stg': 0, 'ev': 0}

    def nxt(k, n):
        v = cnt[k] % n
        cnt[k] += 1
        return v
    for i in range(len(wt)):
        if i + 2 < len(wt):
            load_w(i + 2)
        c0, ncol, kind, dstn, d0 = wt[i]
        dst = od[dstn]
        s = i % NW
        if kind == 'fm':
            for cb in range((ncol + 127) // 128):
                m = min(128, ncol - cb * 128)
                for tg in range(NTG):
                    b = nxt('ps', 6)
                    for kc in range(KC):
                        MM(P, psb[b][:m, :], wbf[s][:, kc, cb * 128:cb * 128 + m], hT[:, kc, tg * 512:(tg + 1) * 512],
                           kc == 0, kc == KC - 1, [('wbf', s), ('hT', kc, tg)], [('ps', b)])
                    ss = nxt('stg', NS)
                    ev = 'act' if nxt('ev', 2) == 0 else 'dve'
                    CP(P, ev, stg[ss][:m, :], psb[b][:m, :], [('ps', b)], [('stg', ss)])
                    r0 = d0 + cb * 128
                    DMA(P, 'sp', dst[r0:r0 + m, t0 + tg * 512:t0 + (tg + 1) * 512], stg[ss][:m, :], [('stg', ss)], [], f'd_o{ss}')
        else:
            for tt in range(NTT):
                b = nxt('ps', 6)
                for kc in range(KC):
                    MM(P, psb[b][:, :ncol], hT[:, kc, tt * 128:(tt + 1) * 128], wbf[s][:, kc, :ncol],
                       kc == 0, kc == KC - 1, [('wbf', s), ('hT', kc, tt // 4)], [('ps', b)])
                r0 = t0 + tt * 128
                if kind == 'tmw':
                    TS(P, 'dve', stgf[:, :], psb[b][:, :16], 1.0 / 32.0, None, ALU.mult, None, [('ps', b)], ['stgf'])
                    DMA(P, 'sp', dst[r0:r0 + 128, :], stgf[:, :], ['stgf'], [], 'd_of')
                    continue
                ss = nxt('stg', NS)
                if kind == 'tmg':
                    ACTV(P, stg[ss][:, :ncol], psb[b][:, :ncol], AF.Silu, [('ps', b)], [('stg', ss)])
                else:
                    CP(P, 'dve', stg[ss][:, :ncol], psb[b][:, :ncol], [('ps', b)], [('stg', ss)])
                DMA(P, 'sp', dst[r0:r0 + 128, d0:d0 + ncol], stg[ss][:, :ncol], [('stg', ss)], [], f'd_o{ss}')


def table_a():
    wt = []
    for i in range(8):
        wt.append((i * 512, 512, 'fm', 'qT' if i < 4 else 'kT', (i % 4) * 512))
    for i in range(2):
        wt.append((8192 + i * 512, 512, 'fm', 'iqT', i * 512))
    wt.append((9216, 64, 'fm', 'ikT', 0))
    for i in range(4):
        wt.append((4096 + i * 512, 512, 'tm', 'v', i * 512))
    for i in range(4):
        wt.append((6144 + i * 512, 512, 'tmg', 'sg', i * 512))
    wt.append((9280, 16, 'tmw', 'iw', 0))
    return wt


def table_b():
    wt = []
    for i in range(8):
        wt.append((i * 512, 512, 'fm', 'zT', i * 512))
    for i in range(12):
        wt.append((4096 + i * 512, 512, 'fm', 'xbcT', i * 512))
    wt.append((10240, 64, 'tm', 'dt', 0))
    return wt


def emit_aout(nc, P, st, uT, xT, W, x1T, t0, NT):
    NTG = NT // 512
    sb = lambda name, shape, dt: st.enter_context(nc.sbuf_tensor(f"p{_PH[0]}_" + name, shape, dt))
    aT = sb("aT", [128, KC, NT], BF16)
    wbf = [sb(f"wbf{i}", [128, KC, 512], BF16) for i in range(2)]
    rs = [sb(f"rs{i}", [128, 512], F32) for i in range(4)]
    psb = [st.enter_context(nc.psum_tensor(f"p{_PH[0]}_ps{i}", [128, 512], F32)) for i in range(6)]
    cnt = {}

    def nxt(k, n):
        v = cnt.get(k, 0)
        cnt[k] = v + 1
        return v % n
    DMA(P, 'sp', aT[:], uT[:, t0:t0 + NT].rearrange("(kc p) t -> p kc t", p=128), [], ['aT'], 'd_a')

    def load_w(i):
        DMA(P, 'pool', wbf[i % 2][:], W[:, i * 512:(i + 1) * 512].rearrange("(kc p) c -> p kc c", p=128),
            [], [('wbf', i % 2)], f'd_w{i % 2}')
    load_w(0)
    for i in range(4):
        if i + 1 < 4:
            load_w(i + 1)
        for cb in range(4):
            ob = i * 4 + cb
            for tg in range(NTG):
                c0 = t0 + tg * 512
                r = nxt('rs', 4)
                DMA(P, 'sp', rs[r][:], xT[ob * 128:(ob + 1) * 128, c0:c0 + 512], [], [('rs', r)], f'd_r{r}')
                b = nxt('ps', 6)
                for kc in range(KC):
                    MM(P, psb[b][:], wbf[i % 2][:, kc, cb * 128:(cb + 1) * 128], aT[:, kc, tg * 512:(tg + 1) * 512],
                       kc == 0, kc == KC - 1, [('wbf', i % 2), 'aT'], [('ps', b)])
                TT(P, 'dve', rs[r][:], psb[b][:], rs[r][:], ALU.add, [('ps', b), ('rs', r)], [('rs', r)])
                DMA(P, 'sp', x1T[ob * 128:(ob + 1) * 128, c0:c0 + 512], rs[r][:], [('rs', r)], [], f'd_o{r}')


def emit_bout(nc, P, st, yT, zT, x1T, bnwd, W, x2T, p0, TP=1024):
    KI = 32
    NTG = TP // 512
    sb = lambda name, shape, dt: st.enter_context(nc.sbuf_tensor(f"p{_PH[0]}_" + name, shape, dt))
    ones = sb("ones", [128, 128], F32)
    bnw = sb("bnws", [128, KI], F32)
    gyb = sb("gyb", [128, KI, TP], BF16)
    rstd = sb("rstd", [128, TP], F32)
    yb = [sb(f"yb{i}", [128, 4, 512], F32) for i in range(2)]
    zb = [sb(f"zb{i}", [128, 4, 512], F32) for i in range(2)]
    sq = [sb(f"sq{i}", [128, 512], F32) for i in range(2)]
    wbf = [sb(f"wbf{i}", [128, KI, 256], BF16) for i in range(3)]
    rs = [sb(f"rs{i}", [128, 512], F32) for i in range(4)]
    t32 = [sb(f"t32{i}", [128, 512], F32) for i in range(2)]
    psb = [st.enter_context(nc.psum_tensor(f"p{_PH[0]}_ps{i}", [128, 512], F32)) for i in range(8)]
    cnt = {}

    def nxt(k, n):
        v = cnt.get(k, 0)
        cnt[k] = v + 1
        return v % n
    P.op('dve', 'memset', dict(ap=ones[:], constant=1.0), [], ['ones'])
    DMA(P, 'sp', bnw[:], bnwd[:, :], [], ['bnw'], 'd_bnw')
    wi = [0]

    def load_w(ci):
        s = wi[0] % 3
        wi[0] += 1
        DMA(P, 'pool', wbf[s][:], W[:, ci * 256:(ci + 1) * 256].rearrange("(kc p) c -> p kc c", p=128),
            [], [('wbf', s)], f'd_w{s}')
        return s
    slots = {0: load_w(0), 1: load_w(1)}
    for tg in range(NTG):
        c0 = p0 + tg * 512
        for k4 in range(KI // 4):
            s = nxt('yz', 2)
            DMA(P, 'sp', yb[s][:], yT[k4 * 512:(k4 + 1) * 512, c0:c0 + 512].rearrange("(k p) t -> p k t", p=128),
                [], [('yb', s)], f'd_y{s}')
            DMA(P, 'sp', zb[s][:], zT[k4 * 512:(k4 + 1) * 512, c0:c0 + 512].rearrange("(k p) t -> p k t", p=128),
                [], [('zb', s)], f'd_z{s}')
            ACTV(P, zb[s][:], zb[s][:], AF.Silu, [('zb', s)], [('zb', s)])
            TT(P, 'dve', yb[s][:], yb[s][:], zb[s][:], ALU.mult, [('yb', s), ('zb', s)], [('yb', s)])
            for k in range(4):
                kc = k4 * 4 + k
                q = nxt('sq', 2)
                ACTV(P, sq[q][:], yb[s][:, k, :], AF.Square, [('yb', s)], [('sq', q)])
                MM(P, psb[7][:], ones[:], sq[q][:], kc == 0, kc == KI - 1, [('sq', q), 'ones'], [('ps', 7)])
                TS(P, 'pool', gyb[:, kc, tg * 512:(tg + 1) * 512], yb[s][:, k, :], bnw[:, kc:kc + 1], 1.0,
                   ALU.mult, ALU.mult, [('yb', s), 'bnw'], [('gyb', kc, tg)])
        rv = rstd[:, tg * 512:(tg + 1) * 512]
        TS(P, 'dve', rv, psb[7][:], 1.0 / 4096.0, EPS, ALU.mult, ALU.add, [('ps', 7)], [('rstd', tg)])
        ACTV(P, rv, rv, AF.Sqrt, [('rstd', tg)], [('rstd', tg)])
        P.op('dve', 'reciprocal', dict(out=rv, in_=rv), [('rstd', tg)], [('rstd', tg)])
    for ci in range(8):
        if ci + 2 < 8:
            slots[ci + 2] = load_w(ci + 2)
        s = slots[ci]
        for cb in range(2):
            ob = ci * 2 + cb
            for tg in range(NTG):
                c0 = p0 + tg * 512
                r = nxt('rs', 4)
                DMA(P, 'sp', rs[r][:], x1T[ob * 128:(ob + 1) * 128, c0:c0 + 512], [], [('rs', r)], f'd_r{r}')
                b = nxt('ps', 6)
                for kc in range(KI):
                    MM(P, psb[b][:], wbf[s][:, kc, cb * 128:(cb + 1) * 128], gyb[:, kc, tg * 512:(tg + 1) * 512],
                       kc == 0, kc == KI - 1, [('wbf', s), ('gyb', kc, tg)], [('ps', b)])
                t = nxt('t32', 2)
                TT(P, 'dve', t32[t][:], psb[b][:], rstd[:, tg * 512:(tg + 1) * 512], ALU.mult,
                   [('ps', b), ('rstd', tg)], [('t32', t)])
                TT(P, 'pool', rs[r][:], t32[t][:], rs[r][:], ALU.add, [('t32', t), ('rs', r)], [('rs', r)])
                DMA(P, 'sp', x2T[ob * 128:(ob + 1) * 128, c0:c0 + 512], rs[r][:], [('rs', r)], [], f'd_o{r}')


def emit_fnorm(nc, P, st, xT, nwd, oT, NT):
    sb = lambda name, shape, dt: st.enter_context(nc.sbuf_tensor(f"p{_PH[0]}_" + name, shape, dt))
    ones = sb("ones", [128, 128], F32)
    nw = sb("nw_sb", [128, KC], F32)
    xt = [sb(f"xt{i}", [128, KC, 512], F32) for i in range(2)]
    sq = [sb(f"sq{i}", [128, 512], F32) for i in range(2)]
    rstd = sb("rstd", [128, 512], F32)
    ps = st.enter_context(nc.psum_tensor(f"p{_PH[0]}_ps0", [128, 512], F32))
    P.op('dve', 'memset', dict(ap=ones[:], constant=1.0), [], ['ones'])
    DMA(P, 'sp', nw[:], nwd[:, :], [], ['nw'], 'd_nw')
    for tg in range(NT // 512):
        xr = ('xt', tg % 2)
        DMA(P, 'sp', xt[tg % 2][:], xT[:, tg * 512:(tg + 1) * 512].rearrange("(kc p) t -> p kc t", p=128),
            [], [xr], f'd_x{tg % 2}')
        rmsnorm_stats(P, xt[tg % 2], ones, ('ps', 0), ps, rstd[:], sq, xr, D, KC)
        for kc in range(KC):
            STT(P, xt[tg % 2][:, kc, :], xt[tg % 2][:, kc, :], nw[:, kc:kc + 1], rstd[:],
                ALU.mult, ALU.mult, [xr, 'rstd', 'nw'], [xr])
        DMA(P, 'sp', oT[:, tg * 512:(tg + 1) * 512].rearrange("(kc p) t -> p kc t", p=128), xt[tg % 2][:],
            [xr], [], f'd_o{tg % 2}')


SEQ = 4096
_DBG = None


def build_fused(phases=None):
    nc = bass.Bass("TRN2", target_bir_lowering=False)
    ein = lambda name, shape, dt: nc.dram_tensor(name, list(shape), dt, kind="ExternalInput").ap()
    scr = lambda name, shape, dt: nc.dram_tensor(name, list(shape), dt, kind="Internal").ap()
    dr = dict(
        xT=ein("xT", [D, SEQ], F32), nwA=ein("nwA", [128, KC], F32), nwB=ein("nwB", [128, KC], F32),
        nwF=ein("nwF", [128, KC], F32), Wa=ein("Wa", [D, 9296], F32), Wo=ein("Wo", [D, D], F32),
        Wb=ein("Wb", [D, 10304], F32), Wbo=ein("Wbo", [4096, D], F32), bnw=ein("bnw", [128, 32], F32),
        bfar=ein("bfar", [128, 16], F32), ident=ein("ident", [128, 128], BF16), cm=ein("cm", [2, 128, 512], F32),
        biasN=ein("biasN", [2, 16, 128, 768], F32), U=ein("U", [128, 128], F32), ones128=ein("ones128", [128, 128], F32),
        sel=ein("sel", [32, 32 * 128], F32), tri=ein("tri", [128, 128], F32), identf=ein("identf", [128, 128], F32),
        cw=ein("cw", [2, 128, NCT * 4], F32), cb=ein("cb", [2, 128, NCT], F32), dtb=ein("dtb", [2, 128, NH], F32),
        alog=ein("alog", [2, 128, NH], F32), Dv=ein("Dv", [2, 128, NH], F32), Dcol=ein("Dcol", [2, 128, 16], F32),
        qT=scr("qT", [2048, SEQ], BF16), kT=scr("kT", [2048, SEQ], BF16), iqT=scr("iqT", [1024, SEQ], BF16),
        ikT=scr("ikT", [64, SEQ], BF16), v=scr("v", [SEQ, 2048], BF16), sg=scr("sg", [SEQ, 2048], BF16),
        iw=scr("iw", [SEQ, 16], F32), uT=scr("uT", [2048, SEQ], BF16), x1T=scr("x1T", [D, SEQ], F32),
        zT=scr("zT", [4096, SEQ], F32), xbcT=scr("xbcT", [6144, SEQ], F32), dt=scr("dt", [SEQ, 64], F32),
        yT=scr("yT", [4096, SEQ], F32), x2T=scr("x2T", [D, SEQ], F32),
    )
    oT = nc.dram_tensor("oT", [D, SEQ], F32, kind="ExternalOutput").ap()
    plist = []
    for hf in range(2):
        plist.append(('projA', lambda P, st, hf=hf: emit_proj(nc, P, st, dr['xT'], dr['Wa'], dr['nwA'], table_a(), dr,
                                                              hf * 2048, 2048, BF16)))
    for par in range(2):
        plist.append(('attn', lambda P, st, par=par: emit_k2(nc, P, st, par, dr)))
    for hf in range(2):
        plist.append(('aout', lambda P, st, hf=hf: emit_aout(nc, P, st, dr['uT'], dr['xT'], dr['Wo'], dr['x1T'], hf * 2048, 2048)))
    for hf in range(2):
        plist.append(('projB', lambda P, st, hf=hf: emit_proj(nc, P, st, dr['x1T'], dr['Wb'], dr['nwB'], table_b(), dr,
                                                              hf * 2048, 2048, F32)))
    for hf in range(2):
        plist.append(('ssd', lambda P, st, hf=hf: emit_k4(nc, P, st, hf, dr)))
    for ps_ in range(4):
        plist.append(('bout', lambda P, st, ps_=ps_: emit_bout(nc, P, st, dr['yT'], dr['zT'], dr['x1T'], dr['bnw'], dr['Wbo'],
                                                               dr['x2T'], ps_ * 1024)))
    plist.append(('fnorm', lambda P, st: emit_fnorm(nc, P, st, dr['x2T'], dr['nwF'], oT, SEQ)))
    with contextlib.ExitStack() as g:
        P = Prog(nc, g)
        for i, (name, fn) in enumerate(plist):
            if phases is not None and i not in phases:
                continue
            _PH[0] = i
            with contextlib.ExitStack() as st:
                fn(P, st)
                P.end_phase(st)
    return nc


def _pk(w, n):
    return np.ascontiguousarray(np.asarray(w, np.float32).reshape(n, 128).T)


def _bc(a):
    a = np.asarray(a, np.float32)
    return np.ascontiguousarray(np.broadcast_to(a[None, :], (128, a.shape[0])))


def host_inputs(b, x, norm_w, a_w_in, a_w_out, rel_bias, b_w_in, b_conv_w, b_conv_b, b_dt_bias, b_a_log, b_d,
                b_norm_w, b_w_out, final_norm_w, shared):
    A = np.ascontiguousarray
    im = dict(shared)
    im["xT"] = A(np.asarray(x[b], np.float32).T)
    return im


def host_shared(norm_w, a_w_in, a_w_out, rel_bias, b_w_in, b_conv_w, b_conv_b, b_dt_bias, b_a_log, b_d,
                b_norm_w, b_w_out, final_norm_w):
    A = np.ascontiguousarray
    f = lambda a: np.asarray(a, np.float32)
    sh = dict(nwA=_pk(norm_w[0], KC), nwB=_pk(norm_w[1], KC), nwF=_pk(final_norm_w, KC), Wa=A(f(a_w_in[0])),
              Wo=A(f(a_w_out[0])), Wb=A(f(b_w_in[0])), Wbo=A(f(b_w_out[0])), bnw=_pk(b_norm_w[0], 32))
    rb = f(rel_bias)
    c0 = k2_consts(rb, 0)
    c1 = k2_consts(rb, 1)
    sh.update(bfar=c0['bfar'], ident=c0['ident'], cm=A(np.stack([c0['cm'], c1['cm']])),
              biasN=A(np.stack([c0['biasN'], c1['biasN']])))
    k4c = k4_consts()
    sh.update(U=k4c['U'], ones128=k4c['ones128'], sel=k4c['sel'], tri=k4c['tri'], identf=k4c['identf'])
    cw = f(b_conv_w[0])
    cbv = f(b_conv_b[0])
    cws, cbs, dtbs, alogs, Dvs = [], [], [], [], []
    for hf in range(2):
        chans = np.concatenate([np.arange(hf * 2048, (hf + 1) * 2048), 4096 + np.arange(hf * 512, (hf + 1) * 512),
                                5120 + np.arange(hf * 512, (hf + 1) * 512)])
        hs = slice(hf * 32, (hf + 1) * 32)
        cws.append(cw[:, chans].T.reshape(NCT, 128, 4).transpose(1, 0, 2).reshape(128, NCT * 4))
        cbs.append(cbv[chans].reshape(NCT, 128).T)
        dtbs.append(_bc(f(b_dt_bias[0])[hs]))
        alogs.append(_bc(f(b_a_log[0])[hs]))
        Dvs.append(_bc(f(b_d[0])[hs]))
    sh.update(cw=A(np.stack(cws)), cb=A(np.stack(cbs)), dtb=A(np.stack(dtbs)), alog=A(np.stack(alogs)), Dv=A(np.stack(Dvs)))
    dcol = [np.repeat(f(b_d[0])[hf * 32:(hf + 1) * 32], 64).reshape(16, 128).T for hf in range(2)]
    sh['Dcol'] = A(np.stack(dcol))
    return sh


NCORES = 8


def kernel(x, norm_w, a_w_in, a_w_out, rel_bias, b_w_in, b_conv_w, b_conv_b, b_dt_bias, b_a_log, b_d,
           b_norm_w, b_w_out, final_norm_w):
    nc = build_fused()
    sh = host_shared(norm_w, a_w_in, a_w_out, rel_bias, b_w_in, b_conv_w, b_conv_b, b_dt_bias, b_a_log, b_d,
                     b_norm_w, b_w_out, final_norm_w)
    ims = []
    for c in range(NCORES):
        im = dict(sh)
        im["xT"] = np.ascontiguousarray(np.asarray(x[c % 4], np.float32).T)
        ims.append(im)
    res = run_bass_kernel_spmd(nc, ims, core_ids=list(range(NCORES)))
    out = np.zeros((4, SEQ, D), np.float32)
    for b in range(4):
        out[b] = np.asarray(res.results[b]["oT"]).T
    return out
```
